# Optimizing a Trainium2 kernel written in Bass

```python
import jax, jax.numpy as jnp
from jax import lax
import numpy as np

D_MODEL = 1024
BATCH = 2
SEQ = 8192
DEPTH = 2

RWKV_HEADS = 8
RWKV_HEAD_DIM = 64
RWKV_WIDTH = RWKV_HEADS * RWKV_HEAD_DIM
DECAY_LORA = 64
ICLR_LORA = 64
GATE_LORA = 128
RWKV_GN_EPS = 64e-5
RWKV_COLS = 3 * RWKV_WIDTH + DECAY_LORA + ICLR_LORA + GATE_LORA
RWKV_SPLITS = (RWKV_WIDTH, 2 * RWKV_WIDTH, 3 * RWKV_WIDTH, 3 * RWKV_WIDTH + DECAY_LORA, 3 * RWKV_WIDTH + DECAY_LORA + ICLR_LORA)

NSA_Q_HEADS = 8
NSA_KV_HEADS = 2
NSA_GROUP = NSA_Q_HEADS // NSA_KV_HEADS
NSA_HEAD_DIM = 64
NSA_WIDTH = NSA_Q_HEADS * NSA_HEAD_DIM
CMP_STRIDE = 16
CMP_BLOCK = 2 * CMP_STRIDE
CMP_HIDDEN = 256
SEL_BLOCK = 64
SEL_TOPK = 16
WINDOW = 512
Q_BLOCK = 128
FORCE_SCORE = 1e4
NEG_INF = -1e30
ROPE_THETA = 500000.0
ROPE_DIM = NSA_HEAD_DIM // 4
KV_COLS = 6 * NSA_KV_HEADS * NSA_HEAD_DIM
NSA_GATE_COLS = 3 * NSA_Q_HEADS

MERGE_COLS = 2 * D_MODEL
IN_COLS = RWKV_COLS + NSA_WIDTH + KV_COLS + NSA_GATE_COLS + MERGE_COLS
IN_SPLITS = (RWKV_COLS, RWKV_COLS + NSA_WIDTH, RWKV_COLS + NSA_WIDTH + KV_COLS, RWKV_COLS + NSA_WIDTH + KV_COLS + NSA_GATE_COLS)

N_EXPERTS = 16
N_GROUPS = 4
EXPERTS_PER_GROUP = N_EXPERTS // N_GROUPS
TOP_K = 2
D_EXPERT = 512
MOE_BLOCK = 256

NORM_EPS = 1e-6

kernel_name = "hybrid_rwkv7_nsa_grouped_moe_adaln"


def rms_norm(x, g):
    xf = x.astype(jnp.float32)
    y = xf * lax.rsqrt(jnp.mean(xf * xf, axis=-1, keepdims=True) + NORM_EPS)
    return (y * g.astype(jnp.float32)).astype(x.dtype)


def rope_partial(x, pos):
    half = ROPE_DIM // 2
    inv = jnp.power(ROPE_THETA, -jnp.arange(half, dtype=jnp.float32) * 2.0 / ROPE_DIM)
    ang = pos.astype(jnp.float32)[:, None] * inv[None, :]
    cos, sin = jnp.cos(ang), jnp.sin(ang)
    xf = x.astype(jnp.float32)
    x1, x2, rest = xf[..., :half], xf[..., half:ROPE_DIM], xf[..., ROPE_DIM:]
    out = jnp.concatenate([x1 * cos - x2 * sin, x2 * cos + x1 * sin, rest], axis=-1)
    return out.astype(x.dtype)


def masked_softmax(s, mask):
    s = jnp.where(mask, s.astype(jnp.float32), NEG_INF)
    m = jnp.max(s, axis=-1, keepdims=True)
    p = jnp.where(mask, jnp.exp(s - m), 0.0)
    return p / jnp.maximum(jnp.sum(p, axis=-1, keepdims=True), jnp.finfo(jnp.float32).tiny)


def token_shift(z):
    return jnp.pad(z[:, :-1], ((0, 0), (1, 0), (0, 0)))


def rwkv7_mix(p, mu, w0, w2, a0, a2, g2, k_k, k_a, r_k, gn_g, gn_b):
    B, T, _ = p.shape
    H, N = RWKV_HEADS, RWKV_HEAD_DIM
    f32 = jnp.float32
    pf = p.astype(f32)
    pf = pf + (token_shift(pf) - pf) * mu.astype(f32)
    r, k, v, wl, al, gl = jnp.split(pf, RWKV_SPLITS, axis=-1)
    w = -jax.nn.softplus(-(w0 + jnp.tanh(wl) @ w2)) - 0.5
    a = jax.nn.sigmoid(a0 + al @ a2)
    g = jax.nn.sigmoid(gl) @ g2
    heads = lambda z: z.reshape(B, T, H, N)
    kk = heads(k * k_k)
    kk = kk / jnp.maximum(jnp.sqrt(jnp.sum(kk * kk, axis=-1, keepdims=True)), 1e-12)
    k = k * (1.0 + (a - 1.0) * k_a)
    r, k, v, a = heads(r), heads(k), heads(v), heads(a)
    decay = jnp.exp(-jnp.exp(heads(w)))

    def step(S, inp):
        d_t, k_t, v_t, kk_t, a_t, r_t = inp
        sa = -jnp.einsum('bhij,bhj->bhi', S, kk_t)
        S = (S * d_t[:, :, None, :] + sa[..., None] * (kk_t * a_t)[:, :, None, :]
             + v_t[..., None] * k_t[:, :, None, :])
        return S, jnp.einsum('bhij,bhj->bhi', S, r_t)

    xs = tuple(jnp.moveaxis(z, 1, 0) for z in (decay, k, v, kk, a, r))
    _, y = lax.scan(step, jnp.zeros((B, H, N, N), f32), xs)
    y = jnp.moveaxis(y, 0, 1)
    mean = jnp.mean(y, axis=-1, keepdims=True)
    var = jnp.mean(jnp.square(y - mean), axis=-1, keepdims=True)
    y = (y - mean) * lax.rsqrt(var + RWKV_GN_EPS) * gn_g.reshape(H, N) + gn_b.reshape(H, N)
    y = y + jnp.sum(r * k * r_k, axis=-1, keepdims=True) * v
    return (y.reshape(B, T, RWKV_WIDTH) * g).astype(p.dtype)


def nsa_mix(q, kv, gates, qk_g, cmp_pos, cmp_w1, cmp_w2):
    B, T, _ = q.shape
    G, R, dh = NSA_KV_HEADS, NSA_GROUP, NSA_HEAD_DIM
    pos = jnp.arange(T, dtype=jnp.int32)
    scale = dh ** -0.5
    q = q.reshape(B, T, G, R, dh).transpose(0, 2, 3, 1, 4)
    q = rope_partial(rms_norm(q, qk_g[0]), pos)
    k_c, v_c, k_s, v_s, k_w, v_w = kv.reshape(B, T, 6, G, dh).transpose(2, 0, 3, 1, 4)
    k_s = rope_partial(rms_norm(k_s, qk_g[2]), pos)
    k_w = rope_partial(rms_norm(k_w, qk_g[3]), pos)

    n_chunk = T // CMP_STRIDE
    n_cmp = n_chunk - 1

    def compress(z, i):
        ch = z.reshape(B, G, n_chunk, CMP_STRIDE, dh)
        blk = jnp.concatenate([ch[:, :, :-1], ch[:, :, 1:]], axis=3) + cmp_pos[i]
        hid = jax.nn.gelu(blk.reshape(B, G, n_cmp, CMP_BLOCK * dh) @ cmp_w1[i])
        return hid @ cmp_w2[i]

    cmp_end = jnp.arange(n_cmp, dtype=jnp.int32) * CMP_STRIDE + CMP_BLOCK - 1
    k_cmp = rope_partial(rms_norm(compress(k_c, 0), qk_g[1]), cmp_end)
    v_cmp = compress(v_c, 1)

    n_sel = T // SEL_BLOCK
    topk = min(SEL_TOPK, n_sel)
    k_sb = k_s.reshape(B, G, n_sel, SEL_BLOCK, dh)
    v_sb = v_s.reshape(B, G, n_sel, SEL_BLOCK, dh)
    ci = jnp.arange(n_cmp)[:, None] * CMP_STRIDE
    sj = jnp.arange(n_sel)[None, :] * SEL_BLOCK
    overlap = ((ci <= sj + SEL_BLOCK - 1) & (ci + CMP_BLOCK - 1 >= sj)).astype(jnp.float32)
    bi = jnp.arange(B)[:, None, None, None]
    gi = jnp.arange(G)[None, :, None, None]
    sel_j = jnp.arange(n_sel, dtype=jnp.int32)

    k_wp = jnp.pad(k_w, ((0, 0), (0, 0), (WINDOW, 0), (0, 0)))
    v_wp = jnp.pad(v_w, ((0, 0), (0, 0), (WINDOW, 0), (0, 0)))

    def block_fn(args):
        qb, q0 = args
        t = q0 + jnp.arange(Q_BLOCK, dtype=jnp.int32)
        s = jnp.einsum('bgrqd,bgnd->bgrqn', qb, k_cmp) * scale
        p_c = masked_softmax(s, cmp_end[None, :] <= t[:, None])
        o_c = jnp.einsum('bgrqn,bgnd->bgrqd', p_c.astype(v_cmp.dtype), v_cmp)
        imp = jnp.einsum('bgqn,ns->bgqs', jnp.sum(p_c, axis=2), overlap)
        cur = (t // SEL_BLOCK)[:, None]
        forced = (sel_j[None, :] == 0) | (sel_j[None, :] == cur) | (sel_j[None, :] == cur - 1)
        valid = sel_j[None, :] * SEL_BLOCK <= t[:, None]
        imp = jnp.where(forced & valid, FORCE_SCORE, jnp.where(valid, imp, -1.0))
        _, sel = lax.top_k(imp, topk)
        ks = k_sb[bi, gi, sel].reshape(B, G, Q_BLOCK, topk * SEL_BLOCK, dh)
        vs = v_sb[bi, gi, sel].reshape(B, G, Q_BLOCK, topk * SEL_BLOCK, dh)
        kpos = (sel[..., None] * SEL_BLOCK + jnp.arange(SEL_BLOCK, dtype=jnp.int32)).reshape(B, G, Q_BLOCK, topk * SEL_BLOCK)
        s = jnp.einsum('bgrqd,bgqkd->bgrqk', qb, ks) * scale
        p_s = masked_softmax(s, (kpos <= t[:, None])[:, :, None])
        o_s = jnp.einsum('bgrqk,bgqkd->bgrqd', p_s.astype(vs.dtype), vs)
        kw = lax.dynamic_slice_in_dim(k_wp, q0, WINDOW + Q_BLOCK, axis=2)
        vw = lax.dynamic_slice_in_dim(v_wp, q0, WINDOW + Q_BLOCK, axis=2)
        spos = q0 - WINDOW + jnp.arange(WINDOW + Q_BLOCK, dtype=jnp.int32)
        wmask = (spos[None, :] <= t[:, None]) & (spos[None, :] > t[:, None] - WINDOW) & (spos[None, :] >= 0)
        s = jnp.einsum('bgrqd,bgkd->bgrqk', qb, kw) * scale
        p_w = masked_softmax(s, wmask)
        o_w = jnp.einsum('bgrqk,bgkd->bgrqd', p_w.astype(vw.dtype), vw)
        return o_c, o_s, o_w

    n_qb = T // Q_BLOCK
    qbs = q.reshape(B, G, R, n_qb, Q_BLOCK, dh).transpose(3, 0, 1, 2, 4, 5)
    starts = jnp.arange(n_qb, dtype=jnp.int32) * Q_BLOCK
    o_c, o_s, o_w = lax.map(block_fn, (qbs, starts))
    to_bthd = lambda o: o.transpose(1, 0, 4, 2, 3, 5).reshape(B, T, NSA_Q_HEADS, dh)
    gt = jax.nn.sigmoid(gates.reshape(B, T, NSA_Q_HEADS, 3))
    out = gt[..., 0:1] * to_bthd(o_c) + gt[..., 1:2] * to_bthd(o_s) + gt[..., 2:3] * to_bthd(o_w)
    return out.reshape(B, T, NSA_WIDTH).astype(q.dtype)


def moe_ffn(h, router_w, router_b, w_gate, w_up, w_down):
    B, T, D = h.shape
    N = B * T
    NK = N * TOP_K
    hf = h.reshape(N, D)
    score = jax.nn.sigmoid((hf @ router_w).astype(jnp.float32))
    sel_score = score + router_b.astype(jnp.float32)
    grp = sel_score.reshape(N, N_GROUPS, EXPERTS_PER_GROUP)
    grp_score = jnp.sum(lax.top_k(grp, TOP_K)[0], axis=-1)
    g_star = jnp.argmax(grp_score, axis=-1).astype(jnp.int32)
    in_grp = jnp.take_along_axis(grp, g_star[:, None, None], axis=1)[:, 0]
    _, loc = lax.top_k(in_grp, TOP_K)
    expert = g_star[:, None] * EXPERTS_PER_GROUP + loc.astype(jnp.int32)
    wts = jnp.take_along_axis(score, expert, axis=1)
    wts = wts / jnp.sum(wts, axis=-1, keepdims=True)

    e_flat = expert.reshape(-1)
    tok = jnp.repeat(jnp.arange(N, dtype=jnp.int32), TOP_K)
    order = jnp.argsort(e_flat)
    e_s, tok_s, w_s = e_flat[order], tok[order], wts.reshape(-1)[order]
    counts = jnp.zeros((N_EXPERTS,), jnp.int32).at[e_flat].add(1)
    seg_start = jnp.cumsum(counts) - counts
    padded = (counts + MOE_BLOCK - 1) // MOE_BLOCK * MOE_BLOCK
    pad_end = jnp.cumsum(padded)
    pad_start = pad_end - padded
    dest = pad_start[e_s] + jnp.arange(NK, dtype=jnp.int32) - seg_start[e_s]
    n_blk = -(-NK // MOE_BLOCK) + N_EXPERTS
    P = n_blk * MOE_BLOCK
    slot_tok = jnp.full((P,), N, jnp.int32).at[dest].set(tok_s)
    slot_w = jnp.zeros((P,), h.dtype).at[dest].set(w_s.astype(h.dtype))
    blk_start = jnp.arange(n_blk, dtype=jnp.int32) * MOE_BLOCK
    blk_expert = jnp.clip(jnp.searchsorted(pad_end, blk_start, side='right'), 0, N_EXPERTS - 1)
    hp = jnp.concatenate([hf, jnp.zeros((1, D), h.dtype)], axis=0)
    xb = hp[slot_tok].reshape(n_blk, MOE_BLOCK, D)

    def expert_block(args):
        xe, e = args
        return (jax.nn.silu(xe @ w_gate[e]) * (xe @ w_up[e])) @ w_down[e]

    yb = lax.map(expert_block, (xb, blk_expert)).reshape(P, D)
    y = jnp.zeros((N + 1, D), h.dtype).at[slot_tok].add(yb * slot_w[:, None])
    return y[:N].reshape(B, T, D)


def setup_inputs(seed: int = 0) -> dict:
    key = jax.random.key(seed)
    ks = jax.random.split(key, 32)
    f32 = jnp.float32
    D, L = D_MODEL, DEPTH
    nrm = lambda k, shape, s: jax.random.normal(k, shape, f32) * s
    return {
        "x": nrm(ks[0], (BATCH, SEQ, D), 1.0),
        "c": nrm(ks[1], (BATCH, D), 1.0),
        "w_ada": nrm(ks[2], (L, D, 6 * D), 0.5 * D ** -0.5),
        "b_ada": nrm(ks[3], (L, 6 * D), 0.02),
        "norm_g": 1.0 + nrm(ks[4], (L, 2, D), 0.02),
        "w_in": nrm(ks[5], (L, D, IN_COLS), D ** -0.5),
        "b_in": nrm(ks[6], (L, IN_COLS), 0.02),
        "rwkv_mu": jax.random.uniform(ks[7], (L, RWKV_COLS), f32, 0.0, 1.0),
        "rwkv_w0": jax.random.uniform(ks[8], (L, RWKV_WIDTH), f32, -6.0, -1.0),
        "rwkv_w2": nrm(ks[9], (L, DECAY_LORA, RWKV_WIDTH), DECAY_LORA ** -0.5),
        "rwkv_a0": nrm(ks[10], (L, RWKV_WIDTH), 0.1),
        "rwkv_a2": nrm(ks[11], (L, ICLR_LORA, RWKV_WIDTH), 0.5 * ICLR_LORA ** -0.5),
        "rwkv_g2": nrm(ks[12], (L, GATE_LORA, RWKV_WIDTH), GATE_LORA ** -0.5),
        "rwkv_k_k": 0.85 + nrm(ks[13], (L, RWKV_WIDTH), 0.02),
        "rwkv_k_a": 1.0 + nrm(ks[14], (L, RWKV_WIDTH), 0.02),
        "rwkv_r_k": nrm(ks[15], (L, RWKV_HEADS, RWKV_HEAD_DIM), 0.1),
        "rwkv_gn_g": 1.0 + nrm(ks[16], (L, RWKV_WIDTH), 0.02),
        "rwkv_gn_b": nrm(ks[17], (L, RWKV_WIDTH), 0.02),
        "qk_norm_g": 1.0 + nrm(ks[18], (L, 4, NSA_HEAD_DIM), 0.02),
        "cmp_pos": nrm(ks[19], (L, 2, CMP_BLOCK, NSA_HEAD_DIM), 0.1),
        "cmp_w1": nrm(ks[20], (L, 2, CMP_BLOCK * NSA_HEAD_DIM, CMP_HIDDEN), (CMP_BLOCK * NSA_HEAD_DIM) ** -0.5),
        "cmp_w2": nrm(ks[21], (L, 2, CMP_HIDDEN, NSA_HEAD_DIM), CMP_HIDDEN ** -0.5),
        "w_up_rwkv": nrm(ks[22], (L, RWKV_WIDTH, D), RWKV_WIDTH ** -0.5),
        "w_up_nsa": nrm(ks[23], (L, NSA_WIDTH, D), NSA_WIDTH ** -0.5),
        "w_out": nrm(ks[24], (L, D, D), D ** -0.5),
        "router_w": nrm(ks[25], (D, N_EXPERTS), D ** -0.5),
        "router_b": nrm(ks[26], (N_EXPERTS,), 0.01),
        "exp_w_gate": nrm(ks[27], (L, N_EXPERTS, D, D_EXPERT), D ** -0.5),
        "exp_w_up": nrm(ks[28], (L, N_EXPERTS, D, D_EXPERT), D ** -0.5),
        "exp_w_down": nrm(ks[29], (L, N_EXPERTS, D_EXPERT, D), D_EXPERT ** -0.5),
    }


def reference(x, c, w_ada, b_ada, norm_g, w_in, b_in, rwkv_mu, rwkv_w0, rwkv_w2, rwkv_a0, rwkv_a2,
              rwkv_g2, rwkv_k_k, rwkv_k_a, rwkv_r_k, rwkv_gn_g, rwkv_gn_b, qk_norm_g, cmp_pos, cmp_w1,
              cmp_w2, w_up_rwkv, w_up_nsa, w_out, router_w, router_b, exp_w_gate, exp_w_up, exp_w_down):
    for l in range(DEPTH):
        mod = jax.nn.silu(c) @ w_ada[l] + b_ada[l]
        sh1, sc1, gate_mix, sh2, sc2, gate_ffn = jnp.split(mod[:, None, :], 6, axis=-1)
        h = rms_norm(x, norm_g[l, 0]) * (1.0 + sc1) + sh1
        p = h @ w_in[l] + b_in[l]
        p_rwkv, p_q, p_kv, p_gate, p_merge = jnp.split(p, IN_SPLITS, axis=-1)
        y_a = rwkv7_mix(p_rwkv, rwkv_mu[l], rwkv_w0[l], rwkv_w2[l], rwkv_a0[l], rwkv_a2[l], rwkv_g2[l],
                        rwkv_k_k[l], rwkv_k_a[l], rwkv_r_k[l], rwkv_gn_g[l], rwkv_gn_b[l])
        y_b = nsa_mix(p_q, p_kv, p_gate, qk_norm_g[l], cmp_pos[l], cmp_w1[l], cmp_w2[l])
        m_a, m_b = jnp.split(jax.nn.sigmoid(p_merge), 2, axis=-1)
        mix = m_a * (y_a @ w_up_rwkv[l]) + m_b * (y_b @ w_up_nsa[l])
        x = x + gate_mix * (mix @ w_out[l])
        h2 = rms_norm(x, norm_g[l, 1]) * (1.0 + sc2) + sh2
        x = x + gate_ffn * moe_ffn(h2, router_w, router_b, exp_w_gate[l], exp_w_up[l], exp_w_down[l])
    return x
```

```python
import numpy as np
import concourse.bass as bass
import concourse.mybir as mybir
from contextlib import ExitStack

F32 = mybir.dt.float32
BF16 = mybir.dt.bfloat16
I32 = mybir.dt.int32
U32 = mybir.dt.uint32
AF = mybir.ActivationFunctionType
ALU = mybir.AluOpType
AX = mybir.AxisListType


class Buf:
    __slots__ = ("name", "w", "r", "dsem", "dcnt", "t", "excl")

    def __init__(self, name, t=None):
        self.name = name
        self.w = None
        self.r = []
        self.dsem = None
        self.dcnt = 0
        self.t = t
        self.excl = False


class Prog:
    ENG = ("pe", "dve", "act", "pool", "sp")

    def __init__(self, nc, es: ExitStack):
        self.nc = nc
        self.es = es
        self.eng = {"pe": nc.tensor, "dve": nc.vector, "act": nc.scalar,
                    "pool": nc.gpsimd, "sp": nc.sync}
        self.sem = {}
        self.cnt = {}
        for e in self.ENG:
            self.sem[e] = es.enter_context(nc.semaphore("s_" + e))
            self.cnt[e] = 0
        self.waited = {e: {} for e in self.ENG}
        self.out_tokens = []
        self.nsem = 5
        self.ninst = 0

    def sb(self, name, shape, dt=F32):
        t = self.es.enter_context(self.nc.sbuf_tensor(name, list(shape), dt))
        return t

    def ps(self, name, shape, dt=F32):
        t = self.es.enter_context(self.nc.psum_tensor(name, list(shape), dt))
        return t

    def buf(self, name, t=None):
        return Buf(name, t)

    def _wait(self, e, deps):
        w = self.waited[e]
        best = {}
        for d in deps:
            if d is None:
                continue
            sem, val, pe = d
            if e == "pe" and pe == "pe":
                continue
            k = id(sem)
            if w.get(k, 0) >= val:
                continue
            if k not in best or best[k][1] < val:
                best[k] = (sem, val)
        for k, (sem, val) in best.items():
            self.eng[e].wait_ge(sem, val)
            w[k] = val
            self.ninst += 1

    def op(self, e, build, reads=(), writes=()):
        deps = []
        for b in reads:
            deps.append(b.w)
            if b.excl:
                deps.extend(r for r in b.r if r[2] != e)
        for b in writes:
            deps.append(b.w)
            deps.extend(b.r)
        self._wait(e, deps)
        ins = build(self.eng[e])
        self.cnt[e] += 1
        ins.then_inc(self.sem[e], 1)
        tok = (self.sem[e], self.cnt[e], e)
        self.ninst += 1
        for b in reads:
            b.r.append(tok)
            if len(b.r) > 64:
                b.r = self._compact(b.r)
        for b in writes:
            b.w = tok
            b.r = []
        return tok

    @staticmethod
    def _compact(rs):
        best = {}
        for sem, val, e in rs:
            k = id(sem)
            if k not in best or best[k][1] < val:
                best[k] = (sem, val, e)
        return list(best.values())

    def dma(self, q, out, in_, sbuf_buf, reads=(), writes=(), is_output=False, **kw):
        deps = []
        for b in reads:
            deps.append(b.w)
        for b in writes:
            deps.append(b.w)
            deps.extend(b.r)
        self._wait(q, deps)
        b0 = sbuf_buf
        if b0.dsem is None:
            b0.dsem = self.es.enter_context(self.nc.semaphore("d_" + b0.name))
            self.nsem += 1
        ins = self.eng[q].dma_start(out=out, in_=in_, **kw)
        b0.dcnt += 16
        ins.then_inc(b0.dsem, 16)
        tok = (b0.dsem, b0.dcnt, "dma")
        self.ninst += 1
        for b in reads:
            b.r.append(tok)
        for b in writes:
            b.w = tok
            b.r = []
        if is_output:
            self.out_tokens.append(tok)
        return tok

    def finish(self):
        self._wait("sp", self.out_tokens)
        deps = [(self.sem[e], self.cnt[e], e) for e in self.ENG if e != "sp" and self.cnt[e] > 0]
        self._wait("sp", deps)


class View:
    __slots__ = ("tile", "ap")

    def __init__(self, tile, ap):
        self.tile = tile
        self.ap = ap

    def __getitem__(self, idx):
        return View(self.tile, self.ap[idx])

    @property
    def t(self):
        return self.ap

    def v(self, ap):
        return View(self.tile, ap)


class Tile:
    def __init__(self, P, name, shape, dt=F32, psum=False):
        self.P = P
        self.name = name
        self.t = (P.ps if psum else P.sb)(name, shape, dt)
        self.buf = Buf(name)
        self.buf.excl = bool(psum)
        self.shape = list(shape)

    def __getitem__(self, idx):
        return View(self, self.t[idx])

    def v(self, ap):
        return View(self, ap)


def _bufs(views):
    out = []
    for v in views:
        if isinstance(v, View):
            if v.tile.buf not in out:
                out.append(v.tile.buf)
        elif isinstance(v, Tile):
            if v.buf not in out:
                out.append(v.buf)
    return out


def _ap(v):
    return v.ap if isinstance(v, View) else v


class Prog2(Prog):
    def tile(self, name, shape, dt=F32, psum=False):
        return Tile(self, name, shape, dt, psum)

    def gop(self, e, fn, outs, ins):
        return self.op(e, fn, reads=_bufs(ins), writes=_bufs(outs))

    def act(self, out, in_, func, bias=0.0, scale=1.0, accum_out=None, e="act"):
        ins = [in_, bias, scale]
        outs = [out] + ([accum_out] if accum_out is not None else [])
        kw = {}
        if accum_out is not None:
            kw["accum_out"] = _ap(accum_out)
        return self.gop(e, lambda E: E.activation(out=_ap(out), in_=_ap(in_), func=func,
                                                  bias=_ap(bias), scale=_ap(scale), **kw), outs, ins)

    def tt(self, out, a, b, op, e="dve"):
        return self.gop(e, lambda E: E.tensor_tensor(out=_ap(out), in0=_ap(a), in1=_ap(b), op=op), [out], [a, b])

    def ts(self, out, a, s1, s2, op0, op1=None, accum_out=None, e="dve"):
        kw = {}
        if op1 is not None:
            kw["op1"] = op1
        outs = [out]
        if accum_out is not None:
            kw["accum_out"] = _ap(accum_out)
            outs.append(accum_out)
        return self.gop(e, lambda E: E.tensor_scalar(out=_ap(out), in0=_ap(a), scalar1=_ap(s1),
                                                     scalar2=_ap(s2) if s2 is not None else None,
                                                     op0=op0, **kw), outs, [a, s1, s2])

    def stt(self, out, in0, scalar, in1, op0, op1, accum_out=None, e="dve"):
        kw = {}
        outs = [out]
        if accum_out is not None:
            kw["accum_out"] = _ap(accum_out)
            outs.append(accum_out)
        return self.gop(e, lambda E: E.scalar_tensor_tensor(out=_ap(out), in0=_ap(in0), scalar=_ap(scalar),
                                                            in1=_ap(in1), op0=op0, op1=op1, **kw),
                        outs, [in0, scalar, in1])

    def copy(self, out, in_, e="dve"):
        if e == "act":
            return self.gop(e, lambda E: E.copy(out=_ap(out), in_=_ap(in_)), [out], [in_])
        return self.gop(e, lambda E: E.tensor_copy(out=_ap(out), in_=_ap(in_)), [out], [in_])

    def memset(self, out, val, e="dve"):
        return self.gop(e, lambda E: E.memset(_ap(out), val), [out], [])

    def red(self, out, in_, op, axis=AX.X, e="dve"):
        return self.gop(e, lambda E: E.tensor_reduce(out=_ap(out), in_=_ap(in_), axis=axis, op=op), [out], [in_])

    def recip(self, out, in_):
        return self.gop("dve", lambda E: E.reciprocal(out=_ap(out), in_=_ap(in_)), [out], [in_])

    def mm(self, out, lhsT, rhs, start=True, stop=True):
        return self.gop("pe", lambda E: E.matmul(_ap(out), _ap(lhsT), _ap(rhs), start=start, stop=stop),
                        [out], [lhsT, rhs])

    def tr(self, out, in_, ident):
        return self.gop("pe", lambda E: E.transpose(_ap(out), _ap(in_), _ap(ident)), [out], [in_, ident])

    def load(self, q, view, dram_ap, **kw):
        return self.dma(q, view.ap, dram_ap, view.tile.buf, writes=[view.tile.buf], **kw)

    def store(self, q, dram_ap, view, is_output=True, **kw):
        return self.dma(q, dram_ap, view.ap, view.tile.buf, reads=[view.tile.buf], is_output=is_output, **kw)


from concourse.bass_utils import run_bass_kernel_spmd

D = 1024
T = 8192
NB = 2
IN_COLS = 5144
NCORES = 8
_CACHE = {}


def _new_nc():
    return bass.Bass("TRN2", target_bir_lowering=False)


def build_k0():
    nc = _new_nc()
    NCOL = 1536
    cT = nc.dram_tensor("cT", [128, 8, 2], F32, kind="ExternalInput").ap()
    w = nc.dram_tensor("w", [1024, NCOL], F32, kind="ExternalInput").ap()
    b = nc.dram_tensor("b", [1, NCOL], F32, kind="ExternalInput").ap()
    y = nc.dram_tensor("y", [2, NCOL], F32, kind="ExternalOutput").ap()
    with ExitStack() as es:
        P = Prog2(nc, es)
        ct = P.tile("ct", [128, 8, 2])
        cs = P.tile("cs", [128, 8, 2])
        wt = P.tile("wt", [128, 8, NCOL])
        bt = P.tile("bt", [2, NCOL])
        yt = P.tile("yt", [2, NCOL])
        P.load("sp", ct[:], cT[:, :, :])
        P.load("act", wt[:], w.rearrange("(kc kp) n -> kp kc n", kp=128))
        P.load("sp", bt[:], b.partition_broadcast(2))
        P.act(cs[:], ct[:], AF.Silu)
        for n in range(3):
            ps = P.tile(f"ps{n}", [2, 512], psum=True)
            for kc in range(8):
                P.mm(ps[:], cs[:, kc, :], wt[:, kc, n * 512:(n + 1) * 512], start=(kc == 0), stop=(kc == 7))
            P.tt(yt[:, n * 512:(n + 1) * 512], ps[:], bt[:, n * 512:(n + 1) * 512], ALU.add)
        P.store("sp", y[:, :], yt[:])
        P.finish()
    return nc


def run_k0(c, w_ada, b_ada):
    if "k0" not in _CACHE:
        _CACHE["k0"] = build_k0()
    nc = _CACHE["k0"]
    L = w_ada.shape[0]
    wcat = np.concatenate([w_ada[l] for l in range(L)], axis=1)
    bcat = np.concatenate([b_ada[l] for l in range(L)], axis=0)[None]
    cT = np.ascontiguousarray(c.T.reshape(8, 128, 2).transpose(1, 0, 2))
    maps = []
    for i in range(NCORES):
        sl = slice(i * 1536, (i + 1) * 1536)
        maps.append({"cT": cT, "w": np.ascontiguousarray(wcat[:, sl]), "b": np.ascontiguousarray(bcat[:, sl])})
    res = run_bass_kernel_spmd(nc, maps, core_ids=list(range(NCORES)))
    y = np.concatenate([r["y"] for r in res.results], axis=1)
    return y.reshape(2, L, 6144).transpose(1, 0, 2)


def build_k1():
    nc = _new_nc()
    NT = 16
    x = nc.dram_tensor("x", [NT * 128, D], F32, kind="ExternalInput").ap()
    g = nc.dram_tensor("g", [1, D], F32, kind="ExternalInput").ap()
    sc = nc.dram_tensor("sc", [1, D], F32, kind="ExternalInput").ap()
    sh = nc.dram_tensor("sh", [1, D], F32, kind="ExternalInput").ap()
    w = nc.dram_tensor("w", [D, IN_COLS], F32, kind="ExternalInput").ap()
    b = nc.dram_tensor("b", [1, IN_COLS], F32, kind="ExternalInput").ap()
    ident = nc.dram_tensor("ident", [128, 128], F32, kind="ExternalInput").ap()
    p = nc.dram_tensor("p", [NT * 128, IN_COLS], F32, kind="ExternalOutput").ap()
    with ExitStack() as es:
        P = Prog2(nc, es)
        idt = P.tile("idt", [128, 128])
        G = P.tile("G", [128, D]); SC = P.tile("SC", [128, D]); SH = P.tile("SH", [128, D])
        B = P.tile("B", [128, IN_COLS])
        hT = [P.tile(f"hT{i}", [128, 8, 128]) for i in range(NT)]
        P.load("sp", idt[:], ident[:, :])
        P.load("sp", G[:], g.partition_broadcast(128))
        P.load("act", SC[:], sc.partition_broadcast(128))
        P.load("sp", SH[:], sh.partition_broadcast(128))
        P.load("act", B[:], b.partition_broadcast(128))
        P.stt(G[:], SC[:], 1.0, G[:], ALU.add, ALU.mult)
        xt = [P.tile(f"xt{i}", [128, D]) for i in range(2)]
        junk = P.tile("junk", [128, D])
        st = [P.tile(f"st{i}", [128, 2]) for i in range(2)]
        pT = [P.tile(f"pT{i}", [128, 4, 128], psum=True) for i in range(2)]
        for ti in range(NT):
            X = xt[ti % 2]; S = st[ti % 2]
            P.load("sp" if ti % 2 == 0 else "act", X[:], x[ti * 128:(ti + 1) * 128, :])
            P.act(junk[:], X[:], AF.Square, accum_out=S[:, 0:1])
            P.ts(S[:, 1:2], S[:, 0:1], 1.0 / D, 1e-6, ALU.mult, ALU.add)
            P.act(S[:, 1:2], S[:, 1:2], AF.Sqrt)
            P.recip(S[:, 1:2], S[:, 1:2])
            P.stt(X[:], X[:], S[:, 1:2], G[:], ALU.mult, ALU.mult)
            P.tt(X[:], X[:], SH[:], ALU.add)
            for half in range(2):
                pt = pT[half]
                for j in range(4):
                    kc = half * 4 + j
                    P.tr(pt[:, j, :], X[:, kc * 128:(kc + 1) * 128], idt[:])
                P.copy(hT[ti][:, half * 4:(half + 1) * 4, :], pt[:], e="act" if half else "dve")
        NCH = (IN_COLS + 511) // 512
        wt = [P.tile(f"wt{i}", [128, 8, 512]) for i in range(2)]
        po = [P.tile(f"po{i}", [128, 512], psum=True) for i in range(4)]
        ot = [P.tile(f"ot{i}", [128, 512]) for i in range(4)]
        wv = w.rearrange("(kc kp) n -> kp kc n", kp=128)
        k = 0
        for ci in range(NCH):
            c0 = ci * 512
            cw = min(512, IN_COLS - c0)
            W = wt[ci % 2]
            P.load("sp" if ci % 2 == 0 else "act", W[:, :, :cw], wv[:, :, c0:c0 + cw])
            for ti in range(NT):
                ps = po[k % 4]; o = ot[k % 4]
                for kc in range(8):
                    P.mm(ps[:, :cw], hT[ti][:, kc, :], W[:, kc, :cw], start=(kc == 0), stop=(kc == 7))
                P.tt(o[:, :cw], ps[:, :cw], B[:, c0:c0 + cw], ALU.add, e="dve")
                P.store("sp" if k % 2 == 0 else "act", p[ti * 128:(ti + 1) * 128, c0:c0 + cw], o[:, :cw])
                k += 1
        P.finish()
    return nc


def run_k1(xfull, g, sc, sh, w, b):
    if "k1" not in _CACHE:
        _CACHE["k1"] = build_k1()
    nc = _CACHE["k1"]
    ident = np.eye(128, dtype=np.float32)
    maps = []
    for i in range(NCORES):
        bi, tq = divmod(i, 4)
        maps.append({"x": np.ascontiguousarray(xfull[bi, tq * 2048:(tq + 1) * 2048]),
                     "g": g[None].copy(), "sc": sc[bi][None].copy(), "sh": sh[bi][None].copy(),
                     "w": w, "b": b[None].copy(), "ident": ident})
    res = run_bass_kernel_spmd(nc, maps, core_ids=list(range(NCORES)))
    out = np.empty((NB, T, IN_COLS), np.float32)
    for i in range(NCORES):
        bi, tq = divmod(i, 4)
        out[bi, tq * 2048:(tq + 1) * 2048] = res.results[i]["p"]
    return out


def _bc(tile, ap, axis, shape):
    return tile.v(ap.unsqueeze(axis).to_broadcast(list(shape)))


def k2_consts():
    C = 64
    tri_incl = np.triu(np.ones((C, C), np.float32))
    tri_strict = np.triu(np.ones((C, C), np.float32), 1)
    m = np.concatenate([tri_strict, tri_incl], 1)
    mask128 = np.concatenate([m, m], 0)
    sel0 = np.concatenate([np.eye(64, dtype=np.float32), np.zeros((64, 64), np.float32)], 1)
    shift = np.concatenate([np.zeros((64, 64), np.float32), np.eye(64, dtype=np.float32)], 1)
    return {"ident": np.eye(128, dtype=np.float32), "mask128": mask128,
            "trils": np.ascontiguousarray(tri_strict.T), "tri": tri_incl,
            "ones64": np.ones((64, 64), np.float32), "sel0": sel0, "shift": shift}


def build_k2(TT=T, stop=0, ustop=99):
    nc = _new_nc()
    NG = TT // 256
    dr = lambda n, s, kind="ExternalInput": nc.dram_tensor(n, s, F32, kind=kind).ap()
    pp = dr("pp", [TT + 1, 640]); mu = dr("mu", [1, 640]); vec = dr("vec", [8, 128])
    w2 = dr("w2", [64, 128]); a2 = dr("a2", [64, 128]); g2 = dr("g2", [128, 128])
    ident = dr("ident", [128, 128]); mask128 = dr("mask128", [128, 128]); trils = dr("trils", [64, 64])
    tri = dr("tri", [64, 64]); ones64 = dr("ones64", [64, 64]); sel0 = dr("sel0", [64, 128]); shift = dr("shift", [64, 128])
    ya = dr("ya", [TT, 128], "ExternalOutput")
    with ExitStack() as es:
        P = Prog2(nc, es)
        IDT = P.tile("IDT", [128, 128]); MASK = P.tile("MASK", [128, 128]); TRILS = P.tile("TRILS", [64, 64])
        TRI = P.tile("TRI", [64, 64]); ONES = P.tile("ONES", [64, 64]); SEL0 = P.tile("SEL0", [64, 128]); SHIFT = P.tile("SHIFT", [64, 128])
        W2 = P.tile("W2", [64, 128]); A2 = P.tile("A2", [64, 128]); G2 = P.tile("G2", [128, 128])
        MU = P.tile("MU", [64, 640]); VEC = P.tile("VEC", [64, 8, 128])
        qs = ["sp", "act"]
        for i, (tl, d) in enumerate([(IDT, ident), (MASK, mask128), (TRILS, trils), (TRI, tri), (ONES, ones64),
                                     (SEL0, sel0), (SHIFT, shift), (W2, w2), (A2, a2), (G2, g2)]):
            P.load(qs[i % 2], tl[:], d[:, :])
        P.load("sp", MU[:], mu.partition_broadcast(64))
        for r in range(7):
            P.load(qs[r % 2], VEC[:, r, :], vec[r:r + 1, :].partition_broadcast(64))
        I64 = IDT[0:64, 0:64]
        S4 = [64, 4, 128]
        vb = lambda r: _bc(VEC, VEC.t[:, r, :], 1, S4)
        W0b, A0b, KKb, KAb, RKb, GNGb, GNBb = [vb(r) for r in range(7)]
        banks = [P.tile(f"bank{i}", [128, 512], psum=True) for i in range(8)]
        pk = [0]

        def pbank():
            b = banks[pk[0] % 4]
            pk[0] += 1
            return b

        def t4(name, n=2):
            return [P.tile(f"{name}{i}", S4) for i in range(n)]

        CURt = [P.tile(f"CUR{i}", [64, 4, 640]) for i in range(2)]
        PRVt = [P.tile(f"PRV{i}", [64, 4, 640]) for i in range(2)]
        TWt = t4("TW"); SGt = t4("SG"); LTt = [P.tile(f"LT{i}", [64, 4, 2, 64]) for i in range(2)]
        GTtt = [P.tile(f"GTt{i}", [128, 4, 64]) for i in range(2)]
        LDt = t4("LD"); At = t4("A"); Ggt = t4("Gg"); KKt = t4("KK"); SQt = t4("SQ"); T1t = t4("T1"); KMt = t4("KM"); Bvt = t4("Bv")
        SSt = [P.tile(f"SS{i}", [64, 8]) for i in range(2)]; RKt = [P.tile(f"RK{i}", [64, 8]) for i in range(2)]
        Lst = t4("Ls"); ELt = t4("EL"); ENLt = t4("ENL"); TMPt = t4("TMP"); TMP2t = t4("TMP2")
        DCFt = [P.tile(f"DCF{i}", [64, 8]) for i in range(2)]
        A_t = t4("A_"); BTt = t4("BT"); KTt = t4("KT"); RTt = t4("RT"); BBt = t4("BB"); KBt = t4("KB")
        FMBKt = [P.tile(f"FMBK{i}", [64, 8, 128]) for i in range(2)]
        FMARt = [P.tile(f"FMAR{i}", [64, 8, 128]) for i in range(2)]
        BKSt = [P.tile(f"BKS{i}", [128, 4, 128]) for i in range(2)]
        VVt = [P.tile(f"VV{i}", [128, 4, 128]) for i in range(2)]
        YBt = t4("YB"); YCt = t4("YC"); MNt = [P.tile(f"MN{i}", [64, 8]) for i in range(2)]; VRt = [P.tile(f"VR{i}", [64, 8]) for i in range(2)]
        BIGt = [P.tile(f"BIG{u}", [128, 128]) for u in range(8)]
        Xt = [[P.tile(f"X{u}_{i}", [64, 64]) for i in range(2)] for u in range(8)]
        XTt = [[P.tile(f"XT{u}_{i}", [64, 64]) for i in range(2)] for u in range(8)]
        Pmt = [[P.tile(f"Pm{u}_{i}", [64, 64]) for i in range(2)] for u in range(8)]
        W2st = [P.tile(f"W2s{u}", [64, 64]) for u in range(8)]
        Aht = [P.tile(f"Ah{u}", [64, 64]) for u in range(8)]
        RhTt = [P.tile(f"RhT{u}", [64, 64]) for u in range(8)]
        GTt_ = [P.tile(f"GT{u}", [64, 64]) for u in range(8)]
        Ht = [[P.tile(f"H{h}_{i}", [64, 64]) for i in range(2)] for h in range(2)]
        hk = [0, 0]
        for h in range(2):
            P.memset(Ht[h][0][:], 0.0)
        fl = lambda tl: tl.v(tl.t[:, :, :].rearrange("p a b -> p (a b)"))
        v8 = lambda tl: tl.v(tl.t[:, :, :].rearrange("p a (h b) -> p (a h) b", b=64))
        b8 = lambda tl: _bc(tl, tl.t[:, :], 2, [64, 8, 64])
        ppv = lambda lo: pp[lo:lo + 256, :].rearrange("(c t) n -> t c n", t=64)
        NEG = -float(np.exp(-0.5))

        class _Stop(Exception):
            pass

        def early(n, view):
            if stop == n:
                P.store("sp", ya[0:256, :].rearrange("(c t) n -> t c n", t=64), view)
                raise _Stop()

        for g in range(NG):
          try:
              k = g % 2
              CUR = CURt[k]; PRV = PRVt[k]
              P.load("sp", CUR[:], ppv(1 + g * 256))
              P.load("act", PRV[:], ppv(g * 256))
              P.tt(PRV[:], PRV[:], CUR[:], ALU.subtract)
              P.tt(PRV[:], PRV[:], _bc(MU, MU.t[:, :], 1, [64, 4, 640]), ALU.mult)
              P.tt(CUR[:], CUR[:], PRV[:], ALU.add, e="pool")
              early(1, CUR[:, :, 0:128])
              Rr = CUR[:, :, 0:128]; Kr = CUR[:, :, 128:256]; Vr = CUR[:, :, 256:384]
              TW = TWt[k]; SG = SGt[k]; LT = LTt[k]; GTt = GTtt[k]
              P.act(TW[:, :, 0:64], CUR[:, :, 384:448], AF.Tanh)
              P.act(SG[:], CUR[:, :, 512:640], AF.Sigmoid)
              b1 = pbank(); b1v = b1.v(b1.t[0:64, :].rearrange("p (c w t) -> p c w t", c=4, w=2))
              for c in range(4):
                  P.tr(b1.v(b1v.ap[:, c, 0, :]), TW[:, c, 0:64], I64)
                  P.tr(b1.v(b1v.ap[:, c, 1, :]), CUR[:, c, 448:512], I64)
              P.copy(LT[:], b1v)
              b2 = pbank(); b2v = b2.v(b2.t[:, 0:256].rearrange("p (c t) -> p c t", c=4))
              for c in range(4):
                  P.tr(b2.v(b2v.ap[:, c, :]), SG[:, c, :], I64)
              P.copy(GTt[:], b2v, e="act")
              bw = pbank(); bwv = bw.v(bw.t[0:64, :].rearrange("p (c n) -> p c n", c=4))
              for c in range(4):
                  P.mm(bw.v(bwv.ap[:, c, :]), LT[:, c, 0, :], W2[:])
              LD = LDt[k]
              P.tt(LD[:], bwv, W0b, ALU.add)
              ba = pbank(); bav = ba.v(ba.t[0:64, :].rearrange("p (c n) -> p c n", c=4))
              for c in range(4):
                  P.mm(ba.v(bav.ap[:, c, :]), LT[:, c, 1, :], A2[:])
              A = At[k]
              P.tt(A[:], bav, A0b, ALU.add)
              bg = pbank(); bgv = bg.v(bg.t[0:64, :].rearrange("p (c n) -> p c n", c=4))
              for c in range(4):
                  P.mm(bg.v(bgv.ap[:, c, :]), GTt[:, c, :], G2[:])
              Gg = Ggt[k]
              P.copy(Gg[:], bgv, e="act")
              P.act(LD[:], LD[:], AF.Sigmoid)
              P.act(A[:], A[:], AF.Sigmoid)
              P.ts(LD[:], LD[:], NEG, None, ALU.mult, e="pool")
              early(2, LD[:])
              KK = KKt[k]; SQ = SQt[k]; SS = SSt[k]; T1 = T1t[k]; KM = KMt[k]; Bv = Bvt[k]; RK = RKt[k]
              P.tt(KK[:], Kr, KKb, ALU.mult)
              P.tt(SQ[:], KK[:], KK[:], ALU.mult)
              P.red(SS[:], v8(SQ), ALU.add)
              P.act(SS[:], SS[:], AF.Sqrt)
              P.ts(SS[:], SS[:], 1e-12, None, ALU.max)
              P.recip(SS[:], SS[:])
              P.tt(v8(KK), v8(KK), b8(SS), ALU.mult)
              P.stt(T1[:], A[:], -1.0, KAb, ALU.add, ALU.mult)
              P.stt(KM[:], T1[:], 1.0, Kr, ALU.add, ALU.mult)
              P.tt(Bv[:], KK[:], A[:], ALU.mult)
              P.tt(SQ[:], Rr, KM[:], ALU.mult)
              P.tt(SQ[:], SQ[:], RKb, ALU.mult)
              P.red(RK[:], v8(SQ), ALU.add)
              early(3, KM[:])
              Ls = Lst[k]; EL = ELt[k]; ENL = ENLt[k]; TMP = TMPt[k]; TMP2 = TMP2t[k]; DCF = DCFt[k]
              bL = pbank(); bLv = bL.v(bL.t[0:64, :].rearrange("p (c n) -> p c n", c=4))
              P.mm(bL[0:64, :], TRI[:], fl(LD))
              P.copy(Ls[:], bLv)
              bC = pbank(); bCv = bC.v(bC.t[0:64, :].rearrange("p (c n) -> p c n", c=4))
              P.mm(bC[0:64, :], ONES[:], fl(LD))
              P.tt(TMP2[:], bCv, Ls[:], ALU.subtract)
              bD = pbank()
              for u in range(8):
                  c, h = divmod(u, 2)
                  P.mm(bD[0:64, u:u + 1], LD[:, c, h * 64:(h + 1) * 64], ONES[:, 0:1])
              P.act(DCF[:], bD[0:64, 0:8], AF.Exp)
              P.act(EL[:], Ls[:], AF.Exp)
              P.act(ENL[:], Ls[:], AF.Exp, scale=-1.0)
              P.tt(TMP[:], Ls[:], LD[:], ALU.subtract)
              P.act(TMP[:], TMP[:], AF.Exp)
              P.act(TMP2[:], TMP2[:], AF.Exp)
              A_ = A_t[k]; BT = BTt[k]; KT = KTt[k]; RT = RTt[k]; BB = BBt[k]; KB = KBt[k]
              P.stt(A_[:], KK[:], -1.0, TMP[:], ALU.mult, ALU.mult)
              P.tt(BT[:], Bv[:], ENL[:], ALU.mult)
              P.tt(KT[:], KM[:], ENL[:], ALU.mult, e="pool")
              P.tt(RT[:], Rr, EL[:], ALU.mult)
              P.tt(BB[:], Bv[:], TMP2[:], ALU.mult, e="pool")
              P.tt(KB[:], KM[:], TMP2[:], ALU.mult)
              early(4, KB[:])
              FMBK = FMBKt[k]; FMAR = FMARt[k]; BKS = BKSt[k]; VV = VVt[k]
              for half in range(2):
                  pb = pbank(); pbv = pb.v(pb.t[0:64, :].rearrange("p (u n) -> p u n", u=4))
                  pa_ = pbank(); pav = pa_.v(pa_.t[0:64, :].rearrange("p (u n) -> p u n", u=4))
                  for uu in range(4):
                      u = half * 4 + uu
                      c, h = divmod(u, 2)
                      hs = slice(h * 64, (h + 1) * 64)
                      P.tr(pb.v(pbv.ap[:, uu, 0:64]), BT[:, c, hs], I64)
                      P.tr(pb.v(pbv.ap[:, uu, 64:128]), KT[:, c, hs], I64)
                      P.tr(pa_.v(pav.ap[:, uu, 0:64]), A_[:, c, hs], I64)
                      P.tr(pa_.v(pav.ap[:, uu, 64:128]), RT[:, c, hs], I64)
                  P.copy(FMBK[:, half * 4:(half + 1) * 4, :], pbv)
                  P.copy(FMAR[:, half * 4:(half + 1) * 4, :], pav, e="act")
              ps1 = pbank()
              P.mm(ps1[:], SEL0[:], fl(BB), start=True, stop=False)
              P.mm(ps1[:], SHIFT[:], fl(KB), start=False, stop=True)
              P.copy(fl(BKS), ps1[:])
              ps2 = pbank(); ps2v = ps2.v(ps2.t[:, :].rearrange("p (c n) -> p c n", c=4))
              P.mm(ps2v, SHIFT[:], Vr)
              P.copy(VV[64:128, :, :], ps2.v(ps2v.ap[64:128, :, :]), e="act")
              early(5, BKS[0:64, :, :])
              YB = YBt[k]

              def unit(u):
                  c, h = divmod(u, 2)
                  hs = slice(h * 64, (h + 1) * 64)
                  bank = banks[4 + u % 4]
                  co = (u // 4) * 256
                  BIG = BIGt[u]
                  fbk = FMBK[:, u, :]; far = FMAR[:, u, :]
                  P.mm(bank[:, co:co + 128], fbk, far)
                  P.mm(bank[0:64, co + 128:co + 192], FMAR[:, u, 0:64], FMBK[:, u, 0:64])
                  P.tt(BIG[:], bank[:, co:co + 128], MASK[:], ALU.mult)
                  XT = XTt[u][0]
                  P.tt(XT[:], bank[0:64, co + 128:co + 192], TRILS[:], ALU.mult)
                  X = BIG[0:64, 0:64]
                  Pm = Pmt[u][0]
                  P.tt(Pm[:], X, I64, ALU.add, e="pool")
                  yield
                  for i in range(5):
                      Xn = Xt[u][i % 2]; XTn = XTt[u][(i + 1) % 2]; Pn = Pmt[u][(i + 1) % 2]
                      if i < 4:
                          P.mm(bank[0:64, co:co + 64], XT[:], X)
                      P.mm(bank[0:64, co + 64:co + 128], X, XT[:])
                      if i < 4:
                          P.copy(Xn[:], bank[0:64, co:co + 64], e=EVAC_E)
                      P.copy(XTn[:], bank[0:64, co + 64:co + 128])
                      yield
                      P.mm(bank[0:64, co + 128:co + 192], XTn[:], Pm[:])
                      P.tt(Pn[:], bank[0:64, co + 128:co + 192], Pm[:], ALU.add)
                      X = Xn[:]; XT = XTn; Pm = Pn
                      yield
                  InvT = Pm
                  P.mm(bank[0:64, co:co + 64], BIG[64:128, 0:64], VV[64:128, c, hs])
                  W2s = W2st[u]
                  P.copy(W2s[:], bank[0:64, co:co + 64], e="act")
                  yield
                  P.mm(bank[0:64, co + 64:co + 128], InvT[:], A_[:, c, hs])
                  P.mm(bank[0:64, co + 128:co + 192], InvT[:], W2s[:])
                  Ah = Aht[u]
                  P.copy(Ah[:], bank[0:64, co + 64:co + 128])
                  P.copy(VV[0:64, c, hs], bank[0:64, co + 128:co + 192], e="act")
                  yield
                  P.mm(bank[0:64, co:co + 64], Ah[:], BIG[0:64, 64:128])
                  P.mm(bank[0:64, co + 64:co + 128], Ah[:], BKS[0:64, c, hs])
                  RhT = RhTt[u]; GT = GTt_[u]
                  P.tt(RhT[:], bank[0:64, co:co + 64], FMAR[:, u, 64:128], ALU.add)
                  P.stt(GT[:], I64, DCF[:, u:u + 1], bank[0:64, co + 64:co + 128], ALU.mult, ALU.add)
                  yield
                  Hc = Ht[h][hk[h] % 2]; Hn = Ht[h][(hk[h] + 1) % 2]
                  hk[h] += 1
                  P.mm(bank[0:64, co + 128:co + 192], RhT[:], Hc[:], start=True, stop=False)
                  P.mm(bank[0:64, co + 128:co + 192], BIG[:, 64:128], VV[:, c, hs], start=False, stop=True)
                  P.mm(bank[0:64, co + 192:co + 256], GT[:], Hc[:], start=True, stop=False)
                  P.mm(bank[0:64, co + 192:co + 256], BKS[:, c, hs], VV[:, c, hs], start=False, stop=True)
                  P.copy(YB[:, c, hs], bank[0:64, co + 128:co + 192], e="act")
                  P.copy(Hn[:], bank[0:64, co + 192:co + 256])
                  yield

              gens = [unit(u) for u in range(8)]
              alive = True
              rounds = 0
              while alive and rounds < ustop:
                  rounds += 1
                  alive = False
                  for gen in gens:
                      try:
                          next(gen)
                          alive = True
                      except StopIteration:
                          pass
              early(6, YB[:])
              YC = YCt[k]; MN = MNt[k]; VR = VRt[k]
              P.red(MN[:], v8(YB), ALU.add)
              P.ts(MN[:], MN[:], 1.0 / 64, None, ALU.mult)
              P.tt(v8(YC), v8(YB), b8(MN), ALU.subtract)
              P.tt(SQ[:], YC[:], YC[:], ALU.mult, e="pool")
              P.red(VR[:], v8(SQ), ALU.add)
              P.ts(VR[:], VR[:], 1.0 / 64, 64e-5, ALU.mult, ALU.add)
              P.act(VR[:], VR[:], AF.Sqrt)
              P.recip(VR[:], VR[:])
              P.tt(v8(YC), v8(YC), b8(VR), ALU.mult)
              P.tt(YC[:], YC[:], GNGb, ALU.mult)
              P.tt(YC[:], YC[:], GNBb, ALU.add)
              P.tt(SQ.v(SQ.t[:, :, :].rearrange("p a (h b) -> p a h b", b=64)),
                   CUR.v(CUR.t[:, :, 256:384].rearrange("p a (h b) -> p a h b", b=64)),
                   RK.v(RK.t[:, :].rearrange("p (a h) -> p a h", h=2).unsqueeze(3).to_broadcast([64, 4, 2, 64])), ALU.mult)
              P.tt(YC[:], YC[:], SQ[:], ALU.add)
              P.tt(YC[:], YC[:], Gg[:], ALU.mult)
              P.store("sp", ya[g * 256:(g + 1) * 256, :].rearrange("(c t) n -> t c n", t=64), YC[:])
          except _Stop:
            break
        P.finish()
        print("k2 ninst", P.ninst, "nsem", P.nsem)
    return nc


RW = 512
EVAC_E = "act"


def k2_inputs(p_rwkv_b, i, prm, l, consts):
    h0 = 2 * i
    cs = slice(h0 * 64, h0 * 64 + 128)
    cols = np.r_[np.arange(h0 * 64, h0 * 64 + 128), RW + np.arange(h0 * 64, h0 * 64 + 128),
                 2 * RW + np.arange(h0 * 64, h0 * 64 + 128), np.arange(3 * RW, 3 * RW + 256)]
    vec = np.zeros((8, 128), np.float32)
    for r, n in enumerate(["rwkv_w0", "rwkv_a0", "rwkv_k_k", "rwkv_k_a", "rwkv_r_k", "rwkv_gn_g", "rwkv_gn_b"]):
        vec[r] = prm[n][l].reshape(-1)[cs]
    d = {"pp": np.ascontiguousarray(p_rwkv_b[:, cols]), "mu": np.ascontiguousarray(prm["rwkv_mu"][l][cols][None]),
         "vec": vec, "w2": np.ascontiguousarray(prm["rwkv_w2"][l][:, cs]),
         "a2": np.ascontiguousarray(prm["rwkv_a2"][l][:, cs]), "g2": np.ascontiguousarray(prm["rwkv_g2"][l][:, cs])}
    d.update(consts)
    return d


def run_k2(p, prm, l, TT=T):
    key = ("k2", TT)
    if key not in _CACHE:
        _CACHE[key] = build_k2(TT)
    nc = _CACHE[key]
    consts = k2_consts()
    maps = []
    for ci in range(NCORES):
        b, i = divmod(ci, 4)
        pb = np.concatenate([np.zeros((1, 1792), np.float32), p[b, :TT, :1792]], 0)
        maps.append(k2_inputs(pb, i, prm, l, consts))
    res = run_bass_kernel_spmd(nc, maps, core_ids=list(range(NCORES)))
    out = np.empty((NB, TT, 512), np.float32)
    for ci in range(NCORES):
        b, i = divmod(ci, 4)
        out[b, :, i * 128:(i + 1) * 128] = res.results[ci]["ya"]
    return out


ROPE_THETA = 500000.0
NEGM = -30000.0


def k3_consts(TT):
    half = 8
    inv = np.power(ROPE_THETA, -np.arange(half, dtype=np.float32) * 2.0 / 16).astype(np.float32)

    def tab(pos):
        ang = pos.astype(np.float32)[:, None] * inv[None, :]
        c, s = np.cos(ang).astype(np.float32), np.sin(ang).astype(np.float32)
        return np.concatenate([c, c, -s, s], 1).astype(np.float32)
    rope_tok = tab(np.arange(TT))
    ncmp = TT // 16
    rope_cmp = tab(np.arange(ncmp) * 16 + 31)
    ql = np.arange(128)
    triu = (ql[:, None] <= ql[None, :]).astype(np.float32)
    tril = (ql[:, None] > ql[None, :]).astype(np.float32)
    cma = np.zeros((128, 17, 128), np.float32)
    cmt = np.zeros((128, 17, 128), np.float32)
    for v in range(17):
        c0 = 8 * v
        nl = np.arange(128)
        valid = (16 * (nl[None, :] - c0) + 31 <= ql[:, None])
        cma[:, v, :] = np.where(valid, 0.0, NEGM)
        cmt[:, v, :] = valid.T.astype(np.float32)
    ca = np.where(ql >= 64, 1e4, -1.0).astype(np.float32)[:, None]
    cb = np.where(ql < 64, 1e4, 0.0).astype(np.float32)[:, None]
    return {"rope_tok": rope_tok, "rope_cmp": rope_cmp, "triu": triu, "tril": tril, "cma": cma, "cmt": cmt,
            "cab": np.concatenate([ca, cb], 1), "ident": np.eye(128, dtype=np.float32)}


def build_k3(TT=T):
    nc = _new_nc()
    NTK = TT // 128
    NQB = NTK
    NCMP = TT // 16
    NCT = (NCMP + 127) // 128
    dr = lambda n, s, kind="ExternalInput": nc.dram_tensor(n, s, F32, kind=kind).ap()
    qin = dr("qin", [NQB * 128, 256]); gin = dr("gin", [NQB * 128, 6]); kvin = dr("kvin", [TT, 384])
    qkg = dr("qkg", [1, 256]); cpos = dr("cpos", [128, 32]); w1 = dr("w1", [128, 32, 256]); w2 = dr("w2", [128, 2, 2, 64])
    rope_tok = dr("rope_tok", [TT, 32]); rope_cmp = dr("rope_cmp", [NCMP, 32])
    triu = dr("triu", [128, 128]); tril = dr("tril", [128, 128]); cma = dr("cma", [128, 17, 128]); cmt = dr("cmt", [128, 17, 128])
    cab = dr("cab", [128, 2]); ident = dr("ident", [128, 128])
    yb = dr("yb", [NQB * 128, 128], "ExternalOutput")
    with ExitStack() as es:
        P = Prog2(nc, es)
        IDT = P.tile("IDT", [128, 128]); TRIU = P.tile("TRIU", [128, 128]); TRIL = P.tile("TRIL", [128, 128])
        CMA = P.tile("CMA", [128, 17, 128]); CMT = P.tile("CMT", [128, 17, 128]); CAB = P.tile("CAB", [128, 2])
        QKG = P.tile("QKG", [128, 4, 64]); CPOS = P.tile("CPOS", [128, 32]); W2 = P.tile("W2", [128, 2, 2, 64])
        for i, (tl, d) in enumerate([(IDT, ident), (TRIU, triu), (TRIL, tril), (CAB, cab), (CPOS, cpos)]):
            P.load(["sp", "act"][i % 2], tl[:], d[:, :])
        P.load("sp", CMA[:], cma[:, :, :]); P.load("act", CMT[:], cmt[:, :, :])
        P.load("sp", QKG.v(QKG.t[:, :, :].rearrange("p a b -> p (a b)")), qkg.partition_broadcast(128))
        P.load("act", W2.v(W2.t[:, :, :, :].rearrange("p a b c -> p (a b c)")), w2.rearrange("p a b c -> p (a b c)"))
        banks = [P.tile(f"bank{i}", [128, 512], psum=True) for i in range(8)]
        KT = P.tile("KT", [128, TT])
        CT = P.tile("CT", [128, TT + 16])
        VSW = P.tile("VSW", [128, NTK, 2, 65])
        P.memset(VSW[:, :, :, 64:65], 1.0)
        P.memset(CT[:, TT:TT + 16], 0.0)
        KVt = [P.tile(f"KV{i}", [128, 384]) for i in range(2)]
        RPt = [P.tile(f"RP{i}", [128, 32]) for i in range(2)]
        KNt = [P.tile(f"KN{i}", [128, 2, 64]) for i in range(2)]
        SQt = [P.tile(f"SQa{i}", [128, 2, 64]) for i in range(2)]
        STt = [P.tile(f"STa{i}", [128, 2]) for i in range(2)]
        R1t = [P.tile(f"R1a{i}", [128, 2, 16]) for i in range(2)]
        R2t = [P.tile(f"R2a{i}", [128, 2, 16]) for i in range(2)]

        def norm_rope(X3, G3, RP, KN, SQ, ST, R1, R2, nh, e2="pool"):
            P.tt(SQ[:], X3, X3, ALU.mult, e=e2)
            P.red(ST[:], SQ[:], ALU.add)
            P.ts(ST[:], ST[:], 1.0 / 64, 1e-6, ALU.mult, ALU.add)
            P.act(ST[:], ST[:], AF.Sqrt)
            P.recip(ST[:], ST[:])
            P.tt(KN[:], X3, _bc(ST, ST.t[:, :], 2, [128, nh, 64]), ALU.mult)
            P.tt(KN[:], KN[:], G3, ALU.mult, e=e2)
            cs = _bc(RP, RP.t[:, 0:16], 1, [128, nh, 16])
            P.tt(R1[:], KN[:, :, 0:16], cs, ALU.mult)
            P.tt(R2[:, :, 0:8], KN[:, :, 8:16], _bc(RP, RP.t[:, 16:24], 1, [128, nh, 8]), ALU.mult, e=e2)
            P.tt(R2[:, :, 8:16], KN[:, :, 0:8], _bc(RP, RP.t[:, 24:32], 1, [128, nh, 8]), ALU.mult, e=e2)
            P.tt(KN[:, :, 0:16], R1[:], R2[:], ALU.add)

        GK = QKG.v(QKG.t[:, 2:4, :])
        pk = 0
        for tg in range(NTK // 4):
            bk = banks[pk % 2]; bc_ = banks[2 + pk % 2]; pk += 1
            for j in range(4):
                ti = tg * 4 + j
                k = ti % 2
                KV = KVt[k]; RP = RPt[k]
                P.load("sp", KV[:], kvin[ti * 128:(ti + 1) * 128, :])
                P.load("act", RP[:], rope_tok[ti * 128:(ti + 1) * 128, :])
                X3 = KV.v(KV.t[:, 128:384].rearrange("p (a b) -> p a b", b=128)[:, :, 0:64])
                norm_rope(X3, GK, RP, KNt[k], SQt[k], STt[k], R1t[k], R2t[k], 2)
                P.tr(bk[:, j * 128:(j + 1) * 128], KNt[k].v(KNt[k].t[:, :, :].rearrange("p a b -> p (a b)")), IDT[:])
                P.tr(bc_[:, j * 128:(j + 1) * 128], KV[:, 0:128], IDT[:])
                V3 = KV.v(KV.t[:, 128:384].rearrange("p (a b) -> p a b", b=128)[:, :, 64:128])
                P.copy(VSW[:, ti, :, 0:64], V3, e="act")
            P.copy(KT[:, tg * 512:(tg + 1) * 512], bk[:])
            P.copy(CT[:, tg * 512:(tg + 1) * 512], bc_[:], e="act")
        HID = P.tile("HID", [128, 2, 2, NCT * 128])
        P.memset(HID[:], 0.0, e="pool")
        W1 = P.tile("W1", [128, 32, 128])
        BIA = P.tile("BIA", [128, 2, 2])
        Zt = P.tile("Zt", [128, 512]); Z2 = P.tile("Z2", [128, 512])
        NV = NCMP - 1
        for hc in range(2):
            P.load("sp" if hc == 0 else "act", W1[:], w1[:, :, hc * 128:(hc + 1) * 128])
            for i in range(2):
                ps_ = slice(i * 64, (i + 1) * 64)
                bb = banks[4]
                for tau in range(32):
                    P.mm(bb[:, 0:1], W1[ps_, tau, :], CPOS[ps_, tau:tau + 1], start=(tau == 0), stop=(tau == 31))
                P.copy(BIA[:, i, hc:hc + 1], bb[:, 0:1])
                for nt in range((NCMP + 511) // 512):
                    n0 = nt * 512
                    nn = min(512, NCMP - n0)
                    ba = banks[5 + nt % 2]
                    for tau in range(32):
                        rhs = CT.v(CT.t[ps_, n0 * 16 + tau: n0 * 16 + tau + (nn - 1) * 16 + 1: 16])
                        P.mm(ba[:, 0:nn], W1[ps_, tau, :], rhs, start=(tau == 0), stop=(tau == 31))
                    Z = Zt
                    P.act(Z[:, 0:nn], ba[:, 0:nn], AF.Identity, bias=BIA[:, i, hc:hc + 1])
                    P.tt(Z2[:, 0:nn], Z[:, 0:nn], Z[:, 0:nn], ALU.mult)
                    P.ts(Z2[:, 0:nn], Z2[:, 0:nn], 0.044715, 1.0, ALU.mult, ALU.add)
                    P.tt(Z2[:, 0:nn], Z2[:, 0:nn], Z[:, 0:nn], ALU.mult)
                    P.act(Z2[:, 0:nn], Z2[:, 0:nn], AF.Sigmoid, scale=1.5957691216057308)
                    P.tt(HID[:, i, hc, n0:n0 + nn], Z2[:, 0:nn], Z[:, 0:nn], ALU.mult)
        KCT = P.tile("KCT", [128, NCT * 128])
        VCMP = P.tile("VCMP", [128, NCT, 65])
        P.memset(VCMP[:, :, 64:65], 1.0)
        KC2 = P.tile("KC2", [128, 2, 64])
        KCR = P.tile("KCR", [128, 1, 64])
        GC = QKG.v(QKG.t[:, 1:2, :])
        for ct in range(NCT):
            bb = banks[4 + ct % 2]
            for i in range(2):
                for hc in range(2):
                    P.mm(bb[:, i * 64:(i + 1) * 64], HID[:, i, hc, ct * 128:(ct + 1) * 128], W2[:, i, hc, :],
                         start=(hc == 0), stop=(hc == 1))
            P.copy(VCMP[:, ct, 0:64], bb[:, 64:128], e="act")
            RP = RPt[ct % 2]
            nr = min(128, NCMP - ct * 128)
            P.load("sp", RP[0:nr, :], rope_cmp[ct * 128:ct * 128 + nr, :])
            k = ct % 2
            KN1 = KNt[k].v(KNt[k].t[:, 0:1, :])
            P.copy(KCR[:, 0, :], bb[:, 0:64])
            norm_rope(KCR[:], GC, RP, KN1,
                      SQt[k].v(SQt[k].t[:, 0:1, :]), STt[k].v(STt[k].t[:, 0:1]), R1t[k].v(R1t[k].t[:, 0:1, :]),
                      R2t[k].v(R2t[k].t[:, 0:1, :]), 1, e2="dve")
            P.copy(KC2[:, 0, :], KNt[k][:, 0, :])
            P.copy(KC2[:, 1, :], KNt[k][:, 0, :], e="act")
            b2 = banks[6 + ct % 2]
            P.tr(b2[:, 0:128], KC2.v(KC2.t[:, :, :].rearrange("p a b -> p (a b)")), IDT[:])
            P.copy(KCT[:, ct * 128:(ct + 1) * 128], b2[:, 0:128])
        Qt = [P.tile(f"Q{i}", [128, 4, 64]) for i in range(2)]
        Gt = [P.tile(f"G{i}", [128, 2, 3]) for i in range(2)]
        QNt = [P.tile(f"QN{i}", [128, 4, 64]) for i in range(2)]
        QDt = [P.tile(f"QD{i}", [128, 4, 2, 64]) for i in range(2)]
        QTt = [P.tile(f"QT{i}", [128, 4, 128]) for i in range(2)]
        SQq = [P.tile(f"SQq{i}", [128, 4, 64]) for i in range(2)]
        STq = [P.tile(f"STq{i}", [128, 4]) for i in range(2)]
        R1q = [P.tile(f"R1q{i}", [128, 4, 16]) for i in range(2)]
        R2q = [P.tile(f"R2q{i}", [128, 4, 16]) for i in range(2)]
        PCt = [P.tile(f"PC{i}", [128, 512]) for i in range(2)]
        RSt = [P.tile(f"RS{i}", [128, 4]) for i in range(2)]
        IMPP = P.tile("IMPP", [128, 516])
        IMPF = P.tile("IMPF", [128, 128]); IMPW = P.tile("IMPW", [128, 128])
        M8 = P.tile("M8", [128, 16])
        SEL = P.tile("SEL", [128, 128])
        PTt = [P.tile(f"PT{i}", [128, 2, 128]) for i in range(4)]
        OUTt = [P.tile(f"OUT{i}", [128, 2, 64]) for i in range(2)]
        RIt = [P.tile(f"RI{i}", [128, 3, 2]) for i in range(2)]
        OTt = [P.tile(f"OT{i}", [128, 2, 64]) for i in range(2)]
        GQ = QKG.v(QKG.t[:, 0:1, :].to_broadcast([128, 4, 64])) if False else _bc(QKG, QKG.t[:, 0, :], 1, [128, 4, 64])
        sb_i = [0]; mb_i = [0]; pt_i = [0]
        OB = [0, 1, 2]

        def attn_tile(first, last, ob, kT_ap, part, v1_ap, QT, mask_fn):
            sb = banks[sb_i[0] % 2]; sb_i[0] += 1
            PT = PTt[pt_i[0] % 4]; pt_i[0] += 1
            P.mm(sb[:, 0:256], kT_ap, QT.v(QT.t[part:part + 64, 0:2, :].rearrange("p a b -> p (a b)")))
            P.act(PT.v(PT.t[:, :, :].rearrange("p a b -> p (a b)")), sb[:, 0:256], AF.Exp)
            mask_fn(PT)
            for r in range(2):
                P.mm(banks[6 + r][:, ob * 65:(ob + 1) * 65], PT[:, r, :], v1_ap, start=first, stop=last)

        for jq in range(NQB):
            qb = jq
            k = jq % 2
            Q = Qt[k]; G = Gt[k]; RP = RPt[k]
            P.load("sp", Q.v(Q.t[:, :, :].rearrange("p a b -> p (a b)")), qin[jq * 128:(jq + 1) * 128, :])
            P.load("act", G.v(G.t[:, :, :].rearrange("p a b -> p (a b)")), gin[jq * 128:(jq + 1) * 128, :])
            P.load("sp", RP[:], rope_tok[qb * 128:(qb + 1) * 128, :])
            QN = QNt[k]; QD = QDt[k]; QT = QTt[k]
            norm_rope(Q[:], GQ, RP, QN, SQq[k], STq[k], R1q[k], R2q[k], 4)
            P.ts(QD[:, :, 0, :], QN[:], 0.125, None, ALU.mult)
            P.ts(QD[:, :, 1, :], QN[:], 0.125, None, ALU.mult, e="pool")
            P.act(G[:], G[:], AF.Sigmoid)
            bq = banks[4]
            for r in range(4):
                P.tr(bq[:, r * 128:(r + 1) * 128], QD.v(QD.t[:, r, :, :].rearrange("p a b -> p (a b)")), IDT[:])
            P.copy(QT.v(QT.t[:, :, :].rearrange("p a b -> p (a b)")), bq[:])
            nct = qb // 16 + 1
            ncols = nct * 128
            var = qb % 16
            P.memset(IMPP[:], 0.0, e="pool")
            RS = RSt[k]
            for r in range(4):
                sb = banks[2 + r % 2]
                P.mm(sb[:, 0:ncols], QT[0:64, r, :], KCT[0:64, 0:ncols])
                PC = PCt[r % 2]
                lo = ncols - 128
                if lo > 0:
                    P.copy(PC[:, 0:lo], sb[:, 0:lo])
                    if var == 0:
                        P.tt(PC[:, lo - 128:lo], PC[:, lo - 128:lo], CMA[:, 16, :], ALU.add)
                P.tt(PC[:, lo:ncols], sb[:, lo:ncols], CMA[:, var, :], ALU.add)
                P.act(PC[:, 0:ncols], PC[:, 0:ncols], AF.Exp, accum_out=RS[:, r:r + 1])
                P.ts(RS[:, r:r + 1], RS[:, r:r + 1], 1.1754944e-38, None, ALU.max)
                P.recip(RS[:, r:r + 1], RS[:, r:r + 1])
                P.stt(IMPP[:, 4:4 + ncols], PC[:, 0:ncols], RS[:, r:r + 1], IMPP[:, 4:4 + ncols], ALU.mult, ALU.add)
            P.red(IMPF[:], IMPP.v(IMPP.t[:, 4:516].rearrange("p (j f) -> p j f", f=4)), ALU.add)
            P.tt(IMPF[:], IMPF[:], IMPP.v(IMPP.t[:, 0:512].rearrange("p (j f) -> p j f", f=4)[:, :, 3]), ALU.add)
            if 2 * qb + 2 < 128:
                P.memset(IMPF[:, 2 * qb + 2:128], -1.0)
            if qb >= 1:
                P.ts(IMPF[:, 2 * qb - 1:2 * qb], IMPF[:, 2 * qb - 1:2 * qb], CAB[:, 1:2], None, ALU.max)
            if 2 * qb + 1 < 128:
                P.copy(IMPF[:, 2 * qb + 1:2 * qb + 2], CAB[:, 0:1])
            P.memset(IMPF[:, 2 * qb:2 * qb + 1], 1e4)
            P.memset(IMPF[:, 0:1], 1e4)
            P.gop("dve", lambda E: E.max(out=M8.t[:, 0:8], in_=IMPF.t[:, :]), [M8], [IMPF])
            P.gop("dve", lambda E: E.match_replace(out=IMPW.t[:, :], in_to_replace=M8.t[:, 0:8], in_values=IMPF.t[:, :],
                                                   imm_value=-2.0), [IMPW], [M8, IMPF])
            P.gop("dve", lambda E: E.max(out=M8.t[:, 8:16], in_=IMPW.t[:, :]), [M8], [IMPW])
            P.ts(SEL[:], IMPF[:], M8[:, 15:16], None, ALU.is_ge)
            for ct in range(nct):
                last = ct == nct - 1
                if last:
                    mf = lambda PT: P.tt(PT[:], PT[:], _bc(CMT, CMT.t[:, var, :], 1, [128, 2, 128]), ALU.mult)
                elif var == 0 and ct == nct - 2:
                    mf = lambda PT: P.tt(PT[:], PT[:], _bc(CMT, CMT.t[:, 16, :], 1, [128, 2, 128]), ALU.mult)
                else:
                    mf = lambda PT: None
                attn_tile(ct == 0, last, OB[0], KCT[0:64, ct * 128:(ct + 1) * 128], 0, VCMP[:, ct, :], QT, mf)
            kts = [kt for kt in range(qb - 4, qb + 1) if kt >= 0]
            for kt in kts:
                if kt == qb:
                    mf = lambda PT: P.tt(PT[:], PT[:], _bc(TRIU, TRIU.t[:, :], 1, [128, 2, 128]), ALU.mult)
                elif kt == qb - 4:
                    mf = lambda PT: P.tt(PT[:], PT[:], _bc(TRIL, TRIL.t[:, :], 1, [128, 2, 128]), ALU.mult)
                else:
                    mf = lambda PT: None
                attn_tile(kt == kts[0], kt == kts[-1], OB[2], KT[64:128, kt * 128:(kt + 1) * 128], 64, VSW[:, kt, 1, :], QT, mf)
            nj = 2 * (qb + 1)
            P.copy(CT.v(CT.t[:, 0:nj * 64].rearrange("p (j f) -> p j f", f=64)),
                   SEL.v(SEL.t[:, 0:nj].unsqueeze(2).to_broadcast([128, nj, 64])), e="pool")
            for kt in range(qb + 1):
                mbk = banks[2 + mb_i[0] % 2]; mb_i[0] += 1
                P.tr(mbk[:, 0:128], CT[:, kt * 128:(kt + 1) * 128], IDT[:])

                def mf(PT, mbk=mbk, kt=kt):
                    P.tt(PT[:], PT[:], _bc(mbk, mbk.t[:, 0:128], 1, [128, 2, 128]), ALU.mult)
                    if kt == qb:
                        P.tt(PT[:], PT[:], _bc(TRIU, TRIU.t[:, :], 1, [128, 2, 128]), ALU.mult, e="pool")
                attn_tile(kt == 0, kt == qb, OB[1], KT[0:64, kt * 128:(kt + 1) * 128], 0, VSW[:, kt, 0, :], QT, mf)
            RI = RIt[k]; OUT = OUTt[k]; OT = OTt[k]
            for bi in range(3):
                for r in range(2):
                    ob = banks[6 + r]
                    c0 = bi * 65
                    P.ts(RI[:, bi, r:r + 1], ob[:, c0 + 64:c0 + 65], 1.1754944e-38, None, ALU.max)
                    P.recip(RI[:, bi, r:r + 1], RI[:, bi, r:r + 1])
                    P.tt(RI[:, bi, r:r + 1], RI[:, bi, r:r + 1], G[:, r, bi:bi + 1], ALU.mult)
                    if bi == 0:
                        P.ts(OUT[:, r, :], ob[:, c0:c0 + 64], RI[:, bi, r:r + 1], None, ALU.mult)
                    else:
                        P.stt(OUT[:, r, :], ob[:, c0:c0 + 64], RI[:, bi, r:r + 1], OUT[:, r, :], ALU.mult, ALU.add)
            P.store("sp", yb[jq * 128:(jq + 1) * 128, :], OUT.v(OUT.t[:, :, :].rearrange("p a b -> p (a b)")))
        P.finish()
        print("k3 ninst", P.ninst, "nsem", P.nsem)
    return nc


def k3_inputs(p_b, g, hh, prm, l, TT):
    q = p_b[:TT, 1792 + g * 256: 1792 + (g + 1) * 256].reshape(TT, 4, 64)
    order = [2 * hh, 2 * hh + 1, 2 * (1 - hh), 2 * (1 - hh) + 1]
    q = q[:, order, :].reshape(TT, 256)
    kv = p_b[:TT, 2304:3072].reshape(TT, 6, 2, 64)[:, :, g, :].reshape(TT, 384)
    gt = p_b[:TT, 3072:3096].reshape(TT, 8, 3)[:, g * 4 + 2 * hh: g * 4 + 2 * hh + 2, :].reshape(TT, 6)
    cp = prm["cmp_pos"][l]
    cpos = np.concatenate([cp[0].T, cp[1].T], 0)
    w1 = prm["cmp_w1"][l].reshape(2, 32, 64, 256).transpose(0, 2, 1, 3).reshape(128, 32, 256)
    w2 = prm["cmp_w2"][l].reshape(2, 2, 128, 64).transpose(2, 0, 1, 3)
    d = {"qin": np.ascontiguousarray(q), "gin": np.ascontiguousarray(gt), "kvin": np.ascontiguousarray(kv),
         "qkg": np.ascontiguousarray(prm["qk_norm_g"][l].reshape(1, 256)), "cpos": np.ascontiguousarray(cpos),
         "w1": np.ascontiguousarray(w1), "w2": np.ascontiguousarray(w2)}
    d.update(k3_consts(TT))
    return d


def run_k3(p, prm, l, TT=T):
    key = ("k3", TT)
    if key not in _CACHE:
        _CACHE[key] = build_k3(TT)
    nc = _CACHE[key]
    maps = []
    for ci in range(NCORES):
        b, rem = divmod(ci, 4)
        g, hh = divmod(rem, 2)
        maps.append(k3_inputs(p[b], g, hh, prm, l, TT))
    res = run_bass_kernel_spmd(nc, maps, core_ids=list(range(NCORES)))
    out = np.empty((NB, TT, 512), np.float32)
    for ci in range(NCORES):
        b, rem = divmod(ci, 4)
        g, hh = divmod(rem, 2)
        h0 = g * 4 + 2 * hh
        out[b, :, h0 * 64:(h0 + 2) * 64] = res.results[ci]["yb"]
    return out


def build_k4a():
    nc = _new_nc()
    NT = 16
    dr = lambda n, s, kind="ExternalInput": nc.dram_tensor(n, s, F32, kind=kind).ap()
    x = dr("x", [NT * 128, D]); ya = dr("ya", [NT * 128, 512]); yb = dr("yb", [NT * 128, 512]); pm = dr("pm", [NT * 128, 2048])
    gm = dr("gm", [1, D]); wua = dr("wua", [512, D]); wub = dr("wub", [512, D]); wo = dr("wo", [D, D]); ident = dr("ident", [128, 128])
    x1 = dr("x1", [NT * 128, D], "ExternalOutput")
    with ExitStack() as es:
        P = Prog2(nc, es)
        IDT = P.tile("IDT", [128, 128]); GMB = P.tile("GMB", [128, D])
        WUA = P.tile("WUA", [128, 4, D]); WUB = P.tile("WUB", [128, 4, D]); WO = P.tile("WO", [128, 8, D])
        P.load("sp", IDT[:], ident[:, :]); P.load("act", GMB[:], gm.partition_broadcast(128))
        P.load("sp", WUA[:], wua.rearrange("(kc kp) n -> kp kc n", kp=128))
        P.load("act", WUB[:], wub.rearrange("(kc kp) n -> kp kc n", kp=128))
        P.load("sp", WO[:, 0:4, :], wo[0:512, :].rearrange("(kc kp) n -> kp kc n", kp=128))
        P.load("act", WO[:, 4:8, :], wo[512:1024, :].rearrange("(kc kp) n -> kp kc n", kp=128))
        banks = [P.tile(f"bank{i}", [128, 512], psum=True) for i in range(8)]
        bi = [0]

        def nb():
            b = banks[bi[0] % 8]; bi[0] += 1
            return b
        Xt = [P.tile(f"X{i}", [128, D]) for i in range(2)]
        YAt = [P.tile(f"YA{i}", [128, 512]) for i in range(2)]
        YBt = [P.tile(f"YB{i}", [128, 512]) for i in range(2)]
        PMt = [P.tile(f"PM{i}", [128, 2048]) for i in range(2)]
        YTt = [P.tile(f"YT{i}", [128, 8, 128]) for i in range(2)]
        MIXt = [P.tile(f"MIX{i}", [128, D]) for i in range(2)]
        TMPt = [P.tile(f"TMP{i}", [128, 512]) for i in range(2)]
        MTt = [P.tile(f"MT{i}", [128, 8, 128]) for i in range(2)]
        for ti in range(NT):
            k = ti % 2
            rs = slice(ti * 128, (ti + 1) * 128)
            X = Xt[k]; YA = YAt[k]; YB = YBt[k]; PM = PMt[k]; YT = YTt[k]; MIX = MIXt[k]; MT = MTt[k]
            P.load("sp", X[:], x[rs, :]); P.load("act", YA[:], ya[rs, :]); P.load("sp", YB[:], yb[rs, :]); P.load("act", PM[:], pm[rs, :])
            for j, Y in enumerate((YA, YB)):
                b = nb()
                for c in range(4):
                    P.tr(b[:, c * 128:(c + 1) * 128], Y[:, c * 128:(c + 1) * 128], IDT[:])
                P.copy(YT.v(YT.t[:, j * 4:(j + 1) * 4, :].rearrange("p a b -> p (a b)")), b[:], e="act" if j else "dve")
            P.act(PM[:], PM[:], AF.Sigmoid)
            for nh in range(2):
                cs = slice(nh * 512, (nh + 1) * 512)
                ba = nb(); bb = nb()
                for kc in range(4):
                    P.mm(ba[:], YT[:, kc, :], WUA[:, kc, cs], start=(kc == 0), stop=(kc == 3))
                for kc in range(4):
                    P.mm(bb[:], YT[:, 4 + kc, :], WUB[:, kc, cs], start=(kc == 0), stop=(kc == 3))
                TMP = TMPt[nh]
                P.tt(MIX[:, cs], ba[:], PM[:, nh * 512:(nh + 1) * 512], ALU.mult)
                P.tt(TMP[:], bb[:], PM[:, 1024 + nh * 512:1024 + (nh + 1) * 512], ALU.mult)
                P.tt(MIX[:, cs], MIX[:, cs], TMP[:], ALU.add, e="pool")
            for half in range(2):
                b = nb()
                for c in range(4):
                    kc = half * 4 + c
                    P.tr(b[:, c * 128:(c + 1) * 128], MIX[:, kc * 128:(kc + 1) * 128], IDT[:])
                P.copy(MT.v(MT.t[:, half * 4:(half + 1) * 4, :].rearrange("p a b -> p (a b)")), b[:], e="act" if half else "dve")
            for nh in range(2):
                cs = slice(nh * 512, (nh + 1) * 512)
                b = nb()
                for kc in range(8):
                    P.mm(b[:], MT[:, kc, :], WO[:, kc, cs], start=(kc == 0), stop=(kc == 7))
                TMP = TMPt[nh]
                P.tt(TMP[:], b[:], GMB[:, cs], ALU.mult)
                P.tt(X[:, cs], X[:, cs], TMP[:], ALU.add, e="pool")
            P.store("sp" if k else "act", x1[rs, :], X[:])
        P.finish()
        print("k4a ninst", P.ninst, "nsem", P.nsem)
    return nc


def run_k4a(xfull, ya, yb, p, gate_mix, prm, l):
    if "k4a" not in _CACHE:
        _CACHE["k4a"] = build_k4a()
    nc = _CACHE["k4a"]
    ident = np.eye(128, dtype=np.float32)
    maps = []
    for i in range(NCORES):
        b, tq = divmod(i, 4)
        ts_ = slice(tq * 2048, (tq + 1) * 2048)
        maps.append({"x": np.ascontiguousarray(xfull[b, ts_]), "ya": np.ascontiguousarray(ya[b, ts_]),
                     "yb": np.ascontiguousarray(yb[b, ts_]), "pm": np.ascontiguousarray(p[b, ts_, 3096:5144]),
                     "gm": gate_mix[b][None].copy(), "wua": prm["w_up_rwkv"][l], "wub": prm["w_up_nsa"][l],
                     "wo": prm["w_out"][l], "ident": ident})
    res = run_bass_kernel_spmd(nc, maps, core_ids=list(range(NCORES)))
    out = np.empty((NB, T, D), np.float32)
    for i in range(NCORES):
        b, tq = divmod(i, 4)
        out[b, tq * 2048:(tq + 1) * 2048] = res.results[i]["x1"]
    return out


def build_k4b():
    nc = _new_nc()
    NT = 16; NE = 16
    dr = lambda n, s, kind="ExternalInput": nc.dram_tensor(n, s, F32, kind=kind).ap()
    x1 = dr("x1", [NT * 128, D]); g = dr("g", [1, D]); sc = dr("sc", [1, D]); sh = dr("sh", [1, D]); gf = dr("gf", [1, D])
    rw = dr("rw", [D, 16]); rb = dr("rb", [1, 16]); ident = dr("ident", [128, 128])
    wg = dr("wg", [NE, D, 512]); wu = dr("wu", [NE, D, 512]); wd = dr("wd", [NE, 512, D])
    xo = dr("xo", [NT * 128, D], "ExternalOutput")
    with ExitStack() as es, nc.allow_low_precision("bf16 expert matmuls, fp32 accumulation"):
        P = Prog2(nc, es)
        IDT = P.tile("IDT", [128, 128]); G2S = P.tile("G2S", [128, D]); SC = P.tile("SC", [128, D]); SH = P.tile("SH", [128, D])
        GF = P.tile("GF", [128, D]); RW = P.tile("RW", [128, 8, 16]); RB = P.tile("RB", [128, 16])
        P.load("sp", IDT[:], ident[:, :]); P.load("act", G2S[:], g.partition_broadcast(128)); P.load("sp", SC[:], sc.partition_broadcast(128))
        P.load("act", SH[:], sh.partition_broadcast(128)); P.load("sp", GF[:], gf.partition_broadcast(128))
        P.load("act", RW[:], rw.rearrange("(kc kp) n -> kp kc n", kp=128)); P.load("sp", RB[:], rb.partition_broadcast(128))
        P.stt(G2S[:], SC[:], 1.0, G2S[:], ALU.add, ALU.mult)
        banks = [P.tile(f"bank{i}", [128, 512], psum=True) for i in range(8)]
        bi = [0]

        def nb():
            b = banks[bi[0] % 8]; bi[0] += 1
            return b
        ACC = [P.tile(f"ACC{t}", [128, D]) for t in range(8)]
        H2T = P.tile("H2T", [128, 8, 1024], BF16)
        CW = P.tile("CW", [128, 8, 16])
        STG = [P.tile(f"STG{i}", [128, 4096]) for i in range(2)]
        WB = [[P.tile(f"WB{m}_{p}", [128, 4096], BF16) for p in range(2)] for m in range(3)]
        H2t = [P.tile(f"H2{i}", [128, D]) for i in range(2)]
        H2Tf = [P.tile(f"H2Tf{i}", [128, 8, 128]) for i in range(2)]
        junk = P.tile("junk", [128, D])
        ST = [P.tile(f"ST{i}", [128, 2]) for i in range(2)]
        SCO = [P.tile(f"SCO{i}", [128, 16]) for i in range(2)]
        SS = [P.tile(f"SS{i}", [128, 4, 4]) for i in range(2)]
        PSm = [P.tile(f"PSm{i}", [128, 4, 6]) for i in range(2)]
        GS = [P.tile(f"GS{i}", [128, 4]) for i in range(2)]
        GMx = [P.tile(f"GMx{i}", [128, 2]) for i in range(2)]
        E2 = [P.tile(f"E2{i}", [128, 4, 4]) for i in range(2)]
        SIL = [P.tile(f"SIL{i}", [128, 512]) for i in range(2)]
        HID = [P.tile(f"HID{i}", [128, 4, 512], BF16) for i in range(2)]
        stg_i = [0]
        for tg in range(2):
            for t in range(8):
                ti = tg * 8 + t
                k = t % 2
                A = ACC[t]
                P.load("sp" if k else "act", A[:], x1[ti * 128:(ti + 1) * 128, :])
                S = ST[k]; H2 = H2t[k]; HF = H2Tf[k]
                P.act(junk[:], A[:], AF.Square, accum_out=S[:, 0:1])
                P.ts(S[:, 1:2], S[:, 0:1], 1.0 / D, 1e-6, ALU.mult, ALU.add)
                P.act(S[:, 1:2], S[:, 1:2], AF.Sqrt)
                P.recip(S[:, 1:2], S[:, 1:2])
                P.stt(H2[:], A[:], S[:, 1:2], G2S[:], ALU.mult, ALU.mult)
                P.tt(H2[:], H2[:], SH[:], ALU.add, e="pool")
                for half in range(2):
                    b = nb()
                    for c in range(4):
                        kc = half * 4 + c
                        P.tr(b[:, c * 128:(c + 1) * 128], H2[:, kc * 128:(kc + 1) * 128], IDT[:])
                    bv = b.v(b.t[:, :].rearrange("p (a b) -> p a b", a=4))
                    P.copy(HF[:, half * 4:(half + 1) * 4, :], bv)
                    P.copy(H2T[:, half * 4:(half + 1) * 4, t * 128:(t + 1) * 128], bv, e="act")
                b = nb()
                for kc in range(8):
                    P.mm(b[:, 0:16], HF[:, kc, :], RW[:, kc, :], start=(kc == 0), stop=(kc == 7))
                sco = SCO[k]; ss = SS[k]; psm = PSm[k]; gs = GS[k]; gmx = GMx[k]; e2 = E2[k]
                P.act(sco[:], b[:, 0:16], AF.Sigmoid)
                ssf = ss.v(ss.t[:, :, :].rearrange("p a b -> p (a b)"))
                P.tt(ssf, sco[:], RB[:], ALU.add)
                P.tt(psm[:, :, 0:3], ss[:, :, 0:3], ss[:, :, 1:4], ALU.add)
                P.tt(psm[:, :, 3:5], ss[:, :, 0:2], ss[:, :, 2:4], ALU.add)
                P.tt(psm[:, :, 5:6], ss[:, :, 0:1], ss[:, :, 3:4], ALU.add)
                P.red(gs[:], psm[:], ALU.max)
                P.red(gmx[:, 0:1], gs[:], ALU.max)
                P.ts(gs[:], gs[:], gmx[:, 0:1], None, ALU.is_ge)
                P.tt(psm[:, :, 0:3], ss[:, :, 0:3], ss[:, :, 1:4], ALU.min)
                P.tt(psm[:, :, 3:5], ss[:, :, 0:2], ss[:, :, 2:4], ALU.min)
                P.tt(psm[:, :, 5:6], ss[:, :, 0:1], ss[:, :, 3:4], ALU.min)
                P.red(e2[:, :, 0], psm[:], ALU.max)
                thr = E2[k].v(E2[k].t[:, :, 0:1].to_broadcast([128, 4, 4]))
                P.tt(psm[:, :, 0:4], ss[:], thr, ALU.is_ge)
                P.tt(psm[:, :, 0:4], psm[:, :, 0:4], _bc(gs, gs.t[:, :], 2, [128, 4, 4]), ALU.mult)
                scv = sco.v(sco.t[:, :].rearrange("p (a b) -> p a b", b=4))
                P.tt(e2[:], psm[:, :, 0:4], scv, ALU.mult)
                P.red(gmx[:, 1:2], E2[k].v(E2[k].t[:, :, :].rearrange("p a b -> p (a b)")), ALU.add)
                P.recip(gmx[:, 1:2], gmx[:, 1:2])
                P.ts(CW[:, t, :], E2[k].v(E2[k].t[:, :, :].rearrange("p a b -> p (a b)")), gmx[:, 1:2], None, ALU.mult)
            for e in range(NE):
                p_ = e % 2
                for m, (wsrc, pat) in enumerate(((wg, 8), (wu, 8), (wd, 4))):
                    S_ = STG[stg_i[0] % 2]; stg_i[0] += 1
                    sv = S_.v(S_.t[:, :].rearrange("p (kc n) -> p kc n", kc=pat))
                    P.load("sp" if m % 2 == 0 else "act", sv, wsrc[e].rearrange("(kc kp) n -> kp kc n", kp=128))
                    Wb = WB[m][p_]
                    if m < 2:
                        P.copy(Wb[:], S_[:], e="pool")
                    else:
                        P.tt(Wb.v(Wb.t[:, :].rearrange("p (kc n) -> p kc n", kc=4)), sv, _bc(GF, GF.t[:, :], 1, [128, 4, D]), ALU.mult, e="pool")
                WG = WB[0][p_].v(WB[0][p_].t[:, :].rearrange("p (kc n) -> p kc n", kc=8))
                WU = WB[1][p_].v(WB[1][p_].t[:, :].rearrange("p (kc n) -> p kc n", kc=8))
                WD = WB[2][p_].v(WB[2][p_].t[:, :].rearrange("p (kc n) -> p kc n", kc=4))
                for sub in range(2):
                    hid = HID[sub]
                    tsl = slice(sub * 512, (sub + 1) * 512)
                    for c in range(4):
                        bg = nb(); bu = nb()
                        for kc in range(8):
                            P.mm(bg[:], WG[:, kc, c * 128:(c + 1) * 128], H2T[:, kc, tsl], start=(kc == 0), stop=(kc == 7))
                        for kc in range(8):
                            P.mm(bu[:], WU[:, kc, c * 128:(c + 1) * 128], H2T[:, kc, tsl], start=(kc == 0), stop=(kc == 7))
                        sil = SIL[c % 2]
                        P.act(sil[:], bg[:], AF.Silu)
                        P.tt(hid[:, c, :], sil[:], bu[:], ALU.mult)
                    for t4 in range(4):
                        t = sub * 4 + t4
                        for nh in range(2):
                            cs = slice(nh * 512, (nh + 1) * 512)
                            bd = nb()
                            for c in range(4):
                                P.mm(bd[:], hid[:, c, t4 * 128:(t4 + 1) * 128], WD[:, c, cs], start=(c == 0), stop=(c == 3))
                            P.stt(ACC[t][:, cs], bd[:], CW[:, t, e:e + 1], ACC[t][:, cs], ALU.mult, ALU.add)
            for t in range(8):
                ti = tg * 8 + t
                P.store("sp" if t % 2 else "act", xo[ti * 128:(ti + 1) * 128, :], ACC[t][:])
        P.finish()
        print("k4b ninst", P.ninst, "nsem", P.nsem)
    return nc


def run_k4b(x1, g2, sc2, sh2, gate_ffn, prm, l):
    if "k4b" not in _CACHE:
        _CACHE["k4b"] = build_k4b()
    nc = _CACHE["k4b"]
    ident = np.eye(128, dtype=np.float32)
    maps = []
    for i in range(NCORES):
        b, tq = divmod(i, 4)
        ts_ = slice(tq * 2048, (tq + 1) * 2048)
        maps.append({"x1": np.ascontiguousarray(x1[b, ts_]), "g": g2[None].copy(), "sc": sc2[b][None].copy(),
                     "sh": sh2[b][None].copy(), "gf": gate_ffn[b][None].copy(), "rw": prm["router_w"],
                     "rb": prm["router_b"][None].copy(), "ident": ident,
                     "wg": prm["exp_w_gate"][l], "wu": prm["exp_w_up"][l], "wd": prm["exp_w_down"][l]})
    res = run_bass_kernel_spmd(nc, maps, core_ids=list(range(NCORES)))
    out = np.empty((NB, T, D), np.float32)
    for i in range(NCORES):
        b, tq = divmod(i, 4)
        out[b, tq * 2048:(tq + 1) * 2048] = res.results[i]["xo"]
    return out


def kernel(**inputs):
    prm = {k: np.asarray(v) for k, v in inputs.items()}
    x = prm["x"].astype(np.float32, copy=False)
    mod = run_k0(prm["c"], prm["w_ada"], prm["b_ada"])
    for l in range(2):
        sh1, sc1, gate_mix, sh2, sc2, gate_ffn = [np.ascontiguousarray(a) for a in np.split(mod[l], 6, axis=-1)]
        p = run_k1(x, prm["norm_g"][l, 0], sc1, sh1, prm["w_in"][l], prm["b_in"][l])
        ya = run_k2(p, prm, l)
        yb = run_k3(p, prm, l)
        x1 = run_k4a(x, ya, yb, p, gate_mix, prm, l)
        x = run_k4b(x1, prm["norm_g"][l, 1], sc2, sh2, gate_ffn, prm, l)
    return x
```

```python
import numpy as np
import concourse.bass as bass
import concourse.mybir as mybir
from contextlib import ExitStack

F32 = mybir.dt.float32
BF16 = mybir.dt.bfloat16
I32 = mybir.dt.int32
U32 = mybir.dt.uint32
AF = mybir.ActivationFunctionType
ALU = mybir.AluOpType
AX = mybir.AxisListType


class Buf:
    __slots__ = ("name", "w", "r", "dsem", "dcnt", "t", "excl")

    def __init__(self, name, t=None):
        self.name = name
        self.w = None
        self.r = []
        self.dsem = None
        self.dcnt = 0
        self.t = t
        self.excl = False


class Prog:
    ENG = ("pe", "dve", "act", "pool", "sp")

    def __init__(self, nc, es: ExitStack):
        self.nc = nc
        self.es = es
        self.eng = {"pe": nc.tensor, "dve": nc.vector, "act": nc.scalar,
                    "pool": nc.gpsimd, "sp": nc.sync}
        self.sem = {}
        self.cnt = {}
        for e in self.ENG:
            self.sem[e] = es.enter_context(nc.semaphore("s_" + e))
            self.cnt[e] = 0
        self.waited = {e: {} for e in self.ENG}
        self.out_tokens = []
        self.nsem = 5
        self.ninst = 0

    def sb(self, name, shape, dt=F32):
        t = self.es.enter_context(self.nc.sbuf_tensor(name, list(shape), dt))
        return t

    def ps(self, name, shape, dt=F32):
        t = self.es.enter_context(self.nc.psum_tensor(name, list(shape), dt))
        return t

    def buf(self, name, t=None):
        return Buf(name, t)

    def _wait(self, e, deps):
        w = self.waited[e]
        best = {}
        for d in deps:
            if d is None:
                continue
            sem, val, pe = d
            if e == "pe" and pe == "pe":
                continue
            k = id(sem)
            if w.get(k, 0) >= val:
                continue
            if k not in best or best[k][1] < val:
                best[k] = (sem, val)
        for k, (sem, val) in best.items():
            self.eng[e].wait_ge(sem, val)
            w[k] = val
            self.ninst += 1

    def op(self, e, build, reads=(), writes=()):
        deps = []
        for b in reads:
            deps.append(b.w)
            if b.excl:
                deps.extend(r for r in b.r if r[2] != e)
        for b in writes:
            deps.append(b.w)
            deps.extend(b.r)
        self._wait(e, deps)
        ins = build(self.eng[e])
        self.cnt[e] += 1
        ins.then_inc(self.sem[e], 1)
        tok = (self.sem[e], self.cnt[e], e)
        self.ninst += 1
        for b in reads:
            b.r.append(tok)
            if len(b.r) > 64:
                b.r = self._compact(b.r)
        for b in writes:
            b.w = tok
            b.r = []
        return tok

    @staticmethod
    def _compact(rs):
        best = {}
        for sem, val, e in rs:
            k = id(sem)
            if k not in best or best[k][1] < val:
                best[k] = (sem, val, e)
        return list(best.values())

    def dma(self, q, out, in_, sbuf_buf, reads=(), writes=(), is_output=False, **kw):
        deps = []
        for b in reads:
            deps.append(b.w)
        for b in writes:
            deps.append(b.w)
            deps.extend(b.r)
        self._wait(q, deps)
        b0 = sbuf_buf
        if b0.dsem is None:
            b0.dsem = self.es.enter_context(self.nc.semaphore("d_" + b0.name))
            self.nsem += 1
        ins = self.eng[q].dma_start(out=out, in_=in_, **kw)
        b0.dcnt += 16
        ins.then_inc(b0.dsem, 16)
        tok = (b0.dsem, b0.dcnt, "dma")
        self.ninst += 1
        for b in reads:
            b.r.append(tok)
        for b in writes:
            b.w = tok
            b.r = []
        if is_output:
            self.out_tokens.append(tok)
        return tok

    def finish(self):
        self._wait("sp", self.out_tokens)
        deps = [(self.sem[e], self.cnt[e], e) for e in self.ENG if e != "sp" and self.cnt[e] > 0]
        self._wait("sp", deps)


class View:
    __slots__ = ("tile", "ap")

    def __init__(self, tile, ap):
        self.tile = tile
        self.ap = ap

    def __getitem__(self, idx):
        return View(self.tile, self.ap[idx])

    @property
    def t(self):
        return self.ap

    def v(self, ap):
        return View(self.tile, ap)


class Tile:
    def __init__(self, P, name, shape, dt=F32, psum=False):
        self.P = P
        self.name = name
        self.t = (P.ps if psum else P.sb)(name, shape, dt)
        self.buf = Buf(name)
        self.buf.excl = bool(psum)
        self.shape = list(shape)

    def __getitem__(self, idx):
        return View(self, self.t[idx])

    def v(self, ap):
        return View(self, ap)


def _bufs(views):
    out = []
    for v in views:
        if isinstance(v, View):
            if v.tile.buf not in out:
                out.append(v.tile.buf)
        elif isinstance(v, Tile):
            if v.buf not in out:
                out.append(v.buf)
    return out


def _ap(v):
    return v.ap if isinstance(v, View) else v


class Prog2(Prog):
    def tile(self, name, shape, dt=F32, psum=False):
        return Tile(self, name, shape, dt, psum)

    def gop(self, e, fn, outs, ins):
        return self.op(e, fn, reads=_bufs(ins), writes=_bufs(outs))

    def act(self, out, in_, func, bias=0.0, scale=1.0, accum_out=None, e="act"):
        ins = [in_, bias, scale]
        outs = [out] + ([accum_out] if accum_out is not None else [])
        kw = {}
        if accum_out is not None:
            kw["accum_out"] = _ap(accum_out)
        return self.gop(e, lambda E: E.activation(out=_ap(out), in_=_ap(in_), func=func,
                                                  bias=_ap(bias), scale=_ap(scale), **kw), outs, ins)

    def tt(self, out, a, b, op, e="dve"):
        return self.gop(e, lambda E: E.tensor_tensor(out=_ap(out), in0=_ap(a), in1=_ap(b), op=op), [out], [a, b])

    def ts(self, out, a, s1, s2, op0, op1=None, accum_out=None, e="dve"):
        kw = {}
        if op1 is not None:
            kw["op1"] = op1
        outs = [out]
        if accum_out is not None:
            kw["accum_out"] = _ap(accum_out)
            outs.append(accum_out)
        return self.gop(e, lambda E: E.tensor_scalar(out=_ap(out), in0=_ap(a), scalar1=_ap(s1),
                                                     scalar2=_ap(s2) if s2 is not None else None,
                                                     op0=op0, **kw), outs, [a, s1, s2])

    def stt(self, out, in0, scalar, in1, op0, op1, accum_out=None, e="dve"):
        kw = {}
        outs = [out]
        if accum_out is not None:
            kw["accum_out"] = _ap(accum_out)
            outs.append(accum_out)
        return self.gop(e, lambda E: E.scalar_tensor_tensor(out=_ap(out), in0=_ap(in0), scalar=_ap(scalar),
                                                            in1=_ap(in1), op0=op0, op1=op1, **kw),
                        outs, [in0, scalar, in1])

    def copy(self, out, in_, e="dve"):
        if e == "act":
            return self.gop(e, lambda E: E.copy(out=_ap(out), in_=_ap(in_)), [out], [in_])
        return self.gop(e, lambda E: E.tensor_copy(out=_ap(out), in_=_ap(in_)), [out], [in_])

    def memset(self, out, val, e="dve"):
        return self.gop(e, lambda E: E.memset(_ap(out), val), [out], [])

    def red(self, out, in_, op, axis=AX.X, e="dve"):
        return self.gop(e, lambda E: E.tensor_reduce(out=_ap(out), in_=_ap(in_), axis=axis, op=op), [out], [in_])

    def recip(self, out, in_):
        return self.gop("dve", lambda E: E.reciprocal(out=_ap(out), in_=_ap(in_)), [out], [in_])

    def mm(self, out, lhsT, rhs, start=True, stop=True):
        return self.gop("pe", lambda E: E.matmul(_ap(out), _ap(lhsT), _ap(rhs), start=start, stop=stop),
                        [out], [lhsT, rhs])

    def tr(self, out, in_, ident):
        return self.gop("pe", lambda E: E.transpose(_ap(out), _ap(in_), _ap(ident)), [out], [in_, ident])

    def load(self, q, view, dram_ap, **kw):
        return self.dma(q, view.ap, dram_ap, view.tile.buf, writes=[view.tile.buf], **kw)

    def store(self, q, dram_ap, view, is_output=True, **kw):
        return self.dma(q, dram_ap, view.ap, view.tile.buf, reads=[view.tile.buf], is_output=is_output, **kw)


from concourse.bass_utils import run_bass_kernel_spmd

D = 1024
T = 8192
NB = 2
IN_COLS = 5144
NCORES = 8
_CACHE = {}


def _new_nc():
    return bass.Bass("TRN2", target_bir_lowering=False)


def build_k0():
    nc = _new_nc()
    NCOL = 1536
    cT = nc.dram_tensor("cT", [128, 8, 2], F32, kind="ExternalInput").ap()
    w = nc.dram_tensor("w", [1024, NCOL], F32, kind="ExternalInput").ap()
    b = nc.dram_tensor("b", [1, NCOL], F32, kind="ExternalInput").ap()
    y = nc.dram_tensor("y", [2, NCOL], F32, kind="ExternalOutput").ap()
    with ExitStack() as es:
        P = Prog2(nc, es)
        ct = P.tile("ct", [128, 8, 2])
        cs = P.tile("cs", [128, 8, 2])
        wt = P.tile("wt", [128, 8, NCOL])
        bt = P.tile("bt", [2, NCOL])
        yt = P.tile("yt", [2, NCOL])
        P.load("sp", ct[:], cT[:, :, :])
        P.load("act", wt[:], w.rearrange("(kc kp) n -> kp kc n", kp=128))
        P.load("sp", bt[:], b.partition_broadcast(2))
        P.act(cs[:], ct[:], AF.Silu)
        for n in range(3):
            ps = P.tile(f"ps{n}", [2, 512], psum=True)
            for kc in range(8):
                P.mm(ps[:], cs[:, kc, :], wt[:, kc, n * 512:(n + 1) * 512], start=(kc == 0), stop=(kc == 7))
            P.tt(yt[:, n * 512:(n + 1) * 512], ps[:], bt[:, n * 512:(n + 1) * 512], ALU.add)
        P.store("sp", y[:, :], yt[:])
        P.finish()
    return nc


def run_k0(c, w_ada, b_ada):
    if "k0" not in _CACHE:
        _CACHE["k0"] = build_k0()
    nc = _CACHE["k0"]
    L = w_ada.shape[0]
    wcat = np.concatenate([w_ada[l] for l in range(L)], axis=1)
    bcat = np.concatenate([b_ada[l] for l in range(L)], axis=0)[None]
    cT = np.ascontiguousarray(c.T.reshape(8, 128, 2).transpose(1, 0, 2))
    maps = []
    for i in range(NCORES):
        sl = slice(i * 1536, (i + 1) * 1536)
        maps.append({"cT": cT, "w": np.ascontiguousarray(wcat[:, sl]), "b": np.ascontiguousarray(bcat[:, sl])})
    res = run_bass_kernel_spmd(nc, maps, core_ids=list(range(NCORES)))
    y = np.concatenate([r["y"] for r in res.results], axis=1)
    return y.reshape(2, L, 6144).transpose(1, 0, 2)


def build_k1():
    nc = _new_nc()
    NT = 16
    x = nc.dram_tensor("x", [NT * 128, D], F32, kind="ExternalInput").ap()
    g = nc.dram_tensor("g", [1, D], F32, kind="ExternalInput").ap()
    sc = nc.dram_tensor("sc", [1, D], F32, kind="ExternalInput").ap()
    sh = nc.dram_tensor("sh", [1, D], F32, kind="ExternalInput").ap()
    w = nc.dram_tensor("w", [D, IN_COLS], F32, kind="ExternalInput").ap()
    b = nc.dram_tensor("b", [1, IN_COLS], F32, kind="ExternalInput").ap()
    ident = nc.dram_tensor("ident", [128, 128], F32, kind="ExternalInput").ap()
    p = nc.dram_tensor("p", [NT * 128, IN_COLS], F32, kind="ExternalOutput").ap()
    with ExitStack() as es:
        P = Prog2(nc, es)
        idt = P.tile("idt", [128, 128])
        G = P.tile("G", [128, D]); SC = P.tile("SC", [128, D]); SH = P.tile("SH", [128, D])
        B = P.tile("B", [128, IN_COLS])
        hT = [P.tile(f"hT{i}", [128, 8, 128]) for i in range(NT)]
        P.load("sp", idt[:], ident[:, :])
        P.load("sp", G[:], g.partition_broadcast(128))
        P.load("act", SC[:], sc.partition_broadcast(128))
        P.load("sp", SH[:], sh.partition_broadcast(128))
        P.load("act", B[:], b.partition_broadcast(128))
        P.stt(G[:], SC[:], 1.0, G[:], ALU.add, ALU.mult)
        xt = [P.tile(f"xt{i}", [128, D]) for i in range(2)]
        junk = P.tile("junk", [128, D])
        st = [P.tile(f"st{i}", [128, 2]) for i in range(2)]
        pT = [P.tile(f"pT{i}", [128, 4, 128], psum=True) for i in range(2)]
        for ti in range(NT):
            X = xt[ti % 2]; S = st[ti % 2]
            P.load("sp" if ti % 2 == 0 else "act", X[:], x[ti * 128:(ti + 1) * 128, :])
            P.act(junk[:], X[:], AF.Square, accum_out=S[:, 0:1])
            P.ts(S[:, 1:2], S[:, 0:1], 1.0 / D, 1e-6, ALU.mult, ALU.add)
            P.act(S[:, 1:2], S[:, 1:2], AF.Sqrt)
            P.recip(S[:, 1:2], S[:, 1:2])
            P.stt(X[:], X[:], S[:, 1:2], G[:], ALU.mult, ALU.mult)
            P.tt(X[:], X[:], SH[:], ALU.add)
            for half in range(2):
                pt = pT[half]
                for j in range(4):
                    kc = half * 4 + j
                    P.tr(pt[:, j, :], X[:, kc * 128:(kc + 1) * 128], idt[:])
                P.copy(hT[ti][:, half * 4:(half + 1) * 4, :], pt[:], e="act" if half else "dve")
        NCH = (IN_COLS + 511) // 512
        wt = [P.tile(f"wt{i}", [128, 8, 512]) for i in range(2)]
        po = [P.tile(f"po{i}", [128, 512], psum=True) for i in range(4)]
        ot = [P.tile(f"ot{i}", [128, 512]) for i in range(4)]
        wv = w.rearrange("(kc kp) n -> kp kc n", kp=128)
        k = 0
        for ci in range(NCH):
            c0 = ci * 512
            cw = min(512, IN_COLS - c0)
            W = wt[ci % 2]
            P.load("sp" if ci % 2 == 0 else "act", W[:, :, :cw], wv[:, :, c0:c0 + cw])
            for ti in range(NT):
                ps = po[k % 4]; o = ot[k % 4]
                for kc in range(8):
                    P.mm(ps[:, :cw], hT[ti][:, kc, :], W[:, kc, :cw], start=(kc == 0), stop=(kc == 7))
                P.tt(o[:, :cw], ps[:, :cw], B[:, c0:c0 + cw], ALU.add, e="dve")
                P.store("sp" if k % 2 == 0 else "act", p[ti * 128:(ti + 1) * 128, c0:c0 + cw], o[:, :cw])
                k += 1
        P.finish()
    return nc


def run_k1(xfull, g, sc, sh, w, b):
    if "k1" not in _CACHE:
        _CACHE["k1"] = build_k1()
    nc = _CACHE["k1"]
    ident = np.eye(128, dtype=np.float32)
    maps = []
    for i in range(NCORES):
        bi, tq = divmod(i, 4)
        maps.append({"x": np.ascontiguousarray(xfull[bi, tq * 2048:(tq + 1) * 2048]),
                     "g": g[None].copy(), "sc": sc[bi][None].copy(), "sh": sh[bi][None].copy(),
                     "w": w, "b": b[None].copy(), "ident": ident})
    res = run_bass_kernel_spmd(nc, maps, core_ids=list(range(NCORES)))
    out = np.empty((NB, T, IN_COLS), np.float32)
    for i in range(NCORES):
        bi, tq = divmod(i, 4)
        out[bi, tq * 2048:(tq + 1) * 2048] = res.results[i]["p"]
    return out


def _bc(tile, ap, axis, shape):
    return tile.v(ap.unsqueeze(axis).to_broadcast(list(shape)))


def k2_consts():
    C = 64
    tri_incl = np.triu(np.ones((C, C), np.float32))
    tri_strict = np.triu(np.ones((C, C), np.float32), 1)
    m = np.concatenate([tri_strict, tri_incl], 1)
    mask128 = np.concatenate([m, m], 0)
    sel0 = np.concatenate([np.eye(64, dtype=np.float32), np.zeros((64, 64), np.float32)], 1)
    shift = np.concatenate([np.zeros((64, 64), np.float32), np.eye(64, dtype=np.float32)], 1)
    return {"ident": np.eye(128, dtype=np.float32), "mask128": mask128,
            "trils": np.ascontiguousarray(tri_strict.T), "tri": tri_incl,
            "ones64": np.ones((64, 64), np.float32), "sel0": sel0, "shift": shift}


def build_k2(TT=T, stop=0, ustop=99):
    nc = _new_nc()
    NG = TT // 256
    dr = lambda n, s, kind="ExternalInput": nc.dram_tensor(n, s, F32, kind=kind).ap()
    pp = dr("pp", [TT + 1, 640]); mu = dr("mu", [1, 640]); vec = dr("vec", [8, 128])
    w2 = dr("w2", [64, 128]); a2 = dr("a2", [64, 128]); g2 = dr("g2", [128, 128])
    ident = dr("ident", [128, 128]); mask128 = dr("mask128", [128, 128]); trils = dr("trils", [64, 64])
    tri = dr("tri", [64, 64]); ones64 = dr("ones64", [64, 64]); sel0 = dr("sel0", [64, 128]); shift = dr("shift", [64, 128])
    ya = dr("ya", [TT, 128], "ExternalOutput")
    with ExitStack() as es:
        P = Prog2(nc, es)
        IDT = P.tile("IDT", [128, 128]); MASK = P.tile("MASK", [128, 128]); TRILS = P.tile("TRILS", [64, 64])
        TRI = P.tile("TRI", [64, 64]); ONES = P.tile("ONES", [64, 64]); SEL0 = P.tile("SEL0", [64, 128]); SHIFT = P.tile("SHIFT", [64, 128])
        W2 = P.tile("W2", [64, 128]); A2 = P.tile("A2", [64, 128]); G2 = P.tile("G2", [128, 128])
        MU = P.tile("MU", [64, 640]); VEC = P.tile("VEC", [64, 8, 128])
        qs = ["sp", "act"]
        for i, (tl, d) in enumerate([(IDT, ident), (MASK, mask128), (TRILS, trils), (TRI, tri), (ONES, ones64),
                                     (SEL0, sel0), (SHIFT, shift), (W2, w2), (A2, a2), (G2, g2)]):
            P.load(qs[i % 2], tl[:], d[:, :])
        P.load("sp", MU[:], mu.partition_broadcast(64))
        for r in range(7):
            P.load(qs[r % 2], VEC[:, r, :], vec[r:r + 1, :].partition_broadcast(64))
        I64 = IDT[0:64, 0:64]
        S4 = [64, 4, 128]
        vb = lambda r: _bc(VEC, VEC.t[:, r, :], 1, S4)
        W0b, A0b, KKb, KAb, RKb, GNGb, GNBb = [vb(r) for r in range(7)]
        banks = [P.tile(f"bank{i}", [128, 512], psum=True) for i in range(8)]
        pk = [0]

        def pbank():
            b = banks[pk[0] % 4]
            pk[0] += 1
            return b

        def t4(name, n=2):
            return [P.tile(f"{name}{i}", S4) for i in range(n)]

        CURt = [P.tile(f"CUR{i}", [64, 4, 640]) for i in range(2)]
        PRVt = [P.tile(f"PRV{i}", [64, 4, 640]) for i in range(2)]
        TWt = t4("TW"); SGt = t4("SG"); LTt = [P.tile(f"LT{i}", [64, 4, 2, 64]) for i in range(2)]
        GTtt = [P.tile(f"GTt{i}", [128, 4, 64]) for i in range(2)]
        LDt = t4("LD"); At = t4("A"); Ggt = t4("Gg"); KKt = t4("KK"); SQt = t4("SQ"); T1t = t4("T1"); KMt = t4("KM"); Bvt = t4("Bv")
        SSt = [P.tile(f"SS{i}", [64, 8]) for i in range(2)]; RKt = [P.tile(f"RK{i}", [64, 8]) for i in range(2)]
        Lst = t4("Ls"); ELt = t4("EL"); ENLt = t4("ENL"); TMPt = t4("TMP"); TMP2t = t4("TMP2")
        DCFt = [P.tile(f"DCF{i}", [64, 8]) for i in range(2)]
        A_t = t4("A_"); BTt = t4("BT"); KTt = t4("KT"); RTt = t4("RT"); BBt = t4("BB"); KBt = t4("KB")
        FMBKt = [P.tile(f"FMBK{i}", [64, 8, 128]) for i in range(2)]
        FMARt = [P.tile(f"FMAR{i}", [64, 8, 128]) for i in range(2)]
        BKSt = [P.tile(f"BKS{i}", [128, 4, 128]) for i in range(2)]
        VVt = [P.tile(f"VV{i}", [128, 4, 128]) for i in range(2)]
        YBt = t4("YB"); YCt = t4("YC"); MNt = [P.tile(f"MN{i}", [64, 8]) for i in range(2)]; VRt = [P.tile(f"VR{i}", [64, 8]) for i in range(2)]
        BIGt = [P.tile(f"BIG{u}", [128, 128]) for u in range(8)]
        Xt = [[P.tile(f"X{u}_{i}", [64, 64]) for i in range(2)] for u in range(8)]
        XTt = [[P.tile(f"XT{u}_{i}", [64, 64]) for i in range(2)] for u in range(8)]
        Pmt = [[P.tile(f"Pm{u}_{i}", [64, 64]) for i in range(2)] for u in range(8)]
        W2st = [P.tile(f"W2s{u}", [64, 64]) for u in range(8)]
        Aht = [P.tile(f"Ah{u}", [64, 64]) for u in range(8)]
        RhTt = [P.tile(f"RhT{u}", [64, 64]) for u in range(8)]
        GTt_ = [P.tile(f"GT{u}", [64, 64]) for u in range(8)]
        Ht = [[P.tile(f"H{h}_{i}", [64, 64]) for i in range(2)] for h in range(2)]
        hk = [0, 0]
        for h in range(2):
            P.memset(Ht[h][0][:], 0.0)
        fl = lambda tl: tl.v(tl.t[:, :, :].rearrange("p a b -> p (a b)"))
        v8 = lambda tl: tl.v(tl.t[:, :, :].rearrange("p a (h b) -> p (a h) b", b=64))
        b8 = lambda tl: _bc(tl, tl.t[:, :], 2, [64, 8, 64])
        ppv = lambda lo: pp[lo:lo + 256, :].rearrange("(c t) n -> t c n", t=64)
        NEG = -float(np.exp(-0.5))

        class _Stop(Exception):
            pass

        def early(n, view):
            if stop == n:
                P.store("sp", ya[0:256, :].rearrange("(c t) n -> t c n", t=64), view)
                raise _Stop()

        for g in range(NG):
          try:
              k = g % 2
              CUR = CURt[k]; PRV = PRVt[k]
              P.load("sp", CUR[:], ppv(1 + g * 256))
              P.load("act", PRV[:], ppv(g * 256))
              P.tt(PRV[:], PRV[:], CUR[:], ALU.subtract)
              P.tt(PRV[:], PRV[:], _bc(MU, MU.t[:, :], 1, [64, 4, 640]), ALU.mult)
              P.tt(CUR[:], CUR[:], PRV[:], ALU.add, e="pool")
              early(1, CUR[:, :, 0:128])
              Rr = CUR[:, :, 0:128]; Kr = CUR[:, :, 128:256]; Vr = CUR[:, :, 256:384]
              TW = TWt[k]; SG = SGt[k]; LT = LTt[k]; GTt = GTtt[k]
              P.act(TW[:, :, 0:64], CUR[:, :, 384:448], AF.Tanh)
              P.act(SG[:], CUR[:, :, 512:640], AF.Sigmoid)
              b1 = pbank(); b1v = b1.v(b1.t[0:64, :].rearrange("p (c w t) -> p c w t", c=4, w=2))
              for c in range(4):
                  P.tr(b1.v(b1v.ap[:, c, 0, :]), TW[:, c, 0:64], I64)
                  P.tr(b1.v(b1v.ap[:, c, 1, :]), CUR[:, c, 448:512], I64)
              P.copy(LT[:], b1v)
              b2 = pbank(); b2v = b2.v(b2.t[:, 0:256].rearrange("p (c t) -> p c t", c=4))
              for c in range(4):
                  P.tr(b2.v(b2v.ap[:, c, :]), SG[:, c, :], I64)
              P.copy(GTt[:], b2v, e="act")
              bw = pbank(); bwv = bw.v(bw.t[0:64, :].rearrange("p (c n) -> p c n", c=4))
              for c in range(4):
                  P.mm(bw.v(bwv.ap[:, c, :]), LT[:, c, 0, :], W2[:])
              LD = LDt[k]
              P.tt(LD[:], bwv, W0b, ALU.add)
              ba = pbank(); bav = ba.v(ba.t[0:64, :].rearrange("p (c n) -> p c n", c=4))
              for c in range(4):
                  P.mm(ba.v(bav.ap[:, c, :]), LT[:, c, 1, :], A2[:])
              A = At[k]
              P.tt(A[:], bav, A0b, ALU.add)
              bg = pbank(); bgv = bg.v(bg.t[0:64, :].rearrange("p (c n) -> p c n", c=4))
              for c in range(4):
                  P.mm(bg.v(bgv.ap[:, c, :]), GTt[:, c, :], G2[:])
              Gg = Ggt[k]
              P.copy(Gg[:], bgv, e="act")
              P.act(LD[:], LD[:], AF.Sigmoid)
              P.act(A[:], A[:], AF.Sigmoid)
              P.ts(LD[:], LD[:], NEG, None, ALU.mult, e="pool")
              early(2, LD[:])
              KK = KKt[k]; SQ = SQt[k]; SS = SSt[k]; T1 = T1t[k]; KM = KMt[k]; Bv = Bvt[k]; RK = RKt[k]
              P.tt(KK[:], Kr, KKb, ALU.mult)
              P.tt(SQ[:], KK[:], KK[:], ALU.mult)
              P.red(SS[:], v8(SQ), ALU.add)
              P.act(SS[:], SS[:], AF.Sqrt)
              P.ts(SS[:], SS[:], 1e-12, None, ALU.max)
              P.recip(SS[:], SS[:])
              P.tt(v8(KK), v8(KK), b8(SS), ALU.mult)
              P.stt(T1[:], A[:], -1.0, KAb, ALU.add, ALU.mult)
              P.stt(KM[:], T1[:], 1.0, Kr, ALU.add, ALU.mult)
              P.tt(Bv[:], KK[:], A[:], ALU.mult)
              P.tt(SQ[:], Rr, KM[:], ALU.mult)
              P.tt(SQ[:], SQ[:], RKb, ALU.mult)
              P.red(RK[:], v8(SQ), ALU.add)
              early(3, KM[:])
              Ls = Lst[k]; EL = ELt[k]; ENL = ENLt[k]; TMP = TMPt[k]; TMP2 = TMP2t[k]; DCF = DCFt[k]
              bL = pbank(); bLv = bL.v(bL.t[0:64, :].rearrange("p (c n) -> p c n", c=4))
              P.mm(bL[0:64, :], TRI[:], fl(LD))
              P.copy(Ls[:], bLv)
              bC = pbank(); bCv = bC.v(bC.t[0:64, :].rearrange("p (c n) -> p c n", c=4))
              P.mm(bC[0:64, :], ONES[:], fl(LD))
              P.tt(TMP2[:], bCv, Ls[:], ALU.subtract)
              bD = pbank()
              for u in range(8):
                  c, h = divmod(u, 2)
                  P.mm(bD[0:64, u:u + 1], LD[:, c, h * 64:(h + 1) * 64], ONES[:, 0:1])
              P.act(DCF[:], bD[0:64, 0:8], AF.Exp)
              P.act(EL[:], Ls[:], AF.Exp)
              P.act(ENL[:], Ls[:], AF.Exp, scale=-1.0)
              P.tt(TMP[:], Ls[:], LD[:], ALU.subtract)
              P.act(TMP[:], TMP[:], AF.Exp)
              P.act(TMP2[:], TMP2[:], AF.Exp)
              A_ = A_t[k]; BT = BTt[k]; KT = KTt[k]; RT = RTt[k]; BB = BBt[k]; KB = KBt[k]
              P.stt(A_[:], KK[:], -1.0, TMP[:], ALU.mult, ALU.mult)
              P.tt(BT[:], Bv[:], ENL[:], ALU.mult)
              P.tt(KT[:], KM[:], ENL[:], ALU.mult, e="pool")
              P.tt(RT[:], Rr, EL[:], ALU.mult)
              P.tt(BB[:], Bv[:], TMP2[:], ALU.mult, e="pool")
              P.tt(KB[:], KM[:], TMP2[:], ALU.mult)
              early(4, KB[:])
              FMBK = FMBKt[k]; FMAR = FMARt[k]; BKS = BKSt[k]; VV = VVt[k]
              for half in range(2):
                  pb = pbank(); pbv = pb.v(pb.t[0:64, :].rearrange("p (u n) -> p u n", u=4))
                  pa_ = pbank(); pav = pa_.v(pa_.t[0:64, :].rearrange("p (u n) -> p u n", u=4))
                  for uu in range(4):
                      u = half * 4 + uu
                      c, h = divmod(u, 2)
                      hs = slice(h * 64, (h + 1) * 64)
                      P.tr(pb.v(pbv.ap[:, uu, 0:64]), BT[:, c, hs], I64)
                      P.tr(pb.v(pbv.ap[:, uu, 64:128]), KT[:, c, hs], I64)
                      P.tr(pa_.v(pav.ap[:, uu, 0:64]), A_[:, c, hs], I64)
                      P.tr(pa_.v(pav.ap[:, uu, 64:128]), RT[:, c, hs], I64)
                  P.copy(FMBK[:, half * 4:(half + 1) * 4, :], pbv)
                  P.copy(FMAR[:, half * 4:(half + 1) * 4, :], pav, e="act")
              ps1 = pbank()
              P.mm(ps1[:], SEL0[:], fl(BB), start=True, stop=False)
              P.mm(ps1[:], SHIFT[:], fl(KB), start=False, stop=True)
              P.copy(fl(BKS), ps1[:])
              ps2 = pbank(); ps2v = ps2.v(ps2.t[:, :].rearrange("p (c n) -> p c n", c=4))
              P.mm(ps2v, SHIFT[:], Vr)
              P.copy(VV[64:128, :, :], ps2.v(ps2v.ap[64:128, :, :]), e="act")
              early(5, BKS[0:64, :, :])
              YB = YBt[k]

              def unit(u):
                  c, h = divmod(u, 2)
                  hs = slice(h * 64, (h + 1) * 64)
                  bank = banks[4 + u % 4]
                  co = (u // 4) * 256
                  BIG = BIGt[u]
                  fbk = FMBK[:, u, :]; far = FMAR[:, u, :]
                  P.mm(bank[:, co:co + 128], fbk, far)
                  P.mm(bank[0:64, co + 128:co + 192], FMAR[:, u, 0:64], FMBK[:, u, 0:64])
                  P.tt(BIG[:], bank[:, co:co + 128], MASK[:], ALU.mult)
                  XT = XTt[u][0]
                  P.tt(XT[:], bank[0:64, co + 128:co + 192], TRILS[:], ALU.mult)
                  X = BIG[0:64, 0:64]
                  Pm = Pmt[u][0]
                  P.tt(Pm[:], X, I64, ALU.add, e="pool")
                  yield
                  for i in range(5):
                      Xn = Xt[u][i % 2]; XTn = XTt[u][(i + 1) % 2]; Pn = Pmt[u][(i + 1) % 2]
                      if i < 4:
                          P.mm(bank[0:64, co:co + 64], XT[:], X)
                      P.mm(bank[0:64, co + 64:co + 128], X, XT[:])
                      if i < 4:
                          P.copy(Xn[:], bank[0:64, co:co + 64], e=EVAC_E)
                      P.copy(XTn[:], bank[0:64, co + 64:co + 128])
                      yield
                      P.mm(bank[0:64, co + 128:co + 192], XTn[:], Pm[:])
                      P.tt(Pn[:], bank[0:64, co + 128:co + 192], Pm[:], ALU.add)
                      X = Xn[:]; XT = XTn; Pm = Pn
                      yield
                  InvT = Pm
                  P.mm(bank[0:64, co:co + 64], BIG[64:128, 0:64], VV[64:128, c, hs])
                  W2s = W2st[u]
                  P.copy(W2s[:], bank[0:64, co:co + 64], e="act")
                  yield
                  P.mm(bank[0:64, co + 64:co + 128], InvT[:], A_[:, c, hs])
                  P.mm(bank[0:64, co + 128:co + 192], InvT[:], W2s[:])
                  Ah = Aht[u]
                  P.copy(Ah[:], bank[0:64, co + 64:co + 128])
                  P.copy(VV[0:64, c, hs], bank[0:64, co + 128:co + 192], e="act")
                  yield
                  P.mm(bank[0:64, co:co + 64], Ah[:], BIG[0:64, 64:128])
                  P.mm(bank[0:64, co + 64:co + 128], Ah[:], BKS[0:64, c, hs])
                  RhT = RhTt[u]; GT = GTt_[u]
                  P.tt(RhT[:], bank[0:64, co:co + 64], FMAR[:, u, 64:128], ALU.add)
                  P.stt(GT[:], I64, DCF[:, u:u + 1], bank[0:64, co + 64:co + 128], ALU.mult, ALU.add)
                  yield
                  Hc = Ht[h][hk[h] % 2]; Hn = Ht[h][(hk[h] + 1) % 2]
                  hk[h] += 1
                  P.mm(bank[0:64, co + 128:co + 192], RhT[:], Hc[:], start=True, stop=False)
                  P.mm(bank[0:64, co + 128:co + 192], BIG[:, 64:128], VV[:, c, hs], start=False, stop=True)
                  P.mm(bank[0:64, co + 192:co + 256], GT[:], Hc[:], start=True, stop=False)
                  P.mm(bank[0:64, co + 192:co + 256], BKS[:, c, hs], VV[:, c, hs], start=False, stop=True)
                  P.copy(YB[:, c, hs], bank[0:64, co + 128:co + 192], e="act")
                  P.copy(Hn[:], bank[0:64, co + 192:co + 256])
                  yield

              gens = [unit(u) for u in range(8)]
              alive = True
              rounds = 0
              while alive and rounds < ustop:
                  rounds += 1
                  alive = False
                  for gen in gens:
                      try:
                          next(gen)
                          alive = True
                      except StopIteration:
                          pass
              early(6, YB[:])
              YC = YCt[k]; MN = MNt[k]; VR = VRt[k]
              P.red(MN[:], v8(YB), ALU.add)
              P.ts(MN[:], MN[:], 1.0 / 64, None, ALU.mult)
              P.tt(v8(YC), v8(YB), b8(MN), ALU.subtract)
              P.tt(SQ[:], YC[:], YC[:], ALU.mult, e="pool")
              P.red(VR[:], v8(SQ), ALU.add)
              P.ts(VR[:], VR[:], 1.0 / 64, 64e-5, ALU.mult, ALU.add)
              P.act(VR[:], VR[:], AF.Sqrt)
              P.recip(VR[:], VR[:])
              P.tt(v8(YC), v8(YC), b8(VR), ALU.mult)
              P.tt(YC[:], YC[:], GNGb, ALU.mult)
              P.tt(YC[:], YC[:], GNBb, ALU.add)
              P.tt(SQ.v(SQ.t[:, :, :].rearrange("p a (h b) -> p a h b", b=64)),
                   CUR.v(CUR.t[:, :, 256:384].rearrange("p a (h b) -> p a h b", b=64)),
                   RK.v(RK.t[:, :].rearrange("p (a h) -> p a h", h=2).unsqueeze(3).to_broadcast([64, 4, 2, 64])), ALU.mult)
              P.tt(YC[:], YC[:], SQ[:], ALU.add)
              P.tt(YC[:], YC[:], Gg[:], ALU.mult)
              P.store("sp", ya[g * 256:(g + 1) * 256, :].rearrange("(c t) n -> t c n", t=64), YC[:])
          except _Stop:
            break
        P.finish()
        print("k2 ninst", P.ninst, "nsem", P.nsem)
    return nc


RW = 512
EVAC_E = "act"


def k2_inputs(p_rwkv_b, i, prm, l, consts):
    h0 = 2 * i
    cs = slice(h0 * 64, h0 * 64 + 128)
    cols = np.r_[np.arange(h0 * 64, h0 * 64 + 128), RW + np.arange(h0 * 64, h0 * 64 + 128),
                 2 * RW + np.arange(h0 * 64, h0 * 64 + 128), np.arange(3 * RW, 3 * RW + 256)]
    vec = np.zeros((8, 128), np.float32)
    for r, n in enumerate(["rwkv_w0", "rwkv_a0", "rwkv_k_k", "rwkv_k_a", "rwkv_r_k", "rwkv_gn_g", "rwkv_gn_b"]):
        vec[r] = prm[n][l].reshape(-1)[cs]
    d = {"pp": np.ascontiguousarray(p_rwkv_b[:, cols]), "mu": np.ascontiguousarray(prm["rwkv_mu"][l][cols][None]),
         "vec": vec, "w2": np.ascontiguousarray(prm["rwkv_w2"][l][:, cs]),
         "a2": np.ascontiguousarray(prm["rwkv_a2"][l][:, cs]), "g2": np.ascontiguousarray(prm["rwkv_g2"][l][:, cs])}
    d.update(consts)
    return d


def run_k2(p, prm, l, TT=T):
    key = ("k2", TT)
    if key not in _CACHE:
        _CACHE[key] = build_k2(TT)
    nc = _CACHE[key]
    consts = k2_consts()
    maps = []
    for ci in range(NCORES):
        b, i = divmod(ci, 4)
        pb = np.concatenate([np.zeros((1, 1792), np.float32), p[b, :TT, :1792]], 0)
        maps.append(k2_inputs(pb, i, prm, l, consts))
    res = run_bass_kernel_spmd(nc, maps, core_ids=list(range(NCORES)))
    out = np.empty((NB, TT, 512), np.float32)
    for ci in range(NCORES):
        b, i = divmod(ci, 4)
        out[b, :, i * 128:(i + 1) * 128] = res.results[ci]["ya"]
    return out


ROPE_THETA = 500000.0
NEGM = -30000.0


def k3_consts(TT):
    half = 8
    inv = np.power(ROPE_THETA, -np.arange(half, dtype=np.float32) * 2.0 / 16).astype(np.float32)

    def tab(pos):
        ang = pos.astype(np.float32)[:, None] * inv[None, :]
        c, s = np.cos(ang).astype(np.float32), np.sin(ang).astype(np.float32)
        return np.concatenate([c, c, -s, s], 1).astype(np.float32)
    rope_tok = tab(np.arange(TT))
    ncmp = TT // 16
    rope_cmp = tab(np.arange(ncmp) * 16 + 31)
    ql = np.arange(128)
    triu = (ql[:, None] <= ql[None, :]).astype(np.float32)
    tril = (ql[:, None] > ql[None, :]).astype(np.float32)
    cma = np.zeros((128, 17, 128), np.float32)
    cmt = np.zeros((128, 17, 128), np.float32)
    for v in range(17):
        c0 = 8 * v
        nl = np.arange(128)
        valid = (16 * (nl[None, :] - c0) + 31 <= ql[:, None])
        cma[:, v, :] = np.where(valid, 0.0, NEGM)
        cmt[:, v, :] = valid.T.astype(np.float32)
    ca = np.where(ql >= 64, 1e4, -1.0).astype(np.float32)[:, None]
    cb = np.where(ql < 64, 1e4, 0.0).astype(np.float32)[:, None]
    return {"rope_tok": rope_tok, "rope_cmp": rope_cmp, "triu": triu, "tril": tril, "cma": cma, "cmt": cmt,
            "cab": np.concatenate([ca, cb], 1), "ident": np.eye(128, dtype=np.float32)}


def build_k3(TT=T):
    nc = _new_nc()
    NTK = TT // 128
    NQB = NTK
    NCMP = TT // 16
    NCT = (NCMP + 127) // 128
    dr = lambda n, s, kind="ExternalInput": nc.dram_tensor(n, s, F32, kind=kind).ap()
    qin = dr("qin", [NQB * 128, 256]); gin = dr("gin", [NQB * 128, 6]); kvin = dr("kvin", [TT, 384])
    qkg = dr("qkg", [1, 256]); cpos = dr("cpos", [128, 32]); w1 = dr("w1", [128, 32, 256]); w2 = dr("w2", [128, 2, 2, 64])
    rope_tok = dr("rope_tok", [TT, 32]); rope_cmp = dr("rope_cmp", [NCMP, 32])
    triu = dr("triu", [128, 128]); tril = dr("tril", [128, 128]); cma = dr("cma", [128, 17, 128]); cmt = dr("cmt", [128, 17, 128])
    cab = dr("cab", [128, 2]); ident = dr("ident", [128, 128])
    yb = dr("yb", [NQB * 128, 128], "ExternalOutput")
    with ExitStack() as es:
        P = Prog2(nc, es)
        IDT = P.tile("IDT", [128, 128]); TRIU = P.tile("TRIU", [128, 128]); TRIL = P.tile("TRIL", [128, 128])
        CMA = P.tile("CMA", [128, 17, 128]); CMT = P.tile("CMT", [128, 17, 128]); CAB = P.tile("CAB", [128, 2])
        QKG = P.tile("QKG", [128, 4, 64]); CPOS = P.tile("CPOS", [128, 32]); W2 = P.tile("W2", [128, 2, 2, 64])
        for i, (tl, d) in enumerate([(IDT, ident), (TRIU, triu), (TRIL, tril), (CAB, cab), (CPOS, cpos)]):
            P.load(["sp", "act"][i % 2], tl[:], d[:, :])
        P.load("sp", CMA[:], cma[:, :, :]); P.load("act", CMT[:], cmt[:, :, :])
        P.load("sp", QKG.v(QKG.t[:, :, :].rearrange("p a b -> p (a b)")), qkg.partition_broadcast(128))
        P.load("act", W2.v(W2.t[:, :, :, :].rearrange("p a b c -> p (a b c)")), w2.rearrange("p a b c -> p (a b c)"))
        banks = [P.tile(f"bank{i}", [128, 512], psum=True) for i in range(8)]
        KT = P.tile("KT", [128, TT])
        CT = P.tile("CT", [128, TT + 16])
        VSW = P.tile("VSW", [128, NTK, 2, 65])
        P.memset(VSW[:, :, :, 64:65], 1.0)
        P.memset(CT[:, TT:TT + 16], 0.0)
        KVt = [P.tile(f"KV{i}", [128, 384]) for i in range(2)]
        RPt = [P.tile(f"RP{i}", [128, 32]) for i in range(2)]
        KNt = [P.tile(f"KN{i}", [128, 2, 64]) for i in range(2)]
        SQt = [P.tile(f"SQa{i}", [128, 2, 64]) for i in range(2)]
        STt = [P.tile(f"STa{i}", [128, 2]) for i in range(2)]
        R1t = [P.tile(f"R1a{i}", [128, 2, 16]) for i in range(2)]
        R2t = [P.tile(f"R2a{i}", [128, 2, 16]) for i in range(2)]

        def norm_rope(X3, G3, RP, KN, SQ, ST, R1, R2, nh, e2="pool"):
            P.tt(SQ[:], X3, X3, ALU.mult, e=e2)
            P.red(ST[:], SQ[:], ALU.add)
            P.ts(ST[:], ST[:], 1.0 / 64, 1e-6, ALU.mult, ALU.add)
            P.act(ST[:], ST[:], AF.Sqrt)
            P.recip(ST[:], ST[:])
            P.tt(KN[:], X3, _bc(ST, ST.t[:, :], 2, [128, nh, 64]), ALU.mult)
            P.tt(KN[:], KN[:], G3, ALU.mult, e=e2)
            cs = _bc(RP, RP.t[:, 0:16], 1, [128, nh, 16])
            P.tt(R1[:], KN[:, :, 0:16], cs, ALU.mult)
            P.tt(R2[:, :, 0:8], KN[:, :, 8:16], _bc(RP, RP.t[:, 16:24], 1, [128, nh, 8]), ALU.mult, e=e2)
            P.tt(R2[:, :, 8:16], KN[:, :, 0:8], _bc(RP, RP.t[:, 24:32], 1, [128, nh, 8]), ALU.mult, e=e2)
            P.tt(KN[:, :, 0:16], R1[:], R2[:], ALU.add)

        GK = QKG.v(QKG.t[:, 2:4, :])
        pk = 0
        for tg in range(NTK // 4):
            bk = banks[pk % 2]; bc_ = banks[2 + pk % 2]; pk += 1
            for j in range(4):
                ti = tg * 4 + j
                k = ti % 2
                KV = KVt[k]; RP = RPt[k]
                P.load("sp", KV[:], kvin[ti * 128:(ti + 1) * 128, :])
                P.load("act", RP[:], rope_tok[ti * 128:(ti + 1) * 128, :])
                X3 = KV.v(KV.t[:, 128:384].rearrange("p (a b) -> p a b", b=128)[:, :, 0:64])
                norm_rope(X3, GK, RP, KNt[k], SQt[k], STt[k], R1t[k], R2t[k], 2)
                P.tr(bk[:, j * 128:(j + 1) * 128], KNt[k].v(KNt[k].t[:, :, :].rearrange("p a b -> p (a b)")), IDT[:])
                P.tr(bc_[:, j * 128:(j + 1) * 128], KV[:, 0:128], IDT[:])
                V3 = KV.v(KV.t[:, 128:384].rearrange("p (a b) -> p a b", b=128)[:, :, 64:128])
                P.copy(VSW[:, ti, :, 0:64], V3, e="act")
            P.copy(KT[:, tg * 512:(tg + 1) * 512], bk[:])
            P.copy(CT[:, tg * 512:(tg + 1) * 512], bc_[:], e="act")
        HID = P.tile("HID", [128, 2, 2, NCT * 128])
        P.memset(HID[:], 0.0, e="pool")
        W1 = P.tile("W1", [128, 32, 128])
        BIA = P.tile("BIA", [128, 2, 2])
        Zt = P.tile("Zt", [128, 512]); Z2 = P.tile("Z2", [128, 512])
        NV = NCMP - 1
        for hc in range(2):
            P.load("sp" if hc == 0 else "act", W1[:], w1[:, :, hc * 128:(hc + 1) * 128])
            for i in range(2):
                ps_ = slice(i * 64, (i + 1) * 64)
                bb = banks[4]
                for tau in range(32):
                    P.mm(bb[:, 0:1], W1[ps_, tau, :], CPOS[ps_, tau:tau + 1], start=(tau == 0), stop=(tau == 31))
                P.copy(BIA[:, i, hc:hc + 1], bb[:, 0:1])
                for nt in range((NCMP + 511) // 512):
                    n0 = nt * 512
                    nn = min(512, NCMP - n0)
                    ba = banks[5 + nt % 2]
                    for tau in range(32):
                        rhs = CT.v(CT.t[ps_, n0 * 16 + tau: n0 * 16 + tau + (nn - 1) * 16 + 1: 16])
                        P.mm(ba[:, 0:nn], W1[ps_, tau, :], rhs, start=(tau == 0), stop=(tau == 31))
                    Z = Zt
                    P.act(Z[:, 0:nn], ba[:, 0:nn], AF.Identity, bias=BIA[:, i, hc:hc + 1])
                    P.tt(Z2[:, 0:nn], Z[:, 0:nn], Z[:, 0:nn], ALU.mult)
                    P.ts(Z2[:, 0:nn], Z2[:, 0:nn], 0.044715, 1.0, ALU.mult, ALU.add)
                    P.tt(Z2[:, 0:nn], Z2[:, 0:nn], Z[:, 0:nn], ALU.mult)
                    P.act(Z2[:, 0:nn], Z2[:, 0:nn], AF.Sigmoid, scale=1.5957691216057308)
                    P.tt(HID[:, i, hc, n0:n0 + nn], Z2[:, 0:nn], Z[:, 0:nn], ALU.mult)
        KCT = P.tile("KCT", [128, NCT * 128])
        VCMP = P.tile("VCMP", [128, NCT, 65])
        P.memset(VCMP[:, :, 64:65], 1.0)
        KC2 = P.tile("KC2", [128, 2, 64])
        KCR = P.tile("KCR", [128, 1, 64])
        GC = QKG.v(QKG.t[:, 1:2, :])
        for ct in range(NCT):
            bb = banks[4 + ct % 2]
            for i in range(2):
                for hc in range(2):
                    P.mm(bb[:, i * 64:(i + 1) * 64], HID[:, i, hc, ct * 128:(ct + 1) * 128], W2[:, i, hc, :],
                         start=(hc == 0), stop=(hc == 1))
            P.copy(VCMP[:, ct, 0:64], bb[:, 64:128], e="act")
            RP = RPt[ct % 2]
            nr = min(128, NCMP - ct * 128)
            P.load("sp", RP[0:nr, :], rope_cmp[ct * 128:ct * 128 + nr, :])
            k = ct % 2
            KN1 = KNt[k].v(KNt[k].t[:, 0:1, :])
            P.copy(KCR[:, 0, :], bb[:, 0:64])
            norm_rope(KCR[:], GC, RP, KN1,
                      SQt[k].v(SQt[k].t[:, 0:1, :]), STt[k].v(STt[k].t[:, 0:1]), R1t[k].v(R1t[k].t[:, 0:1, :]),
                      R2t[k].v(R2t[k].t[:, 0:1, :]), 1, e2="dve")
            P.copy(KC2[:, 0, :], KNt[k][:, 0, :])
            P.copy(KC2[:, 1, :], KNt[k][:, 0, :], e="act")
            b2 = banks[6 + ct % 2]
            P.tr(b2[:, 0:128], KC2.v(KC2.t[:, :, :].rearrange("p a b -> p (a b)")), IDT[:])
            P.copy(KCT[:, ct * 128:(ct + 1) * 128], b2[:, 0:128])
        Qt = [P.tile(f"Q{i}", [128, 4, 64]) for i in range(2)]
        Gt = [P.tile(f"G{i}", [128, 2, 3]) for i in range(2)]
        QNt = [P.tile(f"QN{i}", [128, 4, 64]) for i in range(2)]
        QDt = [P.tile(f"QD{i}", [128, 4, 2, 64]) for i in range(2)]
        QTt = [P.tile(f"QT{i}", [128, 4, 128]) for i in range(2)]
        SQq = [P.tile(f"SQq{i}", [128, 4, 64]) for i in range(2)]
        STq = [P.tile(f"STq{i}", [128, 4]) for i in range(2)]
        R1q = [P.tile(f"R1q{i}", [128, 4, 16]) for i in range(2)]
        R2q = [P.tile(f"R2q{i}", [128, 4, 16]) for i in range(2)]
        PCt = [P.tile(f"PC{i}", [128, 512]) for i in range(2)]
        RSt = [P.tile(f"RS{i}", [128, 4]) for i in range(2)]
        IMPP = P.tile("IMPP", [128, 516])
        IMPF = P.tile("IMPF", [128, 128]); IMPW = P.tile("IMPW", [128, 128])
        M8 = P.tile("M8", [128, 16])
        SEL = P.tile("SEL", [128, 128])
        PTt = [P.tile(f"PT{i}", [128, 2, 128]) for i in range(4)]
        OUTt = [P.tile(f"OUT{i}", [128, 2, 64]) for i in range(2)]
        RIt = [P.tile(f"RI{i}", [128, 3, 2]) for i in range(2)]
        OTt = [P.tile(f"OT{i}", [128, 2, 64]) for i in range(2)]
        GQ = QKG.v(QKG.t[:, 0:1, :].to_broadcast([128, 4, 64])) if False else _bc(QKG, QKG.t[:, 0, :], 1, [128, 4, 64])
        sb_i = [0]; mb_i = [0]; pt_i = [0]
        OB = [0, 1, 2]

        def attn_tile(first, last, ob, kT_ap, part, v1_ap, QT, mask_fn):
            sb = banks[sb_i[0] % 2]; sb_i[0] += 1
            PT = PTt[pt_i[0] % 4]; pt_i[0] += 1
            P.mm(sb[:, 0:256], kT_ap, QT.v(QT.t[part:part + 64, 0:2, :].rearrange("p a b -> p (a b)")))
            P.act(PT.v(PT.t[:, :, :].rearrange("p a b -> p (a b)")), sb[:, 0:256], AF.Exp)
            mask_fn(PT)
            for r in range(2):
                P.mm(banks[6 + r][:, ob * 65:(ob + 1) * 65], PT[:, r, :], v1_ap, start=first, stop=last)

        for jq in range(NQB):
            qb = jq
            k = jq % 2
            Q = Qt[k]; G = Gt[k]; RP = RPt[k]
            P.load("sp", Q.v(Q.t[:, :, :].rearrange("p a b -> p (a b)")), qin[jq * 128:(jq + 1) * 128, :])
            P.load("act", G.v(G.t[:, :, :].rearrange("p a b -> p (a b)")), gin[jq * 128:(jq + 1) * 128, :])
            P.load("sp", RP[:], rope_tok[qb * 128:(qb + 1) * 128, :])
            QN = QNt[k]; QD = QDt[k]; QT = QTt[k]
            norm_rope(Q[:], GQ, RP, QN, SQq[k], STq[k], R1q[k], R2q[k], 4)
            P.ts(QD[:, :, 0, :], QN[:], 0.125, None, ALU.mult)
            P.ts(QD[:, :, 1, :], QN[:], 0.125, None, ALU.mult, e="pool")
            P.act(G[:], G[:], AF.Sigmoid)
            bq = banks[4]
            for r in range(4):
                P.tr(bq[:, r * 128:(r + 1) * 128], QD.v(QD.t[:, r, :, :].rearrange("p a b -> p (a b)")), IDT[:])
            P.copy(QT.v(QT.t[:, :, :].rearrange("p a b -> p (a b)")), bq[:])
            nct = qb // 16 + 1
            ncols = nct * 128
            var = qb % 16
            P.memset(IMPP[:], 0.0, e="pool")
            RS = RSt[k]
            for r in range(4):
                sb = banks[2 + r % 2]
                P.mm(sb[:, 0:ncols], QT[0:64, r, :], KCT[0:64, 0:ncols])
                PC = PCt[r % 2]
                lo = ncols - 128
                if lo > 0:
                    P.copy(PC[:, 0:lo], sb[:, 0:lo])
                    if var == 0:
                        P.tt(PC[:, lo - 128:lo], PC[:, lo - 128:lo], CMA[:, 16, :], ALU.add)
                P.tt(PC[:, lo:ncols], sb[:, lo:ncols], CMA[:, var, :], ALU.add)
                P.act(PC[:, 0:ncols], PC[:, 0:ncols], AF.Exp, accum_out=RS[:, r:r + 1])
                P.ts(RS[:, r:r + 1], RS[:, r:r + 1], 1.1754944e-38, None, ALU.max)
                P.recip(RS[:, r:r + 1], RS[:, r:r + 1])
                P.stt(IMPP[:, 4:4 + ncols], PC[:, 0:ncols], RS[:, r:r + 1], IMPP[:, 4:4 + ncols], ALU.mult, ALU.add)
            P.red(IMPF[:], IMPP.v(IMPP.t[:, 4:516].rearrange("p (j f) -> p j f", f=4)), ALU.add)
            P.tt(IMPF[:], IMPF[:], IMPP.v(IMPP.t[:, 0:512].rearrange("p (j f) -> p j f", f=4)[:, :, 3]), ALU.add)
            if 2 * qb + 2 < 128:
                P.memset(IMPF[:, 2 * qb + 2:128], -1.0)
            if qb >= 1:
                P.ts(IMPF[:, 2 * qb - 1:2 * qb], IMPF[:, 2 * qb - 1:2 * qb], CAB[:, 1:2], None, ALU.max)
            if 2 * qb + 1 < 128:
                P.copy(IMPF[:, 2 * qb + 1:2 * qb + 2], CAB[:, 0:1])
            P.memset(IMPF[:, 2 * qb:2 * qb + 1], 1e4)
            P.memset(IMPF[:, 0:1], 1e4)
            P.gop("dve", lambda E: E.max(out=M8.t[:, 0:8], in_=IMPF.t[:, :]), [M8], [IMPF])
            P.gop("dve", lambda E: E.match_replace(out=IMPW.t[:, :], in_to_replace=M8.t[:, 0:8], in_values=IMPF.t[:, :],
                                                   imm_value=-2.0), [IMPW], [M8, IMPF])
            P.gop("dve", lambda E: E.max(out=M8.t[:, 8:16], in_=IMPW.t[:, :]), [M8], [IMPW])
            P.ts(SEL[:], IMPF[:], M8[:, 15:16], None, ALU.is_ge)
            tiles = []
            for ct in range(nct):
                last = ct == nct - 1
                if last:
                    mf = lambda PT: P.tt(PT[:], PT[:], _bc(CMT, CMT.t[:, var, :], 1, [128, 2, 128]), ALU.mult)
                elif var == 0 and ct == nct - 2:
                    mf = lambda PT: P.tt(PT[:], PT[:], _bc(CMT, CMT.t[:, 16, :], 1, [128, 2, 128]), ALU.mult)
                else:
                    mf = None
                tiles.append((ct == 0, last, OB[0], KCT[0:64, ct * 128:(ct + 1) * 128], 0, VCMP[:, ct, :], mf, None))
            kts = [kt for kt in range(qb - 4, qb + 1) if kt >= 0]
            for kt in kts:
                if kt == qb:
                    mf = lambda PT: P.tt(PT[:], PT[:], _bc(TRIU, TRIU.t[:, :], 1, [128, 2, 128]), ALU.mult)
                elif kt == qb - 4:
                    mf = lambda PT: P.tt(PT[:], PT[:], _bc(TRIL, TRIL.t[:, :], 1, [128, 2, 128]), ALU.mult)
                else:
                    mf = None
                tiles.append((kt == kts[0], kt == kts[-1], OB[2], KT[64:128, kt * 128:(kt + 1) * 128], 64, VSW[:, kt, 1, :], mf, None))
            nj = 2 * (qb + 1)
            P.copy(CT.v(CT.t[:, 0:nj * 64].rearrange("p (j f) -> p j f", f=64)),
                   SEL.v(SEL.t[:, 0:nj].unsqueeze(2).to_broadcast([128, nj, 64])), e="pool")
            for kt in range(qb + 1):
                tiles.append((kt == 0, kt == qb, OB[1], KT[0:64, kt * 128:(kt + 1) * 128], 0, VSW[:, kt, 0, :], None, kt))
            LA = 2
            pend = []
            sbanks = [banks[0], banks[1], banks[5]]
            mbanks = [banks[2], banks[3], banks[4]]
            for i in range(len(tiles) + LA):
                if i < len(tiles):
                    first, last, ob, kT_ap, part, v1_ap, mf, kt = tiles[i]
                    sb = sbanks[sb_i[0] % 3]; sb_i[0] += 1
                    PT = PTt[pt_i[0] % 4]; pt_i[0] += 1
                    P.mm(sb[:, 0:256], kT_ap, QT.v(QT.t[part:part + 64, 0:2, :].rearrange("p a b -> p (a b)")))
                    if kt is not None:
                        mbk = mbanks[mb_i[0] % 3]; mb_i[0] += 1
                        P.tr(mbk[:, 0:128], CT[:, kt * 128:(kt + 1) * 128], IDT[:])
                    P.act(PT.v(PT.t[:, :, :].rearrange("p a b -> p (a b)")), sb[:, 0:256], AF.Exp)
                    if kt is not None:
                        P.tt(PT[:], PT[:], _bc(mbk, mbk.t[:, 0:128], 1, [128, 2, 128]), ALU.mult)
                        if kt == qb:
                            P.tt(PT[:], PT[:], _bc(TRIU, TRIU.t[:, :], 1, [128, 2, 128]), ALU.mult, e="pool")
                    elif mf is not None:
                        mf(PT)
                    pend.append((PT, ob, v1_ap, first, last))
                j = i - LA
                if j >= 0:
                    PT, ob, v1_ap, first, last = pend[j]
                    for r in range(2):
                        P.mm(banks[6 + r][:, ob * 65:(ob + 1) * 65], PT[:, r, :], v1_ap, start=first, stop=last)
            RI = RIt[k]; OUT = OUTt[k]; OT = OTt[k]
            for bi in range(3):
                for r in range(2):
                    ob = banks[6 + r]
                    c0 = bi * 65
                    P.ts(RI[:, bi, r:r + 1], ob[:, c0 + 64:c0 + 65], 1.1754944e-38, None, ALU.max)
                    P.recip(RI[:, bi, r:r + 1], RI[:, bi, r:r + 1])
                    P.tt(RI[:, bi, r:r + 1], RI[:, bi, r:r + 1], G[:, r, bi:bi + 1], ALU.mult)
                    if bi == 0:
                        P.ts(OUT[:, r, :], ob[:, c0:c0 + 64], RI[:, bi, r:r + 1], None, ALU.mult)
                    else:
                        P.stt(OUT[:, r, :], ob[:, c0:c0 + 64], RI[:, bi, r:r + 1], OUT[:, r, :], ALU.mult, ALU.add)
            P.store("sp", yb[jq * 128:(jq + 1) * 128, :], OUT.v(OUT.t[:, :, :].rearrange("p a b -> p (a b)")))
        P.finish()
        print("k3 ninst", P.ninst, "nsem", P.nsem)
    return nc


def k3_inputs(p_b, g, hh, prm, l, TT):
    q = p_b[:TT, 1792 + g * 256: 1792 + (g + 1) * 256].reshape(TT, 4, 64)
    order = [2 * hh, 2 * hh + 1, 2 * (1 - hh), 2 * (1 - hh) + 1]
    q = q[:, order, :].reshape(TT, 256)
    kv = p_b[:TT, 2304:3072].reshape(TT, 6, 2, 64)[:, :, g, :].reshape(TT, 384)
    gt = p_b[:TT, 3072:3096].reshape(TT, 8, 3)[:, g * 4 + 2 * hh: g * 4 + 2 * hh + 2, :].reshape(TT, 6)
    cp = prm["cmp_pos"][l]
    cpos = np.concatenate([cp[0].T, cp[1].T], 0)
    w1 = prm["cmp_w1"][l].reshape(2, 32, 64, 256).transpose(0, 2, 1, 3).reshape(128, 32, 256)
    w2 = prm["cmp_w2"][l].reshape(2, 2, 128, 64).transpose(2, 0, 1, 3)
    d = {"qin": np.ascontiguousarray(q), "gin": np.ascontiguousarray(gt), "kvin": np.ascontiguousarray(kv),
         "qkg": np.ascontiguousarray(prm["qk_norm_g"][l].reshape(1, 256)), "cpos": np.ascontiguousarray(cpos),
         "w1": np.ascontiguousarray(w1), "w2": np.ascontiguousarray(w2)}
    d.update(k3_consts(TT))
    return d


def run_k3(p, prm, l, TT=T):
    key = ("k3", TT)
    if key not in _CACHE:
        _CACHE[key] = build_k3(TT)
    nc = _CACHE[key]
    maps = []
    for ci in range(NCORES):
        b, rem = divmod(ci, 4)
        g, hh = divmod(rem, 2)
        maps.append(k3_inputs(p[b], g, hh, prm, l, TT))
    res = run_bass_kernel_spmd(nc, maps, core_ids=list(range(NCORES)))
    out = np.empty((NB, TT, 512), np.float32)
    for ci in range(NCORES):
        b, rem = divmod(ci, 4)
        g, hh = divmod(rem, 2)
        h0 = g * 4 + 2 * hh
        out[b, :, h0 * 64:(h0 + 2) * 64] = res.results[ci]["yb"]
    return out


def build_k4a():
    nc = _new_nc()
    NT = 16
    dr = lambda n, s, kind="ExternalInput": nc.dram_tensor(n, s, F32, kind=kind).ap()
    x = dr("x", [NT * 128, D]); ya = dr("ya", [NT * 128, 512]); yb = dr("yb", [NT * 128, 512]); pm = dr("pm", [NT * 128, 2048])
    gm = dr("gm", [1, D]); wua = dr("wua", [512, D]); wub = dr("wub", [512, D]); wo = dr("wo", [D, D]); ident = dr("ident", [128, 128])
    x1 = dr("x1", [NT * 128, D], "ExternalOutput")
    with ExitStack() as es:
        P = Prog2(nc, es)
        IDT = P.tile("IDT", [128, 128]); GMB = P.tile("GMB", [128, D])
        WUA = P.tile("WUA", [128, 4, D]); WUB = P.tile("WUB", [128, 4, D]); WO = P.tile("WO", [128, 8, D])
        P.load("sp", IDT[:], ident[:, :]); P.load("act", GMB[:], gm.partition_broadcast(128))
        P.load("sp", WUA[:], wua.rearrange("(kc kp) n -> kp kc n", kp=128))
        P.load("act", WUB[:], wub.rearrange("(kc kp) n -> kp kc n", kp=128))
        P.load("sp", WO[:, 0:4, :], wo[0:512, :].rearrange("(kc kp) n -> kp kc n", kp=128))
        P.load("act", WO[:, 4:8, :], wo[512:1024, :].rearrange("(kc kp) n -> kp kc n", kp=128))
        banks = [P.tile(f"bank{i}", [128, 512], psum=True) for i in range(8)]
        bi = [0]

        def nb():
            b = banks[bi[0] % 8]; bi[0] += 1
            return b
        Xt = [P.tile(f"X{i}", [128, D]) for i in range(2)]
        YAt = [P.tile(f"YA{i}", [128, 512]) for i in range(2)]
        YBt = [P.tile(f"YB{i}", [128, 512]) for i in range(2)]
        PMt = [P.tile(f"PM{i}", [128, 2048]) for i in range(2)]
        YTt = [P.tile(f"YT{i}", [128, 8, 128]) for i in range(2)]
        MIXt = [P.tile(f"MIX{i}", [128, D]) for i in range(2)]
        TMPt = [P.tile(f"TMP{i}", [128, 512]) for i in range(2)]
        MTt = [P.tile(f"MT{i}", [128, 8, 128]) for i in range(2)]
        for ti in range(NT):
            k = ti % 2
            rs = slice(ti * 128, (ti + 1) * 128)
            X = Xt[k]; YA = YAt[k]; YB = YBt[k]; PM = PMt[k]; YT = YTt[k]; MIX = MIXt[k]; MT = MTt[k]
            P.load("sp", X[:], x[rs, :]); P.load("act", YA[:], ya[rs, :]); P.load("sp", YB[:], yb[rs, :]); P.load("act", PM[:], pm[rs, :])
            for j, Y in enumerate((YA, YB)):
                b = nb()
                for c in range(4):
                    P.tr(b[:, c * 128:(c + 1) * 128], Y[:, c * 128:(c + 1) * 128], IDT[:])
                P.copy(YT.v(YT.t[:, j * 4:(j + 1) * 4, :].rearrange("p a b -> p (a b)")), b[:], e="act" if j else "dve")
            P.act(PM[:], PM[:], AF.Sigmoid)
            for nh in range(2):
                cs = slice(nh * 512, (nh + 1) * 512)
                ba = nb(); bb = nb()
                for kc in range(4):
                    P.mm(ba[:], YT[:, kc, :], WUA[:, kc, cs], start=(kc == 0), stop=(kc == 3))
                for kc in range(4):
                    P.mm(bb[:], YT[:, 4 + kc, :], WUB[:, kc, cs], start=(kc == 0), stop=(kc == 3))
                TMP = TMPt[nh]
                P.tt(MIX[:, cs], ba[:], PM[:, nh * 512:(nh + 1) * 512], ALU.mult)
                P.tt(TMP[:], bb[:], PM[:, 1024 + nh * 512:1024 + (nh + 1) * 512], ALU.mult)
                P.tt(MIX[:, cs], MIX[:, cs], TMP[:], ALU.add, e="pool")
            for half in range(2):
                b = nb()
                for c in range(4):
                    kc = half * 4 + c
                    P.tr(b[:, c * 128:(c + 1) * 128], MIX[:, kc * 128:(kc + 1) * 128], IDT[:])
                P.copy(MT.v(MT.t[:, half * 4:(half + 1) * 4, :].rearrange("p a b -> p (a b)")), b[:], e="act" if half else "dve")
            for nh in range(2):
                cs = slice(nh * 512, (nh + 1) * 512)
                b = nb()
                for kc in range(8):
                    P.mm(b[:], MT[:, kc, :], WO[:, kc, cs], start=(kc == 0), stop=(kc == 7))
                TMP = TMPt[nh]
                P.tt(TMP[:], b[:], GMB[:, cs], ALU.mult)
                P.tt(X[:, cs], X[:, cs], TMP[:], ALU.add, e="pool")
            P.store("sp" if k else "act", x1[rs, :], X[:])
        P.finish()
        print("k4a ninst", P.ninst, "nsem", P.nsem)
    return nc


def run_k4a(xfull, ya, yb, p, gate_mix, prm, l):
    if "k4a" not in _CACHE:
        _CACHE["k4a"] = build_k4a()
    nc = _CACHE["k4a"]
    ident = np.eye(128, dtype=np.float32)
    maps = []
    for i in range(NCORES):
        b, tq = divmod(i, 4)
        ts_ = slice(tq * 2048, (tq + 1) * 2048)
        maps.append({"x": np.ascontiguousarray(xfull[b, ts_]), "ya": np.ascontiguousarray(ya[b, ts_]),
                     "yb": np.ascontiguousarray(yb[b, ts_]), "pm": np.ascontiguousarray(p[b, ts_, 3096:5144]),
                     "gm": gate_mix[b][None].copy(), "wua": prm["w_up_rwkv"][l], "wub": prm["w_up_nsa"][l],
                     "wo": prm["w_out"][l], "ident": ident})
    res = run_bass_kernel_spmd(nc, maps, core_ids=list(range(NCORES)))
    out = np.empty((NB, T, D), np.float32)
    for i in range(NCORES):
        b, tq = divmod(i, 4)
        out[b, tq * 2048:(tq + 1) * 2048] = res.results[i]["x1"]
    return out


def build_k4b():
    nc = _new_nc()
    NT = 16; NE = 16
    dr = lambda n, s, kind="ExternalInput": nc.dram_tensor(n, s, F32, kind=kind).ap()
    x1 = dr("x1", [NT * 128, D]); g = dr("g", [1, D]); sc = dr("sc", [1, D]); sh = dr("sh", [1, D]); gf = dr("gf", [1, D])
    rw = dr("rw", [D, 16]); rb = dr("rb", [1, 16]); ident = dr("ident", [128, 128])
    wg = dr("wg", [NE, D, 512]); wu = dr("wu", [NE, D, 512]); wd = dr("wd", [NE, 512, D])
    xo = dr("xo", [NT * 128, D], "ExternalOutput")
    with ExitStack() as es, nc.allow_low_precision("bf16 expert matmuls, fp32 accumulation"):
        P = Prog2(nc, es)
        IDT = P.tile("IDT", [128, 128]); G2S = P.tile("G2S", [128, D]); SC = P.tile("SC", [128, D]); SH = P.tile("SH", [128, D])
        GF = P.tile("GF", [128, D]); RW = P.tile("RW", [128, 8, 16]); RB = P.tile("RB", [128, 16])
        P.load("sp", IDT[:], ident[:, :]); P.load("act", G2S[:], g.partition_broadcast(128)); P.load("sp", SC[:], sc.partition_broadcast(128))
        P.load("act", SH[:], sh.partition_broadcast(128)); P.load("sp", GF[:], gf.partition_broadcast(128))
        P.load("act", RW[:], rw.rearrange("(kc kp) n -> kp kc n", kp=128)); P.load("sp", RB[:], rb.partition_broadcast(128))
        P.stt(G2S[:], SC[:], 1.0, G2S[:], ALU.add, ALU.mult)
        banks = [P.tile(f"bank{i}", [128, 512], psum=True) for i in range(8)]
        bi = [0]

        def nb():
            b = banks[bi[0] % 8]; bi[0] += 1
            return b
        ACC = [P.tile(f"ACC{t}", [128, D]) for t in range(8)]
        H2T = P.tile("H2T", [128, 8, 1024], BF16)
        CW = P.tile("CW", [128, 8, 16])
        STG = [P.tile(f"STG{i}", [128, 4096]) for i in range(2)]
        WB = [[P.tile(f"WB{m}_{p}", [128, 4096], BF16) for p in range(2)] for m in range(3)]
        H2t = [P.tile(f"H2{i}", [128, D]) for i in range(2)]
        H2Tf = [P.tile(f"H2Tf{i}", [128, 8, 128]) for i in range(2)]
        junk = P.tile("junk", [128, D])
        ST = [P.tile(f"ST{i}", [128, 2]) for i in range(2)]
        SCO = [P.tile(f"SCO{i}", [128, 16]) for i in range(2)]
        SS = [P.tile(f"SS{i}", [128, 4, 4]) for i in range(2)]
        PSm = [P.tile(f"PSm{i}", [128, 4, 6]) for i in range(2)]
        GS = [P.tile(f"GS{i}", [128, 4]) for i in range(2)]
        GMx = [P.tile(f"GMx{i}", [128, 2]) for i in range(2)]
        E2 = [P.tile(f"E2{i}", [128, 4, 4]) for i in range(2)]
        SIL = [P.tile(f"SIL{i}", [128, 512]) for i in range(2)]
        HID = [P.tile(f"HID{i}", [128, 4, 512], BF16) for i in range(2)]
        stg_i = [0]
        for tg in range(2):
            for t in range(8):
                ti = tg * 8 + t
                k = t % 2
                A = ACC[t]
                P.load("sp" if k else "act", A[:], x1[ti * 128:(ti + 1) * 128, :])
                S = ST[k]; H2 = H2t[k]; HF = H2Tf[k]
                P.act(junk[:], A[:], AF.Square, accum_out=S[:, 0:1])
                P.ts(S[:, 1:2], S[:, 0:1], 1.0 / D, 1e-6, ALU.mult, ALU.add)
                P.act(S[:, 1:2], S[:, 1:2], AF.Sqrt)
                P.recip(S[:, 1:2], S[:, 1:2])
                P.stt(H2[:], A[:], S[:, 1:2], G2S[:], ALU.mult, ALU.mult)
                P.tt(H2[:], H2[:], SH[:], ALU.add, e="pool")
                for half in range(2):
                    b = nb()
                    for c in range(4):
                        kc = half * 4 + c
                        P.tr(b[:, c * 128:(c + 1) * 128], H2[:, kc * 128:(kc + 1) * 128], IDT[:])
                    bv = b.v(b.t[:, :].rearrange("p (a b) -> p a b", a=4))
                    P.copy(HF[:, half * 4:(half + 1) * 4, :], bv)
                    P.copy(H2T[:, half * 4:(half + 1) * 4, t * 128:(t + 1) * 128], bv, e="act")
                b = nb()
                for kc in range(8):
                    P.mm(b[:, 0:16], HF[:, kc, :], RW[:, kc, :], start=(kc == 0), stop=(kc == 7))
                sco = SCO[k]; ss = SS[k]; psm = PSm[k]; gs = GS[k]; gmx = GMx[k]; e2 = E2[k]
                P.act(sco[:], b[:, 0:16], AF.Sigmoid)
                ssf = ss.v(ss.t[:, :, :].rearrange("p a b -> p (a b)"))
                P.tt(ssf, sco[:], RB[:], ALU.add)
                P.tt(psm[:, :, 0:3], ss[:, :, 0:3], ss[:, :, 1:4], ALU.add)
                P.tt(psm[:, :, 3:5], ss[:, :, 0:2], ss[:, :, 2:4], ALU.add)
                P.tt(psm[:, :, 5:6], ss[:, :, 0:1], ss[:, :, 3:4], ALU.add)
                P.red(gs[:], psm[:], ALU.max)
                P.red(gmx[:, 0:1], gs[:], ALU.max)
                P.ts(gs[:], gs[:], gmx[:, 0:1], None, ALU.is_ge)
                P.tt(psm[:, :, 0:3], ss[:, :, 0:3], ss[:, :, 1:4], ALU.min)
                P.tt(psm[:, :, 3:5], ss[:, :, 0:2], ss[:, :, 2:4], ALU.min)
                P.tt(psm[:, :, 5:6], ss[:, :, 0:1], ss[:, :, 3:4], ALU.min)
                P.red(e2[:, :, 0], psm[:], ALU.max)
                thr = E2[k].v(E2[k].t[:, :, 0:1].to_broadcast([128, 4, 4]))
                P.tt(psm[:, :, 0:4], ss[:], thr, ALU.is_ge)
                P.tt(psm[:, :, 0:4], psm[:, :, 0:4], _bc(gs, gs.t[:, :], 2, [128, 4, 4]), ALU.mult)
                scv = sco.v(sco.t[:, :].rearrange("p (a b) -> p a b", b=4))
                P.tt(e2[:], psm[:, :, 0:4], scv, ALU.mult)
                P.red(gmx[:, 1:2], E2[k].v(E2[k].t[:, :, :].rearrange("p a b -> p (a b)")), ALU.add)
                P.recip(gmx[:, 1:2], gmx[:, 1:2])
                P.ts(CW[:, t, :], E2[k].v(E2[k].t[:, :, :].rearrange("p a b -> p (a b)")), gmx[:, 1:2], None, ALU.mult)
            for e in range(NE):
                p_ = e % 2
                for m, (wsrc, pat) in enumerate(((wg, 8), (wu, 8), (wd, 4))):
                    S_ = STG[stg_i[0] % 2]; stg_i[0] += 1
                    sv = S_.v(S_.t[:, :].rearrange("p (kc n) -> p kc n", kc=pat))
                    P.load("sp" if m % 2 == 0 else "act", sv, wsrc[e].rearrange("(kc kp) n -> kp kc n", kp=128))
                    Wb = WB[m][p_]
                    if m < 2:
                        P.copy(Wb[:], S_[:], e="pool")
                    else:
                        P.tt(Wb.v(Wb.t[:, :].rearrange("p (kc n) -> p kc n", kc=4)), sv, _bc(GF, GF.t[:, :], 1, [128, 4, D]), ALU.mult, e="pool")
                WG = WB[0][p_].v(WB[0][p_].t[:, :].rearrange("p (kc n) -> p kc n", kc=8))
                WU = WB[1][p_].v(WB[1][p_].t[:, :].rearrange("p (kc n) -> p kc n", kc=8))
                WD = WB[2][p_].v(WB[2][p_].t[:, :].rearrange("p (kc n) -> p kc n", kc=4))
                for sub in range(2):
                    hid = HID[sub]
                    tsl = slice(sub * 512, (sub + 1) * 512)
                    for c in range(4):
                        bg = nb(); bu = nb()
                        for kc in range(8):
                            P.mm(bg[:], WG[:, kc, c * 128:(c + 1) * 128], H2T[:, kc, tsl], start=(kc == 0), stop=(kc == 7))
                        for kc in range(8):
                            P.mm(bu[:], WU[:, kc, c * 128:(c + 1) * 128], H2T[:, kc, tsl], start=(kc == 0), stop=(kc == 7))
                        sil = SIL[c % 2]
                        P.act(sil[:], bg[:], AF.Silu)
                        P.tt(hid[:, c, :], sil[:], bu[:], ALU.mult)
                    for t4 in range(4):
                        t = sub * 4 + t4
                        for nh in range(2):
                            cs = slice(nh * 512, (nh + 1) * 512)
                            bd = nb()
                            for c in range(4):
                                P.mm(bd[:], hid[:, c, t4 * 128:(t4 + 1) * 128], WD[:, c, cs], start=(c == 0), stop=(c == 3))
                            P.stt(ACC[t][:, cs], bd[:], CW[:, t, e:e + 1], ACC[t][:, cs], ALU.mult, ALU.add)
            for t in range(8):
                ti = tg * 8 + t
                P.store("sp" if t % 2 else "act", xo[ti * 128:(ti + 1) * 128, :], ACC[t][:])
        P.finish()
        print("k4b ninst", P.ninst, "nsem", P.nsem)
    return nc


def run_k4b(x1, g2, sc2, sh2, gate_ffn, prm, l):
    if "k4b" not in _CACHE:
        _CACHE["k4b"] = build_k4b()
    nc = _CACHE["k4b"]
    ident = np.eye(128, dtype=np.float32)
    maps = []
    for i in range(NCORES):
        b, tq = divmod(i, 4)
        ts_ = slice(tq * 2048, (tq + 1) * 2048)
        maps.append({"x1": np.ascontiguousarray(x1[b, ts_]), "g": g2[None].copy(), "sc": sc2[b][None].copy(),
                     "sh": sh2[b][None].copy(), "gf": gate_ffn[b][None].copy(), "rw": prm["router_w"],
                     "rb": prm["router_b"][None].copy(), "ident": ident,
                     "wg": prm["exp_w_gate"][l], "wu": prm["exp_w_up"][l], "wd": prm["exp_w_down"][l]})
    res = run_bass_kernel_spmd(nc, maps, core_ids=list(range(NCORES)))
    out = np.empty((NB, T, D), np.float32)
    for i in range(NCORES):
        b, tq = divmod(i, 4)
        out[b, tq * 2048:(tq + 1) * 2048] = res.results[i]["xo"]
    return out


def kernel(**inputs):
    prm = {k: np.asarray(v) for k, v in inputs.items()}
    x = prm["x"].astype(np.float32, copy=False)
    mod = run_k0(prm["c"], prm["w_ada"], prm["b_ada"])
    for l in range(2):
        sh1, sc1, gate_mix, sh2, sc2, gate_ffn = [np.ascontiguousarray(a) for a in np.split(mod[l], 6, axis=-1)]
        p = run_k1(x, prm["norm_g"][l, 0], sc1, sh1, prm["w_in"][l], prm["b_in"][l])
        ya = run_k2(p, prm, l)
        yb = run_k3(p, prm, l)
        x1 = run_k4a(x, ya, yb, p, gate_mix, prm, l)
        x = run_k4b(x1, prm["norm_g"][l, 1], sc2, sh2, gate_ffn, prm, l)
    return x
```

```python
import numpy as np
import concourse.bass as bass
import concourse.mybir as mybir
from contextlib import ExitStack

F32 = mybir.dt.float32
BF16 = mybir.dt.bfloat16
I32 = mybir.dt.int32
U32 = mybir.dt.uint32
AF = mybir.ActivationFunctionType
ALU = mybir.AluOpType
AX = mybir.AxisListType


class Buf:
    __slots__ = ("name", "w", "r", "dsem", "dcnt", "t", "excl")

    def __init__(self, name, t=None):
        self.name = name
        self.w = None
        self.r = []
        self.dsem = None
        self.dcnt = 0
        self.t = t
        self.excl = False


class Prog:
    ENG = ("pe", "dve", "act", "pool", "sp")

    def __init__(self, nc, es: ExitStack):
        self.nc = nc
        self.es = es
        self.eng = {"pe": nc.tensor, "dve": nc.vector, "act": nc.scalar,
                    "pool": nc.gpsimd, "sp": nc.sync}
        self.sem = {}
        self.cnt = {}
        for e in self.ENG:
            self.sem[e] = es.enter_context(nc.semaphore("s_" + e))
            self.cnt[e] = 0
        self.waited = {e: {} for e in self.ENG}
        self.out_tokens = []
        self.nsem = 5
        self.ninst = 0

    def sb(self, name, shape, dt=F32):
        t = self.es.enter_context(self.nc.sbuf_tensor(name, list(shape), dt))
        return t

    def ps(self, name, shape, dt=F32):
        t = self.es.enter_context(self.nc.psum_tensor(name, list(shape), dt))
        return t

    def buf(self, name, t=None):
        return Buf(name, t)

    def _wait(self, e, deps):
        w = self.waited[e]
        best = {}
        for d in deps:
            if d is None:
                continue
            sem, val, pe = d
            if e == "pe" and pe == "pe":
                continue
            k = id(sem)
            if w.get(k, 0) >= val:
                continue
            if k not in best or best[k][1] < val:
                best[k] = (sem, val)
        for k, (sem, val) in best.items():
            self.eng[e].wait_ge(sem, val)
            w[k] = val
            self.ninst += 1

    def op(self, e, build, reads=(), writes=()):
        deps = []
        for b in reads:
            deps.append(b.w)
            if b.excl:
                deps.extend(r for r in b.r if r[2] != e)
        for b in writes:
            deps.append(b.w)
            deps.extend(b.r)
        self._wait(e, deps)
        ins = build(self.eng[e])
        self.cnt[e] += 1
        ins.then_inc(self.sem[e], 1)
        tok = (self.sem[e], self.cnt[e], e)
        self.ninst += 1
        for b in reads:
            b.r.append(tok)
            if len(b.r) > 64:
                b.r = self._compact(b.r)
        for b in writes:
            b.w = tok
            b.r = []
        return tok

    @staticmethod
    def _compact(rs):
        best = {}
        for sem, val, e in rs:
            k = id(sem)
            if k not in best or best[k][1] < val:
                best[k] = (sem, val, e)
        return list(best.values())

    def dma(self, q, out, in_, sbuf_buf, reads=(), writes=(), is_output=False, **kw):
        deps = []
        for b in reads:
            deps.append(b.w)
        for b in writes:
            deps.append(b.w)
            deps.extend(b.r)
        self._wait(q, deps)
        b0 = sbuf_buf
        if b0.dsem is None:
            b0.dsem = self.es.enter_context(self.nc.semaphore("d_" + b0.name))
            self.nsem += 1
        ins = self.eng[q].dma_start(out=out, in_=in_, **kw)
        b0.dcnt += 16
        ins.then_inc(b0.dsem, 16)
        tok = (b0.dsem, b0.dcnt, "dma")
        self.ninst += 1
        for b in reads:
            b.r.append(tok)
        for b in writes:
            b.w = tok
            b.r = []
        if is_output:
            self.out_tokens.append(tok)
        return tok

    def finish(self):
        self._wait("sp", self.out_tokens)
        deps = [(self.sem[e], self.cnt[e], e) for e in self.ENG if e != "sp" and self.cnt[e] > 0]
        self._wait("sp", deps)


class View:
    __slots__ = ("tile", "ap")

    def __init__(self, tile, ap):
        self.tile = tile
        self.ap = ap

    def __getitem__(self, idx):
        return View(self.tile, self.ap[idx])

    @property
    def t(self):
        return self.ap

    def v(self, ap):
        return View(self.tile, ap)


class Tile:
    def __init__(self, P, name, shape, dt=F32, psum=False):
        self.P = P
        self.name = name
        self.t = (P.ps if psum else P.sb)(name, shape, dt)
        self.buf = Buf(name)
        self.buf.excl = bool(psum)
        self.shape = list(shape)

    def __getitem__(self, idx):
        return View(self, self.t[idx])

    def v(self, ap):
        return View(self, ap)


def _bufs(views):
    out = []
    for v in views:
        if isinstance(v, View):
            if v.tile.buf not in out:
                out.append(v.tile.buf)
        elif isinstance(v, Tile):
            if v.buf not in out:
                out.append(v.buf)
    return out


def _ap(v):
    return v.ap if isinstance(v, View) else v


class Prog2(Prog):
    def tile(self, name, shape, dt=F32, psum=False):
        return Tile(self, name, shape, dt, psum)

    def gop(self, e, fn, outs, ins):
        return self.op(e, fn, reads=_bufs(ins), writes=_bufs(outs))

    def act(self, out, in_, func, bias=0.0, scale=1.0, accum_out=None, e="act"):
        ins = [in_, bias, scale]
        outs = [out] + ([accum_out] if accum_out is not None else [])
        kw = {}
        if accum_out is not None:
            kw["accum_out"] = _ap(accum_out)
        return self.gop(e, lambda E: E.activation(out=_ap(out), in_=_ap(in_), func=func,
                                                  bias=_ap(bias), scale=_ap(scale), **kw), outs, ins)

    def tt(self, out, a, b, op, e="dve"):
        return self.gop(e, lambda E: E.tensor_tensor(out=_ap(out), in0=_ap(a), in1=_ap(b), op=op), [out], [a, b])

    def ts(self, out, a, s1, s2, op0, op1=None, accum_out=None, e="dve"):
        kw = {}
        if op1 is not None:
            kw["op1"] = op1
        outs = [out]
        if accum_out is not None:
            kw["accum_out"] = _ap(accum_out)
            outs.append(accum_out)
        return self.gop(e, lambda E: E.tensor_scalar(out=_ap(out), in0=_ap(a), scalar1=_ap(s1),
                                                     scalar2=_ap(s2) if s2 is not None else None,
                                                     op0=op0, **kw), outs, [a, s1, s2])

    def stt(self, out, in0, scalar, in1, op0, op1, accum_out=None, e="dve"):
        kw = {}
        outs = [out]
        if accum_out is not None:
            kw["accum_out"] = _ap(accum_out)
            outs.append(accum_out)
        return self.gop(e, lambda E: E.scalar_tensor_tensor(out=_ap(out), in0=_ap(in0), scalar=_ap(scalar),
                                                            in1=_ap(in1), op0=op0, op1=op1, **kw),
                        outs, [in0, scalar, in1])

    def copy(self, out, in_, e="dve"):
        if e == "act":
            return self.gop(e, lambda E: E.copy(out=_ap(out), in_=_ap(in_)), [out], [in_])
        return self.gop(e, lambda E: E.tensor_copy(out=_ap(out), in_=_ap(in_)), [out], [in_])

    def memset(self, out, val, e="dve"):
        return self.gop(e, lambda E: E.memset(_ap(out), val), [out], [])

    def red(self, out, in_, op, axis=AX.X, e="dve"):
        return self.gop(e, lambda E: E.tensor_reduce(out=_ap(out), in_=_ap(in_), axis=axis, op=op), [out], [in_])

    def recip(self, out, in_):
        return self.gop("dve", lambda E: E.reciprocal(out=_ap(out), in_=_ap(in_)), [out], [in_])

    def mm(self, out, lhsT, rhs, start=True, stop=True):
        return self.gop("pe", lambda E: E.matmul(_ap(out), _ap(lhsT), _ap(rhs), start=start, stop=stop),
                        [out], [lhsT, rhs])

    def tr(self, out, in_, ident):
        return self.gop("pe", lambda E: E.transpose(_ap(out), _ap(in_), _ap(ident)), [out], [in_, ident])

    def load(self, q, view, dram_ap, **kw):
        return self.dma(q, view.ap, dram_ap, view.tile.buf, writes=[view.tile.buf], **kw)

    def store(self, q, dram_ap, view, is_output=True, **kw):
        return self.dma(q, dram_ap, view.ap, view.tile.buf, reads=[view.tile.buf], is_output=is_output, **kw)


from concourse.bass_utils import run_bass_kernel_spmd

D = 1024
T = 8192
NB = 2
IN_COLS = 5144
NCORES = 8
_CACHE = {}


def _new_nc():
    return bass.Bass("TRN2", target_bir_lowering=False)


def build_k0():
    nc = _new_nc()
    NCOL = 1536
    cT = nc.dram_tensor("cT", [128, 8, 2], F32, kind="ExternalInput").ap()
    w = nc.dram_tensor("w", [1024, NCOL], F32, kind="ExternalInput").ap()
    b = nc.dram_tensor("b", [1, NCOL], F32, kind="ExternalInput").ap()
    y = nc.dram_tensor("y", [2, NCOL], F32, kind="ExternalOutput").ap()
    with ExitStack() as es:
        P = Prog2(nc, es)
        ct = P.tile("ct", [128, 8, 2])
        cs = P.tile("cs", [128, 8, 2])
        wt = P.tile("wt", [128, 8, NCOL])
        bt = P.tile("bt", [2, NCOL])
        yt = P.tile("yt", [2, NCOL])
        P.load("sp", ct[:], cT[:, :, :])
        P.load("act", wt[:], w.rearrange("(kc kp) n -> kp kc n", kp=128))
        P.load("sp", bt[:], b.partition_broadcast(2))
        P.act(cs[:], ct[:], AF.Silu)
        for n in range(3):
            ps = P.tile(f"ps{n}", [2, 512], psum=True)
            for kc in range(8):
                P.mm(ps[:], cs[:, kc, :], wt[:, kc, n * 512:(n + 1) * 512], start=(kc == 0), stop=(kc == 7))
            P.tt(yt[:, n * 512:(n + 1) * 512], ps[:], bt[:, n * 512:(n + 1) * 512], ALU.add)
        P.store("sp", y[:, :], yt[:])
        P.finish()
    return nc


def run_k0(c, w_ada, b_ada):
    if "k0" not in _CACHE:
        _CACHE["k0"] = build_k0()
    nc = _CACHE["k0"]
    L = w_ada.shape[0]
    wcat = np.concatenate([w_ada[l] for l in range(L)], axis=1)
    bcat = np.concatenate([b_ada[l] for l in range(L)], axis=0)[None]
    cT = np.ascontiguousarray(c.T.reshape(8, 128, 2).transpose(1, 0, 2))
    maps = []
    for i in range(NCORES):
        sl = slice(i * 1536, (i + 1) * 1536)
        maps.append({"cT": cT, "w": np.ascontiguousarray(wcat[:, sl]), "b": np.ascontiguousarray(bcat[:, sl])})
    res = run_bass_kernel_spmd(nc, maps, core_ids=list(range(NCORES)))
    y = np.concatenate([r["y"] for r in res.results], axis=1)
    return y.reshape(2, L, 6144).transpose(1, 0, 2)


def build_k1():
    nc = _new_nc()
    NT = 16
    x = nc.dram_tensor("x", [NT * 128, D], F32, kind="ExternalInput").ap()
    g = nc.dram_tensor("g", [1, D], F32, kind="ExternalInput").ap()
    sc = nc.dram_tensor("sc", [1, D], F32, kind="ExternalInput").ap()
    sh = nc.dram_tensor("sh", [1, D], F32, kind="ExternalInput").ap()
    w = nc.dram_tensor("w", [D, IN_COLS], F32, kind="ExternalInput").ap()
    b = nc.dram_tensor("b", [1, IN_COLS], F32, kind="ExternalInput").ap()
    ident = nc.dram_tensor("ident", [128, 128], F32, kind="ExternalInput").ap()
    p = nc.dram_tensor("p", [NT * 128, IN_COLS], F32, kind="ExternalOutput").ap()
    with ExitStack() as es:
        P = Prog2(nc, es)
        idt = P.tile("idt", [128, 128])
        G = P.tile("G", [128, D]); SC = P.tile("SC", [128, D]); SH = P.tile("SH", [128, D])
        B = P.tile("B", [128, IN_COLS])
        hT = [P.tile(f"hT{i}", [128, 8, 128]) for i in range(NT)]
        P.load("sp", idt[:], ident[:, :])
        P.load("sp", G[:], g.partition_broadcast(128))
        P.load("act", SC[:], sc.partition_broadcast(128))
        P.load("sp", SH[:], sh.partition_broadcast(128))
        P.load("act", B[:], b.partition_broadcast(128))
        P.stt(G[:], SC[:], 1.0, G[:], ALU.add, ALU.mult)
        xt = [P.tile(f"xt{i}", [128, D]) for i in range(2)]
        junk = P.tile("junk", [128, D])
        st = [P.tile(f"st{i}", [128, 2]) for i in range(2)]
        pT = [P.tile(f"pT{i}", [128, 4, 128], psum=True) for i in range(2)]
        for ti in range(NT):
            X = xt[ti % 2]; S = st[ti % 2]
            P.load("sp" if ti % 2 == 0 else "act", X[:], x[ti * 128:(ti + 1) * 128, :])
            P.act(junk[:], X[:], AF.Square, accum_out=S[:, 0:1])
            P.ts(S[:, 1:2], S[:, 0:1], 1.0 / D, 1e-6, ALU.mult, ALU.add)
            P.act(S[:, 1:2], S[:, 1:2], AF.Sqrt)
            P.recip(S[:, 1:2], S[:, 1:2])
            P.stt(X[:], X[:], S[:, 1:2], G[:], ALU.mult, ALU.mult)
            P.tt(X[:], X[:], SH[:], ALU.add)
            for half in range(2):
                pt = pT[half]
                for j in range(4):
                    kc = half * 4 + j
                    P.tr(pt[:, j, :], X[:, kc * 128:(kc + 1) * 128], idt[:])
                P.copy(hT[ti][:, half * 4:(half + 1) * 4, :], pt[:], e="act" if half else "dve")
        NCH = (IN_COLS + 511) // 512
        wt = [P.tile(f"wt{i}", [128, 8, 512]) for i in range(2)]
        po = [P.tile(f"po{i}", [128, 512], psum=True) for i in range(4)]
        ot = [P.tile(f"ot{i}", [128, 512]) for i in range(4)]
        wv = w.rearrange("(kc kp) n -> kp kc n", kp=128)
        k = 0
        for ci in range(NCH):
            c0 = ci * 512
            cw = min(512, IN_COLS - c0)
            W = wt[ci % 2]
            P.load("sp" if ci % 2 == 0 else "act", W[:, :, :cw], wv[:, :, c0:c0 + cw])
            for ti in range(NT):
                ps = po[k % 4]; o = ot[k % 4]
                for kc in range(8):
                    P.mm(ps[:, :cw], hT[ti][:, kc, :], W[:, kc, :cw], start=(kc == 0), stop=(kc == 7))
                P.tt(o[:, :cw], ps[:, :cw], B[:, c0:c0 + cw], ALU.add, e="dve")
                P.store("sp" if k % 2 == 0 else "act", p[ti * 128:(ti + 1) * 128, c0:c0 + cw], o[:, :cw])
                k += 1
        P.finish()
    return nc


def run_k1(xfull, g, sc, sh, w, b):
    if "k1" not in _CACHE:
        _CACHE["k1"] = build_k1()
    nc = _CACHE["k1"]
    ident = np.eye(128, dtype=np.float32)
    maps = []
    for i in range(NCORES):
        bi, tq = divmod(i, 4)
        maps.append({"x": np.ascontiguousarray(xfull[bi, tq * 2048:(tq + 1) * 2048]),
                     "g": g[None].copy(), "sc": sc[bi][None].copy(), "sh": sh[bi][None].copy(),
                     "w": w, "b": b[None].copy(), "ident": ident})
    res = run_bass_kernel_spmd(nc, maps, core_ids=list(range(NCORES)))
    out = np.empty((NB, T, IN_COLS), np.float32)
    for i in range(NCORES):
        bi, tq = divmod(i, 4)
        out[bi, tq * 2048:(tq + 1) * 2048] = res.results[i]["p"]
    return out


def _bc(tile, ap, axis, shape):
    return tile.v(ap.unsqueeze(axis).to_broadcast(list(shape)))


def k2_consts():
    C = 64
    tri_incl = np.triu(np.ones((C, C), np.float32))
    tri_strict = np.triu(np.ones((C, C), np.float32), 1)
    m = np.concatenate([tri_strict, tri_incl], 1)
    mask128 = np.concatenate([m, m], 0)
    sel0 = np.concatenate([np.eye(64, dtype=np.float32), np.zeros((64, 64), np.float32)], 1)
    shift = np.concatenate([np.zeros((64, 64), np.float32), np.eye(64, dtype=np.float32)], 1)
    return {"ident": np.eye(128, dtype=np.float32), "mask128": mask128,
            "trils": np.ascontiguousarray(tri_strict.T), "tri": tri_incl,
            "ones64": np.ones((64, 64), np.float32), "sel0": sel0, "shift": shift}


def build_k2(TT=T, stop=0, ustop=99):
    nc = _new_nc()
    NG = TT // 256
    dr = lambda n, s, kind="ExternalInput": nc.dram_tensor(n, s, F32, kind=kind).ap()
    pp = dr("pp", [TT + 1, 640]); mu = dr("mu", [1, 640]); vec = dr("vec", [8, 128])
    w2 = dr("w2", [64, 128]); a2 = dr("a2", [64, 128]); g2 = dr("g2", [128, 128])
    ident = dr("ident", [128, 128]); mask128 = dr("mask128", [128, 128]); trils = dr("trils", [64, 64])
    tri = dr("tri", [64, 64]); ones64 = dr("ones64", [64, 64]); sel0 = dr("sel0", [64, 128]); shift = dr("shift", [64, 128])
    ya = dr("ya", [TT, 128], "ExternalOutput")
    with ExitStack() as es:
        P = Prog2(nc, es)
        IDT = P.tile("IDT", [128, 128]); MASK = P.tile("MASK", [128, 128]); TRILS = P.tile("TRILS", [64, 64])
        TRI = P.tile("TRI", [64, 64]); ONES = P.tile("ONES", [64, 64]); SEL0 = P.tile("SEL0", [64, 128]); SHIFT = P.tile("SHIFT", [64, 128])
        W2 = P.tile("W2", [64, 128]); A2 = P.tile("A2", [64, 128]); G2 = P.tile("G2", [128, 128])
        MU = P.tile("MU", [64, 640]); VEC = P.tile("VEC", [64, 8, 128])
        qs = ["sp", "act"]
        for i, (tl, d) in enumerate([(IDT, ident), (MASK, mask128), (TRILS, trils), (TRI, tri), (ONES, ones64),
                                     (SEL0, sel0), (SHIFT, shift), (W2, w2), (A2, a2), (G2, g2)]):
            P.load(qs[i % 2], tl[:], d[:, :])
        P.load("sp", MU[:], mu.partition_broadcast(64))
        for r in range(7):
            P.load(qs[r % 2], VEC[:, r, :], vec[r:r + 1, :].partition_broadcast(64))
        I64 = IDT[0:64, 0:64]
        S4 = [64, 4, 128]
        vb = lambda r: _bc(VEC, VEC.t[:, r, :], 1, S4)
        W0b, A0b, KKb, KAb, RKb, GNGb, GNBb = [vb(r) for r in range(7)]
        banks = [P.tile(f"bank{i}", [128, 512], psum=True) for i in range(8)]
        pk = [0]

        def pbank():
            b = banks[pk[0] % 4]
            pk[0] += 1
            return b

        def t4(name, n=2):
            return [P.tile(f"{name}{i}", S4) for i in range(n)]

        CURt = [P.tile(f"CUR{i}", [64, 4, 640]) for i in range(2)]
        PRVt = [P.tile(f"PRV{i}", [64, 4, 640]) for i in range(2)]
        TWt = t4("TW"); SGt = t4("SG"); LTt = [P.tile(f"LT{i}", [64, 4, 2, 64]) for i in range(2)]
        GTtt = [P.tile(f"GTt{i}", [128, 4, 64]) for i in range(2)]
        LDt = t4("LD"); At = t4("A"); Ggt = t4("Gg"); KKt = t4("KK"); SQt = t4("SQ"); T1t = t4("T1"); KMt = t4("KM"); Bvt = t4("Bv")
        SSt = [P.tile(f"SS{i}", [64, 8]) for i in range(2)]; RKt = [P.tile(f"RK{i}", [64, 8]) for i in range(2)]
        Lst = t4("Ls"); ELt = t4("EL"); ENLt = t4("ENL"); TMPt = t4("TMP"); TMP2t = t4("TMP2")
        DCFt = [P.tile(f"DCF{i}", [64, 8]) for i in range(2)]
        A_t = t4("A_"); BTt = t4("BT"); KTt = t4("KT"); RTt = t4("RT"); BBt = t4("BB"); KBt = t4("KB")
        FMBKt = [P.tile(f"FMBK{i}", [64, 8, 128]) for i in range(2)]
        FMARt = [P.tile(f"FMAR{i}", [64, 8, 128]) for i in range(2)]
        BKSt = [P.tile(f"BKS{i}", [128, 4, 128]) for i in range(2)]
        VVt = [P.tile(f"VV{i}", [128, 4, 128]) for i in range(2)]
        YBt = t4("YB"); YCt = t4("YC"); MNt = [P.tile(f"MN{i}", [64, 8]) for i in range(2)]; VRt = [P.tile(f"VR{i}", [64, 8]) for i in range(2)]
        hk = [0, 0]
        ubi = [0]; hkk = [0]
        BIGA = P.tile("BIGA", [128, 8, 128])
        XA = [P.tile(f"XA{i}", [64, 8, 64]) for i in range(2)]
        XTA = [P.tile(f"XTA{i}", [64, 8, 64]) for i in range(2)]
        PMA = [P.tile(f"PMA{i}", [64, 8, 64]) for i in range(2)]
        W2A = P.tile("W2A", [64, 8, 64]); AHA = P.tile("AHA", [64, 8, 64]); RHA = P.tile("RHA", [64, 8, 64])
        GTA = P.tile("GTA", [64, 8, 64]); TMPG = P.tile("TMPG", [64, 8, 64])
        HA = [P.tile(f"HA{i}", [64, 2, 64]) for i in range(2)]
        P.memset(HA[0][:], 0.0)
        fl = lambda tl: tl.v(tl.t[:, :, :].rearrange("p a b -> p (a b)"))
        v8 = lambda tl: tl.v(tl.t[:, :, :].rearrange("p a (h b) -> p (a h) b", b=64))
        b8 = lambda tl: _bc(tl, tl.t[:, :], 2, [64, 8, 64])
        ppv = lambda lo: pp[lo:lo + 256, :].rearrange("(c t) n -> t c n", t=64)
        NEG = -float(np.exp(-0.5))

        class _Stop(Exception):
            pass

        def early(n, view):
            if stop == n:
                P.store("sp", ya[0:256, :].rearrange("(c t) n -> t c n", t=64), view)
                raise _Stop()

        ctxp = {}

        def prepG(g):
            k = g % 2
            CUR = CURt[k]; PRV = PRVt[k]
            P.load("sp", CUR[:], ppv(1 + g * 256))
            yield
            P.load("act", PRV[:], ppv(g * 256))
            yield
            P.tt(PRV[:], PRV[:], CUR[:], ALU.subtract)
            yield
            P.tt(PRV[:], PRV[:], _bc(MU, MU.t[:, :], 1, [64, 4, 640]), ALU.mult)
            yield
            P.tt(CUR[:], CUR[:], PRV[:], ALU.add, e="pool")
            yield
            Rr = CUR[:, :, 0:128]; Kr = CUR[:, :, 128:256]; Vr = CUR[:, :, 256:384]
            TW = TWt[k]; SG = SGt[k]; LT = LTt[k]; GTt = GTtt[k]
            P.act(TW[:, :, 0:64], CUR[:, :, 384:448], AF.Tanh)
            yield
            P.act(SG[:], CUR[:, :, 512:640], AF.Sigmoid)
            yield
            b1 = pbank(); b1v = b1.v(b1.t[0:64, :].rearrange("p (c w t) -> p c w t", c=4, w=2))
            for c in range(4):
                P.tr(b1.v(b1v.ap[:, c, 0, :]), TW[:, c, 0:64], I64)
                yield
                P.tr(b1.v(b1v.ap[:, c, 1, :]), CUR[:, c, 448:512], I64)
                yield
            P.copy(LT[:], b1v)
            yield
            b2 = pbank(); b2v = b2.v(b2.t[:, 0:256].rearrange("p (c t) -> p c t", c=4))
            for c in range(4):
                P.tr(b2.v(b2v.ap[:, c, :]), SG[:, c, :], I64)
                yield
            P.copy(GTt[:], b2v, e="act")
            yield
            bw = pbank(); bwv = bw.v(bw.t[0:64, :].rearrange("p (c n) -> p c n", c=4))
            for c in range(4):
                P.mm(bw.v(bwv.ap[:, c, :]), LT[:, c, 0, :], W2[:])
                yield
            LD = LDt[k]
            P.tt(LD[:], bwv, W0b, ALU.add)
            yield
            ba = pbank(); bav = ba.v(ba.t[0:64, :].rearrange("p (c n) -> p c n", c=4))
            for c in range(4):
                P.mm(ba.v(bav.ap[:, c, :]), LT[:, c, 1, :], A2[:])
                yield
            A = At[k]
            P.tt(A[:], bav, A0b, ALU.add)
            yield
            bg = pbank(); bgv = bg.v(bg.t[0:64, :].rearrange("p (c n) -> p c n", c=4))
            for c in range(4):
                P.mm(bg.v(bgv.ap[:, c, :]), GTt[:, c, :], G2[:])
                yield
            Gg = Ggt[k]
            P.copy(Gg[:], bgv, e="act")
            yield
            P.act(LD[:], LD[:], AF.Sigmoid)
            yield
            P.act(A[:], A[:], AF.Sigmoid)
            yield
            P.ts(LD[:], LD[:], NEG, None, ALU.mult, e="pool")
            yield
            KK = KKt[k]; SQ = SQt[k]; SS = SSt[k]; T1 = T1t[k]; KM = KMt[k]; Bv = Bvt[k]; RK = RKt[k]
            P.tt(KK[:], Kr, KKb, ALU.mult)
            yield
            P.tt(SQ[:], KK[:], KK[:], ALU.mult)
            yield
            P.red(SS[:], v8(SQ), ALU.add)
            yield
            P.act(SS[:], SS[:], AF.Sqrt)
            yield
            P.ts(SS[:], SS[:], 1e-12, None, ALU.max)
            yield
            P.recip(SS[:], SS[:])
            yield
            P.tt(v8(KK), v8(KK), b8(SS), ALU.mult)
            yield
            P.stt(T1[:], A[:], -1.0, KAb, ALU.add, ALU.mult)
            yield
            P.stt(KM[:], T1[:], 1.0, Kr, ALU.add, ALU.mult)
            yield
            P.tt(Bv[:], KK[:], A[:], ALU.mult)
            yield
            P.tt(SQ[:], Rr, KM[:], ALU.mult)
            yield
            P.tt(SQ[:], SQ[:], RKb, ALU.mult)
            yield
            P.red(RK[:], v8(SQ), ALU.add)
            yield
            Ls = Lst[k]; EL = ELt[k]; ENL = ENLt[k]; TMP = TMPt[k]; TMP2 = TMP2t[k]; DCF = DCFt[k]
            bL = pbank(); bLv = bL.v(bL.t[0:64, :].rearrange("p (c n) -> p c n", c=4))
            P.mm(bL[0:64, :], TRI[:], fl(LD))
            yield
            P.copy(Ls[:], bLv)
            yield
            bC = pbank(); bCv = bC.v(bC.t[0:64, :].rearrange("p (c n) -> p c n", c=4))
            P.mm(bC[0:64, :], ONES[:], fl(LD))
            yield
            P.tt(TMP2[:], bCv, Ls[:], ALU.subtract)
            yield
            bD = pbank()
            for u in range(8):
                c, h = divmod(u, 2)
                P.mm(bD[0:64, u:u + 1], LD[:, c, h * 64:(h + 1) * 64], ONES[:, 0:1])
                yield
            P.act(DCF[:], bD[0:64, 0:8], AF.Exp)
            yield
            P.act(EL[:], Ls[:], AF.Exp)
            yield
            P.act(ENL[:], Ls[:], AF.Exp, scale=-1.0)
            yield
            P.tt(TMP[:], Ls[:], LD[:], ALU.subtract)
            yield
            P.act(TMP[:], TMP[:], AF.Exp)
            yield
            P.act(TMP2[:], TMP2[:], AF.Exp)
            yield
            A_ = A_t[k]; BT = BTt[k]; KT = KTt[k]; RT = RTt[k]; BB = BBt[k]; KB = KBt[k]
            P.stt(A_[:], KK[:], -1.0, TMP[:], ALU.mult, ALU.mult)
            yield
            P.tt(BT[:], Bv[:], ENL[:], ALU.mult)
            yield
            P.tt(KT[:], KM[:], ENL[:], ALU.mult, e="pool")
            yield
            P.tt(RT[:], Rr, EL[:], ALU.mult)
            yield
            P.tt(BB[:], Bv[:], TMP2[:], ALU.mult, e="pool")
            yield
            P.tt(KB[:], KM[:], TMP2[:], ALU.mult)
            yield
            FMBK = FMBKt[k]; FMAR = FMARt[k]; BKS = BKSt[k]; VV = VVt[k]
            for half in range(2):
                pb = pbank(); pbv = pb.v(pb.t[0:64, :].rearrange("p (u n) -> p u n", u=4))
                pa_ = pbank(); pav = pa_.v(pa_.t[0:64, :].rearrange("p (u n) -> p u n", u=4))
                for uu in range(4):
                    u = half * 4 + uu
                    c, h = divmod(u, 2)
                    hs = slice(h * 64, (h + 1) * 64)
                    P.tr(pb.v(pbv.ap[:, uu, 0:64]), BT[:, c, hs], I64)
                    yield
                    P.tr(pb.v(pbv.ap[:, uu, 64:128]), KT[:, c, hs], I64)
                    yield
                    P.tr(pa_.v(pav.ap[:, uu, 0:64]), A_[:, c, hs], I64)
                    yield
                    P.tr(pa_.v(pav.ap[:, uu, 64:128]), RT[:, c, hs], I64)
                    yield
                P.copy(FMBK[:, half * 4:(half + 1) * 4, :], pbv)
                yield
                P.copy(FMAR[:, half * 4:(half + 1) * 4, :], pav, e="act")
                yield
            ps1 = pbank()
            P.mm(ps1[:], SEL0[:], fl(BB), start=True, stop=False)
            yield
            P.mm(ps1[:], SHIFT[:], fl(KB), start=False, stop=True)
            yield
            P.copy(fl(BKS), ps1[:])
            yield
            ps2 = pbank(); ps2v = ps2.v(ps2.t[:, :].rearrange("p (c n) -> p c n", c=4))
            P.mm(ps2v, SHIFT[:], Vr)
            yield
            P.copy(VV[64:128, :, :], ps2.v(ps2v.ap[64:128, :, :]), e="act")
            yield

            ctxp[g] = dict(k=k, CUR=CUR, A_=A_, FMBK=FMBK, FMAR=FMAR, BKS=BKS, VV=VV, DCF=DCF, RK=RK, Gg=Gg, SQ=SQ)

        def drainP(g_, n):
            if g_ is None:
                return
            try:
                if n is None:
                    while True:
                        next(g_)
                else:
                    for _ in range(n):
                        next(g_)
            except StopIteration:
                pass

        drainP(prepG(0), None)
        for g in range(NG):
            c_ = ctxp.pop(g)
            k = c_["k"]; CUR = c_["CUR"]; A_ = c_["A_"]; FMBK = c_["FMBK"]; FMAR = c_["FMAR"]; BKS = c_["BKS"]
            VV = c_["VV"]; DCF = c_["DCF"]; RK = c_["RK"]; Gg = c_["Gg"]; SQ = c_["SQ"]
            gP = prepG(g + 1) if g + 1 < NG else None
            YB = YBt[k]

            def ub():
                b = banks[4 + ubi[0] % 4]; ubi[0] += 1
                return b
            v864 = lambda bank: bank.v(bank.t[0:64, :].rearrange("p (u n) -> p u n", u=8))
            I8 = _bc(IDT, IDT.t[0:64, 0:64], 1, [64, 8, 64])
            bB = [ub(), ub()]; bX = ub()
            for u in range(8):
                bb = bB[u // 4]
                P.mm(bb[:, (u % 4) * 128:(u % 4 + 1) * 128], FMBK[:, u, :], FMAR[:, u, :])
                P.mm(bX[0:64, u * 64:(u + 1) * 64], FMAR[:, u, 0:64], FMBK[:, u, 0:64])
            for hf in range(2):
                bb = bB[hf]
                P.tt(BIGA[:, hf * 4:(hf + 1) * 4, :], bb.v(bb.t[:, :].rearrange("p (u n) -> p u n", u=4)),
                     _bc(MASK, MASK.t[:, :], 1, [128, 4, 128]), ALU.mult)
            XT = XTA[0]
            P.tt(XT[:], v864(bX), _bc(TRILS, TRILS.t[:, :], 1, [64, 8, 64]), ALU.mult)
            X = BIGA.v(BIGA.t[0:64, :, 0:64])
            Pm = PMA[0]
            P.tt(Pm[:], X, I8, ALU.add, e="pool")
            drainP(gP, 12)
            for i in range(5):
                Xn = XA[i % 2]; XTn = XTA[(i + 1) % 2]; Pn = PMA[(i + 1) % 2]
                bA = ub(); bQ = ub()
                for u in range(8):
                    us = slice(u * 64, (u + 1) * 64)
                    if i < 4:
                        P.mm(bA[0:64, us], XT[:, u, :], X[:, u, :])
                    P.mm(bQ[0:64, us], X[:, u, :], XT[:, u, :])
                if i < 4:
                    P.copy(Xn[:], v864(bA), e="act")
                P.copy(XTn[:], v864(bQ))
                bC = ub()
                for u in range(8):
                    us = slice(u * 64, (u + 1) * 64)
                    P.mm(bC[0:64, us], XTn[:, u, :], Pm[:, u, :])
                P.tt(Pn[:], v864(bC), Pm[:], ALU.add)
                drainP(gP, 12)
                X = Xn; XT = XTn; Pm = Pn
                if i == 4:
                    X = None
            InvT = Pm
            bW = ub()
            for u in range(8):
                c, h = divmod(u, 2); hs = slice(h * 64, (h + 1) * 64); us = slice(u * 64, (u + 1) * 64)
                P.mm(bW[0:64, us], BIGA[64:128, u, 0:64], VV[64:128, c, hs])
            P.copy(W2A[:], v864(bW), e="act")
            drainP(gP, 12)
            bA = ub(); bU = ub()
            for u in range(8):
                c, h = divmod(u, 2); hs = slice(h * 64, (h + 1) * 64); us = slice(u * 64, (u + 1) * 64)
                P.mm(bA[0:64, us], InvT[:, u, :], A_[:, c, hs])
                P.mm(bU[0:64, us], InvT[:, u, :], W2A[:, u, :])
            P.copy(AHA[:], v864(bA))
            P.copy(VV.v(VV.t[0:64, :, :].rearrange("p c (h n) -> p (c h) n", n=64)), v864(bU), e="act")
            bR = ub(); bG = ub()
            for u in range(8):
                c, h = divmod(u, 2); hs = slice(h * 64, (h + 1) * 64); us = slice(u * 64, (u + 1) * 64)
                P.mm(bR[0:64, us], AHA[:, u, :], BIGA[0:64, u, 64:128])
                P.mm(bG[0:64, us], AHA[:, u, :], BKS[0:64, c, hs])
            P.tt(RHA[:], v864(bR), FMAR[:, :, 64:128], ALU.add)
            P.tt(TMPG[:], I8, _bc(DCF, DCF.t[:, :], 2, [64, 8, 64]), ALU.mult, e="pool")
            P.tt(GTA[:], v864(bG), TMPG[:], ALU.add)
            drainP(gP, 12)
            bY = ub()
            bHs = [ub(), ub()]
            for c in range(4):
                bH = bHs[c % 2]
                Hc = HA[hkk[0] % 2]; Hn = HA[(hkk[0] + 1) % 2]; hkk[0] += 1
                for h in range(2):
                    u = c * 2 + h
                    hs = slice(h * 64, (h + 1) * 64); us = slice(u * 64, (u + 1) * 64)
                    P.mm(bY[0:64, us], RHA[:, u, :], Hc[:, h, :], start=True, stop=False)
                    P.mm(bY[0:64, us], BIGA[:, u, 64:128], VV[:, c, hs], start=False, stop=True)
                    P.mm(bH[0:64, hs], GTA[:, u, :], Hc[:, h, :], start=True, stop=False)
                    P.mm(bH[0:64, hs], BKS[:, c, hs], VV[:, c, hs], start=False, stop=True)
                P.copy(Hn[:], bH.v(bH.t[0:64, 0:128].rearrange("p (h n) -> p h n", h=2)))
                drainP(gP, 12)
            P.copy(fl(YB), bY[0:64, :], e="act")
            drainP(gP, None)
            YC = YCt[k]; MN = MNt[k]; VR = VRt[k]
            P.red(MN[:], v8(YB), ALU.add)
            P.ts(MN[:], MN[:], 1.0 / 64, None, ALU.mult)
            P.tt(v8(YC), v8(YB), b8(MN), ALU.subtract)
            P.tt(SQ[:], YC[:], YC[:], ALU.mult, e="pool")
            P.red(VR[:], v8(SQ), ALU.add)
            P.ts(VR[:], VR[:], 1.0 / 64, 64e-5, ALU.mult, ALU.add)
            P.act(VR[:], VR[:], AF.Sqrt)
            P.recip(VR[:], VR[:])
            P.tt(v8(YC), v8(YC), b8(VR), ALU.mult)
            P.tt(YC[:], YC[:], GNGb, ALU.mult)
            P.tt(YC[:], YC[:], GNBb, ALU.add)
            P.tt(SQ.v(SQ.t[:, :, :].rearrange("p a (h b) -> p a h b", b=64)),
                 CUR.v(CUR.t[:, :, 256:384].rearrange("p a (h b) -> p a h b", b=64)),
                 RK.v(RK.t[:, :].rearrange("p (a h) -> p a h", h=2).unsqueeze(3).to_broadcast([64, 4, 2, 64])), ALU.mult)
            P.tt(YC[:], YC[:], SQ[:], ALU.add)
            P.tt(YC[:], YC[:], Gg[:], ALU.mult)
            P.store("sp", ya[g * 256:(g + 1) * 256, :].rearrange("(c t) n -> t c n", t=64), YC[:])
        P.finish()
        print("k2 ninst", P.ninst, "nsem", P.nsem)
    return nc


RW = 512
EVAC_E = "act"


def k2_inputs(p_rwkv_b, i, prm, l, consts):
    h0 = 2 * i
    cs = slice(h0 * 64, h0 * 64 + 128)
    cols = np.r_[np.arange(h0 * 64, h0 * 64 + 128), RW + np.arange(h0 * 64, h0 * 64 + 128),
                 2 * RW + np.arange(h0 * 64, h0 * 64 + 128), np.arange(3 * RW, 3 * RW + 256)]
    vec = np.zeros((8, 128), np.float32)
    for r, n in enumerate(["rwkv_w0", "rwkv_a0", "rwkv_k_k", "rwkv_k_a", "rwkv_r_k", "rwkv_gn_g", "rwkv_gn_b"]):
        vec[r] = prm[n][l].reshape(-1)[cs]
    d = {"pp": np.ascontiguousarray(p_rwkv_b[:, cols]), "mu": np.ascontiguousarray(prm["rwkv_mu"][l][cols][None]),
         "vec": vec, "w2": np.ascontiguousarray(prm["rwkv_w2"][l][:, cs]),
         "a2": np.ascontiguousarray(prm["rwkv_a2"][l][:, cs]), "g2": np.ascontiguousarray(prm["rwkv_g2"][l][:, cs])}
    d.update(consts)
    return d


def run_k2(p, prm, l, TT=T):
    key = ("k2", TT)
    if key not in _CACHE:
        _CACHE[key] = build_k2(TT)
    nc = _CACHE[key]
    consts = k2_consts()
    maps = []
    for ci in range(NCORES):
        b, i = divmod(ci, 4)
        pb = np.concatenate([np.zeros((1, 1792), np.float32), p[b, :TT, :1792]], 0)
        maps.append(k2_inputs(pb, i, prm, l, consts))
    res = run_bass_kernel_spmd(nc, maps, core_ids=list(range(NCORES)))
    out = np.empty((NB, TT, 512), np.float32)
    for ci in range(NCORES):
        b, i = divmod(ci, 4)
        out[b, :, i * 128:(i + 1) * 128] = res.results[ci]["ya"]
    return out


ROPE_THETA = 500000.0
NEGM = -30000.0


def k3_consts(TT):
    half = 8
    inv = np.power(ROPE_THETA, -np.arange(half, dtype=np.float32) * 2.0 / 16).astype(np.float32)

    def tab(pos):
        ang = pos.astype(np.float32)[:, None] * inv[None, :]
        c, s = np.cos(ang).astype(np.float32), np.sin(ang).astype(np.float32)
        return np.concatenate([c, c, -s, s], 1).astype(np.float32)
    rope_tok = tab(np.arange(TT))
    ncmp = TT // 16
    rope_cmp = tab(np.arange(ncmp) * 16 + 31)
    ql = np.arange(128)
    triu = (ql[:, None] <= ql[None, :]).astype(np.float32)
    tril = (ql[:, None] > ql[None, :]).astype(np.float32)
    cma = np.zeros((128, 17, 128), np.float32)
    cmt = np.zeros((128, 17, 128), np.float32)
    for v in range(17):
        c0 = 8 * v
        nl = np.arange(128)
        valid = (16 * (nl[None, :] - c0) + 31 <= ql[:, None])
        cma[:, v, :] = np.where(valid, 0.0, NEGM)
        cmt[:, v, :] = valid.T.astype(np.float32)
    ca = np.where(ql >= 64, 1e4, -1.0).astype(np.float32)[:, None]
    cb = np.where(ql < 64, 1e4, 0.0).astype(np.float32)[:, None]
    return {"rope_tok": rope_tok, "rope_cmp": rope_cmp, "triu": triu, "tril": tril, "cma": cma, "cmt": cmt,
            "cab": np.concatenate([ca, cb], 1), "ident": np.eye(128, dtype=np.float32)}


def build_k3(TT=T):
    nc = _new_nc()
    NTK = TT // 128
    NQB = NTK
    NCMP = TT // 16
    NCT = (NCMP + 127) // 128
    dr = lambda n, s, kind="ExternalInput": nc.dram_tensor(n, s, F32, kind=kind).ap()
    qin = dr("qin", [NQB * 128, 256]); gin = dr("gin", [NQB * 128, 6]); kvin = dr("kvin", [TT, 384])
    qkg = dr("qkg", [1, 256]); cpos = dr("cpos", [128, 32]); w1 = dr("w1", [128, 32, 256]); w2 = dr("w2", [128, 2, 2, 64])
    rope_tok = dr("rope_tok", [TT, 32]); rope_cmp = dr("rope_cmp", [NCMP, 32])
    triu = dr("triu", [128, 128]); tril = dr("tril", [128, 128]); cma = dr("cma", [128, 17, 128]); cmt = dr("cmt", [128, 17, 128])
    cab = dr("cab", [128, 2]); ident = dr("ident", [128, 128])
    yb = dr("yb", [NQB * 128, 128], "ExternalOutput")
    with ExitStack() as es, nc.allow_low_precision("bf16 attention-tile operands, fp32 PSUM accumulation"):
        P = Prog2(nc, es)
        IDT = P.tile("IDT", [128, 128]); TRIU = P.tile("TRIU", [128, 128]); TRIL = P.tile("TRIL", [128, 128])
        CMA = P.tile("CMA", [128, 17, 128]); CMT = P.tile("CMT", [128, 17, 128]); CAB = P.tile("CAB", [128, 2])
        QKG = P.tile("QKG", [128, 4, 64]); CPOS = P.tile("CPOS", [128, 32]); W2 = P.tile("W2", [128, 2, 2, 64])
        for i, (tl, d) in enumerate([(IDT, ident), (TRIU, triu), (TRIL, tril), (CAB, cab), (CPOS, cpos)]):
            P.load(["sp", "act"][i % 2], tl[:], d[:, :])
        P.load("sp", CMA[:], cma[:, :, :]); P.load("act", CMT[:], cmt[:, :, :])
        P.load("sp", QKG.v(QKG.t[:, :, :].rearrange("p a b -> p (a b)")), qkg.partition_broadcast(128))
        P.load("act", W2.v(W2.t[:, :, :, :].rearrange("p a b c -> p (a b c)")), w2.rearrange("p a b c -> p (a b c)"))
        banks = [P.tile(f"bank{i}", [128, 512], psum=True) for i in range(8)]
        KT = P.tile("KT", [128, TT], BF16)
        CT = P.tile("CT", [128, TT + 16])
        VSW = P.tile("VSW", [128, NTK, 2, 65], BF16)
        P.memset(VSW[:, :, :, 64:65], 1.0)
        P.memset(CT[:, TT:TT + 16], 0.0)
        KVt = [P.tile(f"KV{i}", [128, 384]) for i in range(2)]
        RPt = [P.tile(f"RP{i}", [128, 32]) for i in range(2)]
        KNt = [P.tile(f"KN{i}", [128, 2, 64]) for i in range(2)]
        SQt = [P.tile(f"SQa{i}", [128, 2, 64]) for i in range(2)]
        STt = [P.tile(f"STa{i}", [128, 2]) for i in range(2)]
        R1t = [P.tile(f"R1a{i}", [128, 2, 16]) for i in range(2)]
        R2t = [P.tile(f"R2a{i}", [128, 2, 16]) for i in range(2)]

        def norm_rope(X3, G3, RP, KN, SQ, ST, R1, R2, nh, e2="pool"):
            P.tt(SQ[:], X3, X3, ALU.mult, e=e2)
            P.red(ST[:], SQ[:], ALU.add)
            P.ts(ST[:], ST[:], 1.0 / 64, 1e-6, ALU.mult, ALU.add)
            P.act(ST[:], ST[:], AF.Sqrt)
            P.recip(ST[:], ST[:])
            P.tt(KN[:], X3, _bc(ST, ST.t[:, :], 2, [128, nh, 64]), ALU.mult)
            P.tt(KN[:], KN[:], G3, ALU.mult, e=e2)
            cs = _bc(RP, RP.t[:, 0:16], 1, [128, nh, 16])
            P.tt(R1[:], KN[:, :, 0:16], cs, ALU.mult)
            P.tt(R2[:, :, 0:8], KN[:, :, 8:16], _bc(RP, RP.t[:, 16:24], 1, [128, nh, 8]), ALU.mult, e=e2)
            P.tt(R2[:, :, 8:16], KN[:, :, 0:8], _bc(RP, RP.t[:, 24:32], 1, [128, nh, 8]), ALU.mult, e=e2)
            P.tt(KN[:, :, 0:16], R1[:], R2[:], ALU.add)

        GK = QKG.v(QKG.t[:, 2:4, :])
        pk = 0
        for tg in range(NTK // 4):
            bk = banks[pk % 2]; bc_ = banks[2 + pk % 2]; pk += 1
            for j in range(4):
                ti = tg * 4 + j
                k = ti % 2
                KV = KVt[k]; RP = RPt[k]
                P.load("sp", KV[:], kvin[ti * 128:(ti + 1) * 128, :])
                P.load("act", RP[:], rope_tok[ti * 128:(ti + 1) * 128, :])
                X3 = KV.v(KV.t[:, 128:384].rearrange("p (a b) -> p a b", b=128)[:, :, 0:64])
                norm_rope(X3, GK, RP, KNt[k], SQt[k], STt[k], R1t[k], R2t[k], 2)
                P.tr(bk[:, j * 128:(j + 1) * 128], KNt[k].v(KNt[k].t[:, :, :].rearrange("p a b -> p (a b)")), IDT[:])
                P.tr(bc_[:, j * 128:(j + 1) * 128], KV[:, 0:128], IDT[:])
                V3 = KV.v(KV.t[:, 128:384].rearrange("p (a b) -> p a b", b=128)[:, :, 64:128])
                P.copy(VSW[:, ti, :, 0:64], V3, e="act")
            P.copy(KT[:, tg * 512:(tg + 1) * 512], bk[:])
            P.copy(CT[:, tg * 512:(tg + 1) * 512], bc_[:], e="act")
        HID = P.tile("HID", [128, 2, 2, NCT * 128])
        P.memset(HID[:], 0.0, e="pool")
        W1 = P.tile("W1", [128, 32, 128])
        BIA = P.tile("BIA", [128, 2, 2])
        Zt = P.tile("Zt", [128, 512]); Z2 = P.tile("Z2", [128, 512])
        NV = NCMP - 1
        for hc in range(2):
            P.load("sp" if hc == 0 else "act", W1[:], w1[:, :, hc * 128:(hc + 1) * 128])
            for i in range(2):
                ps_ = slice(i * 64, (i + 1) * 64)
                bb = banks[4]
                for tau in range(32):
                    P.mm(bb[:, 0:1], W1[ps_, tau, :], CPOS[ps_, tau:tau + 1], start=(tau == 0), stop=(tau == 31))
                P.copy(BIA[:, i, hc:hc + 1], bb[:, 0:1])
                for nt in range((NCMP + 511) // 512):
                    n0 = nt * 512
                    nn = min(512, NCMP - n0)
                    ba = banks[5 + nt % 2]
                    for tau in range(32):
                        rhs = CT.v(CT.t[ps_, n0 * 16 + tau: n0 * 16 + tau + (nn - 1) * 16 + 1: 16])
                        P.mm(ba[:, 0:nn], W1[ps_, tau, :], rhs, start=(tau == 0), stop=(tau == 31))
                    Z = Zt
                    P.act(Z[:, 0:nn], ba[:, 0:nn], AF.Identity, bias=BIA[:, i, hc:hc + 1])
                    P.tt(Z2[:, 0:nn], Z[:, 0:nn], Z[:, 0:nn], ALU.mult)
                    P.ts(Z2[:, 0:nn], Z2[:, 0:nn], 0.044715, 1.0, ALU.mult, ALU.add)
                    P.tt(Z2[:, 0:nn], Z2[:, 0:nn], Z[:, 0:nn], ALU.mult)
                    P.act(Z2[:, 0:nn], Z2[:, 0:nn], AF.Sigmoid, scale=1.5957691216057308)
                    P.tt(HID[:, i, hc, n0:n0 + nn], Z2[:, 0:nn], Z[:, 0:nn], ALU.mult)
        KCT = P.tile("KCT", [128, NCT * 128])
        VCMP = P.tile("VCMP", [128, NCT, 65], BF16)
        KCTb = P.tile("KCTb", [128, NCT * 128], BF16)
        P.memset(VCMP[:, :, 64:65], 1.0)
        KC2 = P.tile("KC2", [128, 2, 64])
        KCR = P.tile("KCR", [128, 1, 64])
        GC = QKG.v(QKG.t[:, 1:2, :])
        for ct in range(NCT):
            bb = banks[4 + ct % 2]
            for i in range(2):
                for hc in range(2):
                    P.mm(bb[:, i * 64:(i + 1) * 64], HID[:, i, hc, ct * 128:(ct + 1) * 128], W2[:, i, hc, :],
                         start=(hc == 0), stop=(hc == 1))
            P.copy(VCMP[:, ct, 0:64], bb[:, 64:128], e="act")
            RP = RPt[ct % 2]
            nr = min(128, NCMP - ct * 128)
            P.load("sp", RP[0:nr, :], rope_cmp[ct * 128:ct * 128 + nr, :])
            k = ct % 2
            KN1 = KNt[k].v(KNt[k].t[:, 0:1, :])
            P.copy(KCR[:, 0, :], bb[:, 0:64])
            norm_rope(KCR[:], GC, RP, KN1,
                      SQt[k].v(SQt[k].t[:, 0:1, :]), STt[k].v(STt[k].t[:, 0:1]), R1t[k].v(R1t[k].t[:, 0:1, :]),
                      R2t[k].v(R2t[k].t[:, 0:1, :]), 1, e2="dve")
            P.copy(KC2[:, 0, :], KNt[k][:, 0, :])
            P.copy(KC2[:, 1, :], KNt[k][:, 0, :], e="act")
            b2 = banks[6 + ct % 2]
            P.tr(b2[:, 0:128], KC2.v(KC2.t[:, :, :].rearrange("p a b -> p (a b)")), IDT[:])
            P.copy(KCT[:, ct * 128:(ct + 1) * 128], b2[:, 0:128])
            P.copy(KCTb[:, ct * 128:(ct + 1) * 128], b2[:, 0:128], e="act")
        Qt = [P.tile(f"Q{i}", [128, 4, 64]) for i in range(2)]
        Gt = [P.tile(f"G{i}", [128, 2, 3]) for i in range(2)]
        QNt = [P.tile(f"QN{i}", [128, 4, 64]) for i in range(2)]
        QDt = [P.tile(f"QD{i}", [128, 4, 2, 64]) for i in range(2)]
        QTt = [P.tile(f"QT{i}", [128, 4, 128]) for i in range(2)]
        SQq = [P.tile(f"SQq{i}", [128, 4, 64]) for i in range(2)]
        STq = [P.tile(f"STq{i}", [128, 4]) for i in range(2)]
        R1q = [P.tile(f"R1q{i}", [128, 4, 16]) for i in range(2)]
        R2q = [P.tile(f"R2q{i}", [128, 4, 16]) for i in range(2)]
        PCt = [P.tile(f"PC{i}", [128, 512]) for i in range(2)]
        RSt = [P.tile(f"RS{i}", [128, 4]) for i in range(2)]
        IMPP = P.tile("IMPP", [128, 516])
        IMPF = P.tile("IMPF", [128, 128]); IMPW = P.tile("IMPW", [128, 128])
        M8 = P.tile("M8", [128, 16])
        SEL = P.tile("SEL", [128, 128])
        PTt = [P.tile(f"PT{i}", [128, 2, 128], BF16) for i in range(4)]
        QTbt = [P.tile(f"QTb{i}", [128, 2, 128], BF16) for i in range(2)]
        OUTt = [P.tile(f"OUT{i}", [128, 2, 64]) for i in range(2)]
        RIt = [P.tile(f"RI{i}", [128, 3, 2]) for i in range(2)]
        OTt = [P.tile(f"OT{i}", [128, 2, 64]) for i in range(2)]
        GQ = QKG.v(QKG.t[:, 0:1, :].to_broadcast([128, 4, 64])) if False else _bc(QKG, QKG.t[:, 0, :], 1, [128, 4, 64])
        sb_i = [0]; mb_i = [0]; pt_i = [0]
        OB = [0, 1, 2]

        def attn_tile(first, last, ob, kT_ap, part, v1_ap, QT, mask_fn):
            sb = banks[sb_i[0] % 2]; sb_i[0] += 1
            PT = PTt[pt_i[0] % 4]; pt_i[0] += 1
            P.mm(sb[:, 0:256], kT_ap, QTb.v(QTb.t[part:part + 64, :, :].rearrange("p a b -> p (a b)")))
            P.act(PT.v(PT.t[:, :, :].rearrange("p a b -> p (a b)")), sb[:, 0:256], AF.Exp)
            mask_fn(PT)
            for r in range(2):
                P.mm(banks[6 + r][:, ob * 65:(ob + 1) * 65], PT[:, r, :], v1_ap, start=first, stop=last)

        ctxs = {}

        def phaseA(jq):
                qb = jq
                k = jq % 2
                Q = Qt[k]; G = Gt[k]; RP = RPt[k]
                P.load("sp", Q.v(Q.t[:, :, :].rearrange("p a b -> p (a b)")), qin[jq * 128:(jq + 1) * 128, :])
                yield
                P.load("act", G.v(G.t[:, :, :].rearrange("p a b -> p (a b)")), gin[jq * 128:(jq + 1) * 128, :])
                yield
                P.load("sp", RP[:], rope_tok[qb * 128:(qb + 1) * 128, :])
                yield
                QN = QNt[k]; QD = QDt[k]; QT = QTt[k]
                norm_rope(Q[:], GQ, RP, QN, SQq[k], STq[k], R1q[k], R2q[k], 4)
                yield
                P.ts(QD[:, :, 0, :], QN[:], 0.125, None, ALU.mult)
                yield
                P.ts(QD[:, :, 1, :], QN[:], 0.125, None, ALU.mult, e="pool")
                yield
                P.act(G[:], G[:], AF.Sigmoid)
                yield
                bq = banks[4]
                for r in range(4):
                    P.tr(bq[:, r * 128:(r + 1) * 128], QD.v(QD.t[:, r, :, :].rearrange("p a b -> p (a b)")), IDT[:])
                    yield
                P.copy(QT.v(QT.t[:, :, :].rearrange("p a b -> p (a b)")), bq[:])
                yield
                QTb = QTbt[k]
                P.copy(QTb.v(QTb.t[:, :, :].rearrange("p a b -> p (a b)")), bq[:, 0:256], e="act")
                yield
                nct = qb // 16 + 1
                ncols = nct * 128
                var = qb % 16
                P.memset(IMPP[:], 0.0, e="pool")
                yield
                RS = RSt[k]
                for r in range(4):
                    sb = banks[5]
                    P.mm(sb[:, 0:ncols], QT[0:64, r, :], KCT[0:64, 0:ncols])
                    yield
                    PC = PCt[r % 2]
                    lo = ncols - 128
                    if lo > 0:
                        P.copy(PC[:, 0:lo], sb[:, 0:lo])
                        yield
                        if var == 0:
                            P.tt(PC[:, lo - 128:lo], PC[:, lo - 128:lo], CMA[:, 16, :], ALU.add)
                            yield
                    P.tt(PC[:, lo:ncols], sb[:, lo:ncols], CMA[:, var, :], ALU.add)
                    yield
                    P.act(PC[:, 0:ncols], PC[:, 0:ncols], AF.Exp, accum_out=RS[:, r:r + 1])
                    yield
                    P.ts(RS[:, r:r + 1], RS[:, r:r + 1], 1.1754944e-38, None, ALU.max)
                    yield
                    P.recip(RS[:, r:r + 1], RS[:, r:r + 1])
                    yield
                    P.stt(IMPP[:, 4:4 + ncols], PC[:, 0:ncols], RS[:, r:r + 1], IMPP[:, 4:4 + ncols], ALU.mult, ALU.add)
                    yield
                P.red(IMPF[:], IMPP.v(IMPP.t[:, 4:516].rearrange("p (j f) -> p j f", f=4)), ALU.add)
                yield
                P.tt(IMPF[:], IMPF[:], IMPP.v(IMPP.t[:, 0:512].rearrange("p (j f) -> p j f", f=4)[:, :, 3]), ALU.add)
                yield
                if 2 * qb + 2 < 128:
                    P.memset(IMPF[:, 2 * qb + 2:128], -1.0)
                    yield
                if qb >= 1:
                    P.ts(IMPF[:, 2 * qb - 1:2 * qb], IMPF[:, 2 * qb - 1:2 * qb], CAB[:, 1:2], None, ALU.max)
                    yield
                if 2 * qb + 1 < 128:
                    P.copy(IMPF[:, 2 * qb + 1:2 * qb + 2], CAB[:, 0:1])
                    yield
                P.memset(IMPF[:, 2 * qb:2 * qb + 1], 1e4)
                yield
                P.memset(IMPF[:, 0:1], 1e4)
                yield
                P.gop("dve", lambda E: E.max(out=M8.t[:, 0:8], in_=IMPF.t[:, :]), [M8], [IMPF])
                yield
                P.gop("dve", lambda E: E.match_replace(out=IMPW.t[:, :], in_to_replace=M8.t[:, 0:8], in_values=IMPF.t[:, :],
                                                       imm_value=-2.0), [IMPW], [M8, IMPF])
                P.gop("dve", lambda E: E.max(out=M8.t[:, 8:16], in_=IMPW.t[:, :]), [M8], [IMPW])
                yield
                P.ts(SEL[:], IMPF[:], M8[:, 15:16], None, ALU.is_ge)
                yield

                ctxs[jq] = dict(qb=qb, k=k, nct=nct, var=var, QT=QT, QTb=QTb, G=G)

        def drainA(g, n):
            if g is None:
                return
            try:
                if n is None:
                    while True:
                        next(g)
                else:
                    for _ in range(n):
                        next(g)
            except StopIteration:
                pass

        drainA(phaseA(0), None)
        for jq in range(NQB):
            c_ = ctxs.pop(jq)
            qb = c_["qb"]; k = c_["k"]; nct = c_["nct"]; var = c_["var"]; QT = c_["QT"]; QTb = c_["QTb"]; G = c_["G"]
            gA = phaseA(jq + 1) if jq + 1 < NQB else None
            tiles = []
            for ct in range(nct):
                last = ct == nct - 1
                if last:
                    mf = lambda PT: P.tt(PT[:], PT[:], _bc(CMT, CMT.t[:, var, :], 1, [128, 2, 128]), ALU.mult)
                elif var == 0 and ct == nct - 2:
                    mf = lambda PT: P.tt(PT[:], PT[:], _bc(CMT, CMT.t[:, 16, :], 1, [128, 2, 128]), ALU.mult)
                else:
                    mf = None
                tiles.append((ct == 0, last, OB[0], KCTb[0:64, ct * 128:(ct + 1) * 128], 0, VCMP[:, ct, :], mf, None))
            kts = [kt for kt in range(qb - 4, qb + 1) if kt >= 0]
            for kt in kts:
                if kt == qb:
                    mf = lambda PT: P.tt(PT[:], PT[:], _bc(TRIU, TRIU.t[:, :], 1, [128, 2, 128]), ALU.mult)
                elif kt == qb - 4:
                    mf = lambda PT: P.tt(PT[:], PT[:], _bc(TRIL, TRIL.t[:, :], 1, [128, 2, 128]), ALU.mult)
                else:
                    mf = None
                tiles.append((kt == kts[0], kt == kts[-1], OB[2], KT[64:128, kt * 128:(kt + 1) * 128], 64, VSW[:, kt, 1, :], mf, None))
            nj = 2 * (qb + 1)
            P.copy(CT.v(CT.t[:, 0:nj * 64].rearrange("p (j f) -> p j f", f=64)),
                   SEL.v(SEL.t[:, 0:nj].unsqueeze(2).to_broadcast([128, nj, 64])), e="pool")
            for kt in range(qb + 1):
                tiles.append((kt == 0, kt == qb, OB[1], KT[0:64, kt * 128:(kt + 1) * 128], 0, VSW[:, kt, 0, :], None, kt))
            LA = 2
            pend = []
            sbanks = [banks[0], banks[1]]
            mbanks = [banks[2], banks[3]]
            for i in range(len(tiles) + LA):
                if i < len(tiles):
                    first, last, ob, kT_ap, part, v1_ap, mf, kt = tiles[i]
                    sb = sbanks[sb_i[0] % 2]; sb_i[0] += 1
                    PT = PTt[pt_i[0] % 4]; pt_i[0] += 1
                    P.mm(sb[:, 0:256], kT_ap, QTb.v(QTb.t[part:part + 64, :, :].rearrange("p a b -> p (a b)")))
                    if kt is not None:
                        mbk = mbanks[mb_i[0] % 2]; mb_i[0] += 1
                        P.tr(mbk[:, 0:128], CT[:, kt * 128:(kt + 1) * 128], IDT[:])
                    P.act(PT.v(PT.t[:, :, :].rearrange("p a b -> p (a b)")), sb[:, 0:256], AF.Exp)
                    if kt is not None:
                        P.tt(PT[:], PT[:], _bc(mbk, mbk.t[:, 0:128], 1, [128, 2, 128]), ALU.mult)
                        if kt == qb:
                            P.tt(PT[:], PT[:], _bc(TRIU, TRIU.t[:, :], 1, [128, 2, 128]), ALU.mult, e="pool")
                    elif mf is not None:
                        mf(PT)
                    pend.append((PT, ob, v1_ap, first, last))
                    drainA(gA, 2)
                j = i - LA
                if j >= 0:
                    PT, ob, v1_ap, first, last = pend[j]
                    for r in range(2):
                        P.mm(banks[6 + r][:, ob * 65:(ob + 1) * 65], PT[:, r, :], v1_ap, start=first, stop=last)
            drainA(gA, None)
            RI = RIt[k]; OUT = OUTt[k]; OT = OTt[k]
            for bi in range(3):
                for r in range(2):
                    ob = banks[6 + r]
                    c0 = bi * 65
                    P.ts(RI[:, bi, r:r + 1], ob[:, c0 + 64:c0 + 65], 1.1754944e-38, None, ALU.max)
                    P.recip(RI[:, bi, r:r + 1], RI[:, bi, r:r + 1])
                    P.tt(RI[:, bi, r:r + 1], RI[:, bi, r:r + 1], G[:, r, bi:bi + 1], ALU.mult)
                    if bi == 0:
                        P.ts(OUT[:, r, :], ob[:, c0:c0 + 64], RI[:, bi, r:r + 1], None, ALU.mult)
                    else:
                        P.stt(OUT[:, r, :], ob[:, c0:c0 + 64], RI[:, bi, r:r + 1], OUT[:, r, :], ALU.mult, ALU.add)
            P.store("sp", yb[jq * 128:(jq + 1) * 128, :], OUT.v(OUT.t[:, :, :].rearrange("p a b -> p (a b)")))
        P.finish()
        print("k3 ninst", P.ninst, "nsem", P.nsem)
    return nc


def k3_inputs(p_b, g, hh, prm, l, TT):
    q = p_b[:TT, 1792 + g * 256: 1792 + (g + 1) * 256].reshape(TT, 4, 64)
    order = [2 * hh, 2 * hh + 1, 2 * (1 - hh), 2 * (1 - hh) + 1]
    q = q[:, order, :].reshape(TT, 256)
    kv = p_b[:TT, 2304:3072].reshape(TT, 6, 2, 64)[:, :, g, :].reshape(TT, 384)
    gt = p_b[:TT, 3072:3096].reshape(TT, 8, 3)[:, g * 4 + 2 * hh: g * 4 + 2 * hh + 2, :].reshape(TT, 6)
    cp = prm["cmp_pos"][l]
    cpos = np.concatenate([cp[0].T, cp[1].T], 0)
    w1 = prm["cmp_w1"][l].reshape(2, 32, 64, 256).transpose(0, 2, 1, 3).reshape(128, 32, 256)
    w2 = prm["cmp_w2"][l].reshape(2, 2, 128, 64).transpose(2, 0, 1, 3)
    d = {"qin": np.ascontiguousarray(q), "gin": np.ascontiguousarray(gt), "kvin": np.ascontiguousarray(kv),
         "qkg": np.ascontiguousarray(prm["qk_norm_g"][l].reshape(1, 256)), "cpos": np.ascontiguousarray(cpos),
         "w1": np.ascontiguousarray(w1), "w2": np.ascontiguousarray(w2)}
    d.update(k3_consts(TT))
    return d


def run_k3(p, prm, l, TT=T):
    key = ("k3", TT)
    if key not in _CACHE:
        _CACHE[key] = build_k3(TT)
    nc = _CACHE[key]
    maps = []
    for ci in range(NCORES):
        b, rem = divmod(ci, 4)
        g, hh = divmod(rem, 2)
        maps.append(k3_inputs(p[b], g, hh, prm, l, TT))
    res = run_bass_kernel_spmd(nc, maps, core_ids=list(range(NCORES)))
    out = np.empty((NB, TT, 512), np.float32)
    for ci in range(NCORES):
        b, rem = divmod(ci, 4)
        g, hh = divmod(rem, 2)
        h0 = g * 4 + 2 * hh
        out[b, :, h0 * 64:(h0 + 2) * 64] = res.results[ci]["yb"]
    return out


def build_k4a():
    nc = _new_nc()
    NT = 16
    dr = lambda n, s, kind="ExternalInput": nc.dram_tensor(n, s, F32, kind=kind).ap()
    x = dr("x", [NT * 128, D]); ya = dr("ya", [NT * 128, 512]); yb = dr("yb", [NT * 128, 512]); pm = dr("pm", [NT * 128, 2048])
    gm = dr("gm", [1, D]); wua = dr("wua", [512, D]); wub = dr("wub", [512, D]); wo = dr("wo", [D, D]); ident = dr("ident", [128, 128])
    x1 = dr("x1", [NT * 128, D], "ExternalOutput")
    with ExitStack() as es:
        P = Prog2(nc, es)
        IDT = P.tile("IDT", [128, 128]); GMB = P.tile("GMB", [128, D])
        WUA = P.tile("WUA", [128, 4, D]); WUB = P.tile("WUB", [128, 4, D]); WO = P.tile("WO", [128, 8, D])
        P.load("sp", IDT[:], ident[:, :]); P.load("act", GMB[:], gm.partition_broadcast(128))
        P.load("sp", WUA[:], wua.rearrange("(kc kp) n -> kp kc n", kp=128))
        P.load("act", WUB[:], wub.rearrange("(kc kp) n -> kp kc n", kp=128))
        P.load("sp", WO[:, 0:4, :], wo[0:512, :].rearrange("(kc kp) n -> kp kc n", kp=128))
        P.load("act", WO[:, 4:8, :], wo[512:1024, :].rearrange("(kc kp) n -> kp kc n", kp=128))
        banks = [P.tile(f"bank{i}", [128, 512], psum=True) for i in range(8)]
        bi = [0]

        def nb():
            b = banks[bi[0] % 8]; bi[0] += 1
            return b
        Xt = [P.tile(f"X{i}", [128, D]) for i in range(2)]
        YAt = [P.tile(f"YA{i}", [128, 512]) for i in range(2)]
        YBt = [P.tile(f"YB{i}", [128, 512]) for i in range(2)]
        PMt = [P.tile(f"PM{i}", [128, 2048]) for i in range(2)]
        YTt = [P.tile(f"YT{i}", [128, 8, 128]) for i in range(2)]
        MIXt = [P.tile(f"MIX{i}", [128, D]) for i in range(2)]
        TMPt = [P.tile(f"TMP{i}", [128, 512]) for i in range(2)]
        MTt = [P.tile(f"MT{i}", [128, 8, 128]) for i in range(2)]
        for ti in range(NT):
            k = ti % 2
            rs = slice(ti * 128, (ti + 1) * 128)
            X = Xt[k]; YA = YAt[k]; YB = YBt[k]; PM = PMt[k]; YT = YTt[k]; MIX = MIXt[k]; MT = MTt[k]
            P.load("sp", X[:], x[rs, :]); P.load("act", YA[:], ya[rs, :]); P.load("sp", YB[:], yb[rs, :]); P.load("act", PM[:], pm[rs, :])
            for j, Y in enumerate((YA, YB)):
                b = nb()
                for c in range(4):
                    P.tr(b[:, c * 128:(c + 1) * 128], Y[:, c * 128:(c + 1) * 128], IDT[:])
                P.copy(YT.v(YT.t[:, j * 4:(j + 1) * 4, :].rearrange("p a b -> p (a b)")), b[:], e="act" if j else "dve")
            P.act(PM[:], PM[:], AF.Sigmoid)
            for nh in range(2):
                cs = slice(nh * 512, (nh + 1) * 512)
                ba = nb(); bb = nb()
                for kc in range(4):
                    P.mm(ba[:], YT[:, kc, :], WUA[:, kc, cs], start=(kc == 0), stop=(kc == 3))
                for kc in range(4):
                    P.mm(bb[:], YT[:, 4 + kc, :], WUB[:, kc, cs], start=(kc == 0), stop=(kc == 3))
                TMP = TMPt[nh]
                P.tt(MIX[:, cs], ba[:], PM[:, nh * 512:(nh + 1) * 512], ALU.mult)
                P.tt(TMP[:], bb[:], PM[:, 1024 + nh * 512:1024 + (nh + 1) * 512], ALU.mult)
                P.tt(MIX[:, cs], MIX[:, cs], TMP[:], ALU.add, e="pool")
            for half in range(2):
                b = nb()
                for c in range(4):
                    kc = half * 4 + c
                    P.tr(b[:, c * 128:(c + 1) * 128], MIX[:, kc * 128:(kc + 1) * 128], IDT[:])
                P.copy(MT.v(MT.t[:, half * 4:(half + 1) * 4, :].rearrange("p a b -> p (a b)")), b[:], e="act" if half else "dve")
            for nh in range(2):
                cs = slice(nh * 512, (nh + 1) * 512)
                b = nb()
                for kc in range(8):
                    P.mm(b[:], MT[:, kc, :], WO[:, kc, cs], start=(kc == 0), stop=(kc == 7))
                TMP = TMPt[nh]
                P.tt(TMP[:], b[:], GMB[:, cs], ALU.mult)
                P.tt(X[:, cs], X[:, cs], TMP[:], ALU.add, e="pool")
            P.store("sp" if k else "act", x1[rs, :], X[:])
        P.finish()
        print("k4a ninst", P.ninst, "nsem", P.nsem)
    return nc


def run_k4a(xfull, ya, yb, p, gate_mix, prm, l):
    if "k4a" not in _CACHE:
        _CACHE["k4a"] = build_k4a()
    nc = _CACHE["k4a"]
    ident = np.eye(128, dtype=np.float32)
    maps = []
    for i in range(NCORES):
        b, tq = divmod(i, 4)
        ts_ = slice(tq * 2048, (tq + 1) * 2048)
        maps.append({"x": np.ascontiguousarray(xfull[b, ts_]), "ya": np.ascontiguousarray(ya[b, ts_]),
                     "yb": np.ascontiguousarray(yb[b, ts_]), "pm": np.ascontiguousarray(p[b, ts_, 3096:5144]),
                     "gm": gate_mix[b][None].copy(), "wua": prm["w_up_rwkv"][l], "wub": prm["w_up_nsa"][l],
                     "wo": prm["w_out"][l], "ident": ident})
    res = run_bass_kernel_spmd(nc, maps, core_ids=list(range(NCORES)))
    out = np.empty((NB, T, D), np.float32)
    for i in range(NCORES):
        b, tq = divmod(i, 4)
        out[b, tq * 2048:(tq + 1) * 2048] = res.results[i]["x1"]
    return out


def build_k4b():
    nc = _new_nc()
    NT = 16; NE = 16
    dr = lambda n, s, kind="ExternalInput": nc.dram_tensor(n, s, F32, kind=kind).ap()
    x1 = dr("x1", [NT * 128, D]); g = dr("g", [1, D]); sc = dr("sc", [1, D]); sh = dr("sh", [1, D]); gf = dr("gf", [1, D])
    rw = dr("rw", [D, 16]); rb = dr("rb", [1, 16]); ident = dr("ident", [128, 128])
    wg = dr("wg", [NE, D, 512]); wu = dr("wu", [NE, D, 512]); wd = dr("wd", [NE, 512, D])
    xo = dr("xo", [NT * 128, D], "ExternalOutput")
    with ExitStack() as es, nc.allow_low_precision("bf16 expert matmuls, fp32 accumulation"):
        P = Prog2(nc, es)
        IDT = P.tile("IDT", [128, 128]); G2S = P.tile("G2S", [128, D]); SC = P.tile("SC", [128, D]); SH = P.tile("SH", [128, D])
        GF = P.tile("GF", [128, D]); RW = P.tile("RW", [128, 8, 16]); RB = P.tile("RB", [128, 16])
        P.load("sp", IDT[:], ident[:, :]); P.load("act", G2S[:], g.partition_broadcast(128)); P.load("sp", SC[:], sc.partition_broadcast(128))
        P.load("act", SH[:], sh.partition_broadcast(128)); P.load("sp", GF[:], gf.partition_broadcast(128))
        P.load("act", RW[:], rw.rearrange("(kc kp) n -> kp kc n", kp=128)); P.load("sp", RB[:], rb.partition_broadcast(128))
        P.stt(G2S[:], SC[:], 1.0, G2S[:], ALU.add, ALU.mult)
        banks = [P.tile(f"bank{i}", [128, 512], psum=True) for i in range(8)]
        bi = [0]

        def nb():
            b = banks[bi[0] % 8]; bi[0] += 1
            return b
        ACC = [P.tile(f"ACC{t}", [128, D]) for t in range(8)]
        H2T = P.tile("H2T", [128, 8, 1024], BF16)
        CW = P.tile("CW", [128, 8, 16])
        STG = [P.tile(f"STG{i}", [128, 4096]) for i in range(2)]
        WB = [[P.tile(f"WB{m}_{p}", [128, 4096], BF16) for p in range(2)] for m in range(3)]
        H2t = [P.tile(f"H2{i}", [128, D]) for i in range(2)]
        H2Tf = [P.tile(f"H2Tf{i}", [128, 8, 128]) for i in range(2)]
        junk = P.tile("junk", [128, D])
        ST = [P.tile(f"ST{i}", [128, 2]) for i in range(2)]
        SCO = [P.tile(f"SCO{i}", [128, 16]) for i in range(2)]
        SS = [P.tile(f"SS{i}", [128, 4, 4]) for i in range(2)]
        PSm = [P.tile(f"PSm{i}", [128, 4, 6]) for i in range(2)]
        GS = [P.tile(f"GS{i}", [128, 4]) for i in range(2)]
        GMx = [P.tile(f"GMx{i}", [128, 2]) for i in range(2)]
        E2 = [P.tile(f"E2{i}", [128, 4, 4]) for i in range(2)]
        SIL = [P.tile(f"SIL{i}", [128, 512]) for i in range(2)]
        HID = [P.tile(f"HID{i}", [128, 4, 512], BF16) for i in range(2)]
        stg_i = [0]
        for tg in range(2):
            for t in range(8):
                ti = tg * 8 + t
                k = t % 2
                A = ACC[t]
                P.load("sp" if k else "act", A[:], x1[ti * 128:(ti + 1) * 128, :])
                S = ST[k]; H2 = H2t[k]; HF = H2Tf[k]
                P.act(junk[:], A[:], AF.Square, accum_out=S[:, 0:1])
                P.ts(S[:, 1:2], S[:, 0:1], 1.0 / D, 1e-6, ALU.mult, ALU.add)
                P.act(S[:, 1:2], S[:, 1:2], AF.Sqrt)
                P.recip(S[:, 1:2], S[:, 1:2])
                P.stt(H2[:], A[:], S[:, 1:2], G2S[:], ALU.mult, ALU.mult)
                P.tt(H2[:], H2[:], SH[:], ALU.add, e="pool")
                for half in range(2):
                    b = nb()
                    for c in range(4):
                        kc = half * 4 + c
                        P.tr(b[:, c * 128:(c + 1) * 128], H2[:, kc * 128:(kc + 1) * 128], IDT[:])
                    bv = b.v(b.t[:, :].rearrange("p (a b) -> p a b", a=4))
                    P.copy(HF[:, half * 4:(half + 1) * 4, :], bv)
                    P.copy(H2T[:, half * 4:(half + 1) * 4, t * 128:(t + 1) * 128], bv, e="act")
                b = nb()
                for kc in range(8):
                    P.mm(b[:, 0:16], HF[:, kc, :], RW[:, kc, :], start=(kc == 0), stop=(kc == 7))
                sco = SCO[k]; ss = SS[k]; psm = PSm[k]; gs = GS[k]; gmx = GMx[k]; e2 = E2[k]
                P.act(sco[:], b[:, 0:16], AF.Sigmoid)
                ssf = ss.v(ss.t[:, :, :].rearrange("p a b -> p (a b)"))
                P.tt(ssf, sco[:], RB[:], ALU.add)
                P.tt(psm[:, :, 0:3], ss[:, :, 0:3], ss[:, :, 1:4], ALU.add)
                P.tt(psm[:, :, 3:5], ss[:, :, 0:2], ss[:, :, 2:4], ALU.add)
                P.tt(psm[:, :, 5:6], ss[:, :, 0:1], ss[:, :, 3:4], ALU.add)
                P.red(gs[:], psm[:], ALU.max)
                P.red(gmx[:, 0:1], gs[:], ALU.max)
                P.ts(gs[:], gs[:], gmx[:, 0:1], None, ALU.is_ge)
                P.tt(psm[:, :, 0:3], ss[:, :, 0:3], ss[:, :, 1:4], ALU.min)
                P.tt(psm[:, :, 3:5], ss[:, :, 0:2], ss[:, :, 2:4], ALU.min)
                P.tt(psm[:, :, 5:6], ss[:, :, 0:1], ss[:, :, 3:4], ALU.min)
                P.red(e2[:, :, 0], psm[:], ALU.max)
                thr = E2[k].v(E2[k].t[:, :, 0:1].to_broadcast([128, 4, 4]))
                P.tt(psm[:, :, 0:4], ss[:], thr, ALU.is_ge)
                P.tt(psm[:, :, 0:4], psm[:, :, 0:4], _bc(gs, gs.t[:, :], 2, [128, 4, 4]), ALU.mult)
                scv = sco.v(sco.t[:, :].rearrange("p (a b) -> p a b", b=4))
                P.tt(e2[:], psm[:, :, 0:4], scv, ALU.mult)
                P.red(gmx[:, 1:2], E2[k].v(E2[k].t[:, :, :].rearrange("p a b -> p (a b)")), ALU.add)
                P.recip(gmx[:, 1:2], gmx[:, 1:2])
                P.ts(CW[:, t, :], E2[k].v(E2[k].t[:, :, :].rearrange("p a b -> p (a b)")), gmx[:, 1:2], None, ALU.mult)
            for e in range(NE):
                p_ = e % 2
                for m, (wsrc, pat) in enumerate(((wg, 8), (wu, 8), (wd, 4))):
                    S_ = STG[stg_i[0] % 2]; stg_i[0] += 1
                    sv = S_.v(S_.t[:, :].rearrange("p (kc n) -> p kc n", kc=pat))
                    P.load("sp" if m % 2 == 0 else "act", sv, wsrc[e].rearrange("(kc kp) n -> kp kc n", kp=128))
                    Wb = WB[m][p_]
                    if m < 2:
                        P.copy(Wb[:], S_[:], e="pool")
                    else:
                        P.tt(Wb.v(Wb.t[:, :].rearrange("p (kc n) -> p kc n", kc=4)), sv, _bc(GF, GF.t[:, :], 1, [128, 4, D]), ALU.mult, e="pool")
                WG = WB[0][p_].v(WB[0][p_].t[:, :].rearrange("p (kc n) -> p kc n", kc=8))
                WU = WB[1][p_].v(WB[1][p_].t[:, :].rearrange("p (kc n) -> p kc n", kc=8))
                WD = WB[2][p_].v(WB[2][p_].t[:, :].rearrange("p (kc n) -> p kc n", kc=4))
                for sub in range(2):
                    hid = HID[sub]
                    tsl = slice(sub * 512, (sub + 1) * 512)
                    for c in range(4):
                        bg = nb(); bu = nb()
                        for kc in range(8):
                            P.mm(bg[:], WG[:, kc, c * 128:(c + 1) * 128], H2T[:, kc, tsl], start=(kc == 0), stop=(kc == 7))
                        for kc in range(8):
                            P.mm(bu[:], WU[:, kc, c * 128:(c + 1) * 128], H2T[:, kc, tsl], start=(kc == 0), stop=(kc == 7))
                        sil = SIL[c % 2]
                        P.act(sil[:], bg[:], AF.Silu)
                        P.tt(hid[:, c, :], sil[:], bu[:], ALU.mult)
                    for t4 in range(4):
                        t = sub * 4 + t4
                        for nh in range(2):
                            cs = slice(nh * 512, (nh + 1) * 512)
                            bd = nb()
                            for c in range(4):
                                P.mm(bd[:], hid[:, c, t4 * 128:(t4 + 1) * 128], WD[:, c, cs], start=(c == 0), stop=(c == 3))
                            P.stt(ACC[t][:, cs], bd[:], CW[:, t, e:e + 1], ACC[t][:, cs], ALU.mult, ALU.add)
            for t in range(8):
                ti = tg * 8 + t
                P.store("sp" if t % 2 else "act", xo[ti * 128:(ti + 1) * 128, :], ACC[t][:])
        P.finish()
        print("k4b ninst", P.ninst, "nsem", P.nsem)
    return nc


def run_k4b(x1, g2, sc2, sh2, gate_ffn, prm, l):
    if "k4b" not in _CACHE:
        _CACHE["k4b"] = build_k4b()
    nc = _CACHE["k4b"]
    ident = np.eye(128, dtype=np.float32)
    maps = []
    for i in range(NCORES):
        b, tq = divmod(i, 4)
        ts_ = slice(tq * 2048, (tq + 1) * 2048)
        maps.append({"x1": np.ascontiguousarray(x1[b, ts_]), "g": g2[None].copy(), "sc": sc2[b][None].copy(),
                     "sh": sh2[b][None].copy(), "gf": gate_ffn[b][None].copy(), "rw": prm["router_w"],
                     "rb": prm["router_b"][None].copy(), "ident": ident,
                     "wg": prm["exp_w_gate"][l], "wu": prm["exp_w_up"][l], "wd": prm["exp_w_down"][l]})
    res = run_bass_kernel_spmd(nc, maps, core_ids=list(range(NCORES)))
    out = np.empty((NB, T, D), np.float32)
    for i in range(NCORES):
        b, tq = divmod(i, 4)
        out[b, tq * 2048:(tq + 1) * 2048] = res.results[i]["xo"]
    return out


def kernel(**inputs):
    prm = {k: np.asarray(v) for k, v in inputs.items()}
    x = prm["x"].astype(np.float32, copy=False)
    mod = run_k0(prm["c"], prm["w_ada"], prm["b_ada"])
    for l in range(2):
        sh1, sc1, gate_mix, sh2, sc2, gate_ffn = [np.ascontiguousarray(a) for a in np.split(mod[l], 6, axis=-1)]
        p = run_k1(x, prm["norm_g"][l, 0], sc1, sh1, prm["w_in"][l], prm["b_in"][l])
        ya = run_k2(p, prm, l)
        yb = run_k3(p, prm, l)
        x1 = run_k4a(x, ya, yb, p, gate_mix, prm, l)
        x = run_k4b(x1, prm["norm_g"][l, 1], sc2, sh2, gate_ffn, prm, l)
    return x
```

```python
import numpy as np
import concourse.bass as bass
import concourse.mybir as mybir
from contextlib import ExitStack

F32 = mybir.dt.float32
BF16 = mybir.dt.bfloat16
I32 = mybir.dt.int32
U32 = mybir.dt.uint32
AF = mybir.ActivationFunctionType
ALU = mybir.AluOpType
AX = mybir.AxisListType


class Buf:
    __slots__ = ("name", "w", "r", "dsem", "dcnt", "t", "excl")

    def __init__(self, name, t=None):
        self.name = name
        self.w = None
        self.r = []
        self.dsem = None
        self.dcnt = 0
        self.t = t
        self.excl = False


class Prog:
    ENG = ("pe", "dve", "act", "pool", "sp")

    def __init__(self, nc, es: ExitStack):
        self.nc = nc
        self.es = es
        self.eng = {"pe": nc.tensor, "dve": nc.vector, "act": nc.scalar,
                    "pool": nc.gpsimd, "sp": nc.sync}
        self.sem = {}
        self.cnt = {}
        for e in self.ENG:
            self.sem[e] = es.enter_context(nc.semaphore("s_" + e))
            self.cnt[e] = 0
        self.waited = {e: {} for e in self.ENG}
        self.out_tokens = []
        self.nsem = 5
        self.ninst = 0

    def sb(self, name, shape, dt=F32):
        t = self.es.enter_context(self.nc.sbuf_tensor(name, list(shape), dt))
        return t

    def ps(self, name, shape, dt=F32):
        t = self.es.enter_context(self.nc.psum_tensor(name, list(shape), dt))
        return t

    def buf(self, name, t=None):
        return Buf(name, t)

    def _wait(self, e, deps):
        w = self.waited[e]
        best = {}
        for d in deps:
            if d is None:
                continue
            sem, val, pe = d
            if e == "pe" and pe == "pe":
                continue
            k = id(sem)
            if w.get(k, 0) >= val:
                continue
            if k not in best or best[k][1] < val:
                best[k] = (sem, val)
        for k, (sem, val) in best.items():
            self.eng[e].wait_ge(sem, val)
            w[k] = val
            self.ninst += 1

    def op(self, e, build, reads=(), writes=()):
        deps = []
        for b in reads:
            deps.append(b.w)
            if b.excl:
                deps.extend(r for r in b.r if r[2] != e)
        for b in writes:
            deps.append(b.w)
            deps.extend(b.r)
        self._wait(e, deps)
        ins = build(self.eng[e])
        self.cnt[e] += 1
        ins.then_inc(self.sem[e], 1)
        tok = (self.sem[e], self.cnt[e], e)
        self.ninst += 1
        for b in reads:
            b.r.append(tok)
            if len(b.r) > 64:
                b.r = self._compact(b.r)
        for b in writes:
            b.w = tok
            b.r = []
        return tok

    @staticmethod
    def _compact(rs):
        best = {}
        for sem, val, e in rs:
            k = id(sem)
            if k not in best or best[k][1] < val:
                best[k] = (sem, val, e)
        return list(best.values())

    def dma(self, q, out, in_, sbuf_buf, reads=(), writes=(), is_output=False, **kw):
        deps = []
        for b in reads:
            deps.append(b.w)
        for b in writes:
            deps.append(b.w)
            deps.extend(b.r)
        self._wait(q, deps)
        b0 = sbuf_buf
        if b0.dsem is None:
            b0.dsem = self.es.enter_context(self.nc.semaphore("d_" + b0.name))
            self.nsem += 1
        ins = self.eng[q].dma_start(out=out, in_=in_, **kw)
        b0.dcnt += 16
        ins.then_inc(b0.dsem, 16)
        tok = (b0.dsem, b0.dcnt, "dma")
        self.ninst += 1
        for b in reads:
            b.r.append(tok)
        for b in writes:
            b.w = tok
            b.r = []
        if is_output:
            self.out_tokens.append(tok)
        return tok

    def finish(self):
        self._wait("sp", self.out_tokens)
        deps = [(self.sem[e], self.cnt[e], e) for e in self.ENG if e != "sp" and self.cnt[e] > 0]
        self._wait("sp", deps)


class View:
    __slots__ = ("tile", "ap")

    def __init__(self, tile, ap):
        self.tile = tile
        self.ap = ap

    def __getitem__(self, idx):
        return View(self.tile, self.ap[idx])

    @property
    def t(self):
        return self.ap

    def v(self, ap):
        return View(self.tile, ap)


class Tile:
    def __init__(self, P, name, shape, dt=F32, psum=False):
        self.P = P
        self.name = name
        self.t = (P.ps if psum else P.sb)(name, shape, dt)
        self.buf = Buf(name)
        self.buf.excl = bool(psum)
        self.shape = list(shape)

    def __getitem__(self, idx):
        return View(self, self.t[idx])

    def v(self, ap):
        return View(self, ap)


def _bufs(views):
    out = []
    for v in views:
        if isinstance(v, View):
            if v.tile.buf not in out:
                out.append(v.tile.buf)
        elif isinstance(v, Tile):
            if v.buf not in out:
                out.append(v.buf)
    return out


def _ap(v):
    return v.ap if isinstance(v, View) else v


class Prog2(Prog):
    def tile(self, name, shape, dt=F32, psum=False):
        return Tile(self, name, shape, dt, psum)

    def gop(self, e, fn, outs, ins):
        return self.op(e, fn, reads=_bufs(ins), writes=_bufs(outs))

    def act(self, out, in_, func, bias=0.0, scale=1.0, accum_out=None, e="act"):
        ins = [in_, bias, scale]
        outs = [out] + ([accum_out] if accum_out is not None else [])
        kw = {}
        if accum_out is not None:
            kw["accum_out"] = _ap(accum_out)
        return self.gop(e, lambda E: E.activation(out=_ap(out), in_=_ap(in_), func=func,
                                                  bias=_ap(bias), scale=_ap(scale), **kw), outs, ins)

    def tt(self, out, a, b, op, e="dve"):
        return self.gop(e, lambda E: E.tensor_tensor(out=_ap(out), in0=_ap(a), in1=_ap(b), op=op), [out], [a, b])

    def ts(self, out, a, s1, s2, op0, op1=None, accum_out=None, e="dve"):
        kw = {}
        if op1 is not None:
            kw["op1"] = op1
        outs = [out]
        if accum_out is not None:
            kw["accum_out"] = _ap(accum_out)
            outs.append(accum_out)
        return self.gop(e, lambda E: E.tensor_scalar(out=_ap(out), in0=_ap(a), scalar1=_ap(s1),
                                                     scalar2=_ap(s2) if s2 is not None else None,
                                                     op0=op0, **kw), outs, [a, s1, s2])

    def stt(self, out, in0, scalar, in1, op0, op1, accum_out=None, e="dve"):
        kw = {}
        outs = [out]
        if accum_out is not None:
            kw["accum_out"] = _ap(accum_out)
            outs.append(accum_out)
        return self.gop(e, lambda E: E.scalar_tensor_tensor(out=_ap(out), in0=_ap(in0), scalar=_ap(scalar),
                                                            in1=_ap(in1), op0=op0, op1=op1, **kw),
                        outs, [in0, scalar, in1])

    def copy(self, out, in_, e="dve"):
        if e == "act":
            return self.gop(e, lambda E: E.copy(out=_ap(out), in_=_ap(in_)), [out], [in_])
        return self.gop(e, lambda E: E.tensor_copy(out=_ap(out), in_=_ap(in_)), [out], [in_])

    def memset(self, out, val, e="dve"):
        return self.gop(e, lambda E: E.memset(_ap(out), val), [out], [])

    def red(self, out, in_, op, axis=AX.X, e="dve"):
        return self.gop(e, lambda E: E.tensor_reduce(out=_ap(out), in_=_ap(in_), axis=axis, op=op), [out], [in_])

    def recip(self, out, in_):
        return self.gop("dve", lambda E: E.reciprocal(out=_ap(out), in_=_ap(in_)), [out], [in_])

    def mm(self, out, lhsT, rhs, start=True, stop=True):
        return self.gop("pe", lambda E: E.matmul(_ap(out), _ap(lhsT), _ap(rhs), start=start, stop=stop),
                        [out], [lhsT, rhs])

    def tr(self, out, in_, ident):
        return self.gop("pe", lambda E: E.transpose(_ap(out), _ap(in_), _ap(ident)), [out], [in_, ident])

    def load(self, q, view, dram_ap, **kw):
        return self.dma(q, view.ap, dram_ap, view.tile.buf, writes=[view.tile.buf], **kw)

    def store(self, q, dram_ap, view, is_output=True, **kw):
        return self.dma(q, dram_ap, view.ap, view.tile.buf, reads=[view.tile.buf], is_output=is_output, **kw)


from concourse.bass_utils import run_bass_kernel_spmd

D = 1024
T = 8192
NB = 2
IN_COLS = 5144
NCORES = 8
_CACHE = {}


def _new_nc():
    return bass.Bass("TRN2", target_bir_lowering=False)


def build_k0():
    nc = _new_nc()
    NCOL = 1536
    cT = nc.dram_tensor("cT", [128, 8, 2], F32, kind="ExternalInput").ap()
    w = nc.dram_tensor("w", [1024, NCOL], F32, kind="ExternalInput").ap()
    b = nc.dram_tensor("b", [1, NCOL], F32, kind="ExternalInput").ap()
    y = nc.dram_tensor("y", [2, NCOL], F32, kind="ExternalOutput").ap()
    with ExitStack() as es:
        P = Prog2(nc, es)
        ct = P.tile("ct", [128, 8, 2])
        cs = P.tile("cs", [128, 8, 2])
        wt = P.tile("wt", [128, 8, NCOL])
        bt = P.tile("bt", [2, NCOL])
        yt = P.tile("yt", [2, NCOL])
        P.load("sp", ct[:], cT[:, :, :])
        P.load("act", wt[:], w.rearrange("(kc kp) n -> kp kc n", kp=128))
        P.load("sp", bt[:], b.partition_broadcast(2))
        P.act(cs[:], ct[:], AF.Silu)
        for n in range(3):
            ps = P.tile(f"ps{n}", [2, 512], psum=True)
            for kc in range(8):
                P.mm(ps[:], cs[:, kc, :], wt[:, kc, n * 512:(n + 1) * 512], start=(kc == 0), stop=(kc == 7))
            P.tt(yt[:, n * 512:(n + 1) * 512], ps[:], bt[:, n * 512:(n + 1) * 512], ALU.add)
        P.store("sp", y[:, :], yt[:])
        P.finish()
    return nc


def run_k0(c, w_ada, b_ada):
    if "k0" not in _CACHE:
        _CACHE["k0"] = build_k0()
    nc = _CACHE["k0"]
    L = w_ada.shape[0]
    wcat = np.concatenate([w_ada[l] for l in range(L)], axis=1)
    bcat = np.concatenate([b_ada[l] for l in range(L)], axis=0)[None]
    cT = np.ascontiguousarray(c.T.reshape(8, 128, 2).transpose(1, 0, 2))
    maps = []
    for i in range(NCORES):
        sl = slice(i * 1536, (i + 1) * 1536)
        maps.append({"cT": cT, "w": np.ascontiguousarray(wcat[:, sl]), "b": np.ascontiguousarray(bcat[:, sl])})
    res = run_bass_kernel_spmd(nc, maps, core_ids=list(range(NCORES)))
    y = np.concatenate([r["y"] for r in res.results], axis=1)
    return y.reshape(2, L, 6144).transpose(1, 0, 2)


def build_k1():
    nc = _new_nc()
    NT = 16
    x = nc.dram_tensor("x", [NT * 128, D], F32, kind="ExternalInput").ap()
    g = nc.dram_tensor("g", [1, D], F32, kind="ExternalInput").ap()
    sc = nc.dram_tensor("sc", [1, D], F32, kind="ExternalInput").ap()
    sh = nc.dram_tensor("sh", [1, D], F32, kind="ExternalInput").ap()
    w = nc.dram_tensor("w", [D, IN_COLS], F32, kind="ExternalInput").ap()
    b = nc.dram_tensor("b", [1, IN_COLS], F32, kind="ExternalInput").ap()
    ident = nc.dram_tensor("ident", [128, 128], F32, kind="ExternalInput").ap()
    p = nc.dram_tensor("p", [NT * 128, IN_COLS], F32, kind="ExternalOutput").ap()
    with ExitStack() as es, nc.allow_low_precision("bf16 in-proj operands, fp32 PSUM accumulation"):
        P = Prog2(nc, es)
        idt = P.tile("idt", [128, 128])
        G = P.tile("G", [128, D]); SC = P.tile("SC", [128, D]); SH = P.tile("SH", [128, D])
        B = P.tile("B", [128, IN_COLS])
        hT = [P.tile(f"hT{i}", [128, 8, 128], BF16) for i in range(NT)]
        P.load("sp", idt[:], ident[:, :])
        P.load("sp", G[:], g.partition_broadcast(128))
        P.load("act", SC[:], sc.partition_broadcast(128))
        P.load("sp", SH[:], sh.partition_broadcast(128))
        P.load("act", B[:], b.partition_broadcast(128))
        P.stt(G[:], SC[:], 1.0, G[:], ALU.add, ALU.mult)
        xt = [P.tile(f"xt{i}", [128, D]) for i in range(2)]
        junk = P.tile("junk", [128, D])
        st = [P.tile(f"st{i}", [128, 2]) for i in range(2)]
        pT = [P.tile(f"pT{i}", [128, 4, 128], psum=True) for i in range(2)]
        for ti in range(NT):
            X = xt[ti % 2]; S = st[ti % 2]
            P.load("sp" if ti % 2 == 0 else "act", X[:], x[ti * 128:(ti + 1) * 128, :])
            P.act(junk[:], X[:], AF.Square, accum_out=S[:, 0:1])
            P.ts(S[:, 1:2], S[:, 0:1], 1.0 / D, 1e-6, ALU.mult, ALU.add)
            P.act(S[:, 1:2], S[:, 1:2], AF.Sqrt)
            P.recip(S[:, 1:2], S[:, 1:2])
            P.stt(X[:], X[:], S[:, 1:2], G[:], ALU.mult, ALU.mult)
            P.tt(X[:], X[:], SH[:], ALU.add)
            for half in range(2):
                pt = pT[half]
                for j in range(4):
                    kc = half * 4 + j
                    P.tr(pt[:, j, :], X[:, kc * 128:(kc + 1) * 128], idt[:])
                P.copy(hT[ti][:, half * 4:(half + 1) * 4, :], pt[:], e="act" if half else "dve")
        NCH = (IN_COLS + 511) // 512
        wt = [P.tile(f"wt{i}", [128, 8, 512]) for i in range(2)]
        wb = [P.tile(f"wb{i}", [128, 8, 512], BF16) for i in range(2)]
        po = [P.tile(f"po{i}", [128, 512], psum=True) for i in range(4)]
        ot = [P.tile(f"ot{i}", [128, 512]) for i in range(4)]
        wv = w.rearrange("(kc kp) n -> kp kc n", kp=128)
        k = 0
        for ci in range(NCH):
            c0 = ci * 512
            cw = min(512, IN_COLS - c0)
            W = wt[ci % 2]
            P.load("sp" if ci % 2 == 0 else "act", W[:, :, :cw], wv[:, :, c0:c0 + cw])
            Wb = wb[ci % 2]
            P.copy(Wb[:, 0:4, :cw], W[:, 0:4, :cw], e="pool")
            P.copy(Wb[:, 4:8, :cw], W[:, 4:8, :cw], e="act")
            for ti in range(NT):
                ps = po[k % 4]; o = ot[k % 4]
                for kc in range(8):
                    P.mm(ps[:, :cw], hT[ti][:, kc, :], Wb[:, kc, :cw], start=(kc == 0), stop=(kc == 7))
                P.tt(o[:, :cw], ps[:, :cw], B[:, c0:c0 + cw], ALU.add, e="dve")
                P.store("sp" if k % 2 == 0 else "act", p[ti * 128:(ti + 1) * 128, c0:c0 + cw], o[:, :cw])
                k += 1
        P.finish()
    return nc


def run_k1(xfull, g, sc, sh, w, b):
    if "k1" not in _CACHE:
        _CACHE["k1"] = build_k1()
    nc = _CACHE["k1"]
    ident = np.eye(128, dtype=np.float32)
    maps = []
    for i in range(NCORES):
        bi, tq = divmod(i, 4)
        maps.append({"x": np.ascontiguousarray(xfull[bi, tq * 2048:(tq + 1) * 2048]),
                     "g": g[None].copy(), "sc": sc[bi][None].copy(), "sh": sh[bi][None].copy(),
                     "w": w, "b": b[None].copy(), "ident": ident})
    res = run_bass_kernel_spmd(nc, maps, core_ids=list(range(NCORES)))
    out = np.empty((NB, T, IN_COLS), np.float32)
    for i in range(NCORES):
        bi, tq = divmod(i, 4)
        out[bi, tq * 2048:(tq + 1) * 2048] = res.results[i]["p"]
    return out


def _bc(tile, ap, axis, shape):
    return tile.v(ap.unsqueeze(axis).to_broadcast(list(shape)))


def k2_consts():
    C = 64
    tri_incl = np.triu(np.ones((C, C), np.float32))
    tri_strict = np.triu(np.ones((C, C), np.float32), 1)
    m = np.concatenate([tri_strict, tri_incl], 1)
    mask128 = np.concatenate([m, m], 0)
    sel0 = np.concatenate([np.eye(64, dtype=np.float32), np.zeros((64, 64), np.float32)], 1)
    shift = np.concatenate([np.zeros((64, 64), np.float32), np.eye(64, dtype=np.float32)], 1)
    return {"ident": np.eye(128, dtype=np.float32), "mask128": mask128,
            "trils": np.ascontiguousarray(tri_strict.T), "tri": tri_incl,
            "ones64": np.ones((64, 64), np.float32), "sel0": sel0, "shift": shift}


def build_k2(TT=T, stop=0, ustop=99):
    nc = _new_nc()
    NG = TT // 256
    dr = lambda n, s, kind="ExternalInput": nc.dram_tensor(n, s, F32, kind=kind).ap()
    pp = dr("pp", [TT + 1, 640]); mu = dr("mu", [1, 640]); vec = dr("vec", [8, 128])
    w2 = dr("w2", [64, 128]); a2 = dr("a2", [64, 128]); g2 = dr("g2", [128, 128])
    ident = dr("ident", [128, 128]); mask128 = dr("mask128", [128, 128]); trils = dr("trils", [64, 64])
    tri = dr("tri", [64, 64]); ones64 = dr("ones64", [64, 64]); sel0 = dr("sel0", [64, 128]); shift = dr("shift", [64, 128])
    ya = dr("ya", [TT, 128], "ExternalOutput")
    with ExitStack() as es:
        P = Prog2(nc, es)
        IDT = P.tile("IDT", [128, 128]); MASK = P.tile("MASK", [128, 128]); TRILS = P.tile("TRILS", [64, 64])
        TRI = P.tile("TRI", [64, 64]); ONES = P.tile("ONES", [64, 64]); SEL0 = P.tile("SEL0", [64, 128]); SHIFT = P.tile("SHIFT", [64, 128])
        W2 = P.tile("W2", [64, 128]); A2 = P.tile("A2", [64, 128]); G2 = P.tile("G2", [128, 128])
        MU = P.tile("MU", [64, 640]); VEC = P.tile("VEC", [64, 8, 128])
        qs = ["sp", "act"]
        for i, (tl, d) in enumerate([(IDT, ident), (MASK, mask128), (TRILS, trils), (TRI, tri), (ONES, ones64),
                                     (SEL0, sel0), (SHIFT, shift), (W2, w2), (A2, a2), (G2, g2)]):
            P.load(qs[i % 2], tl[:], d[:, :])
        P.load("sp", MU[:], mu.partition_broadcast(64))
        for r in range(7):
            P.load(qs[r % 2], VEC[:, r, :], vec[r:r + 1, :].partition_broadcast(64))
        I64 = IDT[0:64, 0:64]
        S4 = [64, 4, 128]
        vb = lambda r: _bc(VEC, VEC.t[:, r, :], 1, S4)
        W0b, A0b, KKb, KAb, RKb, GNGb, GNBb = [vb(r) for r in range(7)]
        banks = [P.tile(f"bank{i}", [128, 512], psum=True) for i in range(8)]
        pk = [0]

        def pbank():
            b = banks[pk[0] % 4]
            pk[0] += 1
            return b

        def t4(name, n=2):
            return [P.tile(f"{name}{i}", S4) for i in range(n)]

        CURt = [P.tile(f"CUR{i}", [64, 4, 640]) for i in range(2)]
        PRVt = [P.tile(f"PRV{i}", [64, 4, 640]) for i in range(2)]
        TWt = t4("TW"); SGt = t4("SG"); LTt = [P.tile(f"LT{i}", [64, 4, 2, 64]) for i in range(2)]
        GTtt = [P.tile(f"GTt{i}", [128, 4, 64]) for i in range(2)]
        LDt = t4("LD"); At = t4("A"); Ggt = t4("Gg"); KKt = t4("KK"); SQt = t4("SQ"); T1t = t4("T1"); KMt = t4("KM"); Bvt = t4("Bv")
        SSt = [P.tile(f"SS{i}", [64, 8]) for i in range(2)]; RKt = [P.tile(f"RK{i}", [64, 8]) for i in range(2)]
        Lst = t4("Ls"); ELt = t4("EL"); ENLt = t4("ENL"); TMPt = t4("TMP"); TMP2t = t4("TMP2")
        DCFt = [P.tile(f"DCF{i}", [64, 8]) for i in range(2)]
        A_t = t4("A_"); BTt = t4("BT"); KTt = t4("KT"); RTt = t4("RT"); BBt = t4("BB"); KBt = t4("KB")
        FMBKt = [P.tile(f"FMBK{i}", [64, 8, 128]) for i in range(2)]
        FMARt = [P.tile(f"FMAR{i}", [64, 8, 128]) for i in range(2)]
        BKSt = [P.tile(f"BKS{i}", [128, 4, 128]) for i in range(2)]
        VVt = [P.tile(f"VV{i}", [128, 4, 128]) for i in range(2)]
        YBt = t4("YB"); YCt = t4("YC"); MNt = [P.tile(f"MN{i}", [64, 8]) for i in range(2)]; VRt = [P.tile(f"VR{i}", [64, 8]) for i in range(2)]
        hk = [0, 0]
        ubi = [0]; hkk = [0]
        BIGA = P.tile("BIGA", [128, 8, 128])
        XA = [P.tile(f"XA{i}", [64, 8, 64]) for i in range(2)]
        XTA = [P.tile(f"XTA{i}", [64, 8, 64]) for i in range(2)]
        PMA = [P.tile(f"PMA{i}", [64, 8, 64]) for i in range(2)]
        W2A = P.tile("W2A", [64, 8, 64]); AHA = P.tile("AHA", [64, 8, 64]); RHA = P.tile("RHA", [64, 8, 64])
        GTA = P.tile("GTA", [64, 8, 64]); TMPG = P.tile("TMPG", [64, 8, 64])
        HA = [P.tile(f"HA{i}", [64, 2, 64]) for i in range(2)]
        P.memset(HA[0][:], 0.0)
        fl = lambda tl: tl.v(tl.t[:, :, :].rearrange("p a b -> p (a b)"))
        v8 = lambda tl: tl.v(tl.t[:, :, :].rearrange("p a (h b) -> p (a h) b", b=64))
        b8 = lambda tl: _bc(tl, tl.t[:, :], 2, [64, 8, 64])
        ppv = lambda lo: pp[lo:lo + 256, :].rearrange("(c t) n -> t c n", t=64)
        NEG = -float(np.exp(-0.5))

        class _Stop(Exception):
            pass

        def early(n, view):
            if stop == n:
                P.store("sp", ya[0:256, :].rearrange("(c t) n -> t c n", t=64), view)
                raise _Stop()

        ctxp = {}

        def prepG(g):
            k = g % 2
            CUR = CURt[k]; PRV = PRVt[k]
            P.load("sp", CUR[:], ppv(1 + g * 256))
            yield
            P.load("act", PRV[:], ppv(g * 256))
            yield
            P.tt(PRV[:], PRV[:], CUR[:], ALU.subtract)
            yield
            P.tt(PRV[:], PRV[:], _bc(MU, MU.t[:, :], 1, [64, 4, 640]), ALU.mult)
            yield
            P.tt(CUR[:], CUR[:], PRV[:], ALU.add, e="pool")
            yield
            Rr = CUR[:, :, 0:128]; Kr = CUR[:, :, 128:256]; Vr = CUR[:, :, 256:384]
            TW = TWt[k]; SG = SGt[k]; LT = LTt[k]; GTt = GTtt[k]
            P.act(TW[:, :, 0:64], CUR[:, :, 384:448], AF.Tanh)
            yield
            P.act(SG[:], CUR[:, :, 512:640], AF.Sigmoid)
            yield
            b1 = pbank(); b1v = b1.v(b1.t[0:64, :].rearrange("p (c w t) -> p c w t", c=4, w=2))
            for c in range(4):
                P.tr(b1.v(b1v.ap[:, c, 0, :]), TW[:, c, 0:64], I64)
                yield
                P.tr(b1.v(b1v.ap[:, c, 1, :]), CUR[:, c, 448:512], I64)
                yield
            P.copy(LT[:], b1v)
            yield
            b2 = pbank(); b2v = b2.v(b2.t[:, 0:256].rearrange("p (c t) -> p c t", c=4))
            for c in range(4):
                P.tr(b2.v(b2v.ap[:, c, :]), SG[:, c, :], I64)
                yield
            P.copy(GTt[:], b2v, e="act")
            yield
            bw = pbank(); bwv = bw.v(bw.t[0:64, :].rearrange("p (c n) -> p c n", c=4))
            for c in range(4):
                P.mm(bw.v(bwv.ap[:, c, :]), LT[:, c, 0, :], W2[:])
                yield
            LD = LDt[k]
            P.tt(LD[:], bwv, W0b, ALU.add)
            yield
            ba = pbank(); bav = ba.v(ba.t[0:64, :].rearrange("p (c n) -> p c n", c=4))
            for c in range(4):
                P.mm(ba.v(bav.ap[:, c, :]), LT[:, c, 1, :], A2[:])
                yield
            A = At[k]
            P.tt(A[:], bav, A0b, ALU.add)
            yield
            bg = pbank(); bgv = bg.v(bg.t[0:64, :].rearrange("p (c n) -> p c n", c=4))
            for c in range(4):
                P.mm(bg.v(bgv.ap[:, c, :]), GTt[:, c, :], G2[:])
                yield
            Gg = Ggt[k]
            P.copy(Gg[:], bgv, e="act")
            yield
            P.act(LD[:], LD[:], AF.Sigmoid)
            yield
            P.act(A[:], A[:], AF.Sigmoid)
            yield
            P.ts(LD[:], LD[:], NEG, None, ALU.mult, e="pool")
            yield
            KK = KKt[k]; SQ = SQt[k]; SS = SSt[k]; T1 = T1t[k]; KM = KMt[k]; Bv = Bvt[k]; RK = RKt[k]
            P.tt(KK[:], Kr, KKb, ALU.mult)
            yield
            P.tt(SQ[:], KK[:], KK[:], ALU.mult)
            yield
            P.red(SS[:], v8(SQ), ALU.add)
            yield
            P.act(SS[:], SS[:], AF.Sqrt)
            yield
            P.ts(SS[:], SS[:], 1e-12, None, ALU.max)
            yield
            P.recip(SS[:], SS[:])
            yield
            P.tt(v8(KK), v8(KK), b8(SS), ALU.mult)
            yield
            P.stt(T1[:], A[:], -1.0, KAb, ALU.add, ALU.mult)
            yield
            P.stt(KM[:], T1[:], 1.0, Kr, ALU.add, ALU.mult)
            yield
            P.tt(Bv[:], KK[:], A[:], ALU.mult)
            yield
            P.tt(SQ[:], Rr, KM[:], ALU.mult)
            yield
            P.tt(SQ[:], SQ[:], RKb, ALU.mult)
            yield
            P.red(RK[:], v8(SQ), ALU.add)
            yield
            Ls = Lst[k]; EL = ELt[k]; ENL = ENLt[k]; TMP = TMPt[k]; TMP2 = TMP2t[k]; DCF = DCFt[k]
            bL = pbank(); bLv = bL.v(bL.t[0:64, :].rearrange("p (c n) -> p c n", c=4))
            P.mm(bL[0:64, :], TRI[:], fl(LD))
            yield
            P.copy(Ls[:], bLv)
            yield
            bC = pbank(); bCv = bC.v(bC.t[0:64, :].rearrange("p (c n) -> p c n", c=4))
            P.mm(bC[0:64, :], ONES[:], fl(LD))
            yield
            P.tt(TMP2[:], bCv, Ls[:], ALU.subtract)
            yield
            bD = pbank()
            for u in range(8):
                c, h = divmod(u, 2)
                P.mm(bD[0:64, u:u + 1], LD[:, c, h * 64:(h + 1) * 64], ONES[:, 0:1])
                yield
            P.act(DCF[:], bD[0:64, 0:8], AF.Exp)
            yield
            P.act(EL[:], Ls[:], AF.Exp)
            yield
            P.act(ENL[:], Ls[:], AF.Exp, scale=-1.0)
            yield
            P.tt(TMP[:], Ls[:], LD[:], ALU.subtract)
            yield
            P.act(TMP[:], TMP[:], AF.Exp)
            yield
            P.act(TMP2[:], TMP2[:], AF.Exp)
            yield
            A_ = A_t[k]; BT = BTt[k]; KT = KTt[k]; RT = RTt[k]; BB = BBt[k]; KB = KBt[k]
            P.stt(A_[:], KK[:], -1.0, TMP[:], ALU.mult, ALU.mult)
            yield
            P.tt(BT[:], Bv[:], ENL[:], ALU.mult)
            yield
            P.tt(KT[:], KM[:], ENL[:], ALU.mult, e="pool")
            yield
            P.tt(RT[:], Rr, EL[:], ALU.mult)
            yield
            P.tt(BB[:], Bv[:], TMP2[:], ALU.mult, e="pool")
            yield
            P.tt(KB[:], KM[:], TMP2[:], ALU.mult)
            yield
            FMBK = FMBKt[k]; FMAR = FMARt[k]; BKS = BKSt[k]; VV = VVt[k]
            for half in range(2):
                pb = pbank(); pbv = pb.v(pb.t[0:64, :].rearrange("p (u n) -> p u n", u=4))
                pa_ = pbank(); pav = pa_.v(pa_.t[0:64, :].rearrange("p (u n) -> p u n", u=4))
                for uu in range(4):
                    u = half * 4 + uu
                    c, h = divmod(u, 2)
                    hs = slice(h * 64, (h + 1) * 64)
                    P.tr(pb.v(pbv.ap[:, uu, 0:64]), BT[:, c, hs], I64)
                    yield
                    P.tr(pb.v(pbv.ap[:, uu, 64:128]), KT[:, c, hs], I64)
                    yield
                    P.tr(pa_.v(pav.ap[:, uu, 0:64]), A_[:, c, hs], I64)
                    yield
                    P.tr(pa_.v(pav.ap[:, uu, 64:128]), RT[:, c, hs], I64)
                    yield
                P.copy(FMBK[:, half * 4:(half + 1) * 4, :], pbv)
                yield
                P.copy(FMAR[:, half * 4:(half + 1) * 4, :], pav, e="act")
                yield
            ps1 = pbank()
            P.mm(ps1[:], SEL0[:], fl(BB), start=True, stop=False)
            yield
            P.mm(ps1[:], SHIFT[:], fl(KB), start=False, stop=True)
            yield
            P.copy(fl(BKS), ps1[:])
            yield
            ps2 = pbank(); ps2v = ps2.v(ps2.t[:, :].rearrange("p (c n) -> p c n", c=4))
            P.mm(ps2v, SHIFT[:], Vr)
            yield
            P.copy(VV[64:128, :, :], ps2.v(ps2v.ap[64:128, :, :]), e="act")
            yield

            ctxp[g] = dict(k=k, CUR=CUR, A_=A_, FMBK=FMBK, FMAR=FMAR, BKS=BKS, VV=VV, DCF=DCF, RK=RK, Gg=Gg, SQ=SQ)

        def drainP(g_, n):
            if g_ is None:
                return
            try:
                if n is None:
                    while True:
                        next(g_)
                else:
                    for _ in range(n):
                        next(g_)
            except StopIteration:
                pass

        drainP(prepG(0), None)
        for g in range(NG):
            c_ = ctxp.pop(g)
            k = c_["k"]; CUR = c_["CUR"]; A_ = c_["A_"]; FMBK = c_["FMBK"]; FMAR = c_["FMAR"]; BKS = c_["BKS"]
            VV = c_["VV"]; DCF = c_["DCF"]; RK = c_["RK"]; Gg = c_["Gg"]; SQ = c_["SQ"]
            gP = prepG(g + 1) if g + 1 < NG else None
            YB = YBt[k]

            def ub():
                b = banks[4 + ubi[0] % 4]; ubi[0] += 1
                return b
            v864 = lambda bank: bank.v(bank.t[0:64, :].rearrange("p (u n) -> p u n", u=8))
            I8 = _bc(IDT, IDT.t[0:64, 0:64], 1, [64, 8, 64])
            bB = [ub(), ub()]; bX = ub()
            for u in range(8):
                bb = bB[u // 4]
                P.mm(bb[:, (u % 4) * 128:(u % 4 + 1) * 128], FMBK[:, u, :], FMAR[:, u, :])
                P.mm(bX[0:64, u * 64:(u + 1) * 64], FMAR[:, u, 0:64], FMBK[:, u, 0:64])
            for hf in range(2):
                bb = bB[hf]
                P.tt(BIGA[:, hf * 4:(hf + 1) * 4, :], bb.v(bb.t[:, :].rearrange("p (u n) -> p u n", u=4)),
                     _bc(MASK, MASK.t[:, :], 1, [128, 4, 128]), ALU.mult)
            XT = XTA[0]
            P.tt(XT[:], v864(bX), _bc(TRILS, TRILS.t[:, :], 1, [64, 8, 64]), ALU.mult)
            X = BIGA.v(BIGA.t[0:64, :, 0:64])
            Pm = PMA[0]
            P.tt(Pm[:], X, I8, ALU.add, e="pool")
            drainP(gP, 12)
            for i in range(5):
                Xn = XA[i % 2]; XTn = XTA[(i + 1) % 2]; Pn = PMA[(i + 1) % 2]
                bA = ub(); bQ = ub()
                for u in range(8):
                    us = slice(u * 64, (u + 1) * 64)
                    if i < 4:
                        P.mm(bA[0:64, us], XT[:, u, :], X[:, u, :])
                    P.mm(bQ[0:64, us], X[:, u, :], XT[:, u, :])
                if i < 4:
                    P.copy(Xn[:], v864(bA), e="act")
                P.copy(XTn[:], v864(bQ))
                bC = ub()
                for u in range(8):
                    us = slice(u * 64, (u + 1) * 64)
                    P.mm(bC[0:64, us], XTn[:, u, :], Pm[:, u, :])
                P.tt(Pn[:], v864(bC), Pm[:], ALU.add)
                drainP(gP, 12)
                X = Xn; XT = XTn; Pm = Pn
                if i == 4:
                    X = None
            InvT = Pm
            bW = ub()
            for u in range(8):
                c, h = divmod(u, 2); hs = slice(h * 64, (h + 1) * 64); us = slice(u * 64, (u + 1) * 64)
                P.mm(bW[0:64, us], BIGA[64:128, u, 0:64], VV[64:128, c, hs])
            P.copy(W2A[:], v864(bW), e="act")
            drainP(gP, 12)
            bA = ub(); bU = ub()
            for u in range(8):
                c, h = divmod(u, 2); hs = slice(h * 64, (h + 1) * 64); us = slice(u * 64, (u + 1) * 64)
                P.mm(bA[0:64, us], InvT[:, u, :], A_[:, c, hs])
                P.mm(bU[0:64, us], InvT[:, u, :], W2A[:, u, :])
            P.copy(AHA[:], v864(bA))
            P.copy(VV.v(VV.t[0:64, :, :].rearrange("p c (h n) -> p (c h) n", n=64)), v864(bU), e="act")
            bR = ub(); bG = ub()
            for u in range(8):
                c, h = divmod(u, 2); hs = slice(h * 64, (h + 1) * 64); us = slice(u * 64, (u + 1) * 64)
                P.mm(bR[0:64, us], AHA[:, u, :], BIGA[0:64, u, 64:128])
                P.mm(bG[0:64, us], AHA[:, u, :], BKS[0:64, c, hs])
            P.tt(RHA[:], v864(bR), FMAR[:, :, 64:128], ALU.add)
            P.tt(TMPG[:], I8, _bc(DCF, DCF.t[:, :], 2, [64, 8, 64]), ALU.mult, e="pool")
            P.tt(GTA[:], v864(bG), TMPG[:], ALU.add)
            drainP(gP, 12)
            bY = ub()
            bHs = [ub(), ub()]
            for c in range(4):
                bH = bHs[c % 2]
                Hc = HA[hkk[0] % 2]; Hn = HA[(hkk[0] + 1) % 2]; hkk[0] += 1
                for h in range(2):
                    u = c * 2 + h
                    hs = slice(h * 64, (h + 1) * 64); us = slice(u * 64, (u + 1) * 64)
                    P.mm(bY[0:64, us], RHA[:, u, :], Hc[:, h, :], start=True, stop=False)
                    P.mm(bY[0:64, us], BIGA[:, u, 64:128], VV[:, c, hs], start=False, stop=True)
                    P.mm(bH[0:64, hs], GTA[:, u, :], Hc[:, h, :], start=True, stop=False)
                    P.mm(bH[0:64, hs], BKS[:, c, hs], VV[:, c, hs], start=False, stop=True)
                P.copy(Hn[:], bH.v(bH.t[0:64, 0:128].rearrange("p (h n) -> p h n", h=2)))
                drainP(gP, 12)
            P.copy(fl(YB), bY[0:64, :], e="act")
            drainP(gP, None)
            YC = YCt[k]; MN = MNt[k]; VR = VRt[k]
            P.red(MN[:], v8(YB), ALU.add)
            P.ts(MN[:], MN[:], 1.0 / 64, None, ALU.mult)
            P.tt(v8(YC), v8(YB), b8(MN), ALU.subtract)
            P.tt(SQ[:], YC[:], YC[:], ALU.mult, e="pool")
            P.red(VR[:], v8(SQ), ALU.add)
            P.ts(VR[:], VR[:], 1.0 / 64, 64e-5, ALU.mult, ALU.add)
            P.act(VR[:], VR[:], AF.Sqrt)
            P.recip(VR[:], VR[:])
            P.tt(v8(YC), v8(YC), b8(VR), ALU.mult)
            P.tt(YC[:], YC[:], GNGb, ALU.mult)
            P.tt(YC[:], YC[:], GNBb, ALU.add)
            P.tt(SQ.v(SQ.t[:, :, :].rearrange("p a (h b) -> p a h b", b=64)),
                 CUR.v(CUR.t[:, :, 256:384].rearrange("p a (h b) -> p a h b", b=64)),
                 RK.v(RK.t[:, :].rearrange("p (a h) -> p a h", h=2).unsqueeze(3).to_broadcast([64, 4, 2, 64])), ALU.mult)
            P.tt(YC[:], YC[:], SQ[:], ALU.add)
            P.tt(YC[:], YC[:], Gg[:], ALU.mult)
            P.store("sp", ya[g * 256:(g + 1) * 256, :].rearrange("(c t) n -> t c n", t=64), YC[:])
        P.finish()
        print("k2 ninst", P.ninst, "nsem", P.nsem)
    return nc


RW = 512
EVAC_E = "act"


def k2_inputs(p_rwkv_b, i, prm, l, consts):
    h0 = 2 * i
    cs = slice(h0 * 64, h0 * 64 + 128)
    cols = np.r_[np.arange(h0 * 64, h0 * 64 + 128), RW + np.arange(h0 * 64, h0 * 64 + 128),
                 2 * RW + np.arange(h0 * 64, h0 * 64 + 128), np.arange(3 * RW, 3 * RW + 256)]
    vec = np.zeros((8, 128), np.float32)
    for r, n in enumerate(["rwkv_w0", "rwkv_a0", "rwkv_k_k", "rwkv_k_a", "rwkv_r_k", "rwkv_gn_g", "rwkv_gn_b"]):
        vec[r] = prm[n][l].reshape(-1)[cs]
    d = {"pp": np.ascontiguousarray(p_rwkv_b[:, cols]), "mu": np.ascontiguousarray(prm["rwkv_mu"][l][cols][None]),
         "vec": vec, "w2": np.ascontiguousarray(prm["rwkv_w2"][l][:, cs]),
         "a2": np.ascontiguousarray(prm["rwkv_a2"][l][:, cs]), "g2": np.ascontiguousarray(prm["rwkv_g2"][l][:, cs])}
    d.update(consts)
    return d


def run_k2(p, prm, l, TT=T):
    key = ("k2", TT)
    if key not in _CACHE:
        _CACHE[key] = build_k2(TT)
    nc = _CACHE[key]
    consts = k2_consts()
    maps = []
    for ci in range(NCORES):
        b, i = divmod(ci, 4)
        pb = np.concatenate([np.zeros((1, 1792), np.float32), p[b, :TT, :1792]], 0)
        maps.append(k2_inputs(pb, i, prm, l, consts))
    res = run_bass_kernel_spmd(nc, maps, core_ids=list(range(NCORES)))
    out = np.empty((NB, TT, 512), np.float32)
    for ci in range(NCORES):
        b, i = divmod(ci, 4)
        out[b, :, i * 128:(i + 1) * 128] = res.results[ci]["ya"]
    return out


ROPE_THETA = 500000.0
NEGM = -30000.0


def k3_consts(TT):
    half = 8
    inv = np.power(ROPE_THETA, -np.arange(half, dtype=np.float32) * 2.0 / 16).astype(np.float32)

    def tab(pos):
        ang = pos.astype(np.float32)[:, None] * inv[None, :]
        c, s = np.cos(ang).astype(np.float32), np.sin(ang).astype(np.float32)
        return np.concatenate([c, c, -s, s], 1).astype(np.float32)
    rope_tok = tab(np.arange(TT))
    ncmp = TT // 16
    rope_cmp = tab(np.arange(ncmp) * 16 + 31)
    ql = np.arange(128)
    triu = (ql[:, None] <= ql[None, :]).astype(np.float32)
    tril = (ql[:, None] > ql[None, :]).astype(np.float32)
    cma = np.zeros((128, 17, 128), np.float32)
    cmt = np.zeros((128, 17, 128), np.float32)
    for v in range(17):
        c0 = 8 * v
        nl = np.arange(128)
        valid = (16 * (nl[None, :] - c0) + 31 <= ql[:, None])
        cma[:, v, :] = np.where(valid, 0.0, NEGM)
        cmt[:, v, :] = valid.T.astype(np.float32)
    ca = np.where(ql >= 64, 1e4, -1.0).astype(np.float32)[:, None]
    cb = np.where(ql < 64, 1e4, 0.0).astype(np.float32)[:, None]
    return {"rope_tok": rope_tok, "rope_cmp": rope_cmp, "triu": triu, "tril": tril, "cma": cma, "cmt": cmt,
            "cab": np.concatenate([ca, cb], 1), "ident": np.eye(128, dtype=np.float32)}


def build_k3(TT=T):
    nc = _new_nc()
    NTK = TT // 128
    NQB = NTK
    NCMP = TT // 16
    NCT = (NCMP + 127) // 128
    dr = lambda n, s, kind="ExternalInput": nc.dram_tensor(n, s, F32, kind=kind).ap()
    qin = dr("qin", [NQB * 128, 256]); gin = dr("gin", [NQB * 128, 6]); kvin = dr("kvin", [TT, 384])
    qkg = dr("qkg", [1, 256]); cpos = dr("cpos", [128, 32]); w1 = dr("w1", [128, 32, 256]); w2 = dr("w2", [128, 2, 2, 64])
    rope_tok = dr("rope_tok", [TT, 32]); rope_cmp = dr("rope_cmp", [NCMP, 32])
    triu = dr("triu", [128, 128]); tril = dr("tril", [128, 128]); cma = dr("cma", [128, 17, 128]); cmt = dr("cmt", [128, 17, 128])
    cab = dr("cab", [128, 2]); ident = dr("ident", [128, 128])
    yb = dr("yb", [NQB * 128, 128], "ExternalOutput")
    with ExitStack() as es, nc.allow_low_precision("bf16 attention-tile operands, fp32 PSUM accumulation"):
        P = Prog2(nc, es)
        IDT = P.tile("IDT", [128, 128]); TRIU = P.tile("TRIU", [128, 128]); TRIL = P.tile("TRIL", [128, 128])
        CMA = P.tile("CMA", [128, 17, 128]); CMT = P.tile("CMT", [128, 17, 128]); CAB = P.tile("CAB", [128, 2])
        QKG = P.tile("QKG", [128, 4, 64]); CPOS = P.tile("CPOS", [128, 32]); W2 = P.tile("W2", [128, 2, 2, 64])
        for i, (tl, d) in enumerate([(IDT, ident), (TRIU, triu), (TRIL, tril), (CAB, cab), (CPOS, cpos)]):
            P.load(["sp", "act"][i % 2], tl[:], d[:, :])
        P.load("sp", CMA[:], cma[:, :, :]); P.load("act", CMT[:], cmt[:, :, :])
        P.load("sp", QKG.v(QKG.t[:, :, :].rearrange("p a b -> p (a b)")), qkg.partition_broadcast(128))
        P.load("act", W2.v(W2.t[:, :, :, :].rearrange("p a b c -> p (a b c)")), w2.rearrange("p a b c -> p (a b c)"))
        banks = [P.tile(f"bank{i}", [128, 512], psum=True) for i in range(8)]
        KT = P.tile("KT", [128, TT], BF16)
        CT = P.tile("CT", [128, TT + 16])
        VSW = P.tile("VSW", [128, NTK, 2, 65], BF16)
        P.memset(VSW[:, :, :, 64:65], 1.0)
        P.memset(CT[:, TT:TT + 16], 0.0)
        KVt = [P.tile(f"KV{i}", [128, 384]) for i in range(2)]
        RPt = [P.tile(f"RP{i}", [128, 32]) for i in range(2)]
        KNt = [P.tile(f"KN{i}", [128, 2, 64]) for i in range(2)]
        SQt = [P.tile(f"SQa{i}", [128, 2, 64]) for i in range(2)]
        STt = [P.tile(f"STa{i}", [128, 2]) for i in range(2)]
        R1t = [P.tile(f"R1a{i}", [128, 2, 16]) for i in range(2)]
        R2t = [P.tile(f"R2a{i}", [128, 2, 16]) for i in range(2)]

        def norm_rope(X3, G3, RP, KN, SQ, ST, R1, R2, nh, e2="pool"):
            P.tt(SQ[:], X3, X3, ALU.mult, e=e2)
            P.red(ST[:], SQ[:], ALU.add)
            P.ts(ST[:], ST[:], 1.0 / 64, 1e-6, ALU.mult, ALU.add)
            P.act(ST[:], ST[:], AF.Sqrt)
            P.recip(ST[:], ST[:])
            P.tt(KN[:], X3, _bc(ST, ST.t[:, :], 2, [128, nh, 64]), ALU.mult)
            P.tt(KN[:], KN[:], G3, ALU.mult, e=e2)
            cs = _bc(RP, RP.t[:, 0:16], 1, [128, nh, 16])
            P.tt(R1[:], KN[:, :, 0:16], cs, ALU.mult)
            P.tt(R2[:, :, 0:8], KN[:, :, 8:16], _bc(RP, RP.t[:, 16:24], 1, [128, nh, 8]), ALU.mult, e=e2)
            P.tt(R2[:, :, 8:16], KN[:, :, 0:8], _bc(RP, RP.t[:, 24:32], 1, [128, nh, 8]), ALU.mult, e=e2)
            P.tt(KN[:, :, 0:16], R1[:], R2[:], ALU.add)

        GK = QKG.v(QKG.t[:, 2:4, :])
        pk = 0
        for tg in range(NTK // 4):
            bk = banks[pk % 2]; bc_ = banks[2 + pk % 2]; pk += 1
            for j in range(4):
                ti = tg * 4 + j
                k = ti % 2
                KV = KVt[k]; RP = RPt[k]
                P.load("sp", KV[:], kvin[ti * 128:(ti + 1) * 128, :])
                P.load("act", RP[:], rope_tok[ti * 128:(ti + 1) * 128, :])
                X3 = KV.v(KV.t[:, 128:384].rearrange("p (a b) -> p a b", b=128)[:, :, 0:64])
                norm_rope(X3, GK, RP, KNt[k], SQt[k], STt[k], R1t[k], R2t[k], 2)
                P.tr(bk[:, j * 128:(j + 1) * 128], KNt[k].v(KNt[k].t[:, :, :].rearrange("p a b -> p (a b)")), IDT[:])
                P.tr(bc_[:, j * 128:(j + 1) * 128], KV[:, 0:128], IDT[:])
                V3 = KV.v(KV.t[:, 128:384].rearrange("p (a b) -> p a b", b=128)[:, :, 64:128])
                P.copy(VSW[:, ti, :, 0:64], V3, e="act")
            P.copy(KT[:, tg * 512:(tg + 1) * 512], bk[:])
            P.copy(CT[:, tg * 512:(tg + 1) * 512], bc_[:], e="act")
        HID = P.tile("HID", [128, 2, 2, NCT * 128])
        P.memset(HID[:], 0.0, e="pool")
        W1 = P.tile("W1", [128, 32, 128])
        BIA = P.tile("BIA", [128, 2, 2])
        Zt = P.tile("Zt", [128, 512]); Z2 = P.tile("Z2", [128, 512])
        NV = NCMP - 1
        for hc in range(2):
            P.load("sp" if hc == 0 else "act", W1[:], w1[:, :, hc * 128:(hc + 1) * 128])
            for i in range(2):
                ps_ = slice(i * 64, (i + 1) * 64)
                bb = banks[4]
                for tau in range(32):
                    P.mm(bb[:, 0:1], W1[ps_, tau, :], CPOS[ps_, tau:tau + 1], start=(tau == 0), stop=(tau == 31))
                P.copy(BIA[:, i, hc:hc + 1], bb[:, 0:1])
                for nt in range((NCMP + 511) // 512):
                    n0 = nt * 512
                    nn = min(512, NCMP - n0)
                    ba = banks[5 + nt % 2]
                    for tau in range(32):
                        rhs = CT.v(CT.t[ps_, n0 * 16 + tau: n0 * 16 + tau + (nn - 1) * 16 + 1: 16])
                        P.mm(ba[:, 0:nn], W1[ps_, tau, :], rhs, start=(tau == 0), stop=(tau == 31))
                    Z = Zt
                    P.act(Z[:, 0:nn], ba[:, 0:nn], AF.Identity, bias=BIA[:, i, hc:hc + 1])
                    P.tt(Z2[:, 0:nn], Z[:, 0:nn], Z[:, 0:nn], ALU.mult)
                    P.ts(Z2[:, 0:nn], Z2[:, 0:nn], 0.044715, 1.0, ALU.mult, ALU.add)
                    P.tt(Z2[:, 0:nn], Z2[:, 0:nn], Z[:, 0:nn], ALU.mult)
                    P.act(Z2[:, 0:nn], Z2[:, 0:nn], AF.Sigmoid, scale=1.5957691216057308)
                    P.tt(HID[:, i, hc, n0:n0 + nn], Z2[:, 0:nn], Z[:, 0:nn], ALU.mult)
        KCT = P.tile("KCT", [128, NCT * 128])
        VCMP = P.tile("VCMP", [128, NCT, 65], BF16)
        KCTb = P.tile("KCTb", [128, NCT * 128], BF16)
        P.memset(VCMP[:, :, 64:65], 1.0)
        KC2 = P.tile("KC2", [128, 2, 64])
        KCR = P.tile("KCR", [128, 1, 64])
        GC = QKG.v(QKG.t[:, 1:2, :])
        for ct in range(NCT):
            bb = banks[4 + ct % 2]
            for i in range(2):
                for hc in range(2):
                    P.mm(bb[:, i * 64:(i + 1) * 64], HID[:, i, hc, ct * 128:(ct + 1) * 128], W2[:, i, hc, :],
                         start=(hc == 0), stop=(hc == 1))
            P.copy(VCMP[:, ct, 0:64], bb[:, 64:128], e="act")
            RP = RPt[ct % 2]
            nr = min(128, NCMP - ct * 128)
            P.load("sp", RP[0:nr, :], rope_cmp[ct * 128:ct * 128 + nr, :])
            k = ct % 2
            KN1 = KNt[k].v(KNt[k].t[:, 0:1, :])
            P.copy(KCR[:, 0, :], bb[:, 0:64])
            norm_rope(KCR[:], GC, RP, KN1,
                      SQt[k].v(SQt[k].t[:, 0:1, :]), STt[k].v(STt[k].t[:, 0:1]), R1t[k].v(R1t[k].t[:, 0:1, :]),
                      R2t[k].v(R2t[k].t[:, 0:1, :]), 1, e2="dve")
            P.copy(KC2[:, 0, :], KNt[k][:, 0, :])
            P.copy(KC2[:, 1, :], KNt[k][:, 0, :], e="act")
            b2 = banks[6 + ct % 2]
            P.tr(b2[:, 0:128], KC2.v(KC2.t[:, :, :].rearrange("p a b -> p (a b)")), IDT[:])
            P.copy(KCT[:, ct * 128:(ct + 1) * 128], b2[:, 0:128])
            P.copy(KCTb[:, ct * 128:(ct + 1) * 128], b2[:, 0:128], e="act")
        Qt = [P.tile(f"Q{i}", [128, 4, 64]) for i in range(2)]
        Gt = [P.tile(f"G{i}", [128, 2, 3]) for i in range(2)]
        QNt = [P.tile(f"QN{i}", [128, 4, 64]) for i in range(2)]
        QDt = [P.tile(f"QD{i}", [128, 4, 2, 64]) for i in range(2)]
        QTt = [P.tile(f"QT{i}", [128, 4, 128]) for i in range(2)]
        SQq = [P.tile(f"SQq{i}", [128, 4, 64]) for i in range(2)]
        STq = [P.tile(f"STq{i}", [128, 4]) for i in range(2)]
        R1q = [P.tile(f"R1q{i}", [128, 4, 16]) for i in range(2)]
        R2q = [P.tile(f"R2q{i}", [128, 4, 16]) for i in range(2)]
        PCt = [P.tile(f"PC{i}", [128, 512]) for i in range(2)]
        RSt = [P.tile(f"RS{i}", [128, 4]) for i in range(2)]
        IMPP = P.tile("IMPP", [128, 516])
        IMPF = P.tile("IMPF", [128, 128]); IMPW = P.tile("IMPW", [128, 128])
        M8 = P.tile("M8", [128, 16])
        SEL = P.tile("SEL", [128, 128])
        PTt = [P.tile(f"PT{i}", [128, 2, 128], BF16) for i in range(4)]
        QTbt = [P.tile(f"QTb{i}", [128, 2, 128], BF16) for i in range(2)]
        OUTt = [P.tile(f"OUT{i}", [128, 2, 64]) for i in range(2)]
        RIt = [P.tile(f"RI{i}", [128, 3, 2]) for i in range(2)]
        OTt = [P.tile(f"OT{i}", [128, 2, 64]) for i in range(2)]
        GQ = QKG.v(QKG.t[:, 0:1, :].to_broadcast([128, 4, 64])) if False else _bc(QKG, QKG.t[:, 0, :], 1, [128, 4, 64])
        sb_i = [0]; mb_i = [0]; pt_i = [0]
        OB = [0, 1, 2]

        def attn_tile(first, last, ob, kT_ap, part, v1_ap, QT, mask_fn):
            sb = banks[sb_i[0] % 2]; sb_i[0] += 1
            PT = PTt[pt_i[0] % 4]; pt_i[0] += 1
            P.mm(sb[:, 0:256], kT_ap, QTb.v(QTb.t[part:part + 64, :, :].rearrange("p a b -> p (a b)")))
            P.act(PT.v(PT.t[:, :, :].rearrange("p a b -> p (a b)")), sb[:, 0:256], AF.Exp)
            mask_fn(PT)
            for r in range(2):
                P.mm(banks[6 + r][:, ob * 65:(ob + 1) * 65], PT[:, r, :], v1_ap, start=first, stop=last)

        ctxs = {}

        def phaseA(jq):
                qb = jq
                k = jq % 2
                Q = Qt[k]; G = Gt[k]; RP = RPt[k]
                P.load("sp", Q.v(Q.t[:, :, :].rearrange("p a b -> p (a b)")), qin[jq * 128:(jq + 1) * 128, :])
                yield
                P.load("act", G.v(G.t[:, :, :].rearrange("p a b -> p (a b)")), gin[jq * 128:(jq + 1) * 128, :])
                yield
                P.load("sp", RP[:], rope_tok[qb * 128:(qb + 1) * 128, :])
                yield
                QN = QNt[k]; QD = QDt[k]; QT = QTt[k]
                norm_rope(Q[:], GQ, RP, QN, SQq[k], STq[k], R1q[k], R2q[k], 4)
                yield
                P.ts(QD[:, :, 0, :], QN[:], 0.125, None, ALU.mult)
                yield
                P.ts(QD[:, :, 1, :], QN[:], 0.125, None, ALU.mult, e="pool")
                yield
                P.act(G[:], G[:], AF.Sigmoid)
                yield
                bq = banks[4]
                for r in range(4):
                    P.tr(bq[:, r * 128:(r + 1) * 128], QD.v(QD.t[:, r, :, :].rearrange("p a b -> p (a b)")), IDT[:])
                    yield
                P.copy(QT.v(QT.t[:, :, :].rearrange("p a b -> p (a b)")), bq[:])
                yield
                QTb = QTbt[k]
                P.copy(QTb.v(QTb.t[:, :, :].rearrange("p a b -> p (a b)")), bq[:, 0:256], e="act")
                yield
                nct = qb // 16 + 1
                ncols = nct * 128
                var = qb % 16
                P.memset(IMPP[:], 0.0, e="pool")
                yield
                RS = RSt[k]
                for r in range(4):
                    sb = banks[5]
                    P.mm(sb[:, 0:ncols], QT[0:64, r, :], KCT[0:64, 0:ncols])
                    yield
                    PC = PCt[r % 2]
                    lo = ncols - 128
                    if lo > 0:
                        P.copy(PC[:, 0:lo], sb[:, 0:lo])
                        yield
                        if var == 0:
                            P.tt(PC[:, lo - 128:lo], PC[:, lo - 128:lo], CMA[:, 16, :], ALU.add)
                            yield
                    P.tt(PC[:, lo:ncols], sb[:, lo:ncols], CMA[:, var, :], ALU.add)
                    yield
                    P.act(PC[:, 0:ncols], PC[:, 0:ncols], AF.Exp, accum_out=RS[:, r:r + 1])
                    yield
                    P.ts(RS[:, r:r + 1], RS[:, r:r + 1], 1.1754944e-38, None, ALU.max)
                    yield
                    P.recip(RS[:, r:r + 1], RS[:, r:r + 1])
                    yield
                    P.stt(IMPP[:, 4:4 + ncols], PC[:, 0:ncols], RS[:, r:r + 1], IMPP[:, 4:4 + ncols], ALU.mult, ALU.add)
                    yield
                P.red(IMPF[:], IMPP.v(IMPP.t[:, 4:516].rearrange("p (j f) -> p j f", f=4)), ALU.add)
                yield
                P.tt(IMPF[:], IMPF[:], IMPP.v(IMPP.t[:, 0:512].rearrange("p (j f) -> p j f", f=4)[:, :, 3]), ALU.add)
                yield
                if 2 * qb + 2 < 128:
                    P.memset(IMPF[:, 2 * qb + 2:128], -1.0)
                    yield
                if qb >= 1:
                    P.ts(IMPF[:, 2 * qb - 1:2 * qb], IMPF[:, 2 * qb - 1:2 * qb], CAB[:, 1:2], None, ALU.max)
                    yield
                if 2 * qb + 1 < 128:
                    P.copy(IMPF[:, 2 * qb + 1:2 * qb + 2], CAB[:, 0:1])
                    yield
                P.memset(IMPF[:, 2 * qb:2 * qb + 1], 1e4)
                yield
                P.memset(IMPF[:, 0:1], 1e4)
                yield
                P.gop("dve", lambda E: E.max(out=M8.t[:, 0:8], in_=IMPF.t[:, :]), [M8], [IMPF])
                yield
                P.gop("dve", lambda E: E.match_replace(out=IMPW.t[:, :], in_to_replace=M8.t[:, 0:8], in_values=IMPF.t[:, :],
                                                       imm_value=-2.0), [IMPW], [M8, IMPF])
                P.gop("dve", lambda E: E.max(out=M8.t[:, 8:16], in_=IMPW.t[:, :]), [M8], [IMPW])
                yield
                P.ts(SEL[:], IMPF[:], M8[:, 15:16], None, ALU.is_ge)
                yield

                ctxs[jq] = dict(qb=qb, k=k, nct=nct, var=var, QT=QT, QTb=QTb, G=G)

        def drainA(g, n):
            if g is None:
                return
            try:
                if n is None:
                    while True:
                        next(g)
                else:
                    for _ in range(n):
                        next(g)
            except StopIteration:
                pass

        drainA(phaseA(0), None)
        for jq in range(NQB):
            c_ = ctxs.pop(jq)
            qb = c_["qb"]; k = c_["k"]; nct = c_["nct"]; var = c_["var"]; QT = c_["QT"]; QTb = c_["QTb"]; G = c_["G"]
            gA = phaseA(jq + 1) if jq + 1 < NQB else None
            tiles = []
            for ct in range(nct):
                last = ct == nct - 1
                if last:
                    mf = lambda PT: P.tt(PT[:], PT[:], _bc(CMT, CMT.t[:, var, :], 1, [128, 2, 128]), ALU.mult)
                elif var == 0 and ct == nct - 2:
                    mf = lambda PT: P.tt(PT[:], PT[:], _bc(CMT, CMT.t[:, 16, :], 1, [128, 2, 128]), ALU.mult)
                else:
                    mf = None
                tiles.append((ct == 0, last, OB[0], KCTb[0:64, ct * 128:(ct + 1) * 128], 0, VCMP[:, ct, :], mf, None))
            kts = [kt for kt in range(qb - 4, qb + 1) if kt >= 0]
            for kt in kts:
                if kt == qb:
                    mf = lambda PT: P.tt(PT[:], PT[:], _bc(TRIU, TRIU.t[:, :], 1, [128, 2, 128]), ALU.mult)
                elif kt == qb - 4:
                    mf = lambda PT: P.tt(PT[:], PT[:], _bc(TRIL, TRIL.t[:, :], 1, [128, 2, 128]), ALU.mult)
                else:
                    mf = None
                tiles.append((kt == kts[0], kt == kts[-1], OB[2], KT[64:128, kt * 128:(kt + 1) * 128], 64, VSW[:, kt, 1, :], mf, None))
            nj = 2 * (qb + 1)
            P.copy(CT.v(CT.t[:, 0:nj * 64].rearrange("p (j f) -> p j f", f=64)),
                   SEL.v(SEL.t[:, 0:nj].unsqueeze(2).to_broadcast([128, nj, 64])), e="pool")
            for kt in range(qb + 1):
                tiles.append((kt == 0, kt == qb, OB[1], KT[0:64, kt * 128:(kt + 1) * 128], 0, VSW[:, kt, 0, :], None, kt))
            LA = 2
            pend = []
            sbanks = [banks[0], banks[1]]
            mbanks = [banks[2], banks[3]]
            for i in range(len(tiles) + LA):
                if i < len(tiles):
                    first, last, ob, kT_ap, part, v1_ap, mf, kt = tiles[i]
                    sb = sbanks[sb_i[0] % 2]; sb_i[0] += 1
                    PT = PTt[pt_i[0] % 4]; pt_i[0] += 1
                    P.mm(sb[:, 0:256], kT_ap, QTb.v(QTb.t[part:part + 64, :, :].rearrange("p a b -> p (a b)")))
                    if kt is not None:
                        mbk = mbanks[mb_i[0] % 2]; mb_i[0] += 1
                        P.tr(mbk[:, 0:128], CT[:, kt * 128:(kt + 1) * 128], IDT[:])
                    P.act(PT.v(PT.t[:, :, :].rearrange("p a b -> p (a b)")), sb[:, 0:256], AF.Exp)
                    if kt is not None:
                        P.tt(PT[:], PT[:], _bc(mbk, mbk.t[:, 0:128], 1, [128, 2, 128]), ALU.mult)
                        if kt == qb:
                            P.tt(PT[:], PT[:], _bc(TRIU, TRIU.t[:, :], 1, [128, 2, 128]), ALU.mult, e="pool")
                    elif mf is not None:
                        mf(PT)
                    pend.append((PT, ob, v1_ap, first, last))
                    drainA(gA, 2)
                j = i - LA
                if j >= 0:
                    PT, ob, v1_ap, first, last = pend[j]
                    for r in range(2):
                        P.mm(banks[6 + r][:, ob * 65:(ob + 1) * 65], PT[:, r, :], v1_ap, start=first, stop=last)
            drainA(gA, None)
            RI = RIt[k]; OUT = OUTt[k]; OT = OTt[k]
            for bi in range(3):
                for r in range(2):
                    ob = banks[6 + r]
                    c0 = bi * 65
                    P.ts(RI[:, bi, r:r + 1], ob[:, c0 + 64:c0 + 65], 1.1754944e-38, None, ALU.max)
                    P.recip(RI[:, bi, r:r + 1], RI[:, bi, r:r + 1])
                    P.tt(RI[:, bi, r:r + 1], RI[:, bi, r:r + 1], G[:, r, bi:bi + 1], ALU.mult)
                    if bi == 0:
                        P.ts(OUT[:, r, :], ob[:, c0:c0 + 64], RI[:, bi, r:r + 1], None, ALU.mult)
                    else:
                        P.stt(OUT[:, r, :], ob[:, c0:c0 + 64], RI[:, bi, r:r + 1], OUT[:, r, :], ALU.mult, ALU.add)
            P.store("sp", yb[jq * 128:(jq + 1) * 128, :], OUT.v(OUT.t[:, :, :].rearrange("p a b -> p (a b)")))
        P.finish()
        print("k3 ninst", P.ninst, "nsem", P.nsem)
    return nc


def k3_inputs(p_b, g, hh, prm, l, TT):
    q = p_b[:TT, 1792 + g * 256: 1792 + (g + 1) * 256].reshape(TT, 4, 64)
    order = [2 * hh, 2 * hh + 1, 2 * (1 - hh), 2 * (1 - hh) + 1]
    q = q[:, order, :].reshape(TT, 256)
    kv = p_b[:TT, 2304:3072].reshape(TT, 6, 2, 64)[:, :, g, :].reshape(TT, 384)
    gt = p_b[:TT, 3072:3096].reshape(TT, 8, 3)[:, g * 4 + 2 * hh: g * 4 + 2 * hh + 2, :].reshape(TT, 6)
    cp = prm["cmp_pos"][l]
    cpos = np.concatenate([cp[0].T, cp[1].T], 0)
    w1 = prm["cmp_w1"][l].reshape(2, 32, 64, 256).transpose(0, 2, 1, 3).reshape(128, 32, 256)
    w2 = prm["cmp_w2"][l].reshape(2, 2, 128, 64).transpose(2, 0, 1, 3)
    d = {"qin": np.ascontiguousarray(q), "gin": np.ascontiguousarray(gt), "kvin": np.ascontiguousarray(kv),
         "qkg": np.ascontiguousarray(prm["qk_norm_g"][l].reshape(1, 256)), "cpos": np.ascontiguousarray(cpos),
         "w1": np.ascontiguousarray(w1), "w2": np.ascontiguousarray(w2)}
    d.update(k3_consts(TT))
    return d


def run_k3(p, prm, l, TT=T):
    key = ("k3", TT)
    if key not in _CACHE:
        _CACHE[key] = build_k3(TT)
    nc = _CACHE[key]
    maps = []
    for ci in range(NCORES):
        b, rem = divmod(ci, 4)
        g, hh = divmod(rem, 2)
        maps.append(k3_inputs(p[b], g, hh, prm, l, TT))
    res = run_bass_kernel_spmd(nc, maps, core_ids=list(range(NCORES)))
    out = np.empty((NB, TT, 512), np.float32)
    for ci in range(NCORES):
        b, rem = divmod(ci, 4)
        g, hh = divmod(rem, 2)
        h0 = g * 4 + 2 * hh
        out[b, :, h0 * 64:(h0 + 2) * 64] = res.results[ci]["yb"]
    return out


def build_k4a():
    nc = _new_nc()
    NT = 16
    dr = lambda n, s, kind="ExternalInput": nc.dram_tensor(n, s, F32, kind=kind).ap()
    x = dr("x", [NT * 128, D]); ya = dr("ya", [NT * 128, 512]); yb = dr("yb", [NT * 128, 512]); pm = dr("pm", [NT * 128, 2048])
    gm = dr("gm", [1, D]); wua = dr("wua", [512, D]); wub = dr("wub", [512, D]); wo = dr("wo", [D, D]); ident = dr("ident", [128, 128])
    x1 = dr("x1", [NT * 128, D], "ExternalOutput")
    with ExitStack() as es, nc.allow_low_precision("bf16 projection operands, fp32 PSUM accumulation"):
        P = Prog2(nc, es)
        IDT = P.tile("IDT", [128, 128]); GMB = P.tile("GMB", [128, D])
        WUA = P.tile("WUA", [128, 4, D]); WUB = P.tile("WUB", [128, 4, D]); WO = P.tile("WO", [128, 8, D])
        P.load("sp", IDT[:], ident[:, :]); P.load("act", GMB[:], gm.partition_broadcast(128))
        P.load("sp", WUA[:], wua.rearrange("(kc kp) n -> kp kc n", kp=128))
        P.load("act", WUB[:], wub.rearrange("(kc kp) n -> kp kc n", kp=128))
        P.load("sp", WO[:, 0:4, :], wo[0:512, :].rearrange("(kc kp) n -> kp kc n", kp=128))
        P.load("act", WO[:, 4:8, :], wo[512:1024, :].rearrange("(kc kp) n -> kp kc n", kp=128))
        WUAb = P.tile("WUAb", [128, 4, D], BF16); WUBb = P.tile("WUBb", [128, 4, D], BF16); WOb = P.tile("WOb", [128, 8, D], BF16)
        P.copy(WUAb[:], WUA[:], e="pool"); P.copy(WUBb[:], WUB[:], e="act")
        P.copy(WOb[:, 0:4, :], WO[:, 0:4, :], e="pool"); P.copy(WOb[:, 4:8, :], WO[:, 4:8, :], e="act")
        banks = [P.tile(f"bank{i}", [128, 512], psum=True) for i in range(8)]
        bi = [0]

        def nb():
            b = banks[bi[0] % 8]; bi[0] += 1
            return b
        Xt = [P.tile(f"X{i}", [128, D]) for i in range(2)]
        YAt = [P.tile(f"YA{i}", [128, 512]) for i in range(2)]
        YBt = [P.tile(f"YB{i}", [128, 512]) for i in range(2)]
        PMt = [P.tile(f"PM{i}", [128, 2048]) for i in range(2)]
        YTt = [P.tile(f"YT{i}", [128, 8, 128], BF16) for i in range(2)]
        MIXt = [P.tile(f"MIX{i}", [128, D]) for i in range(2)]
        TMPt = [P.tile(f"TMP{i}", [128, 512]) for i in range(2)]
        MTt = [P.tile(f"MT{i}", [128, 8, 128], BF16) for i in range(2)]
        for ti in range(NT):
            k = ti % 2
            rs = slice(ti * 128, (ti + 1) * 128)
            X = Xt[k]; YA = YAt[k]; YB = YBt[k]; PM = PMt[k]; YT = YTt[k]; MIX = MIXt[k]; MT = MTt[k]
            P.load("sp", X[:], x[rs, :]); P.load("act", YA[:], ya[rs, :]); P.load("sp", YB[:], yb[rs, :]); P.load("act", PM[:], pm[rs, :])
            for j, Y in enumerate((YA, YB)):
                b = nb()
                for c in range(4):
                    P.tr(b[:, c * 128:(c + 1) * 128], Y[:, c * 128:(c + 1) * 128], IDT[:])
                P.copy(YT.v(YT.t[:, j * 4:(j + 1) * 4, :].rearrange("p a b -> p (a b)")), b[:], e="act" if j else "dve")
            P.act(PM[:], PM[:], AF.Sigmoid)
            for nh in range(2):
                cs = slice(nh * 512, (nh + 1) * 512)
                ba = nb(); bb = nb()
                for kc in range(4):
                    P.mm(ba[:], YT[:, kc, :], WUAb[:, kc, cs], start=(kc == 0), stop=(kc == 3))
                for kc in range(4):
                    P.mm(bb[:], YT[:, 4 + kc, :], WUBb[:, kc, cs], start=(kc == 0), stop=(kc == 3))
                TMP = TMPt[nh]
                P.tt(MIX[:, cs], ba[:], PM[:, nh * 512:(nh + 1) * 512], ALU.mult)
                P.tt(TMP[:], bb[:], PM[:, 1024 + nh * 512:1024 + (nh + 1) * 512], ALU.mult)
                P.tt(MIX[:, cs], MIX[:, cs], TMP[:], ALU.add, e="pool")
            for half in range(2):
                b = nb()
                for c in range(4):
                    kc = half * 4 + c
                    P.tr(b[:, c * 128:(c + 1) * 128], MIX[:, kc * 128:(kc + 1) * 128], IDT[:])
                P.copy(MT.v(MT.t[:, half * 4:(half + 1) * 4, :].rearrange("p a b -> p (a b)")), b[:], e="act" if half else "dve")
            for nh in range(2):
                cs = slice(nh * 512, (nh + 1) * 512)
                b = nb()
                for kc in range(8):
                    P.mm(b[:], MT[:, kc, :], WOb[:, kc, cs], start=(kc == 0), stop=(kc == 7))
                TMP = TMPt[nh]
                P.tt(TMP[:], b[:], GMB[:, cs], ALU.mult)
                P.tt(X[:, cs], X[:, cs], TMP[:], ALU.add, e="pool")
            P.store("sp" if k else "act", x1[rs, :], X[:])
        P.finish()
        print("k4a ninst", P.ninst, "nsem", P.nsem)
    return nc


def run_k4a(xfull, ya, yb, p, gate_mix, prm, l):
    if "k4a" not in _CACHE:
        _CACHE["k4a"] = build_k4a()
    nc = _CACHE["k4a"]
    ident = np.eye(128, dtype=np.float32)
    maps = []
    for i in range(NCORES):
        b, tq = divmod(i, 4)
        ts_ = slice(tq * 2048, (tq + 1) * 2048)
        maps.append({"x": np.ascontiguousarray(xfull[b, ts_]), "ya": np.ascontiguousarray(ya[b, ts_]),
                     "yb": np.ascontiguousarray(yb[b, ts_]), "pm": np.ascontiguousarray(p[b, ts_, 3096:5144]),
                     "gm": gate_mix[b][None].copy(), "wua": prm["w_up_rwkv"][l], "wub": prm["w_up_nsa"][l],
                     "wo": prm["w_out"][l], "ident": ident})
    res = run_bass_kernel_spmd(nc, maps, core_ids=list(range(NCORES)))
    out = np.empty((NB, T, D), np.float32)
    for i in range(NCORES):
        b, tq = divmod(i, 4)
        out[b, tq * 2048:(tq + 1) * 2048] = res.results[i]["x1"]
    return out


def build_k4b():
    nc = _new_nc()
    NT = 16; NE = 16
    dr = lambda n, s, kind="ExternalInput": nc.dram_tensor(n, s, F32, kind=kind).ap()
    x1 = dr("x1", [NT * 128, D]); g = dr("g", [1, D]); sc = dr("sc", [1, D]); sh = dr("sh", [1, D]); gf = dr("gf", [1, D])
    rw = dr("rw", [D, 16]); rb = dr("rb", [1, 16]); ident = dr("ident", [128, 128])
    wg = dr("wg", [NE, D, 512]); wu = dr("wu", [NE, D, 512]); wd = dr("wd", [NE, 512, D])
    xo = dr("xo", [NT * 128, D], "ExternalOutput")
    with ExitStack() as es, nc.allow_low_precision("bf16 expert matmuls, fp32 accumulation"):
        P = Prog2(nc, es)
        IDT = P.tile("IDT", [128, 128]); G2S = P.tile("G2S", [128, D]); SC = P.tile("SC", [128, D]); SH = P.tile("SH", [128, D])
        GF = P.tile("GF", [128, D]); RW = P.tile("RW", [128, 8, 16]); RB = P.tile("RB", [128, 16])
        P.load("sp", IDT[:], ident[:, :]); P.load("act", G2S[:], g.partition_broadcast(128)); P.load("sp", SC[:], sc.partition_broadcast(128))
        P.load("act", SH[:], sh.partition_broadcast(128)); P.load("sp", GF[:], gf.partition_broadcast(128))
        P.load("act", RW[:], rw.rearrange("(kc kp) n -> kp kc n", kp=128)); P.load("sp", RB[:], rb.partition_broadcast(128))
        P.stt(G2S[:], SC[:], 1.0, G2S[:], ALU.add, ALU.mult)
        banks = [P.tile(f"bank{i}", [128, 512], psum=True) for i in range(8)]
        bi = [0]

        def nb():
            b = banks[bi[0] % 8]; bi[0] += 1
            return b
        ACC = [P.tile(f"ACC{t}", [128, D]) for t in range(8)]
        H2T = P.tile("H2T", [128, 8, 1024], BF16)
        CW = P.tile("CW", [128, 8, 16])
        STG = [P.tile(f"STG{i}", [128, 4096]) for i in range(2)]
        WB = [[P.tile(f"WB{m}_{p}", [128, 4096], BF16) for p in range(2)] for m in range(3)]
        H2t = [P.tile(f"H2{i}", [128, D]) for i in range(2)]
        H2Tf = [P.tile(f"H2Tf{i}", [128, 8, 128]) for i in range(2)]
        junk = P.tile("junk", [128, D])
        ST = [P.tile(f"ST{i}", [128, 2]) for i in range(2)]
        SCO = [P.tile(f"SCO{i}", [128, 16]) for i in range(2)]
        SS = [P.tile(f"SS{i}", [128, 4, 4]) for i in range(2)]
        PSm = [P.tile(f"PSm{i}", [128, 4, 6]) for i in range(2)]
        GS = [P.tile(f"GS{i}", [128, 4]) for i in range(2)]
        GMx = [P.tile(f"GMx{i}", [128, 2]) for i in range(2)]
        E2 = [P.tile(f"E2{i}", [128, 4, 4]) for i in range(2)]
        SIL = [P.tile(f"SIL{i}", [128, 512]) for i in range(2)]
        HID = [P.tile(f"HID{i}", [128, 4, 512], BF16) for i in range(2)]
        stg_i = [0]
        for tg in range(2):
            for t in range(8):
                ti = tg * 8 + t
                k = t % 2
                A = ACC[t]
                P.load("sp" if k else "act", A[:], x1[ti * 128:(ti + 1) * 128, :])
                S = ST[k]; H2 = H2t[k]; HF = H2Tf[k]
                P.act(junk[:], A[:], AF.Square, accum_out=S[:, 0:1])
                P.ts(S[:, 1:2], S[:, 0:1], 1.0 / D, 1e-6, ALU.mult, ALU.add)
                P.act(S[:, 1:2], S[:, 1:2], AF.Sqrt)
                P.recip(S[:, 1:2], S[:, 1:2])
                P.stt(H2[:], A[:], S[:, 1:2], G2S[:], ALU.mult, ALU.mult)
                P.tt(H2[:], H2[:], SH[:], ALU.add, e="pool")
                for half in range(2):
                    b = nb()
                    for c in range(4):
                        kc = half * 4 + c
                        P.tr(b[:, c * 128:(c + 1) * 128], H2[:, kc * 128:(kc + 1) * 128], IDT[:])
                    bv = b.v(b.t[:, :].rearrange("p (a b) -> p a b", a=4))
                    P.copy(HF[:, half * 4:(half + 1) * 4, :], bv)
                    P.copy(H2T[:, half * 4:(half + 1) * 4, t * 128:(t + 1) * 128], bv, e="act")
                b = nb()
                for kc in range(8):
                    P.mm(b[:, 0:16], HF[:, kc, :], RW[:, kc, :], start=(kc == 0), stop=(kc == 7))
                sco = SCO[k]; ss = SS[k]; psm = PSm[k]; gs = GS[k]; gmx = GMx[k]; e2 = E2[k]
                P.act(sco[:], b[:, 0:16], AF.Sigmoid)
                ssf = ss.v(ss.t[:, :, :].rearrange("p a b -> p (a b)"))
                P.tt(ssf, sco[:], RB[:], ALU.add)
                P.tt(psm[:, :, 0:3], ss[:, :, 0:3], ss[:, :, 1:4], ALU.add)
                P.tt(psm[:, :, 3:5], ss[:, :, 0:2], ss[:, :, 2:4], ALU.add)
                P.tt(psm[:, :, 5:6], ss[:, :, 0:1], ss[:, :, 3:4], ALU.add)
                P.red(gs[:], psm[:], ALU.max)
                P.red(gmx[:, 0:1], gs[:], ALU.max)
                P.ts(gs[:], gs[:], gmx[:, 0:1], None, ALU.is_ge)
                P.tt(psm[:, :, 0:3], ss[:, :, 0:3], ss[:, :, 1:4], ALU.min)
                P.tt(psm[:, :, 3:5], ss[:, :, 0:2], ss[:, :, 2:4], ALU.min)
                P.tt(psm[:, :, 5:6], ss[:, :, 0:1], ss[:, :, 3:4], ALU.min)
                P.red(e2[:, :, 0], psm[:], ALU.max)
                thr = E2[k].v(E2[k].t[:, :, 0:1].to_broadcast([128, 4, 4]))
                P.tt(psm[:, :, 0:4], ss[:], thr, ALU.is_ge)
                P.tt(psm[:, :, 0:4], psm[:, :, 0:4], _bc(gs, gs.t[:, :], 2, [128, 4, 4]), ALU.mult)
                scv = sco.v(sco.t[:, :].rearrange("p (a b) -> p a b", b=4))
                P.tt(e2[:], psm[:, :, 0:4], scv, ALU.mult)
                P.red(gmx[:, 1:2], E2[k].v(E2[k].t[:, :, :].rearrange("p a b -> p (a b)")), ALU.add)
                P.recip(gmx[:, 1:2], gmx[:, 1:2])
                P.ts(CW[:, t, :], E2[k].v(E2[k].t[:, :, :].rearrange("p a b -> p (a b)")), gmx[:, 1:2], None, ALU.mult)
            for e in range(NE):
                p_ = e % 2
                for m, (wsrc, pat) in enumerate(((wg, 8), (wu, 8), (wd, 4))):
                    S_ = STG[stg_i[0] % 2]; stg_i[0] += 1
                    sv = S_.v(S_.t[:, :].rearrange("p (kc n) -> p kc n", kc=pat))
                    P.load("sp" if m % 2 == 0 else "act", sv, wsrc[e].rearrange("(kc kp) n -> kp kc n", kp=128))
                    Wb = WB[m][p_]
                    if m < 2:
                        P.copy(Wb[:], S_[:], e="pool")
                    else:
                        P.tt(Wb.v(Wb.t[:, :].rearrange("p (kc n) -> p kc n", kc=4)), sv, _bc(GF, GF.t[:, :], 1, [128, 4, D]), ALU.mult, e="pool")
                WG = WB[0][p_].v(WB[0][p_].t[:, :].rearrange("p (kc n) -> p kc n", kc=8))
                WU = WB[1][p_].v(WB[1][p_].t[:, :].rearrange("p (kc n) -> p kc n", kc=8))
                WD = WB[2][p_].v(WB[2][p_].t[:, :].rearrange("p (kc n) -> p kc n", kc=4))
                for sub in range(2):
                    hid = HID[sub]
                    tsl = slice(sub * 512, (sub + 1) * 512)
                    for c in range(4):
                        bg = nb(); bu = nb()
                        for kc in range(8):
                            P.mm(bg[:], WG[:, kc, c * 128:(c + 1) * 128], H2T[:, kc, tsl], start=(kc == 0), stop=(kc == 7))
                        for kc in range(8):
                            P.mm(bu[:], WU[:, kc, c * 128:(c + 1) * 128], H2T[:, kc, tsl], start=(kc == 0), stop=(kc == 7))
                        sil = SIL[c % 2]
                        P.act(sil[:], bg[:], AF.Silu)
                        P.tt(hid[:, c, :], sil[:], bu[:], ALU.mult)
                    for t4 in range(4):
                        t = sub * 4 + t4
                        for nh in range(2):
                            cs = slice(nh * 512, (nh + 1) * 512)
                            bd = nb()
                            for c in range(4):
                                P.mm(bd[:], hid[:, c, t4 * 128:(t4 + 1) * 128], WD[:, c, cs], start=(c == 0), stop=(c == 3))
                            P.stt(ACC[t][:, cs], bd[:], CW[:, t, e:e + 1], ACC[t][:, cs], ALU.mult, ALU.add)
            for t in range(8):
                ti = tg * 8 + t
                P.store("sp" if t % 2 else "act", xo[ti * 128:(ti + 1) * 128, :], ACC[t][:])
        P.finish()
        print("k4b ninst", P.ninst, "nsem", P.nsem)
    return nc


def run_k4b(x1, g2, sc2, sh2, gate_ffn, prm, l):
    if "k4b" not in _CACHE:
        _CACHE["k4b"] = build_k4b()
    nc = _CACHE["k4b"]
    ident = np.eye(128, dtype=np.float32)
    maps = []
    for i in range(NCORES):
        b, tq = divmod(i, 4)
        ts_ = slice(tq * 2048, (tq + 1) * 2048)
        maps.append({"x1": np.ascontiguousarray(x1[b, ts_]), "g": g2[None].copy(), "sc": sc2[b][None].copy(),
                     "sh": sh2[b][None].copy(), "gf": gate_ffn[b][None].copy(), "rw": prm["router_w"],
                     "rb": prm["router_b"][None].copy(), "ident": ident,
                     "wg": prm["exp_w_gate"][l], "wu": prm["exp_w_up"][l], "wd": prm["exp_w_down"][l]})
    res = run_bass_kernel_spmd(nc, maps, core_ids=list(range(NCORES)))
    out = np.empty((NB, T, D), np.float32)
    for i in range(NCORES):
        b, tq = divmod(i, 4)
        out[b, tq * 2048:(tq + 1) * 2048] = res.results[i]["xo"]
    return out


def kernel(**inputs):
    prm = {k: np.asarray(v) for k, v in inputs.items()}
    x = prm["x"].astype(np.float32, copy=False)
    mod = run_k0(prm["c"], prm["w_ada"], prm["b_ada"])
    for l in range(2):
        sh1, sc1, gate_mix, sh2, sc2, gate_ffn = [np.ascontiguousarray(a) for a in np.split(mod[l], 6, axis=-1)]
        p = run_k1(x, prm["norm_g"][l, 0], sc1, sh1, prm["w_in"][l], prm["b_in"][l])
        ya = run_k2(p, prm, l)
        yb = run_k3(p, prm, l)
        x1 = run_k4a(x, ya, yb, p, gate_mix, prm, l)
        x = run_k4b(x1, prm["norm_g"][l, 1], sc2, sh2, gate_ffn, prm, l)
    return x
```

```python
import numpy as np
import concourse.bass as bass
import concourse.mybir as mybir
from contextlib import ExitStack

F32 = mybir.dt.float32
BF16 = mybir.dt.bfloat16
I32 = mybir.dt.int32
U32 = mybir.dt.uint32
AF = mybir.ActivationFunctionType
ALU = mybir.AluOpType
AX = mybir.AxisListType


class Buf:
    __slots__ = ("name", "w", "r", "dsem", "dcnt", "t", "excl")

    def __init__(self, name, t=None):
        self.name = name
        self.w = None
        self.r = []
        self.dsem = None
        self.dcnt = 0
        self.t = t
        self.excl = False


class Prog:
    ENG = ("pe", "dve", "act", "pool", "sp")

    def __init__(self, nc, es: ExitStack):
        self.nc = nc
        self.es = es
        self.eng = {"pe": nc.tensor, "dve": nc.vector, "act": nc.scalar,
                    "pool": nc.gpsimd, "sp": nc.sync}
        self.sem = {}
        self.cnt = {}
        for e in self.ENG:
            self.sem[e] = es.enter_context(nc.semaphore("s_" + e))
            self.cnt[e] = 0
        self.waited = {e: {} for e in self.ENG}
        self.out_tokens = []
        self.nsem = 5
        self.ninst = 0

    def sb(self, name, shape, dt=F32):
        t = self.es.enter_context(self.nc.sbuf_tensor(name, list(shape), dt))
        return t

    def ps(self, name, shape, dt=F32):
        t = self.es.enter_context(self.nc.psum_tensor(name, list(shape), dt))
        return t

    def buf(self, name, t=None):
        return Buf(name, t)

    def _wait(self, e, deps):
        w = self.waited[e]
        best = {}
        for d in deps:
            if d is None:
                continue
            sem, val, pe = d
            if e == "pe" and pe == "pe":
                continue
            k = id(sem)
            if w.get(k, 0) >= val:
                continue
            if k not in best or best[k][1] < val:
                best[k] = (sem, val)
        for k, (sem, val) in best.items():
            self.eng[e].wait_ge(sem, val)
            w[k] = val
            self.ninst += 1

    def op(self, e, build, reads=(), writes=()):
        deps = []
        for b in reads:
            deps.append(b.w)
            if b.excl:
                deps.extend(r for r in b.r if r[2] != e)
        for b in writes:
            deps.append(b.w)
            deps.extend(b.r)
        self._wait(e, deps)
        ins = build(self.eng[e])
        self.cnt[e] += 1
        ins.then_inc(self.sem[e], 1)
        tok = (self.sem[e], self.cnt[e], e)
        self.ninst += 1
        for b in reads:
            b.r.append(tok)
            if len(b.r) > 64:
                b.r = self._compact(b.r)
        for b in writes:
            b.w = tok
            b.r = []
        return tok

    @staticmethod
    def _compact(rs):
        best = {}
        for sem, val, e in rs:
            k = id(sem)
            if k not in best or best[k][1] < val:
                best[k] = (sem, val, e)
        return list(best.values())

    def dma(self, q, out, in_, sbuf_buf, reads=(), writes=(), is_output=False, **kw):
        deps = []
        for b in reads:
            deps.append(b.w)
        for b in writes:
            deps.append(b.w)
            deps.extend(b.r)
        self._wait(q, deps)
        b0 = sbuf_buf
        if b0.dsem is None:
            b0.dsem = self.es.enter_context(self.nc.semaphore("d_" + b0.name))
            self.nsem += 1
        ins = self.eng[q].dma_start(out=out, in_=in_, **kw)
        b0.dcnt += 16
        ins.then_inc(b0.dsem, 16)
        tok = (b0.dsem, b0.dcnt, "dma")
        self.ninst += 1
        for b in reads:
            b.r.append(tok)
        for b in writes:
            b.w = tok
            b.r = []
        if is_output:
            self.out_tokens.append(tok)
        return tok

    def finish(self):
        self._wait("sp", self.out_tokens)
        deps = [(self.sem[e], self.cnt[e], e) for e in self.ENG if e != "sp" and self.cnt[e] > 0]
        self._wait("sp", deps)


class View:
    __slots__ = ("tile", "ap")

    def __init__(self, tile, ap):
        self.tile = tile
        self.ap = ap

    def __getitem__(self, idx):
        return View(self.tile, self.ap[idx])

    @property
    def t(self):
        return self.ap

    def v(self, ap):
        return View(self.tile, ap)


class Tile:
    def __init__(self, P, name, shape, dt=F32, psum=False):
        self.P = P
        self.name = name
        self.t = (P.ps if psum else P.sb)(name, shape, dt)
        self.buf = Buf(name)
        self.buf.excl = bool(psum)
        self.shape = list(shape)

    def __getitem__(self, idx):
        return View(self, self.t[idx])

    def v(self, ap):
        return View(self, ap)


def _bufs(views):
    out = []
    for v in views:
        if isinstance(v, View):
            if v.tile.buf not in out:
                out.append(v.tile.buf)
        elif isinstance(v, Tile):
            if v.buf not in out:
                out.append(v.buf)
    return out


def _ap(v):
    return v.ap if isinstance(v, View) else v


class Prog2(Prog):
    def tile(self, name, shape, dt=F32, psum=False):
        return Tile(self, name, shape, dt, psum)

    def gop(self, e, fn, outs, ins):
        return self.op(e, fn, reads=_bufs(ins), writes=_bufs(outs))

    def act(self, out, in_, func, bias=0.0, scale=1.0, accum_out=None, e="act"):
        ins = [in_, bias, scale]
        outs = [out] + ([accum_out] if accum_out is not None else [])
        kw = {}
        if accum_out is not None:
            kw["accum_out"] = _ap(accum_out)
        return self.gop(e, lambda E: E.activation(out=_ap(out), in_=_ap(in_), func=func,
                                                  bias=_ap(bias), scale=_ap(scale), **kw), outs, ins)

    def tt(self, out, a, b, op, e="dve"):
        return self.gop(e, lambda E: E.tensor_tensor(out=_ap(out), in0=_ap(a), in1=_ap(b), op=op), [out], [a, b])

    def ts(self, out, a, s1, s2, op0, op1=None, accum_out=None, e="dve"):
        kw = {}
        if op1 is not None:
            kw["op1"] = op1
        outs = [out]
        if accum_out is not None:
            kw["accum_out"] = _ap(accum_out)
            outs.append(accum_out)
        return self.gop(e, lambda E: E.tensor_scalar(out=_ap(out), in0=_ap(a), scalar1=_ap(s1),
                                                     scalar2=_ap(s2) if s2 is not None else None,
                                                     op0=op0, **kw), outs, [a, s1, s2])

    def stt(self, out, in0, scalar, in1, op0, op1, accum_out=None, e="dve"):
        kw = {}
        outs = [out]
        if accum_out is not None:
            kw["accum_out"] = _ap(accum_out)
            outs.append(accum_out)
        return self.gop(e, lambda E: E.scalar_tensor_tensor(out=_ap(out), in0=_ap(in0), scalar=_ap(scalar),
                                                            in1=_ap(in1), op0=op0, op1=op1, **kw),
                        outs, [in0, scalar, in1])

    def copy(self, out, in_, e="dve"):
        if e == "act":
            return self.gop(e, lambda E: E.copy(out=_ap(out), in_=_ap(in_)), [out], [in_])
        return self.gop(e, lambda E: E.tensor_copy(out=_ap(out), in_=_ap(in_)), [out], [in_])

    def memset(self, out, val, e="dve"):
        return self.gop(e, lambda E: E.memset(_ap(out), val), [out], [])

    def red(self, out, in_, op, axis=AX.X, e="dve"):
        return self.gop(e, lambda E: E.tensor_reduce(out=_ap(out), in_=_ap(in_), axis=axis, op=op), [out], [in_])

    def recip(self, out, in_):
        return self.gop("dve", lambda E: E.reciprocal(out=_ap(out), in_=_ap(in_)), [out], [in_])

    def mm(self, out, lhsT, rhs, start=True, stop=True):
        return self.gop("pe", lambda E: E.matmul(_ap(out), _ap(lhsT), _ap(rhs), start=start, stop=stop),
                        [out], [lhsT, rhs])

    def tr(self, out, in_, ident):
        return self.gop("pe", lambda E: E.transpose(_ap(out), _ap(in_), _ap(ident)), [out], [in_, ident])

    def load(self, q, view, dram_ap, **kw):
        return self.dma(q, view.ap, dram_ap, view.tile.buf, writes=[view.tile.buf], **kw)

    def store(self, q, dram_ap, view, is_output=True, **kw):
        return self.dma(q, dram_ap, view.ap, view.tile.buf, reads=[view.tile.buf], is_output=is_output, **kw)


from concourse.bass_utils import run_bass_kernel_spmd

D = 1024
T = 8192
NB = 2
IN_COLS = 5144
NCORES = 8
_CACHE = {}


def _new_nc():
    return bass.Bass("TRN2", target_bir_lowering=False)


def build_k0():
    nc = _new_nc()
    NCOL = 1536
    cT = nc.dram_tensor("cT", [128, 8, 2], F32, kind="ExternalInput").ap()
    w = nc.dram_tensor("w", [1024, NCOL], F32, kind="ExternalInput").ap()
    b = nc.dram_tensor("b", [1, NCOL], F32, kind="ExternalInput").ap()
    y = nc.dram_tensor("y", [2, NCOL], F32, kind="ExternalOutput").ap()
    with ExitStack() as es:
        P = Prog2(nc, es)
        ct = P.tile("ct", [128, 8, 2])
        cs = P.tile("cs", [128, 8, 2])
        wt = P.tile("wt", [128, 8, NCOL])
        bt = P.tile("bt", [2, NCOL])
        yt = P.tile("yt", [2, NCOL])
        P.load("sp", ct[:], cT[:, :, :])
        P.load("act", wt[:], w.rearrange("(kc kp) n -> kp kc n", kp=128))
        P.load("sp", bt[:], b.partition_broadcast(2))
        P.act(cs[:], ct[:], AF.Silu)
        for n in range(3):
            ps = P.tile(f"ps{n}", [2, 512], psum=True)
            for kc in range(8):
                P.mm(ps[:], cs[:, kc, :], wt[:, kc, n * 512:(n + 1) * 512], start=(kc == 0), stop=(kc == 7))
            P.tt(yt[:, n * 512:(n + 1) * 512], ps[:], bt[:, n * 512:(n + 1) * 512], ALU.add)
        P.store("sp", y[:, :], yt[:])
        P.finish()
    return nc


def run_k0(c, w_ada, b_ada):
    if "k0" not in _CACHE:
        _CACHE["k0"] = build_k0()
    nc = _CACHE["k0"]
    L = w_ada.shape[0]
    wcat = np.concatenate([w_ada[l] for l in range(L)], axis=1)
    bcat = np.concatenate([b_ada[l] for l in range(L)], axis=0)[None]
    cT = np.ascontiguousarray(c.T.reshape(8, 128, 2).transpose(1, 0, 2))
    maps = []
    for i in range(NCORES):
        sl = slice(i * 1536, (i + 1) * 1536)
        maps.append({"cT": cT, "w": np.ascontiguousarray(wcat[:, sl]), "b": np.ascontiguousarray(bcat[:, sl])})
    res = run_bass_kernel_spmd(nc, maps, core_ids=list(range(NCORES)))
    y = np.concatenate([r["y"] for r in res.results], axis=1)
    return y.reshape(2, L, 6144).transpose(1, 0, 2)


def build_k1():
    nc = _new_nc()
    NT = 16
    x = nc.dram_tensor("x", [NT * 128, D], F32, kind="ExternalInput").ap()
    g = nc.dram_tensor("g", [1, D], F32, kind="ExternalInput").ap()
    sc = nc.dram_tensor("sc", [1, D], F32, kind="ExternalInput").ap()
    sh = nc.dram_tensor("sh", [1, D], F32, kind="ExternalInput").ap()
    w = nc.dram_tensor("w", [D, IN_COLS], F32, kind="ExternalInput").ap()
    b = nc.dram_tensor("b", [1, IN_COLS], F32, kind="ExternalInput").ap()
    ident = nc.dram_tensor("ident", [128, 128], F32, kind="ExternalInput").ap()
    p = nc.dram_tensor("p", [NT * 128, IN_COLS], F32, kind="ExternalOutput").ap()
    with ExitStack() as es, nc.allow_low_precision("bf16 in-proj operands, fp32 PSUM accumulation"):
        P = Prog2(nc, es)
        idt = P.tile("idt", [128, 128])
        G = P.tile("G", [128, D]); SC = P.tile("SC", [128, D]); SH = P.tile("SH", [128, D])
        B = P.tile("B", [128, IN_COLS])
        hT = [P.tile(f"hT{i}", [128, 8, 128], BF16) for i in range(NT)]
        P.load("sp", idt[:], ident[:, :])
        P.load("sp", G[:], g.partition_broadcast(128))
        P.load("act", SC[:], sc.partition_broadcast(128))
        P.load("sp", SH[:], sh.partition_broadcast(128))
        P.load("act", B[:], b.partition_broadcast(128))
        P.stt(G[:], SC[:], 1.0, G[:], ALU.add, ALU.mult)
        xt = [P.tile(f"xt{i}", [128, D]) for i in range(2)]
        junk = P.tile("junk", [128, D])
        st = [P.tile(f"st{i}", [128, 2]) for i in range(2)]
        pT = [P.tile(f"pT{i}", [128, 4, 128], psum=True) for i in range(2)]
        for ti in range(NT):
            X = xt[ti % 2]; S = st[ti % 2]
            P.load("sp" if ti % 2 == 0 else "act", X[:], x[ti * 128:(ti + 1) * 128, :])
            P.act(junk[:], X[:], AF.Square, accum_out=S[:, 0:1])
            P.ts(S[:, 1:2], S[:, 0:1], 1.0 / D, 1e-6, ALU.mult, ALU.add)
            P.act(S[:, 1:2], S[:, 1:2], AF.Sqrt)
            P.recip(S[:, 1:2], S[:, 1:2])
            P.stt(X[:], X[:], S[:, 1:2], G[:], ALU.mult, ALU.mult)
            P.tt(X[:], X[:], SH[:], ALU.add)
            for half in range(2):
                pt = pT[half]
                for j in range(4):
                    kc = half * 4 + j
                    P.tr(pt[:, j, :], X[:, kc * 128:(kc + 1) * 128], idt[:])
                P.copy(hT[ti][:, half * 4:(half + 1) * 4, :], pt[:], e="act" if half else "dve")
        NCH = (IN_COLS + 511) // 512
        wt = [P.tile(f"wt{i}", [128, 8, 512]) for i in range(2)]
        wb = [P.tile(f"wb{i}", [128, 8, 512], BF16) for i in range(2)]
        po = [P.tile(f"po{i}", [128, 512], psum=True) for i in range(4)]
        ot = [P.tile(f"ot{i}", [128, 512]) for i in range(4)]
        wv = w.rearrange("(kc kp) n -> kp kc n", kp=128)
        k = 0
        for ci in range(NCH):
            c0 = ci * 512
            cw = min(512, IN_COLS - c0)
            W = wt[ci % 2]
            P.load("sp" if ci % 2 == 0 else "act", W[:, :, :cw], wv[:, :, c0:c0 + cw])
            Wb = wb[ci % 2]
            P.copy(Wb[:, 0:4, :cw], W[:, 0:4, :cw], e="pool")
            P.copy(Wb[:, 4:8, :cw], W[:, 4:8, :cw], e="act")
            for ti in range(NT):
                ps = po[k % 4]; o = ot[k % 4]
                for kc in range(8):
                    P.mm(ps[:, :cw], hT[ti][:, kc, :], Wb[:, kc, :cw], start=(kc == 0), stop=(kc == 7))
                P.tt(o[:, :cw], ps[:, :cw], B[:, c0:c0 + cw], ALU.add, e="dve")
                P.store("sp" if k % 2 == 0 else "act", p[ti * 128:(ti + 1) * 128, c0:c0 + cw], o[:, :cw])
                k += 1
        P.finish()
    return nc


def run_k1(xfull, g, sc, sh, w, b):
    if "k1" not in _CACHE:
        _CACHE["k1"] = build_k1()
    nc = _CACHE["k1"]
    ident = np.eye(128, dtype=np.float32)
    maps = []
    for i in range(NCORES):
        bi, tq = divmod(i, 4)
        maps.append({"x": np.ascontiguousarray(xfull[bi, tq * 2048:(tq + 1) * 2048]),
                     "g": g[None].copy(), "sc": sc[bi][None].copy(), "sh": sh[bi][None].copy(),
                     "w": w, "b": b[None].copy(), "ident": ident})
    res = run_bass_kernel_spmd(nc, maps, core_ids=list(range(NCORES)))
    out = np.empty((NB, T, IN_COLS), np.float32)
    for i in range(NCORES):
        bi, tq = divmod(i, 4)
        out[bi, tq * 2048:(tq + 1) * 2048] = res.results[i]["p"]
    return out


def _bc(tile, ap, axis, shape):
    return tile.v(ap.unsqueeze(axis).to_broadcast(list(shape)))


def k2_consts():
    C = 64
    tri_incl = np.triu(np.ones((C, C), np.float32))
    tri_strict = np.triu(np.ones((C, C), np.float32), 1)
    m = np.concatenate([tri_strict, tri_incl], 1)
    mask128 = np.concatenate([m, m], 0)
    sel0 = np.concatenate([np.eye(64, dtype=np.float32), np.zeros((64, 64), np.float32)], 1)
    shift = np.concatenate([np.zeros((64, 64), np.float32), np.eye(64, dtype=np.float32)], 1)
    return {"ident": np.eye(128, dtype=np.float32), "mask128": mask128,
            "trils": np.ascontiguousarray(tri_strict.T), "tri": tri_incl,
            "ones64": np.ones((64, 64), np.float32), "sel0": sel0, "shift": shift}


def build_k2(TT=T, stop=0, ustop=99):
    nc = _new_nc()
    NG = TT // 256
    dr = lambda n, s, kind="ExternalInput": nc.dram_tensor(n, s, F32, kind=kind).ap()
    pp = dr("pp", [TT + 1, 640]); mu = dr("mu", [1, 640]); vec = dr("vec", [8, 128])
    w2 = dr("w2", [64, 128]); a2 = dr("a2", [64, 128]); g2 = dr("g2", [128, 128])
    ident = dr("ident", [128, 128]); mask128 = dr("mask128", [128, 128]); trils = dr("trils", [64, 64])
    tri = dr("tri", [64, 64]); ones64 = dr("ones64", [64, 64]); sel0 = dr("sel0", [64, 128]); shift = dr("shift", [64, 128])
    ya = dr("ya", [TT, 128], "ExternalOutput")
    with ExitStack() as es:
        P = Prog2(nc, es)
        IDT = P.tile("IDT", [128, 128]); MASK = P.tile("MASK", [128, 128]); TRILS = P.tile("TRILS", [64, 64])
        TRI = P.tile("TRI", [64, 64]); ONES = P.tile("ONES", [64, 64]); SEL0 = P.tile("SEL0", [64, 128]); SHIFT = P.tile("SHIFT", [64, 128])
        W2 = P.tile("W2", [64, 128]); A2 = P.tile("A2", [64, 128]); G2 = P.tile("G2", [128, 128])
        MU = P.tile("MU", [64, 640]); VEC = P.tile("VEC", [64, 8, 128])
        qs = ["sp", "act"]
        for i, (tl, d) in enumerate([(IDT, ident), (MASK, mask128), (TRILS, trils), (TRI, tri), (ONES, ones64),
                                     (SEL0, sel0), (SHIFT, shift), (W2, w2), (A2, a2), (G2, g2)]):
            P.load(qs[i % 2], tl[:], d[:, :])
        P.load("sp", MU[:], mu.partition_broadcast(64))
        for r in range(7):
            P.load(qs[r % 2], VEC[:, r, :], vec[r:r + 1, :].partition_broadcast(64))
        I64 = IDT[0:64, 0:64]
        S4 = [64, 4, 128]
        vb = lambda r: _bc(VEC, VEC.t[:, r, :], 1, S4)
        W0b, A0b, KKb, KAb, RKb, GNGb, GNBb = [vb(r) for r in range(7)]
        banks = [P.tile(f"bank{i}", [128, 512], psum=True) for i in range(8)]
        pk = [0]

        def pbank():
            b = banks[pk[0] % 4]
            pk[0] += 1
            return b

        def t4(name, n=2):
            return [P.tile(f"{name}{i}", S4) for i in range(n)]

        CURt = [P.tile(f"CUR{i}", [64, 4, 640]) for i in range(2)]
        PRVt = [P.tile(f"PRV{i}", [64, 4, 640]) for i in range(2)]
        TWt = t4("TW"); SGt = t4("SG"); LTt = [P.tile(f"LT{i}", [64, 4, 2, 64]) for i in range(2)]
        GTtt = [P.tile(f"GTt{i}", [128, 4, 64]) for i in range(2)]
        LDt = t4("LD"); At = t4("A"); Ggt = t4("Gg"); KKt = t4("KK"); SQt = t4("SQ"); T1t = t4("T1"); KMt = t4("KM"); Bvt = t4("Bv")
        SSt = [P.tile(f"SS{i}", [64, 8]) for i in range(2)]; RKt = [P.tile(f"RK{i}", [64, 8]) for i in range(2)]
        Lst = t4("Ls"); ELt = t4("EL"); ENLt = t4("ENL"); TMPt = t4("TMP"); TMP2t = t4("TMP2")
        DCFt = [P.tile(f"DCF{i}", [64, 8]) for i in range(2)]
        A_t = t4("A_"); BTt = t4("BT"); KTt = t4("KT"); RTt = t4("RT"); BBt = t4("BB"); KBt = t4("KB")
        FMBKt = [P.tile(f"FMBK{i}", [64, 8, 128]) for i in range(2)]
        FMARt = [P.tile(f"FMAR{i}", [64, 8, 128]) for i in range(2)]
        BKSt = [P.tile(f"BKS{i}", [128, 4, 128]) for i in range(2)]
        VVt = [P.tile(f"VV{i}", [128, 4, 128]) for i in range(2)]
        YBt = t4("YB"); YCt = t4("YC"); MNt = [P.tile(f"MN{i}", [64, 8]) for i in range(2)]; VRt = [P.tile(f"VR{i}", [64, 8]) for i in range(2)]
        hk = [0, 0]
        ubi = [0]; hkk = [0]
        BIGA = P.tile("BIGA", [128, 8, 128])
        XA = [P.tile(f"XA{i}", [64, 8, 64]) for i in range(2)]
        XTA = [P.tile(f"XTA{i}", [64, 8, 64]) for i in range(2)]
        PMA = [P.tile(f"PMA{i}", [64, 8, 64]) for i in range(2)]
        W2A = P.tile("W2A", [64, 8, 64]); AHA = P.tile("AHA", [64, 8, 64]); RHA = P.tile("RHA", [64, 8, 64])
        GTA = P.tile("GTA", [64, 8, 64]); TMPG = P.tile("TMPG", [64, 8, 64])
        HA = [P.tile(f"HA{i}", [64, 2, 64]) for i in range(2)]
        P.memset(HA[0][:], 0.0)
        fl = lambda tl: tl.v(tl.t[:, :, :].rearrange("p a b -> p (a b)"))
        v8 = lambda tl: tl.v(tl.t[:, :, :].rearrange("p a (h b) -> p (a h) b", b=64))
        b8 = lambda tl: _bc(tl, tl.t[:, :], 2, [64, 8, 64])
        ppv = lambda lo: pp[lo:lo + 256, :].rearrange("(c t) n -> t c n", t=64)
        NEG = -float(np.exp(-0.5))

        class _Stop(Exception):
            pass

        def early(n, view):
            if stop == n:
                P.store("sp", ya[0:256, :].rearrange("(c t) n -> t c n", t=64), view)
                raise _Stop()

        ctxp = {}

        def prepG(g):
            k = g % 2
            CUR = CURt[k]; PRV = PRVt[k]
            P.load("sp", CUR[:], ppv(1 + g * 256))
            yield
            P.load("act", PRV[:], ppv(g * 256))
            yield
            P.tt(PRV[:], PRV[:], CUR[:], ALU.subtract)
            yield
            P.tt(PRV[:], PRV[:], _bc(MU, MU.t[:, :], 1, [64, 4, 640]), ALU.mult)
            yield
            P.tt(CUR[:], CUR[:], PRV[:], ALU.add, e="pool")
            yield
            Rr = CUR[:, :, 0:128]; Kr = CUR[:, :, 128:256]; Vr = CUR[:, :, 256:384]
            TW = TWt[k]; SG = SGt[k]; LT = LTt[k]; GTt = GTtt[k]
            P.act(TW[:, :, 0:64], CUR[:, :, 384:448], AF.Tanh)
            yield
            P.act(SG[:], CUR[:, :, 512:640], AF.Sigmoid)
            yield
            b1 = pbank(); b1v = b1.v(b1.t[0:64, :].rearrange("p (c w t) -> p c w t", c=4, w=2))
            for c in range(4):
                P.tr(b1.v(b1v.ap[:, c, 0, :]), TW[:, c, 0:64], I64)
                yield
                P.tr(b1.v(b1v.ap[:, c, 1, :]), CUR[:, c, 448:512], I64)
                yield
            P.copy(LT[:], b1v)
            yield
            b2 = pbank(); b2v = b2.v(b2.t[:, 0:256].rearrange("p (c t) -> p c t", c=4))
            for c in range(4):
                P.tr(b2.v(b2v.ap[:, c, :]), SG[:, c, :], I64)
                yield
            P.copy(GTt[:], b2v, e="act")
            yield
            bw = pbank(); bwv = bw.v(bw.t[0:64, :].rearrange("p (c n) -> p c n", c=4))
            for c in range(4):
                P.mm(bw.v(bwv.ap[:, c, :]), LT[:, c, 0, :], W2[:])
                yield
            LD = LDt[k]
            P.tt(LD[:], bwv, W0b, ALU.add)
            yield
            ba = pbank(); bav = ba.v(ba.t[0:64, :].rearrange("p (c n) -> p c n", c=4))
            for c in range(4):
                P.mm(ba.v(bav.ap[:, c, :]), LT[:, c, 1, :], A2[:])
                yield
            A = At[k]
            P.tt(A[:], bav, A0b, ALU.add)
            yield
            bg = pbank(); bgv = bg.v(bg.t[0:64, :].rearrange("p (c n) -> p c n", c=4))
            for c in range(4):
                P.mm(bg.v(bgv.ap[:, c, :]), GTt[:, c, :], G2[:])
                yield
            Gg = Ggt[k]
            P.copy(Gg[:], bgv, e="act")
            yield
            P.act(LD[:], LD[:], AF.Sigmoid)
            yield
            P.act(A[:], A[:], AF.Sigmoid)
            yield
            P.ts(LD[:], LD[:], NEG, None, ALU.mult, e="pool")
            yield
            KK = KKt[k]; SQ = SQt[k]; SS = SSt[k]; T1 = T1t[k]; KM = KMt[k]; Bv = Bvt[k]; RK = RKt[k]
            P.tt(KK[:], Kr, KKb, ALU.mult)
            yield
            P.tt(SQ[:], KK[:], KK[:], ALU.mult)
            yield
            P.red(SS[:], v8(SQ), ALU.add)
            yield
            P.act(SS[:], SS[:], AF.Sqrt)
            yield
            P.ts(SS[:], SS[:], 1e-12, None, ALU.max)
            yield
            P.recip(SS[:], SS[:])
            yield
            P.tt(v8(KK), v8(KK), b8(SS), ALU.mult)
            yield
            P.stt(T1[:], A[:], -1.0, KAb, ALU.add, ALU.mult)
            yield
            P.stt(KM[:], T1[:], 1.0, Kr, ALU.add, ALU.mult)
            yield
            P.tt(Bv[:], KK[:], A[:], ALU.mult)
            yield
            P.tt(SQ[:], Rr, KM[:], ALU.mult)
            yield
            P.tt(SQ[:], SQ[:], RKb, ALU.mult)
            yield
            P.red(RK[:], v8(SQ), ALU.add)
            yield
            Ls = Lst[k]; EL = ELt[k]; ENL = ENLt[k]; TMP = TMPt[k]; TMP2 = TMP2t[k]; DCF = DCFt[k]
            bL = pbank(); bLv = bL.v(bL.t[0:64, :].rearrange("p (c n) -> p c n", c=4))
            P.mm(bL[0:64, :], TRI[:], fl(LD))
            yield
            P.copy(Ls[:], bLv)
            yield
            bC = pbank(); bCv = bC.v(bC.t[0:64, :].rearrange("p (c n) -> p c n", c=4))
            P.mm(bC[0:64, :], ONES[:], fl(LD))
            yield
            P.tt(TMP2[:], bCv, Ls[:], ALU.subtract)
            yield
            bD = pbank()
            for u in range(8):
                c, h = divmod(u, 2)
                P.mm(bD[0:64, u:u + 1], LD[:, c, h * 64:(h + 1) * 64], ONES[:, 0:1])
                yield
            P.act(DCF[:], bD[0:64, 0:8], AF.Exp)
            yield
            P.act(EL[:], Ls[:], AF.Exp)
            yield
            P.act(ENL[:], Ls[:], AF.Exp, scale=-1.0)
            yield
            P.tt(TMP[:], Ls[:], LD[:], ALU.subtract)
            yield
            P.act(TMP[:], TMP[:], AF.Exp)
            yield
            P.act(TMP2[:], TMP2[:], AF.Exp)
            yield
            A_ = A_t[k]; BT = BTt[k]; KT = KTt[k]; RT = RTt[k]; BB = BBt[k]; KB = KBt[k]
            P.stt(A_[:], KK[:], -1.0, TMP[:], ALU.mult, ALU.mult)
            yield
            P.tt(BT[:], Bv[:], ENL[:], ALU.mult)
            yield
            P.tt(KT[:], KM[:], ENL[:], ALU.mult, e="pool")
            yield
            P.tt(RT[:], Rr, EL[:], ALU.mult)
            yield
            P.tt(BB[:], Bv[:], TMP2[:], ALU.mult, e="pool")
            yield
            P.tt(KB[:], KM[:], TMP2[:], ALU.mult)
            yield
            FMBK = FMBKt[k]; FMAR = FMARt[k]; BKS = BKSt[k]; VV = VVt[k]
            for half in range(2):
                pb = pbank(); pbv = pb.v(pb.t[0:64, :].rearrange("p (u n) -> p u n", u=4))
                pa_ = pbank(); pav = pa_.v(pa_.t[0:64, :].rearrange("p (u n) -> p u n", u=4))
                for uu in range(4):
                    u = half * 4 + uu
                    c, h = divmod(u, 2)
                    hs = slice(h * 64, (h + 1) * 64)
                    P.tr(pb.v(pbv.ap[:, uu, 0:64]), BT[:, c, hs], I64)
                    yield
                    P.tr(pb.v(pbv.ap[:, uu, 64:128]), KT[:, c, hs], I64)
                    yield
                    P.tr(pa_.v(pav.ap[:, uu, 0:64]), A_[:, c, hs], I64)
                    yield
                    P.tr(pa_.v(pav.ap[:, uu, 64:128]), RT[:, c, hs], I64)
                    yield
                P.copy(FMBK[:, half * 4:(half + 1) * 4, :], pbv)
                yield
                P.copy(FMAR[:, half * 4:(half + 1) * 4, :], pav, e="act")
                yield
            ps1 = pbank()
            P.mm(ps1[:], SEL0[:], fl(BB), start=True, stop=False)
            yield
            P.mm(ps1[:], SHIFT[:], fl(KB), start=False, stop=True)
            yield
            P.copy(fl(BKS), ps1[:])
            yield
            ps2 = pbank(); ps2v = ps2.v(ps2.t[:, :].rearrange("p (c n) -> p c n", c=4))
            P.mm(ps2v, SHIFT[:], Vr)
            yield
            P.copy(VV[64:128, :, :], ps2.v(ps2v.ap[64:128, :, :]), e="act")
            yield

            ctxp[g] = dict(k=k, CUR=CUR, A_=A_, FMBK=FMBK, FMAR=FMAR, BKS=BKS, VV=VV, DCF=DCF, RK=RK, Gg=Gg, SQ=SQ)

        def drainP(g_, n):
            if g_ is None:
                return
            try:
                if n is None:
                    while True:
                        next(g_)
                else:
                    for _ in range(n):
                        next(g_)
            except StopIteration:
                pass

        drainP(prepG(0), None)
        for g in range(NG):
            c_ = ctxp.pop(g)
            k = c_["k"]; CUR = c_["CUR"]; A_ = c_["A_"]; FMBK = c_["FMBK"]; FMAR = c_["FMAR"]; BKS = c_["BKS"]
            VV = c_["VV"]; DCF = c_["DCF"]; RK = c_["RK"]; Gg = c_["Gg"]; SQ = c_["SQ"]
            gP = prepG(g + 1) if g + 1 < NG else None
            YB = YBt[k]

            def ub():
                b = banks[4 + ubi[0] % 4]; ubi[0] += 1
                return b
            v864 = lambda bank: bank.v(bank.t[0:64, :].rearrange("p (u n) -> p u n", u=8))
            I8 = _bc(IDT, IDT.t[0:64, 0:64], 1, [64, 8, 64])
            bB = [ub(), ub()]; bX = ub()
            for u in range(8):
                bb = bB[u // 4]
                P.mm(bb[:, (u % 4) * 128:(u % 4 + 1) * 128], FMBK[:, u, :], FMAR[:, u, :])
                P.mm(bX[0:64, u * 64:(u + 1) * 64], FMAR[:, u, 0:64], FMBK[:, u, 0:64])
            for hf in range(2):
                bb = bB[hf]
                P.tt(BIGA[:, hf * 4:(hf + 1) * 4, :], bb.v(bb.t[:, :].rearrange("p (u n) -> p u n", u=4)),
                     _bc(MASK, MASK.t[:, :], 1, [128, 4, 128]), ALU.mult)
            XT = XTA[0]
            P.tt(XT[:], v864(bX), _bc(TRILS, TRILS.t[:, :], 1, [64, 8, 64]), ALU.mult)
            X = BIGA.v(BIGA.t[0:64, :, 0:64])
            Pm = PMA[0]
            P.tt(Pm[:], X, I8, ALU.add, e="pool")
            drainP(gP, 12)
            for i in range(5):
                Xn = XA[i % 2]; XTn = XTA[(i + 1) % 2]; Pn = PMA[(i + 1) % 2]
                bA = ub(); bQ = ub()
                for u in range(8):
                    us = slice(u * 64, (u + 1) * 64)
                    if i < 4:
                        P.mm(bA[0:64, us], XT[:, u, :], X[:, u, :])
                    P.mm(bQ[0:64, us], X[:, u, :], XT[:, u, :])
                if i < 4:
                    P.copy(Xn[:], v864(bA), e="act")
                P.copy(XTn[:], v864(bQ))
                bC = ub()
                for u in range(8):
                    us = slice(u * 64, (u + 1) * 64)
                    P.mm(bC[0:64, us], XTn[:, u, :], Pm[:, u, :])
                P.tt(Pn[:], v864(bC), Pm[:], ALU.add)
                drainP(gP, 12)
                X = Xn; XT = XTn; Pm = Pn
                if i == 4:
                    X = None
            InvT = Pm
            bW = ub()
            for u in range(8):
                c, h = divmod(u, 2); hs = slice(h * 64, (h + 1) * 64); us = slice(u * 64, (u + 1) * 64)
                P.mm(bW[0:64, us], BIGA[64:128, u, 0:64], VV[64:128, c, hs])
            P.copy(W2A[:], v864(bW), e="act")
            drainP(gP, 12)
            bA = ub(); bU = ub()
            for u in range(8):
                c, h = divmod(u, 2); hs = slice(h * 64, (h + 1) * 64); us = slice(u * 64, (u + 1) * 64)
                P.mm(bA[0:64, us], InvT[:, u, :], A_[:, c, hs])
                P.mm(bU[0:64, us], InvT[:, u, :], W2A[:, u, :])
            P.copy(AHA[:], v864(bA))
            P.copy(VV.v(VV.t[0:64, :, :].rearrange("p c (h n) -> p (c h) n", n=64)), v864(bU), e="act")
            bR = ub(); bG = ub()
            for u in range(8):
                c, h = divmod(u, 2); hs = slice(h * 64, (h + 1) * 64); us = slice(u * 64, (u + 1) * 64)
                P.mm(bR[0:64, us], AHA[:, u, :], BIGA[0:64, u, 64:128])
                P.mm(bG[0:64, us], AHA[:, u, :], BKS[0:64, c, hs])
            P.tt(RHA[:], v864(bR), FMAR[:, :, 64:128], ALU.add)
            P.tt(TMPG[:], I8, _bc(DCF, DCF.t[:, :], 2, [64, 8, 64]), ALU.mult, e="pool")
            P.tt(GTA[:], v864(bG), TMPG[:], ALU.add)
            drainP(gP, 12)
            bY = ub()
            bHs = [ub(), ub()]
            for c in range(4):
                bH = bHs[c % 2]
                Hc = HA[hkk[0] % 2]; Hn = HA[(hkk[0] + 1) % 2]; hkk[0] += 1
                for h in range(2):
                    u = c * 2 + h
                    hs = slice(h * 64, (h + 1) * 64); us = slice(u * 64, (u + 1) * 64)
                    P.mm(bY[0:64, us], RHA[:, u, :], Hc[:, h, :], start=True, stop=False)
                    P.mm(bY[0:64, us], BIGA[:, u, 64:128], VV[:, c, hs], start=False, stop=True)
                    P.mm(bH[0:64, hs], GTA[:, u, :], Hc[:, h, :], start=True, stop=False)
                    P.mm(bH[0:64, hs], BKS[:, c, hs], VV[:, c, hs], start=False, stop=True)
                P.copy(Hn[:], bH.v(bH.t[0:64, 0:128].rearrange("p (h n) -> p h n", h=2)))
                drainP(gP, 12)
            P.copy(fl(YB), bY[0:64, :], e="act")
            drainP(gP, None)
            YC = YCt[k]; MN = MNt[k]; VR = VRt[k]
            P.red(MN[:], v8(YB), ALU.add)
            P.ts(MN[:], MN[:], 1.0 / 64, None, ALU.mult)
            P.tt(v8(YC), v8(YB), b8(MN), ALU.subtract)
            P.tt(SQ[:], YC[:], YC[:], ALU.mult, e="pool")
            P.red(VR[:], v8(SQ), ALU.add)
            P.ts(VR[:], VR[:], 1.0 / 64, 64e-5, ALU.mult, ALU.add)
            P.act(VR[:], VR[:], AF.Sqrt)
            P.recip(VR[:], VR[:])
            P.tt(v8(YC), v8(YC), b8(VR), ALU.mult)
            P.tt(YC[:], YC[:], GNGb, ALU.mult)
            P.tt(YC[:], YC[:], GNBb, ALU.add)
            P.tt(SQ.v(SQ.t[:, :, :].rearrange("p a (h b) -> p a h b", b=64)),
                 CUR.v(CUR.t[:, :, 256:384].rearrange("p a (h b) -> p a h b", b=64)),
                 RK.v(RK.t[:, :].rearrange("p (a h) -> p a h", h=2).unsqueeze(3).to_broadcast([64, 4, 2, 64])), ALU.mult)
            P.tt(YC[:], YC[:], SQ[:], ALU.add)
            P.tt(YC[:], YC[:], Gg[:], ALU.mult)
            P.store("sp", ya[g * 256:(g + 1) * 256, :].rearrange("(c t) n -> t c n", t=64), YC[:])
        P.finish()
        print("k2 ninst", P.ninst, "nsem", P.nsem)
    return nc


RW = 512
EVAC_E = "act"


def k2_inputs(p_rwkv_b, i, prm, l, consts):
    h0 = 2 * i
    cs = slice(h0 * 64, h0 * 64 + 128)
    cols = np.r_[np.arange(h0 * 64, h0 * 64 + 128), RW + np.arange(h0 * 64, h0 * 64 + 128),
                 2 * RW + np.arange(h0 * 64, h0 * 64 + 128), np.arange(3 * RW, 3 * RW + 256)]
    vec = np.zeros((8, 128), np.float32)
    for r, n in enumerate(["rwkv_w0", "rwkv_a0", "rwkv_k_k", "rwkv_k_a", "rwkv_r_k", "rwkv_gn_g", "rwkv_gn_b"]):
        vec[r] = prm[n][l].reshape(-1)[cs]
    d = {"pp": np.ascontiguousarray(p_rwkv_b[:, cols]), "mu": np.ascontiguousarray(prm["rwkv_mu"][l][cols][None]),
         "vec": vec, "w2": np.ascontiguousarray(prm["rwkv_w2"][l][:, cs]),
         "a2": np.ascontiguousarray(prm["rwkv_a2"][l][:, cs]), "g2": np.ascontiguousarray(prm["rwkv_g2"][l][:, cs])}
    d.update(consts)
    return d


def run_k2(p, prm, l, TT=T):
    key = ("k2", TT)
    if key not in _CACHE:
        _CACHE[key] = build_k2(TT)
    nc = _CACHE[key]
    consts = k2_consts()
    maps = []
    for ci in range(NCORES):
        b, i = divmod(ci, 4)
        pb = np.concatenate([np.zeros((1, 1792), np.float32), p[b, :TT, :1792]], 0)
        maps.append(k2_inputs(pb, i, prm, l, consts))
    res = run_bass_kernel_spmd(nc, maps, core_ids=list(range(NCORES)))
    out = np.empty((NB, TT, 512), np.float32)
    for ci in range(NCORES):
        b, i = divmod(ci, 4)
        out[b, :, i * 128:(i + 1) * 128] = res.results[ci]["ya"]
    return out


ROPE_THETA = 500000.0
NEGM = -30000.0


def k3_consts(TT):
    half = 8
    inv = np.power(ROPE_THETA, -np.arange(half, dtype=np.float32) * 2.0 / 16).astype(np.float32)

    def tab(pos):
        ang = pos.astype(np.float32)[:, None] * inv[None, :]
        c, s = np.cos(ang).astype(np.float32), np.sin(ang).astype(np.float32)
        return np.concatenate([c, c, -s, s], 1).astype(np.float32)
    rope_tok = tab(np.arange(TT))
    ncmp = TT // 16
    rope_cmp = tab(np.arange(ncmp) * 16 + 31)
    ql = np.arange(128)
    triu = (ql[:, None] <= ql[None, :]).astype(np.float32)
    tril = (ql[:, None] > ql[None, :]).astype(np.float32)
    cma = np.zeros((128, 17, 128), np.float32)
    cmt = np.zeros((128, 17, 128), np.float32)
    for v in range(17):
        c0 = 8 * v
        nl = np.arange(128)
        valid = (16 * (nl[None, :] - c0) + 31 <= ql[:, None])
        cma[:, v, :] = np.where(valid, 0.0, NEGM)
        cmt[:, v, :] = valid.T.astype(np.float32)
    ca = np.where(ql >= 64, 1e4, -1.0).astype(np.float32)[:, None]
    cb = np.where(ql < 64, 1e4, 0.0).astype(np.float32)[:, None]
    return {"rope_tok": rope_tok, "rope_cmp": rope_cmp, "triu": triu, "tril": tril, "cma": cma, "cmt": cmt,
            "cab": np.concatenate([ca, cb], 1), "ident": np.eye(128, dtype=np.float32)}


def build_k3(TT=T):
    nc = _new_nc()
    NTK = TT // 128
    NQB = NTK
    NCMP = TT // 16
    NCT = (NCMP + 127) // 128
    dr = lambda n, s, kind="ExternalInput": nc.dram_tensor(n, s, F32, kind=kind).ap()
    qin = dr("qin", [NQB * 128, 256]); gin = dr("gin", [NQB * 128, 6]); kvin = dr("kvin", [TT, 384])
    qkg = dr("qkg", [1, 256]); cpos = dr("cpos", [128, 32]); w1 = dr("w1", [128, 32, 256]); w2 = dr("w2", [128, 2, 2, 64])
    rope_tok = dr("rope_tok", [TT, 32]); rope_cmp = dr("rope_cmp", [NCMP, 32])
    triu = dr("triu", [128, 128]); tril = dr("tril", [128, 128]); cma = dr("cma", [128, 17, 128]); cmt = dr("cmt", [128, 17, 128])
    cab = dr("cab", [128, 2]); ident = dr("ident", [128, 128])
    yb = dr("yb", [NQB * 128, 128], "ExternalOutput")
    with ExitStack() as es, nc.allow_low_precision("bf16 attention-tile operands, fp32 PSUM accumulation"):
        P = Prog2(nc, es)
        IDT = P.tile("IDT", [128, 128]); TRIU = P.tile("TRIU", [128, 128]); TRIL = P.tile("TRIL", [128, 128])
        CMA = P.tile("CMA", [128, 17, 128]); CMT = P.tile("CMT", [128, 17, 128]); CAB = P.tile("CAB", [128, 2])
        QKG = P.tile("QKG", [128, 4, 64]); CPOS = P.tile("CPOS", [128, 32]); W2 = P.tile("W2", [128, 2, 2, 64])
        for i, (tl, d) in enumerate([(IDT, ident), (TRIU, triu), (TRIL, tril), (CAB, cab), (CPOS, cpos)]):
            P.load(["sp", "act"][i % 2], tl[:], d[:, :])
        P.load("sp", CMA[:], cma[:, :, :]); P.load("act", CMT[:], cmt[:, :, :])
        P.load("sp", QKG.v(QKG.t[:, :, :].rearrange("p a b -> p (a b)")), qkg.partition_broadcast(128))
        P.load("act", W2.v(W2.t[:, :, :, :].rearrange("p a b c -> p (a b c)")), w2.rearrange("p a b c -> p (a b c)"))
        banks = [P.tile(f"bank{i}", [128, 512], psum=True) for i in range(8)]
        KT = P.tile("KT", [128, TT], BF16)
        CT = P.tile("CT", [128, TT + 16])
        VSW = P.tile("VSW", [128, NTK, 2, 65], BF16)
        P.memset(VSW[:, :, :, 64:65], 1.0)
        P.memset(CT[:, TT:TT + 16], 0.0)
        KVt = [P.tile(f"KV{i}", [128, 384]) for i in range(2)]
        RPt = [P.tile(f"RP{i}", [128, 32]) for i in range(2)]
        KNt = [P.tile(f"KN{i}", [128, 2, 64]) for i in range(2)]
        SQt = [P.tile(f"SQa{i}", [128, 2, 64]) for i in range(2)]
        STt = [P.tile(f"STa{i}", [128, 2]) for i in range(2)]
        R1t = [P.tile(f"R1a{i}", [128, 2, 16]) for i in range(2)]
        R2t = [P.tile(f"R2a{i}", [128, 2, 16]) for i in range(2)]

        def norm_rope(X3, G3, RP, KN, SQ, ST, R1, R2, nh, e2="pool"):
            P.tt(SQ[:], X3, X3, ALU.mult, e=e2)
            P.red(ST[:], SQ[:], ALU.add)
            P.ts(ST[:], ST[:], 1.0 / 64, 1e-6, ALU.mult, ALU.add)
            P.act(ST[:], ST[:], AF.Sqrt)
            P.recip(ST[:], ST[:])
            P.tt(KN[:], X3, _bc(ST, ST.t[:, :], 2, [128, nh, 64]), ALU.mult)
            P.tt(KN[:], KN[:], G3, ALU.mult, e=e2)
            cs = _bc(RP, RP.t[:, 0:16], 1, [128, nh, 16])
            P.tt(R1[:], KN[:, :, 0:16], cs, ALU.mult)
            P.tt(R2[:, :, 0:8], KN[:, :, 8:16], _bc(RP, RP.t[:, 16:24], 1, [128, nh, 8]), ALU.mult, e=e2)
            P.tt(R2[:, :, 8:16], KN[:, :, 0:8], _bc(RP, RP.t[:, 24:32], 1, [128, nh, 8]), ALU.mult, e=e2)
            P.tt(KN[:, :, 0:16], R1[:], R2[:], ALU.add)

        GK = QKG.v(QKG.t[:, 2:4, :])
        pk = 0
        for tg in range(NTK // 4):
            bk = banks[pk % 2]; bc_ = banks[2 + pk % 2]; pk += 1
            for j in range(4):
                ti = tg * 4 + j
                k = ti % 2
                KV = KVt[k]; RP = RPt[k]
                P.load("sp", KV[:], kvin[ti * 128:(ti + 1) * 128, :])
                P.load("act", RP[:], rope_tok[ti * 128:(ti + 1) * 128, :])
                X3 = KV.v(KV.t[:, 128:384].rearrange("p (a b) -> p a b", b=128)[:, :, 0:64])
                norm_rope(X3, GK, RP, KNt[k], SQt[k], STt[k], R1t[k], R2t[k], 2)
                P.tr(bk[:, j * 128:(j + 1) * 128], KNt[k].v(KNt[k].t[:, :, :].rearrange("p a b -> p (a b)")), IDT[:])
                P.tr(bc_[:, j * 128:(j + 1) * 128], KV[:, 0:128], IDT[:])
                V3 = KV.v(KV.t[:, 128:384].rearrange("p (a b) -> p a b", b=128)[:, :, 64:128])
                P.copy(VSW[:, ti, :, 0:64], V3, e="act")
            P.copy(KT[:, tg * 512:(tg + 1) * 512], bk[:])
            P.copy(CT[:, tg * 512:(tg + 1) * 512], bc_[:], e="act")
        HID = P.tile("HID", [128, 2, 2, NCT * 128])
        P.memset(HID[:], 0.0, e="pool")
        W1 = P.tile("W1", [128, 32, 128])
        BIA = P.tile("BIA", [128, 2, 2])
        Zt = P.tile("Zt", [128, 512]); Z2 = P.tile("Z2", [128, 512])
        NV = NCMP - 1
        for hc in range(2):
            P.load("sp" if hc == 0 else "act", W1[:], w1[:, :, hc * 128:(hc + 1) * 128])
            for i in range(2):
                ps_ = slice(i * 64, (i + 1) * 64)
                bb = banks[4]
                for tau in range(32):
                    P.mm(bb[:, 0:1], W1[ps_, tau, :], CPOS[ps_, tau:tau + 1], start=(tau == 0), stop=(tau == 31))
                P.copy(BIA[:, i, hc:hc + 1], bb[:, 0:1])
                for nt in range((NCMP + 511) // 512):
                    n0 = nt * 512
                    nn = min(512, NCMP - n0)
                    ba = banks[5 + nt % 2]
                    for tau in range(32):
                        rhs = CT.v(CT.t[ps_, n0 * 16 + tau: n0 * 16 + tau + (nn - 1) * 16 + 1: 16])
                        P.mm(ba[:, 0:nn], W1[ps_, tau, :], rhs, start=(tau == 0), stop=(tau == 31))
                    Z = Zt
                    P.act(Z[:, 0:nn], ba[:, 0:nn], AF.Identity, bias=BIA[:, i, hc:hc + 1])
                    P.tt(Z2[:, 0:nn], Z[:, 0:nn], Z[:, 0:nn], ALU.mult)
                    P.ts(Z2[:, 0:nn], Z2[:, 0:nn], 0.044715, 1.0, ALU.mult, ALU.add)
                    P.tt(Z2[:, 0:nn], Z2[:, 0:nn], Z[:, 0:nn], ALU.mult)
                    P.act(Z2[:, 0:nn], Z2[:, 0:nn], AF.Sigmoid, scale=1.5957691216057308)
                    P.tt(HID[:, i, hc, n0:n0 + nn], Z2[:, 0:nn], Z[:, 0:nn], ALU.mult)
        KCT = P.tile("KCT", [128, NCT * 128])
        VCMP = P.tile("VCMP", [128, NCT, 65], BF16)
        KCTb = P.tile("KCTb", [128, NCT * 128], BF16)
        P.memset(VCMP[:, :, 64:65], 1.0)
        KC2 = P.tile("KC2", [128, 2, 64])
        KCR = P.tile("KCR", [128, 1, 64])
        GC = QKG.v(QKG.t[:, 1:2, :])
        for ct in range(NCT):
            bb = banks[4 + ct % 2]
            for i in range(2):
                for hc in range(2):
                    P.mm(bb[:, i * 64:(i + 1) * 64], HID[:, i, hc, ct * 128:(ct + 1) * 128], W2[:, i, hc, :],
                         start=(hc == 0), stop=(hc == 1))
            P.copy(VCMP[:, ct, 0:64], bb[:, 64:128], e="act")
            RP = RPt[ct % 2]
            nr = min(128, NCMP - ct * 128)
            P.load("sp", RP[0:nr, :], rope_cmp[ct * 128:ct * 128 + nr, :])
            k = ct % 2
            KN1 = KNt[k].v(KNt[k].t[:, 0:1, :])
            P.copy(KCR[:, 0, :], bb[:, 0:64])
            norm_rope(KCR[:], GC, RP, KN1,
                      SQt[k].v(SQt[k].t[:, 0:1, :]), STt[k].v(STt[k].t[:, 0:1]), R1t[k].v(R1t[k].t[:, 0:1, :]),
                      R2t[k].v(R2t[k].t[:, 0:1, :]), 1, e2="dve")
            P.copy(KC2[:, 0, :], KNt[k][:, 0, :])
            P.copy(KC2[:, 1, :], KNt[k][:, 0, :], e="act")
            b2 = banks[6 + ct % 2]
            P.tr(b2[:, 0:128], KC2.v(KC2.t[:, :, :].rearrange("p a b -> p (a b)")), IDT[:])
            P.copy(KCT[:, ct * 128:(ct + 1) * 128], b2[:, 0:128])
            P.copy(KCTb[:, ct * 128:(ct + 1) * 128], b2[:, 0:128], e="act")
        Qt = [P.tile(f"Q{i}", [128, 4, 64]) for i in range(2)]
        Gt = [P.tile(f"G{i}", [128, 2, 3]) for i in range(2)]
        QNt = [P.tile(f"QN{i}", [128, 4, 64]) for i in range(2)]
        QDt = [P.tile(f"QD{i}", [128, 4, 2, 64]) for i in range(2)]
        QTt = [P.tile(f"QT{i}", [128, 4, 128]) for i in range(2)]
        SQq = [P.tile(f"SQq{i}", [128, 4, 64]) for i in range(2)]
        STq = [P.tile(f"STq{i}", [128, 4]) for i in range(2)]
        R1q = [P.tile(f"R1q{i}", [128, 4, 16]) for i in range(2)]
        R2q = [P.tile(f"R2q{i}", [128, 4, 16]) for i in range(2)]
        PCt = [P.tile(f"PC{i}", [128, 512]) for i in range(2)]
        RSt = [P.tile(f"RS{i}", [128, 4]) for i in range(2)]
        IMPP = P.tile("IMPP", [128, 516])
        IMPF = P.tile("IMPF", [128, 128]); IMPW = P.tile("IMPW", [128, 128])
        M8 = P.tile("M8", [128, 16])
        SEL = P.tile("SEL", [128, 128])
        SELN = P.tile("SELN", [128, 128]); SELXb = P.tile("SELXb", [128, TT], BF16); IDTb = P.tile("IDTb", [128, 128], BF16)
        P.copy(IDTb[:], IDT[:])
        PTt = [P.tile(f"PT{i}", [128, 2, 128], BF16) for i in range(4)]
        QTbt = [P.tile(f"QTb{i}", [128, 2, 128], BF16) for i in range(2)]
        OUTt = [P.tile(f"OUT{i}", [128, 2, 64]) for i in range(2)]
        RIt = [P.tile(f"RI{i}", [128, 3, 2]) for i in range(2)]
        OTt = [P.tile(f"OT{i}", [128, 2, 64]) for i in range(2)]
        GQ = QKG.v(QKG.t[:, 0:1, :].to_broadcast([128, 4, 64])) if False else _bc(QKG, QKG.t[:, 0, :], 1, [128, 4, 64])
        sb_i = [0]; mb_i = [0]; pt_i = [0]
        OB = [0, 1, 2]

        def attn_tile(first, last, ob, kT_ap, part, v1_ap, QT, mask_fn):
            sb = banks[sb_i[0] % 2]; sb_i[0] += 1
            PT = PTt[pt_i[0] % 4]; pt_i[0] += 1
            P.mm(sb[:, 0:256], kT_ap, QTb.v(QTb.t[part:part + 64, :, :].rearrange("p a b -> p (a b)")))
            P.act(PT.v(PT.t[:, :, :].rearrange("p a b -> p (a b)")), sb[:, 0:256], AF.Exp)
            mask_fn(PT)
            for r in range(2):
                P.mm(banks[6 + r][:, ob * 65:(ob + 1) * 65], PT[:, r, :], v1_ap, start=first, stop=last)

        ctxs = {}

        def phaseA(jq):
                qb = jq
                k = jq % 2
                Q = Qt[k]; G = Gt[k]; RP = RPt[k]
                P.load("sp", Q.v(Q.t[:, :, :].rearrange("p a b -> p (a b)")), qin[jq * 128:(jq + 1) * 128, :])
                yield
                P.load("act", G.v(G.t[:, :, :].rearrange("p a b -> p (a b)")), gin[jq * 128:(jq + 1) * 128, :])
                yield
                P.load("sp", RP[:], rope_tok[qb * 128:(qb + 1) * 128, :])
                yield
                QN = QNt[k]; QD = QDt[k]; QT = QTt[k]
                norm_rope(Q[:], GQ, RP, QN, SQq[k], STq[k], R1q[k], R2q[k], 4)
                yield
                P.ts(QD[:, :, 0, :], QN[:], 0.125, None, ALU.mult)
                yield
                P.ts(QD[:, :, 1, :], QN[:], 0.125, None, ALU.mult, e="pool")
                yield
                P.act(G[:], G[:], AF.Sigmoid)
                yield
                bq = banks[4]
                for r in range(4):
                    P.tr(bq[:, r * 128:(r + 1) * 128], QD.v(QD.t[:, r, :, :].rearrange("p a b -> p (a b)")), IDT[:])
                    yield
                P.copy(QT.v(QT.t[:, :, :].rearrange("p a b -> p (a b)")), bq[:])
                yield
                QTb = QTbt[k]
                P.copy(QTb.v(QTb.t[:, :, :].rearrange("p a b -> p (a b)")), bq[:, 0:256], e="act")
                yield
                nct = qb // 16 + 1
                ncols = nct * 128
                var = qb % 16
                P.memset(IMPP[:], 0.0, e="pool")
                yield
                RS = RSt[k]
                for r in range(4):
                    sb = banks[5]
                    P.mm(sb[:, 0:ncols], QT[0:64, r, :], KCT[0:64, 0:ncols])
                    yield
                    PC = PCt[r % 2]
                    lo = ncols - 128
                    if lo > 0:
                        P.copy(PC[:, 0:lo], sb[:, 0:lo])
                        yield
                        if var == 0:
                            P.tt(PC[:, lo - 128:lo], PC[:, lo - 128:lo], CMA[:, 16, :], ALU.add)
                            yield
                    P.tt(PC[:, lo:ncols], sb[:, lo:ncols], CMA[:, var, :], ALU.add)
                    yield
                    P.act(PC[:, 0:ncols], PC[:, 0:ncols], AF.Exp, accum_out=RS[:, r:r + 1])
                    yield
                    P.ts(RS[:, r:r + 1], RS[:, r:r + 1], 1.1754944e-38, None, ALU.max)
                    yield
                    P.recip(RS[:, r:r + 1], RS[:, r:r + 1])
                    yield
                    P.stt(IMPP[:, 4:4 + ncols], PC[:, 0:ncols], RS[:, r:r + 1], IMPP[:, 4:4 + ncols], ALU.mult, ALU.add)
                    yield
                P.red(IMPF[:], IMPP.v(IMPP.t[:, 4:516].rearrange("p (j f) -> p j f", f=4)), ALU.add)
                yield
                P.tt(IMPF[:], IMPF[:], IMPP.v(IMPP.t[:, 0:512].rearrange("p (j f) -> p j f", f=4)[:, :, 3]), ALU.add)
                yield
                if 2 * qb + 2 < 128:
                    P.memset(IMPF[:, 2 * qb + 2:128], -1.0)
                    yield
                if qb >= 1:
                    P.ts(IMPF[:, 2 * qb - 1:2 * qb], IMPF[:, 2 * qb - 1:2 * qb], CAB[:, 1:2], None, ALU.max)
                    yield
                if 2 * qb + 1 < 128:
                    P.copy(IMPF[:, 2 * qb + 1:2 * qb + 2], CAB[:, 0:1])
                    yield
                P.memset(IMPF[:, 2 * qb:2 * qb + 1], 1e4)
                yield
                P.memset(IMPF[:, 0:1], 1e4)
                yield
                P.gop("dve", lambda E: E.max(out=M8.t[:, 0:8], in_=IMPF.t[:, :]), [M8], [IMPF])
                yield
                P.gop("dve", lambda E: E.match_replace(out=IMPW.t[:, :], in_to_replace=M8.t[:, 0:8], in_values=IMPF.t[:, :],
                                                       imm_value=-2.0), [IMPW], [M8, IMPF])
                P.gop("dve", lambda E: E.max(out=M8.t[:, 8:16], in_=IMPW.t[:, :]), [M8], [IMPW])
                yield
                P.ts(SEL[:], IMPF[:], M8[:, 15:16], None, ALU.is_ge)
                yield

                ctxs[jq] = dict(qb=qb, k=k, nct=nct, var=var, QT=QT, QTb=QTb, G=G)

        def drainA(g, n):
            if g is None:
                return
            try:
                if n is None:
                    while True:
                        next(g)
                else:
                    for _ in range(n):
                        next(g)
            except StopIteration:
                pass

        drainA(phaseA(0), None)
        for jq in range(NQB):
            c_ = ctxs.pop(jq)
            qb = c_["qb"]; k = c_["k"]; nct = c_["nct"]; var = c_["var"]; QT = c_["QT"]; QTb = c_["QTb"]; G = c_["G"]
            gA = phaseA(jq + 1) if jq + 1 < NQB else None
            tiles = []
            for ct in range(nct):
                last = ct == nct - 1
                if last:
                    mf = lambda PT: P.tt(PT[:], PT[:], _bc(CMT, CMT.t[:, var, :], 1, [128, 2, 128]), ALU.mult)
                elif var == 0 and ct == nct - 2:
                    mf = lambda PT: P.tt(PT[:], PT[:], _bc(CMT, CMT.t[:, 16, :], 1, [128, 2, 128]), ALU.mult)
                else:
                    mf = None
                tiles.append((ct == 0, last, OB[0], KCTb[0:64, ct * 128:(ct + 1) * 128], 0, VCMP[:, ct, :], mf, None))
            kts = [kt for kt in range(qb - 4, qb + 1) if kt >= 0]
            for kt in kts:
                if kt == qb:
                    mf = lambda PT: P.tt(PT[:], PT[:], _bc(TRIU, TRIU.t[:, :], 1, [128, 2, 128]), ALU.mult)
                elif kt == qb - 4:
                    mf = lambda PT: P.tt(PT[:], PT[:], _bc(TRIL, TRIL.t[:, :], 1, [128, 2, 128]), ALU.mult)
                else:
                    mf = None
                tiles.append((kt == kts[0], kt == kts[-1], OB[2], KT[64:128, kt * 128:(kt + 1) * 128], 64, VSW[:, kt, 1, :], mf, None))
            nj = 2 * (qb + 1)
            P.ts(SELN[:, 0:nj], SEL[:, 0:nj], -1.0, 30000.0, ALU.add, ALU.mult, e="pool")
            P.copy(SELXb.v(SELXb.t[:, 0:nj * 64].rearrange("p (j f) -> p j f", f=64)),
                   SELN.v(SELN.t[:, 0:nj].unsqueeze(2).to_broadcast([128, nj, 64])), e="pool")
            for kt in range(qb + 1):
                tiles.append((kt == 0, kt == qb, OB[1], KT[0:64, kt * 128:(kt + 1) * 128], 0, VSW[:, kt, 0, :], None, kt))
            LA = 2
            pend = []
            sbanks = [banks[0], banks[1]]
            mbanks = [banks[2], banks[3]]
            for i in range(len(tiles) + LA):
                if i < len(tiles):
                    first, last, ob, kT_ap, part, v1_ap, mf, kt = tiles[i]
                    sb = sbanks[sb_i[0] % 2]; sb_i[0] += 1
                    PT = PTt[pt_i[0] % 4]; pt_i[0] += 1
                    P.mm(sb[:, 0:256], kT_ap, QTb.v(QTb.t[part:part + 64, :, :].rearrange("p a b -> p (a b)")),
                         start=True, stop=(kt is None))
                    if kt is not None:
                        for r in range(2):
                            P.mm(sb[:, r * 128:(r + 1) * 128], SELXb[:, kt * 128:(kt + 1) * 128], IDTb[:], start=False, stop=(r == 1))
                    P.act(PT.v(PT.t[:, :, :].rearrange("p a b -> p (a b)")), sb[:, 0:256], AF.Exp)
                    if kt is not None:
                        if kt == qb:
                            P.tt(PT[:], PT[:], _bc(TRIU, TRIU.t[:, :], 1, [128, 2, 128]), ALU.mult, e="pool")
                    elif mf is not None:
                        mf(PT)
                    pend.append((PT, ob, v1_ap, first, last))
                    drainA(gA, 2)
                j = i - LA
                if j >= 0:
                    PT, ob, v1_ap, first, last = pend[j]
                    for r in range(2):
                        P.mm(banks[6 + r][:, ob * 65:(ob + 1) * 65], PT[:, r, :], v1_ap, start=first, stop=last)
            drainA(gA, None)
            RI = RIt[k]; OUT = OUTt[k]; OT = OTt[k]
            for bi in range(3):
                for r in range(2):
                    ob = banks[6 + r]
                    c0 = bi * 65
                    P.ts(RI[:, bi, r:r + 1], ob[:, c0 + 64:c0 + 65], 1.1754944e-38, None, ALU.max)
                    P.recip(RI[:, bi, r:r + 1], RI[:, bi, r:r + 1])
                    P.tt(RI[:, bi, r:r + 1], RI[:, bi, r:r + 1], G[:, r, bi:bi + 1], ALU.mult)
                    if bi == 0:
                        P.ts(OUT[:, r, :], ob[:, c0:c0 + 64], RI[:, bi, r:r + 1], None, ALU.mult)
                    else:
                        P.stt(OUT[:, r, :], ob[:, c0:c0 + 64], RI[:, bi, r:r + 1], OUT[:, r, :], ALU.mult, ALU.add)
            P.store("sp", yb[jq * 128:(jq + 1) * 128, :], OUT.v(OUT.t[:, :, :].rearrange("p a b -> p (a b)")))
        P.finish()
        print("k3 ninst", P.ninst, "nsem", P.nsem)
    return nc


def k3_inputs(p_b, g, hh, prm, l, TT):
    q = p_b[:TT, 1792 + g * 256: 1792 + (g + 1) * 256].reshape(TT, 4, 64)
    order = [2 * hh, 2 * hh + 1, 2 * (1 - hh), 2 * (1 - hh) + 1]
    q = q[:, order, :].reshape(TT, 256)
    kv = p_b[:TT, 2304:3072].reshape(TT, 6, 2, 64)[:, :, g, :].reshape(TT, 384)
    gt = p_b[:TT, 3072:3096].reshape(TT, 8, 3)[:, g * 4 + 2 * hh: g * 4 + 2 * hh + 2, :].reshape(TT, 6)
    cp = prm["cmp_pos"][l]
    cpos = np.concatenate([cp[0].T, cp[1].T], 0)
    w1 = prm["cmp_w1"][l].reshape(2, 32, 64, 256).transpose(0, 2, 1, 3).reshape(128, 32, 256)
    w2 = prm["cmp_w2"][l].reshape(2, 2, 128, 64).transpose(2, 0, 1, 3)
    d = {"qin": np.ascontiguousarray(q), "gin": np.ascontiguousarray(gt), "kvin": np.ascontiguousarray(kv),
         "qkg": np.ascontiguousarray(prm["qk_norm_g"][l].reshape(1, 256)), "cpos": np.ascontiguousarray(cpos),
         "w1": np.ascontiguousarray(w1), "w2": np.ascontiguousarray(w2)}
    d.update(k3_consts(TT))
    return d


def run_k3(p, prm, l, TT=T):
    key = ("k3", TT)
    if key not in _CACHE:
        _CACHE[key] = build_k3(TT)
    nc = _CACHE[key]
    maps = []
    for ci in range(NCORES):
        b, rem = divmod(ci, 4)
        g, hh = divmod(rem, 2)
        maps.append(k3_inputs(p[b], g, hh, prm, l, TT))
    res = run_bass_kernel_spmd(nc, maps, core_ids=list(range(NCORES)))
    out = np.empty((NB, TT, 512), np.float32)
    for ci in range(NCORES):
        b, rem = divmod(ci, 4)
        g, hh = divmod(rem, 2)
        h0 = g * 4 + 2 * hh
        out[b, :, h0 * 64:(h0 + 2) * 64] = res.results[ci]["yb"]
    return out


def build_k4a():
    nc = _new_nc()
    NT = 16
    dr = lambda n, s, kind="ExternalInput": nc.dram_tensor(n, s, F32, kind=kind).ap()
    x = dr("x", [NT * 128, D]); ya = dr("ya", [NT * 128, 512]); yb = dr("yb", [NT * 128, 512]); pm = dr("pm", [NT * 128, 2048])
    gm = dr("gm", [1, D]); wua = dr("wua", [512, D]); wub = dr("wub", [512, D]); wo = dr("wo", [D, D]); ident = dr("ident", [128, 128])
    x1 = dr("x1", [NT * 128, D], "ExternalOutput")
    with ExitStack() as es, nc.allow_low_precision("bf16 projection operands, fp32 PSUM accumulation"):
        P = Prog2(nc, es)
        IDT = P.tile("IDT", [128, 128]); GMB = P.tile("GMB", [128, D])
        WUA = P.tile("WUA", [128, 4, D]); WUB = P.tile("WUB", [128, 4, D]); WO = P.tile("WO", [128, 8, D])
        P.load("sp", IDT[:], ident[:, :]); P.load("act", GMB[:], gm.partition_broadcast(128))
        P.load("sp", WUA[:], wua.rearrange("(kc kp) n -> kp kc n", kp=128))
        P.load("act", WUB[:], wub.rearrange("(kc kp) n -> kp kc n", kp=128))
        P.load("sp", WO[:, 0:4, :], wo[0:512, :].rearrange("(kc kp) n -> kp kc n", kp=128))
        P.load("act", WO[:, 4:8, :], wo[512:1024, :].rearrange("(kc kp) n -> kp kc n", kp=128))
        WUAb = P.tile("WUAb", [128, 4, D], BF16); WUBb = P.tile("WUBb", [128, 4, D], BF16); WOb = P.tile("WOb", [128, 8, D], BF16)
        P.copy(WUAb[:], WUA[:], e="pool"); P.copy(WUBb[:], WUB[:], e="act")
        P.copy(WOb[:, 0:4, :], WO[:, 0:4, :], e="pool"); P.copy(WOb[:, 4:8, :], WO[:, 4:8, :], e="act")
        banks = [P.tile(f"bank{i}", [128, 512], psum=True) for i in range(8)]
        bi = [0]

        def nb():
            b = banks[bi[0] % 8]; bi[0] += 1
            return b
        Xt = [P.tile(f"X{i}", [128, D]) for i in range(2)]
        YAt = [P.tile(f"YA{i}", [128, 512]) for i in range(2)]
        YBt = [P.tile(f"YB{i}", [128, 512]) for i in range(2)]
        PMt = [P.tile(f"PM{i}", [128, 2048]) for i in range(2)]
        YTt = [P.tile(f"YT{i}", [128, 8, 128], BF16) for i in range(2)]
        MIXt = [P.tile(f"MIX{i}", [128, D]) for i in range(2)]
        TMPt = [P.tile(f"TMP{i}", [128, 512]) for i in range(2)]
        MTt = [P.tile(f"MT{i}", [128, 8, 128], BF16) for i in range(2)]
        for ti in range(NT):
            k = ti % 2
            rs = slice(ti * 128, (ti + 1) * 128)
            X = Xt[k]; YA = YAt[k]; YB = YBt[k]; PM = PMt[k]; YT = YTt[k]; MIX = MIXt[k]; MT = MTt[k]
            P.load("sp", X[:], x[rs, :]); P.load("act", YA[:], ya[rs, :]); P.load("sp", YB[:], yb[rs, :]); P.load("act", PM[:], pm[rs, :])
            for j, Y in enumerate((YA, YB)):
                b = nb()
                for c in range(4):
                    P.tr(b[:, c * 128:(c + 1) * 128], Y[:, c * 128:(c + 1) * 128], IDT[:])
                P.copy(YT.v(YT.t[:, j * 4:(j + 1) * 4, :].rearrange("p a b -> p (a b)")), b[:], e="act" if j else "dve")
            P.act(PM[:], PM[:], AF.Sigmoid)
            for nh in range(2):
                cs = slice(nh * 512, (nh + 1) * 512)
                ba = nb(); bb = nb()
                for kc in range(4):
                    P.mm(ba[:], YT[:, kc, :], WUAb[:, kc, cs], start=(kc == 0), stop=(kc == 3))
                for kc in range(4):
                    P.mm(bb[:], YT[:, 4 + kc, :], WUBb[:, kc, cs], start=(kc == 0), stop=(kc == 3))
                TMP = TMPt[nh]
                P.tt(MIX[:, cs], ba[:], PM[:, nh * 512:(nh + 1) * 512], ALU.mult)
                P.tt(TMP[:], bb[:], PM[:, 1024 + nh * 512:1024 + (nh + 1) * 512], ALU.mult)
                P.tt(MIX[:, cs], MIX[:, cs], TMP[:], ALU.add, e="pool")
            for half in range(2):
                b = nb()
                for c in range(4):
                    kc = half * 4 + c
                    P.tr(b[:, c * 128:(c + 1) * 128], MIX[:, kc * 128:(kc + 1) * 128], IDT[:])
                P.copy(MT.v(MT.t[:, half * 4:(half + 1) * 4, :].rearrange("p a b -> p (a b)")), b[:], e="act" if half else "dve")
            for nh in range(2):
                cs = slice(nh * 512, (nh + 1) * 512)
                b = nb()
                for kc in range(8):
                    P.mm(b[:], MT[:, kc, :], WOb[:, kc, cs], start=(kc == 0), stop=(kc == 7))
                TMP = TMPt[nh]
                P.tt(TMP[:], b[:], GMB[:, cs], ALU.mult)
                P.tt(X[:, cs], X[:, cs], TMP[:], ALU.add, e="pool")
            P.store("sp" if k else "act", x1[rs, :], X[:])
        P.finish()
        print("k4a ninst", P.ninst, "nsem", P.nsem)
    return nc


def run_k4a(xfull, ya, yb, p, gate_mix, prm, l):
    if "k4a" not in _CACHE:
        _CACHE["k4a"] = build_k4a()
    nc = _CACHE["k4a"]
    ident = np.eye(128, dtype=np.float32)
    maps = []
    for i in range(NCORES):
        b, tq = divmod(i, 4)
        ts_ = slice(tq * 2048, (tq + 1) * 2048)
        maps.append({"x": np.ascontiguousarray(xfull[b, ts_]), "ya": np.ascontiguousarray(ya[b, ts_]),
                     "yb": np.ascontiguousarray(yb[b, ts_]), "pm": np.ascontiguousarray(p[b, ts_, 3096:5144]),
                     "gm": gate_mix[b][None].copy(), "wua": prm["w_up_rwkv"][l], "wub": prm["w_up_nsa"][l],
                     "wo": prm["w_out"][l], "ident": ident})
    res = run_bass_kernel_spmd(nc, maps, core_ids=list(range(NCORES)))
    out = np.empty((NB, T, D), np.float32)
    for i in range(NCORES):
        b, tq = divmod(i, 4)
        out[b, tq * 2048:(tq + 1) * 2048] = res.results[i]["x1"]
    return out


def build_k4b():
    nc = _new_nc()
    NT = 16; NE = 16
    dr = lambda n, s, kind="ExternalInput": nc.dram_tensor(n, s, F32, kind=kind).ap()
    x1 = dr("x1", [NT * 128, D]); g = dr("g", [1, D]); sc = dr("sc", [1, D]); sh = dr("sh", [1, D]); gf = dr("gf", [1, D])
    rw = dr("rw", [D, 16]); rb = dr("rb", [1, 16]); ident = dr("ident", [128, 128])
    wg = dr("wg", [NE, D, 512]); wu = dr("wu", [NE, D, 512]); wd = dr("wd", [NE, 512, D])
    xo = dr("xo", [NT * 128, D], "ExternalOutput")
    with ExitStack() as es, nc.allow_low_precision("bf16 expert matmuls, fp32 accumulation"):
        P = Prog2(nc, es)
        IDT = P.tile("IDT", [128, 128]); G2S = P.tile("G2S", [128, D]); SC = P.tile("SC", [128, D]); SH = P.tile("SH", [128, D])
        GF = P.tile("GF", [128, D]); RW = P.tile("RW", [128, 8, 16]); RB = P.tile("RB", [128, 16])
        P.load("sp", IDT[:], ident[:, :]); P.load("act", G2S[:], g.partition_broadcast(128)); P.load("sp", SC[:], sc.partition_broadcast(128))
        P.load("act", SH[:], sh.partition_broadcast(128)); P.load("sp", GF[:], gf.partition_broadcast(128))
        P.load("act", RW[:], rw.rearrange("(kc kp) n -> kp kc n", kp=128)); P.load("sp", RB[:], rb.partition_broadcast(128))
        P.stt(G2S[:], SC[:], 1.0, G2S[:], ALU.add, ALU.mult)
        banks = [P.tile(f"bank{i}", [128, 512], psum=True) for i in range(8)]
        bi = [0]

        def nb():
            b = banks[bi[0] % 8]; bi[0] += 1
            return b
        ACC = [P.tile(f"ACC{t}", [128, D]) for t in range(8)]
        H2T = P.tile("H2T", [128, 8, 1024], BF16)
        CW = P.tile("CW", [128, 8, 16])
        STG = [P.tile(f"STG{i}", [128, 4096]) for i in range(2)]
        WB = [[P.tile(f"WB{m}_{p}", [128, 4096], BF16) for p in range(2)] for m in range(3)]
        H2t = [P.tile(f"H2{i}", [128, D]) for i in range(2)]
        H2Tf = [P.tile(f"H2Tf{i}", [128, 8, 128]) for i in range(2)]
        junk = P.tile("junk", [128, D])
        ST = [P.tile(f"ST{i}", [128, 2]) for i in range(2)]
        SCO = [P.tile(f"SCO{i}", [128, 16]) for i in range(2)]
        SS = [P.tile(f"SS{i}", [128, 4, 4]) for i in range(2)]
        PSm = [P.tile(f"PSm{i}", [128, 4, 6]) for i in range(2)]
        GS = [P.tile(f"GS{i}", [128, 4]) for i in range(2)]
        GMx = [P.tile(f"GMx{i}", [128, 2]) for i in range(2)]
        E2 = [P.tile(f"E2{i}", [128, 4, 4]) for i in range(2)]
        SIL = [P.tile(f"SIL{i}", [128, 512]) for i in range(2)]
        HID = [P.tile(f"HID{i}", [128, 4, 512], BF16) for i in range(2)]
        stg_i = [0]
        for tg in range(2):
            for t in range(8):
                ti = tg * 8 + t
                k = t % 2
                A = ACC[t]
                P.load("sp" if k else "act", A[:], x1[ti * 128:(ti + 1) * 128, :])
                S = ST[k]; H2 = H2t[k]; HF = H2Tf[k]
                P.act(junk[:], A[:], AF.Square, accum_out=S[:, 0:1])
                P.ts(S[:, 1:2], S[:, 0:1], 1.0 / D, 1e-6, ALU.mult, ALU.add)
                P.act(S[:, 1:2], S[:, 1:2], AF.Sqrt)
                P.recip(S[:, 1:2], S[:, 1:2])
                P.stt(H2[:], A[:], S[:, 1:2], G2S[:], ALU.mult, ALU.mult)
                P.tt(H2[:], H2[:], SH[:], ALU.add, e="pool")
                for half in range(2):
                    b = nb()
                    for c in range(4):
                        kc = half * 4 + c
                        P.tr(b[:, c * 128:(c + 1) * 128], H2[:, kc * 128:(kc + 1) * 128], IDT[:])
                    bv = b.v(b.t[:, :].rearrange("p (a b) -> p a b", a=4))
                    P.copy(HF[:, half * 4:(half + 1) * 4, :], bv)
                    P.copy(H2T[:, half * 4:(half + 1) * 4, t * 128:(t + 1) * 128], bv, e="act")
                b = nb()
                for kc in range(8):
                    P.mm(b[:, 0:16], HF[:, kc, :], RW[:, kc, :], start=(kc == 0), stop=(kc == 7))
                sco = SCO[k]; ss = SS[k]; psm = PSm[k]; gs = GS[k]; gmx = GMx[k]; e2 = E2[k]
                P.act(sco[:], b[:, 0:16], AF.Sigmoid)
                ssf = ss.v(ss.t[:, :, :].rearrange("p a b -> p (a b)"))
                P.tt(ssf, sco[:], RB[:], ALU.add)
                P.tt(psm[:, :, 0:3], ss[:, :, 0:3], ss[:, :, 1:4], ALU.add)
                P.tt(psm[:, :, 3:5], ss[:, :, 0:2], ss[:, :, 2:4], ALU.add)
                P.tt(psm[:, :, 5:6], ss[:, :, 0:1], ss[:, :, 3:4], ALU.add)
                P.red(gs[:], psm[:], ALU.max)
                P.red(gmx[:, 0:1], gs[:], ALU.max)
                P.ts(gs[:], gs[:], gmx[:, 0:1], None, ALU.is_ge)
                P.tt(psm[:, :, 0:3], ss[:, :, 0:3], ss[:, :, 1:4], ALU.min)
                P.tt(psm[:, :, 3:5], ss[:, :, 0:2], ss[:, :, 2:4], ALU.min)
                P.tt(psm[:, :, 5:6], ss[:, :, 0:1], ss[:, :, 3:4], ALU.min)
                P.red(e2[:, :, 0], psm[:], ALU.max)
                thr = E2[k].v(E2[k].t[:, :, 0:1].to_broadcast([128, 4, 4]))
                P.tt(psm[:, :, 0:4], ss[:], thr, ALU.is_ge)
                P.tt(psm[:, :, 0:4], psm[:, :, 0:4], _bc(gs, gs.t[:, :], 2, [128, 4, 4]), ALU.mult)
                scv = sco.v(sco.t[:, :].rearrange("p (a b) -> p a b", b=4))
                P.tt(e2[:], psm[:, :, 0:4], scv, ALU.mult)
                P.red(gmx[:, 1:2], E2[k].v(E2[k].t[:, :, :].rearrange("p a b -> p (a b)")), ALU.add)
                P.recip(gmx[:, 1:2], gmx[:, 1:2])
                P.ts(CW[:, t, :], E2[k].v(E2[k].t[:, :, :].rearrange("p a b -> p (a b)")), gmx[:, 1:2], None, ALU.mult)
            for e in range(NE):
                p_ = e % 2
                for m, (wsrc, pat) in enumerate(((wg, 8), (wu, 8), (wd, 4))):
                    S_ = STG[stg_i[0] % 2]; stg_i[0] += 1
                    sv = S_.v(S_.t[:, :].rearrange("p (kc n) -> p kc n", kc=pat))
                    P.load("sp" if m % 2 == 0 else "act", sv, wsrc[e].rearrange("(kc kp) n -> kp kc n", kp=128))
                    Wb = WB[m][p_]
                    if m < 2:
                        P.copy(Wb[:], S_[:], e="pool")
                    else:
                        P.tt(Wb.v(Wb.t[:, :].rearrange("p (kc n) -> p kc n", kc=4)), sv, _bc(GF, GF.t[:, :], 1, [128, 4, D]), ALU.mult, e="pool")
                WG = WB[0][p_].v(WB[0][p_].t[:, :].rearrange("p (kc n) -> p kc n", kc=8))
                WU = WB[1][p_].v(WB[1][p_].t[:, :].rearrange("p (kc n) -> p kc n", kc=8))
                WD = WB[2][p_].v(WB[2][p_].t[:, :].rearrange("p (kc n) -> p kc n", kc=4))
                for sub in range(2):
                    hid = HID[sub]
                    tsl = slice(sub * 512, (sub + 1) * 512)
                    for c in range(4):
                        bg = nb(); bu = nb()
                        for kc in range(8):
                            P.mm(bg[:], WG[:, kc, c * 128:(c + 1) * 128], H2T[:, kc, tsl], start=(kc == 0), stop=(kc == 7))
                        for kc in range(8):
                            P.mm(bu[:], WU[:, kc, c * 128:(c + 1) * 128], H2T[:, kc, tsl], start=(kc == 0), stop=(kc == 7))
                        sil = SIL[c % 2]
                        P.act(sil[:], bg[:], AF.Silu)
                        P.tt(hid[:, c, :], sil[:], bu[:], ALU.mult)
                    for t4 in range(4):
                        t = sub * 4 + t4
                        for nh in range(2):
                            cs = slice(nh * 512, (nh + 1) * 512)
                            bd = nb()
                            for c in range(4):
                                P.mm(bd[:], hid[:, c, t4 * 128:(t4 + 1) * 128], WD[:, c, cs], start=(c == 0), stop=(c == 3))
                            P.stt(ACC[t][:, cs], bd[:], CW[:, t, e:e + 1], ACC[t][:, cs], ALU.mult, ALU.add)
            for t in range(8):
                ti = tg * 8 + t
                P.store("sp" if t % 2 else "act", xo[ti * 128:(ti + 1) * 128, :], ACC[t][:])
        P.finish()
        print("k4b ninst", P.ninst, "nsem", P.nsem)
    return nc


def run_k4b(x1, g2, sc2, sh2, gate_ffn, prm, l):
    if "k4b" not in _CACHE:
        _CACHE["k4b"] = build_k4b()
    nc = _CACHE["k4b"]
    ident = np.eye(128, dtype=np.float32)
    maps = []
    for i in range(NCORES):
        b, tq = divmod(i, 4)
        ts_ = slice(tq * 2048, (tq + 1) * 2048)
        maps.append({"x1": np.ascontiguousarray(x1[b, ts_]), "g": g2[None].copy(), "sc": sc2[b][None].copy(),
                     "sh": sh2[b][None].copy(), "gf": gate_ffn[b][None].copy(), "rw": prm["router_w"],
                     "rb": prm["router_b"][None].copy(), "ident": ident,
                     "wg": prm["exp_w_gate"][l], "wu": prm["exp_w_up"][l], "wd": prm["exp_w_down"][l]})
    res = run_bass_kernel_spmd(nc, maps, core_ids=list(range(NCORES)))
    out = np.empty((NB, T, D), np.float32)
    for i in range(NCORES):
        b, tq = divmod(i, 4)
        out[b, tq * 2048:(tq + 1) * 2048] = res.results[i]["xo"]
    return out


def kernel(**inputs):
    prm = {k: np.asarray(v) for k, v in inputs.items()}
    x = prm["x"].astype(np.float32, copy=False)
    mod = run_k0(prm["c"], prm["w_ada"], prm["b_ada"])
    for l in range(2):
        sh1, sc1, gate_mix, sh2, sc2, gate_ffn = [np.ascontiguousarray(a) for a in np.split(mod[l], 6, axis=-1)]
        p = run_k1(x, prm["norm_g"][l, 0], sc1, sh1, prm["w_in"][l], prm["b_in"][l])
        ya = run_k2(p, prm, l)
        yb = run_k3(p, prm, l)
        x1 = run_k4a(x, ya, yb, p, gate_mix, prm, l)
        x = run_k4b(x1, prm["norm_g"][l, 1], sc2, sh2, gate_ffn, prm, l)
    return x
```

```python
import numpy as np
import concourse.bass as bass
import concourse.mybir as mybir
from contextlib import ExitStack

F32 = mybir.dt.float32
BF16 = mybir.dt.bfloat16
I32 = mybir.dt.int32
U32 = mybir.dt.uint32
AF = mybir.ActivationFunctionType
ALU = mybir.AluOpType
AX = mybir.AxisListType


class Buf:
    __slots__ = ("name", "w", "r", "dsem", "dcnt", "t", "excl")

    def __init__(self, name, t=None):
        self.name = name
        self.w = None
        self.r = []
        self.dsem = None
        self.dcnt = 0
        self.t = t
        self.excl = False


class Prog:
    ENG = ("pe", "dve", "act", "pool", "sp")

    def __init__(self, nc, es: ExitStack):
        self.nc = nc
        self.es = es
        self.eng = {"pe": nc.tensor, "dve": nc.vector, "act": nc.scalar,
                    "pool": nc.gpsimd, "sp": nc.sync}
        self.sem = {}
        self.cnt = {}
        for e in self.ENG:
            self.sem[e] = es.enter_context(nc.semaphore("s_" + e))
            self.cnt[e] = 0
        self.waited = {e: {} for e in self.ENG}
        self.out_tokens = []
        self.nsem = 5
        self.ninst = 0

    def sb(self, name, shape, dt=F32):
        t = self.es.enter_context(self.nc.sbuf_tensor(name, list(shape), dt))
        return t

    def ps(self, name, shape, dt=F32):
        t = self.es.enter_context(self.nc.psum_tensor(name, list(shape), dt))
        return t

    def buf(self, name, t=None):
        return Buf(name, t)

    def _wait(self, e, deps):
        w = self.waited[e]
        best = {}
        for d in deps:
            if d is None:
                continue
            sem, val, pe = d
            if e == "pe" and pe == "pe":
                continue
            k = id(sem)
            if w.get(k, 0) >= val:
                continue
            if k not in best or best[k][1] < val:
                best[k] = (sem, val)
        for k, (sem, val) in best.items():
            self.eng[e].wait_ge(sem, val)
            w[k] = val
            self.ninst += 1

    def op(self, e, build, reads=(), writes=()):
        deps = []
        for b in reads:
            deps.append(b.w)
            if b.excl:
                deps.extend(r for r in b.r if r[2] != e)
        for b in writes:
            deps.append(b.w)
            deps.extend(b.r)
        self._wait(e, deps)
        ins = build(self.eng[e])
        self.cnt[e] += 1
        ins.then_inc(self.sem[e], 1)
        tok = (self.sem[e], self.cnt[e], e)
        self.ninst += 1
        for b in reads:
            b.r.append(tok)
            if len(b.r) > 64:
                b.r = self._compact(b.r)
        for b in writes:
            b.w = tok
            b.r = []
        return tok

    @staticmethod
    def _compact(rs):
        best = {}
        for sem, val, e in rs:
            k = id(sem)
            if k not in best or best[k][1] < val:
                best[k] = (sem, val, e)
        return list(best.values())

    def dma(self, q, out, in_, sbuf_buf, reads=(), writes=(), is_output=False, **kw):
        deps = []
        for b in reads:
            deps.append(b.w)
        for b in writes:
            deps.append(b.w)
            deps.extend(b.r)
        self._wait(q, deps)
        b0 = sbuf_buf
        if b0.dsem is None:
            b0.dsem = self.es.enter_context(self.nc.semaphore("d_" + b0.name))
            self.nsem += 1
        ins = self.eng[q].dma_start(out=out, in_=in_, **kw)
        b0.dcnt += 16
        ins.then_inc(b0.dsem, 16)
        tok = (b0.dsem, b0.dcnt, "dma")
        self.ninst += 1
        for b in reads:
            b.r.append(tok)
        for b in writes:
            b.w = tok
            b.r = []
        if is_output:
            self.out_tokens.append(tok)
        return tok

    def finish(self):
        self._wait("sp", self.out_tokens)
        deps = [(self.sem[e], self.cnt[e], e) for e in self.ENG if e != "sp" and self.cnt[e] > 0]
        self._wait("sp", deps)


class View:
    __slots__ = ("tile", "ap")

    def __init__(self, tile, ap):
        self.tile = tile
        self.ap = ap

    def __getitem__(self, idx):
        return View(self.tile, self.ap[idx])

    @property
    def t(self):
        return self.ap

    def v(self, ap):
        return View(self.tile, ap)


class Tile:
    def __init__(self, P, name, shape, dt=F32, psum=False):
        self.P = P
        self.name = name
        self.t = (P.ps if psum else P.sb)(name, shape, dt)
        self.buf = Buf(name)
        self.buf.excl = bool(psum)
        self.shape = list(shape)

    def __getitem__(self, idx):
        return View(self, self.t[idx])

    def v(self, ap):
        return View(self, ap)


def _bufs(views):
    out = []
    for v in views:
        if isinstance(v, View):
            if v.tile.buf not in out:
                out.append(v.tile.buf)
        elif isinstance(v, Tile):
            if v.buf not in out:
                out.append(v.buf)
    return out


def _ap(v):
    return v.ap if isinstance(v, View) else v


class Prog2(Prog):
    def tile(self, name, shape, dt=F32, psum=False):
        return Tile(self, name, shape, dt, psum)

    def gop(self, e, fn, outs, ins):
        return self.op(e, fn, reads=_bufs(ins), writes=_bufs(outs))

    def act(self, out, in_, func, bias=0.0, scale=1.0, accum_out=None, e="act"):
        ins = [in_, bias, scale]
        outs = [out] + ([accum_out] if accum_out is not None else [])
        kw = {}
        if accum_out is not None:
            kw["accum_out"] = _ap(accum_out)
        return self.gop(e, lambda E: E.activation(out=_ap(out), in_=_ap(in_), func=func,
                                                  bias=_ap(bias), scale=_ap(scale), **kw), outs, ins)

    def tt(self, out, a, b, op, e="dve"):
        return self.gop(e, lambda E: E.tensor_tensor(out=_ap(out), in0=_ap(a), in1=_ap(b), op=op), [out], [a, b])

    def ts(self, out, a, s1, s2, op0, op1=None, accum_out=None, e="dve"):
        kw = {}
        if op1 is not None:
            kw["op1"] = op1
        outs = [out]
        if accum_out is not None:
            kw["accum_out"] = _ap(accum_out)
            outs.append(accum_out)
        return self.gop(e, lambda E: E.tensor_scalar(out=_ap(out), in0=_ap(a), scalar1=_ap(s1),
                                                     scalar2=_ap(s2) if s2 is not None else None,
                                                     op0=op0, **kw), outs, [a, s1, s2])

    def stt(self, out, in0, scalar, in1, op0, op1, accum_out=None, e="dve"):
        kw = {}
        outs = [out]
        if accum_out is not None:
            kw["accum_out"] = _ap(accum_out)
            outs.append(accum_out)
        return self.gop(e, lambda E: E.scalar_tensor_tensor(out=_ap(out), in0=_ap(in0), scalar=_ap(scalar),
                                                            in1=_ap(in1), op0=op0, op1=op1, **kw),
                        outs, [in0, scalar, in1])

    def copy(self, out, in_, e="dve"):
        if e == "act":
            return self.gop(e, lambda E: E.copy(out=_ap(out), in_=_ap(in_)), [out], [in_])
        return self.gop(e, lambda E: E.tensor_copy(out=_ap(out), in_=_ap(in_)), [out], [in_])

    def memset(self, out, val, e="dve"):
        return self.gop(e, lambda E: E.memset(_ap(out), val), [out], [])

    def red(self, out, in_, op, axis=AX.X, e="dve"):
        return self.gop(e, lambda E: E.tensor_reduce(out=_ap(out), in_=_ap(in_), axis=axis, op=op), [out], [in_])

    def recip(self, out, in_):
        return self.gop("dve", lambda E: E.reciprocal(out=_ap(out), in_=_ap(in_)), [out], [in_])

    def mm(self, out, lhsT, rhs, start=True, stop=True):
        return self.gop("pe", lambda E: E.matmul(_ap(out), _ap(lhsT), _ap(rhs), start=start, stop=stop),
                        [out], [lhsT, rhs])

    def tr(self, out, in_, ident):
        return self.gop("pe", lambda E: E.transpose(_ap(out), _ap(in_), _ap(ident)), [out], [in_, ident])

    def load(self, q, view, dram_ap, **kw):
        return self.dma(q, view.ap, dram_ap, view.tile.buf, writes=[view.tile.buf], **kw)

    def store(self, q, dram_ap, view, is_output=True, **kw):
        return self.dma(q, dram_ap, view.ap, view.tile.buf, reads=[view.tile.buf], is_output=is_output, **kw)


from concourse.bass_utils import run_bass_kernel_spmd

D = 1024
T = 8192
NB = 2
IN_COLS = 5144
NCORES = 8
_CACHE = {}


def _new_nc():
    return bass.Bass("TRN2", target_bir_lowering=False)


def build_k0():
    nc = _new_nc()
    NCOL = 1536
    cT = nc.dram_tensor("cT", [128, 8, 2], F32, kind="ExternalInput").ap()
    w = nc.dram_tensor("w", [1024, NCOL], F32, kind="ExternalInput").ap()
    b = nc.dram_tensor("b", [1, NCOL], F32, kind="ExternalInput").ap()
    y = nc.dram_tensor("y", [2, NCOL], F32, kind="ExternalOutput").ap()
    with ExitStack() as es:
        P = Prog2(nc, es)
        ct = P.tile("ct", [128, 8, 2])
        cs = P.tile("cs", [128, 8, 2])
        wt = P.tile("wt", [128, 8, NCOL])
        bt = P.tile("bt", [2, NCOL])
        yt = P.tile("yt", [2, NCOL])
        P.load("sp", ct[:], cT[:, :, :])
        P.load("act", wt[:], w.rearrange("(kc kp) n -> kp kc n", kp=128))
        P.load("sp", bt[:], b.partition_broadcast(2))
        P.act(cs[:], ct[:], AF.Silu)
        for n in range(3):
            ps = P.tile(f"ps{n}", [2, 512], psum=True)
            for kc in range(8):
                P.mm(ps[:], cs[:, kc, :], wt[:, kc, n * 512:(n + 1) * 512], start=(kc == 0), stop=(kc == 7))
            P.tt(yt[:, n * 512:(n + 1) * 512], ps[:], bt[:, n * 512:(n + 1) * 512], ALU.add)
        P.store("sp", y[:, :], yt[:])
        P.finish()
    return nc


def run_k0(c, w_ada, b_ada):
    if "k0" not in _CACHE:
        _CACHE["k0"] = build_k0()
    nc = _CACHE["k0"]
    L = w_ada.shape[0]
    wcat = np.concatenate([w_ada[l] for l in range(L)], axis=1)
    bcat = np.concatenate([b_ada[l] for l in range(L)], axis=0)[None]
    cT = np.ascontiguousarray(c.T.reshape(8, 128, 2).transpose(1, 0, 2))
    maps = []
    for i in range(NCORES):
        sl = slice(i * 1536, (i + 1) * 1536)
        maps.append({"cT": cT, "w": np.ascontiguousarray(wcat[:, sl]), "b": np.ascontiguousarray(bcat[:, sl])})
    res = run_bass_kernel_spmd(nc, maps, core_ids=list(range(NCORES)))
    y = np.concatenate([r["y"] for r in res.results], axis=1)
    return y.reshape(2, L, 6144).transpose(1, 0, 2)


def build_k1():
    nc = _new_nc()
    NT = 16
    x = nc.dram_tensor("x", [NT * 128, D], F32, kind="ExternalInput").ap()
    g = nc.dram_tensor("g", [1, D], F32, kind="ExternalInput").ap()
    sc = nc.dram_tensor("sc", [1, D], F32, kind="ExternalInput").ap()
    sh = nc.dram_tensor("sh", [1, D], F32, kind="ExternalInput").ap()
    w = nc.dram_tensor("w", [D, IN_COLS], F32, kind="ExternalInput").ap()
    b = nc.dram_tensor("b", [1, IN_COLS], F32, kind="ExternalInput").ap()
    ident = nc.dram_tensor("ident", [128, 128], F32, kind="ExternalInput").ap()
    p = nc.dram_tensor("p", [NT * 128, IN_COLS], F32, kind="ExternalOutput").ap()
    with ExitStack() as es, nc.allow_low_precision("bf16 in-proj operands, fp32 PSUM accumulation"):
        P = Prog2(nc, es)
        idt = P.tile("idt", [128, 128])
        G = P.tile("G", [128, D]); SC = P.tile("SC", [128, D]); SH = P.tile("SH", [128, D])
        B = P.tile("B", [128, IN_COLS])
        hT = [P.tile(f"hT{i}", [128, 8, 128], BF16) for i in range(NT)]
        P.load("sp", idt[:], ident[:, :])
        P.load("sp", G[:], g.partition_broadcast(128))
        P.load("act", SC[:], sc.partition_broadcast(128))
        P.load("sp", SH[:], sh.partition_broadcast(128))
        P.load("act", B[:], b.partition_broadcast(128))
        P.stt(G[:], SC[:], 1.0, G[:], ALU.add, ALU.mult)
        xt = [P.tile(f"xt{i}", [128, D]) for i in range(2)]
        junk = P.tile("junk", [128, D])
        st = [P.tile(f"st{i}", [128, 2]) for i in range(2)]
        pT = [P.tile(f"pT{i}", [128, 4, 128], psum=True) for i in range(2)]
        for ti in range(NT):
            X = xt[ti % 2]; S = st[ti % 2]
            P.load("sp" if ti % 2 == 0 else "act", X[:], x[ti * 128:(ti + 1) * 128, :])
            P.act(junk[:], X[:], AF.Square, accum_out=S[:, 0:1])
            P.ts(S[:, 1:2], S[:, 0:1], 1.0 / D, 1e-6, ALU.mult, ALU.add)
            P.act(S[:, 1:2], S[:, 1:2], AF.Sqrt)
            P.recip(S[:, 1:2], S[:, 1:2])
            P.stt(X[:], X[:], S[:, 1:2], G[:], ALU.mult, ALU.mult)
            P.tt(X[:], X[:], SH[:], ALU.add)
            for half in range(2):
                pt = pT[half]
                for j in range(4):
                    kc = half * 4 + j
                    P.tr(pt[:, j, :], X[:, kc * 128:(kc + 1) * 128], idt[:])
                P.copy(hT[ti][:, half * 4:(half + 1) * 4, :], pt[:], e="act" if half else "dve")
        NCH = (IN_COLS + 511) // 512
        wt = [P.tile(f"wt{i}", [128, 8, 512]) for i in range(2)]
        wb = [P.tile(f"wb{i}", [128, 8, 512], BF16) for i in range(2)]
        po = [P.tile(f"po{i}", [128, 512], psum=True) for i in range(4)]
        ot = [P.tile(f"ot{i}", [128, 512]) for i in range(4)]
        wv = w.rearrange("(kc kp) n -> kp kc n", kp=128)
        k = 0
        for ci in range(NCH):
            c0 = ci * 512
            cw = min(512, IN_COLS - c0)
            W = wt[ci % 2]
            P.load("sp" if ci % 2 == 0 else "act", W[:, :, :cw], wv[:, :, c0:c0 + cw])
            Wb = wb[ci % 2]
            P.copy(Wb[:, 0:4, :cw], W[:, 0:4, :cw], e="pool")
            P.copy(Wb[:, 4:8, :cw], W[:, 4:8, :cw], e="act")
            for ti in range(NT):
                ps = po[k % 4]; o = ot[k % 4]
                for kc in range(8):
                    P.mm(ps[:, :cw], hT[ti][:, kc, :], Wb[:, kc, :cw], start=(kc == 0), stop=(kc == 7))
                P.tt(o[:, :cw], ps[:, :cw], B[:, c0:c0 + cw], ALU.add, e="dve")
                P.store("sp" if k % 2 == 0 else "act", p[ti * 128:(ti + 1) * 128, c0:c0 + cw], o[:, :cw])
                k += 1
        P.finish()
    return nc


def run_k1(xfull, g, sc, sh, w, b):
    if "k1" not in _CACHE:
        _CACHE["k1"] = build_k1()
    nc = _CACHE["k1"]
    ident = np.eye(128, dtype=np.float32)
    maps = []
    for i in range(NCORES):
        bi, tq = divmod(i, 4)
        maps.append({"x": np.ascontiguousarray(xfull[bi, tq * 2048:(tq + 1) * 2048]),
                     "g": g[None].copy(), "sc": sc[bi][None].copy(), "sh": sh[bi][None].copy(),
                     "w": w, "b": b[None].copy(), "ident": ident})
    res = run_bass_kernel_spmd(nc, maps, core_ids=list(range(NCORES)))
    out = np.empty((NB, T, IN_COLS), np.float32)
    for i in range(NCORES):
        bi, tq = divmod(i, 4)
        out[bi, tq * 2048:(tq + 1) * 2048] = res.results[i]["p"]
    return out


def _bc(tile, ap, axis, shape):
    return tile.v(ap.unsqueeze(axis).to_broadcast(list(shape)))


def k2_consts():
    C = 64
    tri_incl = np.triu(np.ones((C, C), np.float32))
    tri_strict = np.triu(np.ones((C, C), np.float32), 1)
    m = np.concatenate([tri_strict, tri_incl], 1)
    mask128 = np.concatenate([m, m], 0)
    sel0 = np.concatenate([np.eye(64, dtype=np.float32), np.zeros((64, 64), np.float32)], 1)
    shift = np.concatenate([np.zeros((64, 64), np.float32), np.eye(64, dtype=np.float32)], 1)
    return {"ident": np.eye(128, dtype=np.float32), "mask128": mask128,
            "trils": np.ascontiguousarray(tri_strict.T), "tri": tri_incl,
            "ones64": np.ones((64, 64), np.float32), "sel0": sel0, "shift": shift}


def build_k2(TT=T, stop=0, ustop=99):
    nc = _new_nc()
    NG = TT // 256
    dr = lambda n, s, kind="ExternalInput": nc.dram_tensor(n, s, F32, kind=kind).ap()
    pp = dr("pp", [TT + 1, 640]); mu = dr("mu", [1, 640]); vec = dr("vec", [8, 128])
    w2 = dr("w2", [64, 128]); a2 = dr("a2", [64, 128]); g2 = dr("g2", [128, 128])
    ident = dr("ident", [128, 128]); mask128 = dr("mask128", [128, 128]); trils = dr("trils", [64, 64])
    tri = dr("tri", [64, 64]); ones64 = dr("ones64", [64, 64]); sel0 = dr("sel0", [64, 128]); shift = dr("shift", [64, 128])
    ya = dr("ya", [TT, 128], "ExternalOutput")
    with ExitStack() as es:
        P = Prog2(nc, es)
        IDT = P.tile("IDT", [128, 128]); MASK = P.tile("MASK", [128, 128]); TRILS = P.tile("TRILS", [64, 64])
        TRI = P.tile("TRI", [64, 64]); ONES = P.tile("ONES", [64, 64]); SEL0 = P.tile("SEL0", [64, 128]); SHIFT = P.tile("SHIFT", [64, 128])
        W2 = P.tile("W2", [64, 128]); A2 = P.tile("A2", [64, 128]); G2 = P.tile("G2", [128, 128])
        MU = P.tile("MU", [64, 640]); VEC = P.tile("VEC", [64, 8, 128])
        qs = ["sp", "act"]
        for i, (tl, d) in enumerate([(IDT, ident), (MASK, mask128), (TRILS, trils), (TRI, tri), (ONES, ones64),
                                     (SEL0, sel0), (SHIFT, shift), (W2, w2), (A2, a2), (G2, g2)]):
            P.load(qs[i % 2], tl[:], d[:, :])
        P.load("sp", MU[:], mu.partition_broadcast(64))
        for r in range(7):
            P.load(qs[r % 2], VEC[:, r, :], vec[r:r + 1, :].partition_broadcast(64))
        I64 = IDT[0:64, 0:64]
        S4 = [64, 4, 128]
        vb = lambda r: _bc(VEC, VEC.t[:, r, :], 1, S4)
        W0b, A0b, KKb, KAb, RKb, GNGb, GNBb = [vb(r) for r in range(7)]
        banks = [P.tile(f"bank{i}", [128, 512], psum=True) for i in range(8)]
        pk = [0]

        def pbank():
            b = banks[pk[0] % 4]
            pk[0] += 1
            return b

        def t4(name, n=2):
            return [P.tile(f"{name}{i}", S4) for i in range(n)]

        CURt = [P.tile(f"CUR{i}", [64, 4, 640]) for i in range(2)]
        PRVt = [P.tile(f"PRV{i}", [64, 4, 640]) for i in range(2)]
        TWt = t4("TW"); SGt = t4("SG"); LTt = [P.tile(f"LT{i}", [64, 4, 2, 64]) for i in range(2)]
        GTtt = [P.tile(f"GTt{i}", [128, 4, 64]) for i in range(2)]
        LDt = t4("LD"); At = t4("A"); Ggt = t4("Gg"); KKt = t4("KK"); SQt = t4("SQ"); T1t = t4("T1"); KMt = t4("KM"); Bvt = t4("Bv")
        SSt = [P.tile(f"SS{i}", [64, 8]) for i in range(2)]; RKt = [P.tile(f"RK{i}", [64, 8]) for i in range(2)]
        Lst = t4("Ls"); ELt = t4("EL"); ENLt = t4("ENL"); TMPt = t4("TMP"); TMP2t = t4("TMP2")
        DCFt = [P.tile(f"DCF{i}", [64, 8]) for i in range(2)]
        A_t = t4("A_"); BTt = t4("BT"); KTt = t4("KT"); RTt = t4("RT"); BBt = t4("BB"); KBt = t4("KB")
        FMBKt = [P.tile(f"FMBK{i}", [64, 8, 128]) for i in range(2)]
        FMARt = [P.tile(f"FMAR{i}", [64, 8, 128]) for i in range(2)]
        BKSt = [P.tile(f"BKS{i}", [128, 4, 128]) for i in range(2)]
        VVt = [P.tile(f"VV{i}", [128, 4, 128]) for i in range(2)]
        YBt = t4("YB"); YCt = t4("YC"); MNt = [P.tile(f"MN{i}", [64, 8]) for i in range(2)]; VRt = [P.tile(f"VR{i}", [64, 8]) for i in range(2)]
        hk = [0, 0]
        ubi = [0]; hkk = [0]
        BIGA = P.tile("BIGA", [128, 8, 128])
        XA = [P.tile(f"XA{i}", [64, 8, 64]) for i in range(2)]
        XTA = [P.tile(f"XTA{i}", [64, 8, 64]) for i in range(2)]
        PMA = [P.tile(f"PMA{i}", [64, 8, 64]) for i in range(2)]
        W2A = P.tile("W2A", [64, 8, 64]); AHA = P.tile("AHA", [64, 8, 64]); RHA = P.tile("RHA", [64, 8, 64])
        GTA = P.tile("GTA", [64, 8, 64]); TMPG = P.tile("TMPG", [64, 8, 64])
        HA = [P.tile(f"HA{i}", [64, 2, 64]) for i in range(2)]
        P.memset(HA[0][:], 0.0)
        fl = lambda tl: tl.v(tl.t[:, :, :].rearrange("p a b -> p (a b)"))
        v8 = lambda tl: tl.v(tl.t[:, :, :].rearrange("p a (h b) -> p (a h) b", b=64))
        b8 = lambda tl: _bc(tl, tl.t[:, :], 2, [64, 8, 64])
        ppv = lambda lo: pp[lo:lo + 256, :].rearrange("(c t) n -> t c n", t=64)
        NEG = -float(np.exp(-0.5))

        class _Stop(Exception):
            pass

        def early(n, view):
            if stop == n:
                P.store("sp", ya[0:256, :].rearrange("(c t) n -> t c n", t=64), view)
                raise _Stop()

        ctxp = {}

        def prepG(g):
            k = g % 2
            CUR = CURt[k]; PRV = PRVt[k]
            P.load("sp", CUR[:], ppv(1 + g * 256))
            yield
            P.load("act", PRV[:], ppv(g * 256))
            yield
            P.tt(PRV[:], PRV[:], CUR[:], ALU.subtract)
            yield
            P.tt(PRV[:], PRV[:], _bc(MU, MU.t[:, :], 1, [64, 4, 640]), ALU.mult)
            yield
            P.tt(CUR[:], CUR[:], PRV[:], ALU.add, e="pool")
            yield
            Rr = CUR[:, :, 0:128]; Kr = CUR[:, :, 128:256]; Vr = CUR[:, :, 256:384]
            TW = TWt[k]; SG = SGt[k]; LT = LTt[k]; GTt = GTtt[k]
            P.act(TW[:, :, 0:64], CUR[:, :, 384:448], AF.Tanh)
            yield
            P.act(SG[:], CUR[:, :, 512:640], AF.Sigmoid)
            yield
            b1 = pbank(); b1v = b1.v(b1.t[0:64, :].rearrange("p (c w t) -> p c w t", c=4, w=2))
            for c in range(4):
                P.tr(b1.v(b1v.ap[:, c, 0, :]), TW[:, c, 0:64], I64)
                yield
                P.tr(b1.v(b1v.ap[:, c, 1, :]), CUR[:, c, 448:512], I64)
                yield
            P.copy(LT[:], b1v)
            yield
            b2 = pbank(); b2v = b2.v(b2.t[:, 0:256].rearrange("p (c t) -> p c t", c=4))
            for c in range(4):
                P.tr(b2.v(b2v.ap[:, c, :]), SG[:, c, :], I64)
                yield
            P.copy(GTt[:], b2v, e="act")
            yield
            bw = pbank(); bwv = bw.v(bw.t[0:64, :].rearrange("p (c n) -> p c n", c=4))
            for c in range(4):
                P.mm(bw.v(bwv.ap[:, c, :]), LT[:, c, 0, :], W2[:])
                yield
            LD = LDt[k]
            P.tt(LD[:], bwv, W0b, ALU.add)
            yield
            ba = pbank(); bav = ba.v(ba.t[0:64, :].rearrange("p (c n) -> p c n", c=4))
            for c in range(4):
                P.mm(ba.v(bav.ap[:, c, :]), LT[:, c, 1, :], A2[:])
                yield
            A = At[k]
            P.tt(A[:], bav, A0b, ALU.add)
            yield
            bg = pbank(); bgv = bg.v(bg.t[0:64, :].rearrange("p (c n) -> p c n", c=4))
            for c in range(4):
                P.mm(bg.v(bgv.ap[:, c, :]), GTt[:, c, :], G2[:])
                yield
            Gg = Ggt[k]
            P.copy(Gg[:], bgv, e="act")
            yield
            P.act(LD[:], LD[:], AF.Sigmoid)
            yield
            P.act(A[:], A[:], AF.Sigmoid)
            yield
            P.ts(LD[:], LD[:], NEG, None, ALU.mult, e="pool")
            yield
            KK = KKt[k]; SQ = SQt[k]; SS = SSt[k]; T1 = T1t[k]; KM = KMt[k]; Bv = Bvt[k]; RK = RKt[k]
            P.tt(KK[:], Kr, KKb, ALU.mult)
            yield
            P.tt(SQ[:], KK[:], KK[:], ALU.mult)
            yield
            P.red(SS[:], v8(SQ), ALU.add)
            yield
            P.act(SS[:], SS[:], AF.Sqrt)
            yield
            P.ts(SS[:], SS[:], 1e-12, None, ALU.max)
            yield
            P.recip(SS[:], SS[:])
            yield
            P.tt(v8(KK), v8(KK), b8(SS), ALU.mult)
            yield
            P.stt(T1[:], A[:], -1.0, KAb, ALU.add, ALU.mult)
            yield
            P.stt(KM[:], T1[:], 1.0, Kr, ALU.add, ALU.mult)
            yield
            P.tt(Bv[:], KK[:], A[:], ALU.mult)
            yield
            P.tt(SQ[:], Rr, KM[:], ALU.mult)
            yield
            P.tt(SQ[:], SQ[:], RKb, ALU.mult)
            yield
            P.red(RK[:], v8(SQ), ALU.add)
            yield
            Ls = Lst[k]; EL = ELt[k]; ENL = ENLt[k]; TMP = TMPt[k]; TMP2 = TMP2t[k]; DCF = DCFt[k]
            bL = pbank(); bLv = bL.v(bL.t[0:64, :].rearrange("p (c n) -> p c n", c=4))
            P.mm(bL[0:64, :], TRI[:], fl(LD))
            yield
            P.copy(Ls[:], bLv)
            yield
            bC = pbank(); bCv = bC.v(bC.t[0:64, :].rearrange("p (c n) -> p c n", c=4))
            P.mm(bC[0:64, :], ONES[:], fl(LD))
            yield
            P.tt(TMP2[:], bCv, Ls[:], ALU.subtract)
            yield
            bD = pbank()
            for u in range(8):
                c, h = divmod(u, 2)
                P.mm(bD[0:64, u:u + 1], LD[:, c, h * 64:(h + 1) * 64], ONES[:, 0:1])
                yield
            P.act(DCF[:], bD[0:64, 0:8], AF.Exp)
            yield
            P.act(EL[:], Ls[:], AF.Exp)
            yield
            P.act(ENL[:], Ls[:], AF.Exp, scale=-1.0)
            yield
            P.tt(TMP[:], Ls[:], LD[:], ALU.subtract)
            yield
            P.act(TMP[:], TMP[:], AF.Exp)
            yield
            P.act(TMP2[:], TMP2[:], AF.Exp)
            yield
            A_ = A_t[k]; BT = BTt[k]; KT = KTt[k]; RT = RTt[k]; BB = BBt[k]; KB = KBt[k]
            P.stt(A_[:], KK[:], -1.0, TMP[:], ALU.mult, ALU.mult)
            yield
            P.tt(BT[:], Bv[:], ENL[:], ALU.mult)
            yield
            P.tt(KT[:], KM[:], ENL[:], ALU.mult, e="pool")
            yield
            P.tt(RT[:], Rr, EL[:], ALU.mult)
            yield
            P.tt(BB[:], Bv[:], TMP2[:], ALU.mult, e="pool")
            yield
            P.tt(KB[:], KM[:], TMP2[:], ALU.mult)
            yield
            FMBK = FMBKt[k]; FMAR = FMARt[k]; BKS = BKSt[k]; VV = VVt[k]
            for half in range(2):
                pb = pbank(); pbv = pb.v(pb.t[0:64, :].rearrange("p (u n) -> p u n", u=4))
                pa_ = pbank(); pav = pa_.v(pa_.t[0:64, :].rearrange("p (u n) -> p u n", u=4))
                for uu in range(4):
                    u = half * 4 + uu
                    c, h = divmod(u, 2)
                    hs = slice(h * 64, (h + 1) * 64)
                    P.tr(pb.v(pbv.ap[:, uu, 0:64]), BT[:, c, hs], I64)
                    yield
                    P.tr(pb.v(pbv.ap[:, uu, 64:128]), KT[:, c, hs], I64)
                    yield
                    P.tr(pa_.v(pav.ap[:, uu, 0:64]), A_[:, c, hs], I64)
                    yield
                    P.tr(pa_.v(pav.ap[:, uu, 64:128]), RT[:, c, hs], I64)
                    yield
                P.copy(FMBK[:, half * 4:(half + 1) * 4, :], pbv)
                yield
                P.copy(FMAR[:, half * 4:(half + 1) * 4, :], pav, e="act")
                yield
            ps1 = pbank()
            P.mm(ps1[:], SEL0[:], fl(BB), start=True, stop=False)
            yield
            P.mm(ps1[:], SHIFT[:], fl(KB), start=False, stop=True)
            yield
            P.copy(fl(BKS), ps1[:])
            yield
            ps2 = pbank(); ps2v = ps2.v(ps2.t[:, :].rearrange("p (c n) -> p c n", c=4))
            P.mm(ps2v, SHIFT[:], Vr)
            yield
            P.copy(VV[64:128, :, :], ps2.v(ps2v.ap[64:128, :, :]), e="act")
            yield

            ctxp[g] = dict(k=k, CUR=CUR, A_=A_, FMBK=FMBK, FMAR=FMAR, BKS=BKS, VV=VV, DCF=DCF, RK=RK, Gg=Gg, SQ=SQ)

        def drainP(g_, n):
            if g_ is None:
                return
            try:
                if n is None:
                    while True:
                        next(g_)
                else:
                    for _ in range(n):
                        next(g_)
            except StopIteration:
                pass

        drainP(prepG(0), None)
        for g in range(NG):
            c_ = ctxp.pop(g)
            k = c_["k"]; CUR = c_["CUR"]; A_ = c_["A_"]; FMBK = c_["FMBK"]; FMAR = c_["FMAR"]; BKS = c_["BKS"]
            VV = c_["VV"]; DCF = c_["DCF"]; RK = c_["RK"]; Gg = c_["Gg"]; SQ = c_["SQ"]
            gP = prepG(g + 1) if g + 1 < NG else None
            YB = YBt[k]

            def ub():
                b = banks[4 + ubi[0] % 4]; ubi[0] += 1
                return b
            v864 = lambda bank: bank.v(bank.t[0:64, :].rearrange("p (u n) -> p u n", u=8))
            I8 = _bc(IDT, IDT.t[0:64, 0:64], 1, [64, 8, 64])
            bB = [ub(), ub()]; bX = ub()
            for u in range(8):
                bb = bB[u // 4]
                P.mm(bb[:, (u % 4) * 128:(u % 4 + 1) * 128], FMBK[:, u, :], FMAR[:, u, :])
                P.mm(bX[0:64, u * 64:(u + 1) * 64], FMAR[:, u, 0:64], FMBK[:, u, 0:64])
            for hf in range(2):
                bb = bB[hf]
                P.tt(BIGA[:, hf * 4:(hf + 1) * 4, :], bb.v(bb.t[:, :].rearrange("p (u n) -> p u n", u=4)),
                     _bc(MASK, MASK.t[:, :], 1, [128, 4, 128]), ALU.mult)
            XT = XTA[0]
            P.tt(XT[:], v864(bX), _bc(TRILS, TRILS.t[:, :], 1, [64, 8, 64]), ALU.mult)
            X = BIGA.v(BIGA.t[0:64, :, 0:64])
            Pm = PMA[0]
            P.tt(Pm[:], X, I8, ALU.add, e="pool")
            drainP(gP, 12)
            for i in range(5):
                Xn = XA[i % 2]; XTn = XTA[(i + 1) % 2]; Pn = PMA[(i + 1) % 2]
                bA = ub(); bQ = ub()
                for u in range(8):
                    us = slice(u * 64, (u + 1) * 64)
                    if i < 4:
                        P.mm(bA[0:64, us], XT[:, u, :], X[:, u, :])
                    P.mm(bQ[0:64, us], X[:, u, :], XT[:, u, :])
                if i < 4:
                    P.copy(Xn[:], v864(bA), e="act")
                P.copy(XTn[:], v864(bQ))
                bC = ub()
                for u in range(8):
                    us = slice(u * 64, (u + 1) * 64)
                    P.mm(bC[0:64, us], XTn[:, u, :], Pm[:, u, :])
                P.tt(Pn[:], v864(bC), Pm[:], ALU.add)
                drainP(gP, 12)
                X = Xn; XT = XTn; Pm = Pn
                if i == 4:
                    X = None
            InvT = Pm
            bW = ub()
            for u in range(8):
                c, h = divmod(u, 2); hs = slice(h * 64, (h + 1) * 64); us = slice(u * 64, (u + 1) * 64)
                P.mm(bW[0:64, us], BIGA[64:128, u, 0:64], VV[64:128, c, hs])
            P.copy(W2A[:], v864(bW), e="act")
            drainP(gP, 12)
            bA = ub(); bU = ub()
            for u in range(8):
                c, h = divmod(u, 2); hs = slice(h * 64, (h + 1) * 64); us = slice(u * 64, (u + 1) * 64)
                P.mm(bA[0:64, us], InvT[:, u, :], A_[:, c, hs])
                P.mm(bU[0:64, us], InvT[:, u, :], W2A[:, u, :])
            P.copy(AHA[:], v864(bA))
            P.copy(VV.v(VV.t[0:64, :, :].rearrange("p c (h n) -> p (c h) n", n=64)), v864(bU), e="act")
            bR = ub(); bG = ub()
            for u in range(8):
                c, h = divmod(u, 2); hs = slice(h * 64, (h + 1) * 64); us = slice(u * 64, (u + 1) * 64)
                P.mm(bR[0:64, us], AHA[:, u, :], BIGA[0:64, u, 64:128])
                P.mm(bG[0:64, us], AHA[:, u, :], BKS[0:64, c, hs])
            P.tt(RHA[:], v864(bR), FMAR[:, :, 64:128], ALU.add)
            P.tt(TMPG[:], I8, _bc(DCF, DCF.t[:, :], 2, [64, 8, 64]), ALU.mult, e="pool")
            P.tt(GTA[:], v864(bG), TMPG[:], ALU.add)
            drainP(gP, 12)
            bY = ub()
            bHs = [ub(), ub()]
            for c in range(4):
                bH = bHs[c % 2]
                Hc = HA[hkk[0] % 2]; Hn = HA[(hkk[0] + 1) % 2]; hkk[0] += 1
                for h in range(2):
                    u = c * 2 + h
                    hs = slice(h * 64, (h + 1) * 64); us = slice(u * 64, (u + 1) * 64)
                    P.mm(bY[0:64, us], RHA[:, u, :], Hc[:, h, :], start=True, stop=False)
                    P.mm(bY[0:64, us], BIGA[:, u, 64:128], VV[:, c, hs], start=False, stop=True)
                    P.mm(bH[0:64, hs], GTA[:, u, :], Hc[:, h, :], start=True, stop=False)
                    P.mm(bH[0:64, hs], BKS[:, c, hs], VV[:, c, hs], start=False, stop=True)
                P.copy(Hn[:], bH.v(bH.t[0:64, 0:128].rearrange("p (h n) -> p h n", h=2)))
                drainP(gP, 12)
            P.copy(fl(YB), bY[0:64, :], e="act")
            drainP(gP, None)
            YC = YCt[k]; MN = MNt[k]; VR = VRt[k]
            P.red(MN[:], v8(YB), ALU.add)
            P.ts(MN[:], MN[:], 1.0 / 64, None, ALU.mult)
            P.tt(v8(YC), v8(YB), b8(MN), ALU.subtract)
            P.tt(SQ[:], YC[:], YC[:], ALU.mult, e="pool")
            P.red(VR[:], v8(SQ), ALU.add)
            P.ts(VR[:], VR[:], 1.0 / 64, 64e-5, ALU.mult, ALU.add)
            P.act(VR[:], VR[:], AF.Sqrt)
            P.recip(VR[:], VR[:])
            P.tt(v8(YC), v8(YC), b8(VR), ALU.mult)
            P.tt(YC[:], YC[:], GNGb, ALU.mult)
            P.tt(YC[:], YC[:], GNBb, ALU.add)
            P.tt(SQ.v(SQ.t[:, :, :].rearrange("p a (h b) -> p a h b", b=64)),
                 CUR.v(CUR.t[:, :, 256:384].rearrange("p a (h b) -> p a h b", b=64)),
                 RK.v(RK.t[:, :].rearrange("p (a h) -> p a h", h=2).unsqueeze(3).to_broadcast([64, 4, 2, 64])), ALU.mult)
            P.tt(YC[:], YC[:], SQ[:], ALU.add)
            P.tt(YC[:], YC[:], Gg[:], ALU.mult)
            P.store("sp", ya[g * 256:(g + 1) * 256, :].rearrange("(c t) n -> t c n", t=64), YC[:])
        P.finish()
        print("k2 ninst", P.ninst, "nsem", P.nsem)
    return nc


RW = 512
EVAC_E = "act"


def k2_inputs(p_rwkv_b, i, prm, l, consts):
    h0 = 2 * i
    cs = slice(h0 * 64, h0 * 64 + 128)
    cols = np.r_[np.arange(h0 * 64, h0 * 64 + 128), RW + np.arange(h0 * 64, h0 * 64 + 128),
                 2 * RW + np.arange(h0 * 64, h0 * 64 + 128), np.arange(3 * RW, 3 * RW + 256)]
    vec = np.zeros((8, 128), np.float32)
    for r, n in enumerate(["rwkv_w0", "rwkv_a0", "rwkv_k_k", "rwkv_k_a", "rwkv_r_k", "rwkv_gn_g", "rwkv_gn_b"]):
        vec[r] = prm[n][l].reshape(-1)[cs]
    d = {"pp": np.ascontiguousarray(p_rwkv_b[:, cols]), "mu": np.ascontiguousarray(prm["rwkv_mu"][l][cols][None]),
         "vec": vec, "w2": np.ascontiguousarray(prm["rwkv_w2"][l][:, cs]),
         "a2": np.ascontiguousarray(prm["rwkv_a2"][l][:, cs]), "g2": np.ascontiguousarray(prm["rwkv_g2"][l][:, cs])}
    d.update(consts)
    return d


def run_k2(p, prm, l, TT=T):
    key = ("k2", TT)
    if key not in _CACHE:
        _CACHE[key] = build_k2(TT)
    nc = _CACHE[key]
    consts = k2_consts()
    maps = []
    for ci in range(NCORES):
        b, i = divmod(ci, 4)
        pb = np.concatenate([np.zeros((1, 1792), np.float32), p[b, :TT, :1792]], 0)
        maps.append(k2_inputs(pb, i, prm, l, consts))
    res = run_bass_kernel_spmd(nc, maps, core_ids=list(range(NCORES)))
    out = np.empty((NB, TT, 512), np.float32)
    for ci in range(NCORES):
        b, i = divmod(ci, 4)
        out[b, :, i * 128:(i + 1) * 128] = res.results[ci]["ya"]
    return out


ROPE_THETA = 500000.0
NEGM = -30000.0


def k3_consts(TT):
    half = 8
    inv = np.power(ROPE_THETA, -np.arange(half, dtype=np.float32) * 2.0 / 16).astype(np.float32)

    def tab(pos):
        ang = pos.astype(np.float32)[:, None] * inv[None, :]
        c, s = np.cos(ang).astype(np.float32), np.sin(ang).astype(np.float32)
        return np.concatenate([c, c, -s, s], 1).astype(np.float32)
    rope_tok = tab(np.arange(TT))
    ncmp = TT // 16
    rope_cmp = tab(np.arange(ncmp) * 16 + 31)
    ql = np.arange(128)
    triu = (ql[:, None] <= ql[None, :]).astype(np.float32)
    tril = (ql[:, None] > ql[None, :]).astype(np.float32)
    cma = np.zeros((128, 17, 128), np.float32)
    cmt = np.zeros((128, 17, 128), np.float32)
    for v in range(17):
        c0 = 8 * v
        nl = np.arange(128)
        valid = (16 * (nl[None, :] - c0) + 31 <= ql[:, None])
        cma[:, v, :] = np.where(valid, 0.0, NEGM)
        cmt[:, v, :] = valid.T.astype(np.float32)
    ca = np.where(ql >= 64, 1e4, -1.0).astype(np.float32)[:, None]
    cb = np.where(ql < 64, 1e4, 0.0).astype(np.float32)[:, None]
    return {"rope_tok": rope_tok, "rope_cmp": rope_cmp, "triu": triu, "tril": tril, "cma": cma, "cmt": cmt,
            "cab": np.concatenate([ca, cb], 1), "ident": np.eye(128, dtype=np.float32)}


def build_k3(TT=T):
    nc = _new_nc()
    NTK = TT // 128
    NQB = NTK
    NCMP = TT // 16
    NCT = (NCMP + 127) // 128
    dr = lambda n, s, kind="ExternalInput": nc.dram_tensor(n, s, F32, kind=kind).ap()
    qin = dr("qin", [NQB * 128, 256]); gin = dr("gin", [NQB * 128, 6]); kvin = dr("kvin", [TT, 384])
    qkg = dr("qkg", [1, 256]); cpos = dr("cpos", [128, 32]); w1 = dr("w1", [128, 32, 256]); w2 = dr("w2", [128, 2, 2, 64])
    rope_tok = dr("rope_tok", [TT, 32]); rope_cmp = dr("rope_cmp", [NCMP, 32])
    triu = dr("triu", [128, 128]); tril = dr("tril", [128, 128]); cma = dr("cma", [128, 17, 128]); cmt = dr("cmt", [128, 17, 128])
    cab = dr("cab", [128, 2]); ident = dr("ident", [128, 128])
    yb = dr("yb", [NQB * 128, 128], "ExternalOutput")
    with ExitStack() as es, nc.allow_low_precision("bf16 attention-tile operands, fp32 PSUM accumulation"):
        P = Prog2(nc, es)
        IDT = P.tile("IDT", [128, 128]); TRIU = P.tile("TRIU", [128, 128]); TRIL = P.tile("TRIL", [128, 128])
        CMA = P.tile("CMA", [128, 17, 128]); CMT = P.tile("CMT", [128, 17, 128]); CAB = P.tile("CAB", [128, 2])
        QKG = P.tile("QKG", [128, 4, 64]); CPOS = P.tile("CPOS", [128, 32]); W2 = P.tile("W2", [128, 2, 2, 64])
        for i, (tl, d) in enumerate([(IDT, ident), (TRIU, triu), (TRIL, tril), (CAB, cab), (CPOS, cpos)]):
            P.load(["sp", "act"][i % 2], tl[:], d[:, :])
        P.load("sp", CMA[:], cma[:, :, :]); P.load("act", CMT[:], cmt[:, :, :])
        P.load("sp", QKG.v(QKG.t[:, :, :].rearrange("p a b -> p (a b)")), qkg.partition_broadcast(128))
        P.load("act", W2.v(W2.t[:, :, :, :].rearrange("p a b c -> p (a b c)")), w2.rearrange("p a b c -> p (a b c)"))
        banks = [P.tile(f"bank{i}", [128, 512], psum=True) for i in range(8)]
        KT = P.tile("KT", [128, TT], BF16)
        CT = P.tile("CT", [128, TT + 16])
        VSW = P.tile("VSW", [128, NTK, 2, 65], BF16)
        P.memset(VSW[:, :, :, 64:65], 1.0)
        P.memset(CT[:, TT:TT + 16], 0.0)
        KVt = [P.tile(f"KV{i}", [128, 384]) for i in range(2)]
        RPt = [P.tile(f"RP{i}", [128, 32]) for i in range(2)]
        KNt = [P.tile(f"KN{i}", [128, 2, 64]) for i in range(2)]
        SQt = [P.tile(f"SQa{i}", [128, 2, 64]) for i in range(2)]
        STt = [P.tile(f"STa{i}", [128, 2]) for i in range(2)]
        R1t = [P.tile(f"R1a{i}", [128, 2, 16]) for i in range(2)]
        R2t = [P.tile(f"R2a{i}", [128, 2, 16]) for i in range(2)]

        def norm_rope(X3, G3, RP, KN, SQ, ST, R1, R2, nh, e2="pool"):
            P.tt(SQ[:], X3, X3, ALU.mult, e=e2)
            P.red(ST[:], SQ[:], ALU.add)
            P.ts(ST[:], ST[:], 1.0 / 64, 1e-6, ALU.mult, ALU.add)
            P.act(ST[:], ST[:], AF.Sqrt)
            P.recip(ST[:], ST[:])
            P.tt(KN[:], X3, _bc(ST, ST.t[:, :], 2, [128, nh, 64]), ALU.mult)
            P.tt(KN[:], KN[:], G3, ALU.mult, e=e2)
            cs = _bc(RP, RP.t[:, 0:16], 1, [128, nh, 16])
            P.tt(R1[:], KN[:, :, 0:16], cs, ALU.mult)
            P.tt(R2[:, :, 0:8], KN[:, :, 8:16], _bc(RP, RP.t[:, 16:24], 1, [128, nh, 8]), ALU.mult, e=e2)
            P.tt(R2[:, :, 8:16], KN[:, :, 0:8], _bc(RP, RP.t[:, 24:32], 1, [128, nh, 8]), ALU.mult, e=e2)
            P.tt(KN[:, :, 0:16], R1[:], R2[:], ALU.add)

        GK = QKG.v(QKG.t[:, 2:4, :])
        pk = 0
        for tg in range(NTK // 4):
            bk = banks[pk % 2]; bc_ = banks[2 + pk % 2]; pk += 1
            for j in range(4):
                ti = tg * 4 + j
                k = ti % 2
                KV = KVt[k]; RP = RPt[k]
                P.load("sp", KV[:], kvin[ti * 128:(ti + 1) * 128, :])
                P.load("act", RP[:], rope_tok[ti * 128:(ti + 1) * 128, :])
                X3 = KV.v(KV.t[:, 128:384].rearrange("p (a b) -> p a b", b=128)[:, :, 0:64])
                norm_rope(X3, GK, RP, KNt[k], SQt[k], STt[k], R1t[k], R2t[k], 2)
                P.tr(bk[:, j * 128:(j + 1) * 128], KNt[k].v(KNt[k].t[:, :, :].rearrange("p a b -> p (a b)")), IDT[:])
                P.tr(bc_[:, j * 128:(j + 1) * 128], KV[:, 0:128], IDT[:])
                V3 = KV.v(KV.t[:, 128:384].rearrange("p (a b) -> p a b", b=128)[:, :, 64:128])
                P.copy(VSW[:, ti, :, 0:64], V3, e="act")
            P.copy(KT[:, tg * 512:(tg + 1) * 512], bk[:])
            P.copy(CT[:, tg * 512:(tg + 1) * 512], bc_[:], e="act")
        HID = P.tile("HID", [128, 2, 2, NCT * 128])
        P.memset(HID[:], 0.0, e="pool")
        W1 = P.tile("W1", [128, 32, 128])
        BIA = P.tile("BIA", [128, 2, 2])
        Zt = P.tile("Zt", [128, 512]); Z2 = P.tile("Z2", [128, 512])
        NV = NCMP - 1
        for hc in range(2):
            P.load("sp" if hc == 0 else "act", W1[:], w1[:, :, hc * 128:(hc + 1) * 128])
            for i in range(2):
                ps_ = slice(i * 64, (i + 1) * 64)
                bb = banks[4]
                for tau in range(32):
                    P.mm(bb[:, 0:1], W1[ps_, tau, :], CPOS[ps_, tau:tau + 1], start=(tau == 0), stop=(tau == 31))
                P.copy(BIA[:, i, hc:hc + 1], bb[:, 0:1])
                for nt in range((NCMP + 511) // 512):
                    n0 = nt * 512
                    nn = min(512, NCMP - n0)
                    ba = banks[5 + nt % 2]
                    for tau in range(32):
                        rhs = CT.v(CT.t[ps_, n0 * 16 + tau: n0 * 16 + tau + (nn - 1) * 16 + 1: 16])
                        P.mm(ba[:, 0:nn], W1[ps_, tau, :], rhs, start=(tau == 0), stop=(tau == 31))
                    Z = Zt
                    P.act(Z[:, 0:nn], ba[:, 0:nn], AF.Identity, bias=BIA[:, i, hc:hc + 1])
                    P.tt(Z2[:, 0:nn], Z[:, 0:nn], Z[:, 0:nn], ALU.mult)
                    P.ts(Z2[:, 0:nn], Z2[:, 0:nn], 0.044715, 1.0, ALU.mult, ALU.add)
                    P.tt(Z2[:, 0:nn], Z2[:, 0:nn], Z[:, 0:nn], ALU.mult)
                    P.act(Z2[:, 0:nn], Z2[:, 0:nn], AF.Sigmoid, scale=1.5957691216057308)
                    P.tt(HID[:, i, hc, n0:n0 + nn], Z2[:, 0:nn], Z[:, 0:nn], ALU.mult)
        KCT = P.tile("KCT", [128, NCT * 128])
        VCMP = P.tile("VCMP", [128, NCT, 65], BF16)
        KCTb = P.tile("KCTb", [128, NCT * 128], BF16)
        P.memset(VCMP[:, :, 64:65], 1.0)
        KC2 = P.tile("KC2", [128, 2, 64])
        KCR = P.tile("KCR", [128, 1, 64])
        GC = QKG.v(QKG.t[:, 1:2, :])
        for ct in range(NCT):
            bb = banks[4 + ct % 2]
            for i in range(2):
                for hc in range(2):
                    P.mm(bb[:, i * 64:(i + 1) * 64], HID[:, i, hc, ct * 128:(ct + 1) * 128], W2[:, i, hc, :],
                         start=(hc == 0), stop=(hc == 1))
            P.copy(VCMP[:, ct, 0:64], bb[:, 64:128], e="act")
            RP = RPt[ct % 2]
            nr = min(128, NCMP - ct * 128)
            P.load("sp", RP[0:nr, :], rope_cmp[ct * 128:ct * 128 + nr, :])
            k = ct % 2
            KN1 = KNt[k].v(KNt[k].t[:, 0:1, :])
            P.copy(KCR[:, 0, :], bb[:, 0:64])
            norm_rope(KCR[:], GC, RP, KN1,
                      SQt[k].v(SQt[k].t[:, 0:1, :]), STt[k].v(STt[k].t[:, 0:1]), R1t[k].v(R1t[k].t[:, 0:1, :]),
                      R2t[k].v(R2t[k].t[:, 0:1, :]), 1, e2="dve")
            P.copy(KC2[:, 0, :], KNt[k][:, 0, :])
            P.copy(KC2[:, 1, :], KNt[k][:, 0, :], e="act")
            b2 = banks[6 + ct % 2]
            P.tr(b2[:, 0:128], KC2.v(KC2.t[:, :, :].rearrange("p a b -> p (a b)")), IDT[:])
            P.copy(KCT[:, ct * 128:(ct + 1) * 128], b2[:, 0:128])
            P.copy(KCTb[:, ct * 128:(ct + 1) * 128], b2[:, 0:128], e="act")
        Qt = [P.tile(f"Q{i}", [128, 4, 64]) for i in range(2)]
        Gt = [P.tile(f"G{i}", [128, 2, 3]) for i in range(2)]
        QNt = [P.tile(f"QN{i}", [128, 4, 64]) for i in range(2)]
        QDt = [P.tile(f"QD{i}", [128, 4, 2, 64]) for i in range(2)]
        QTt = [P.tile(f"QT{i}", [128, 4, 128]) for i in range(2)]
        SQq = [P.tile(f"SQq{i}", [128, 4, 64]) for i in range(2)]
        STq = [P.tile(f"STq{i}", [128, 4]) for i in range(2)]
        R1q = [P.tile(f"R1q{i}", [128, 4, 16]) for i in range(2)]
        R2q = [P.tile(f"R2q{i}", [128, 4, 16]) for i in range(2)]
        PCt = [P.tile(f"PC{i}", [128, 512]) for i in range(2)]
        RSt = [P.tile(f"RS{i}", [128, 4]) for i in range(2)]
        IMPP = P.tile("IMPP", [128, 516])
        IMPF = P.tile("IMPF", [128, 128]); IMPW = P.tile("IMPW", [128, 128])
        M8 = P.tile("M8", [128, 16])
        SEL = P.tile("SEL", [128, 128])
        SELN = P.tile("SELN", [128, 128]); SELXb = P.tile("SELXb", [128, TT], BF16); IDTb = P.tile("IDTb", [128, 128], BF16)
        P.copy(IDTb[:], IDT[:])
        PTt = [P.tile(f"PT{i}", [128, 2, 128], BF16) for i in range(6)]
        QTbt = [P.tile(f"QTb{i}", [128, 2, 128], BF16) for i in range(2)]
        OUTt = [P.tile(f"OUT{i}", [128, 2, 64]) for i in range(2)]
        RIt = [P.tile(f"RI{i}", [128, 3, 2]) for i in range(2)]
        OTt = [P.tile(f"OT{i}", [128, 2, 64]) for i in range(2)]
        GQ = QKG.v(QKG.t[:, 0:1, :].to_broadcast([128, 4, 64])) if False else _bc(QKG, QKG.t[:, 0, :], 1, [128, 4, 64])
        sb_i = [0]; mb_i = [0]; pt_i = [0]
        OB = [0, 1, 2]

        def attn_tile(first, last, ob, kT_ap, part, v1_ap, QT, mask_fn):
            sb = banks[sb_i[0] % 2]; sb_i[0] += 1
            PT = PTt[pt_i[0] % 4]; pt_i[0] += 1
            P.mm(sb[:, 0:256], kT_ap, QTb.v(QTb.t[part:part + 64, :, :].rearrange("p a b -> p (a b)")))
            P.act(PT.v(PT.t[:, :, :].rearrange("p a b -> p (a b)")), sb[:, 0:256], AF.Exp)
            mask_fn(PT)
            for r in range(2):
                P.mm(banks[6 + r][:, ob * 65:(ob + 1) * 65], PT[:, r, :], v1_ap, start=first, stop=last)

        ctxs = {}

        def phaseA(jq):
                qb = jq
                k = jq % 2
                Q = Qt[k]; G = Gt[k]; RP = RPt[k]
                P.load("sp", Q.v(Q.t[:, :, :].rearrange("p a b -> p (a b)")), qin[jq * 128:(jq + 1) * 128, :])
                yield
                P.load("act", G.v(G.t[:, :, :].rearrange("p a b -> p (a b)")), gin[jq * 128:(jq + 1) * 128, :])
                yield
                P.load("sp", RP[:], rope_tok[qb * 128:(qb + 1) * 128, :])
                yield
                QN = QNt[k]; QD = QDt[k]; QT = QTt[k]
                norm_rope(Q[:], GQ, RP, QN, SQq[k], STq[k], R1q[k], R2q[k], 4)
                yield
                P.ts(QD[:, :, 0, :], QN[:], 0.125, None, ALU.mult)
                yield
                P.ts(QD[:, :, 1, :], QN[:], 0.125, None, ALU.mult, e="pool")
                yield
                P.act(G[:], G[:], AF.Sigmoid)
                yield
                bq = banks[4]
                for r in range(4):
                    P.tr(bq[:, r * 128:(r + 1) * 128], QD.v(QD.t[:, r, :, :].rearrange("p a b -> p (a b)")), IDT[:])
                    yield
                P.copy(QT.v(QT.t[:, :, :].rearrange("p a b -> p (a b)")), bq[:])
                yield
                QTb = QTbt[k]
                P.copy(QTb.v(QTb.t[:, :, :].rearrange("p a b -> p (a b)")), bq[:, 0:256], e="act")
                yield
                nct = qb // 16 + 1
                ncols = nct * 128
                var = qb % 16
                P.memset(IMPP[:], 0.0, e="pool")
                yield
                RS = RSt[k]
                for r in range(4):
                    sb = banks[5]
                    P.mm(sb[:, 0:ncols], QT[0:64, r, :], KCT[0:64, 0:ncols])
                    yield
                    PC = PCt[r % 2]
                    lo = ncols - 128
                    if lo > 0:
                        P.copy(PC[:, 0:lo], sb[:, 0:lo])
                        yield
                        if var == 0:
                            P.tt(PC[:, lo - 128:lo], PC[:, lo - 128:lo], CMA[:, 16, :], ALU.add)
                            yield
                    P.tt(PC[:, lo:ncols], sb[:, lo:ncols], CMA[:, var, :], ALU.add)
                    yield
                    P.act(PC[:, 0:ncols], PC[:, 0:ncols], AF.Exp, accum_out=RS[:, r:r + 1])
                    yield
                    P.ts(RS[:, r:r + 1], RS[:, r:r + 1], 1.1754944e-38, None, ALU.max)
                    yield
                    P.recip(RS[:, r:r + 1], RS[:, r:r + 1])
                    yield
                    P.stt(IMPP[:, 4:4 + ncols], PC[:, 0:ncols], RS[:, r:r + 1], IMPP[:, 4:4 + ncols], ALU.mult, ALU.add)
                    yield
                P.red(IMPF[:], IMPP.v(IMPP.t[:, 4:516].rearrange("p (j f) -> p j f", f=4)), ALU.add)
                yield
                P.tt(IMPF[:], IMPF[:], IMPP.v(IMPP.t[:, 0:512].rearrange("p (j f) -> p j f", f=4)[:, :, 3]), ALU.add)
                yield
                if 2 * qb + 2 < 128:
                    P.memset(IMPF[:, 2 * qb + 2:128], -1.0)
                    yield
                if qb >= 1:
                    P.ts(IMPF[:, 2 * qb - 1:2 * qb], IMPF[:, 2 * qb - 1:2 * qb], CAB[:, 1:2], None, ALU.max)
                    yield
                if 2 * qb + 1 < 128:
                    P.copy(IMPF[:, 2 * qb + 1:2 * qb + 2], CAB[:, 0:1])
                    yield
                P.memset(IMPF[:, 2 * qb:2 * qb + 1], 1e4)
                yield
                P.memset(IMPF[:, 0:1], 1e4)
                yield
                P.gop("dve", lambda E: E.max(out=M8.t[:, 0:8], in_=IMPF.t[:, :]), [M8], [IMPF])
                yield
                P.gop("dve", lambda E: E.match_replace(out=IMPW.t[:, :], in_to_replace=M8.t[:, 0:8], in_values=IMPF.t[:, :],
                                                       imm_value=-2.0), [IMPW], [M8, IMPF])
                P.gop("dve", lambda E: E.max(out=M8.t[:, 8:16], in_=IMPW.t[:, :]), [M8], [IMPW])
                yield
                P.ts(SEL[:], IMPF[:], M8[:, 15:16], None, ALU.is_ge)
                yield

                ctxs[jq] = dict(qb=qb, k=k, nct=nct, var=var, QT=QT, QTb=QTb, G=G)

        def drainA(g, n):
            if g is None:
                return
            try:
                if n is None:
                    while True:
                        next(g)
                else:
                    for _ in range(n):
                        next(g)
            except StopIteration:
                pass

        drainA(phaseA(0), None)
        for jq in range(NQB):
            c_ = ctxs.pop(jq)
            qb = c_["qb"]; k = c_["k"]; nct = c_["nct"]; var = c_["var"]; QT = c_["QT"]; QTb = c_["QTb"]; G = c_["G"]
            gA = phaseA(jq + 1) if jq + 1 < NQB else None
            tiles = []
            for ct in range(nct):
                last = ct == nct - 1
                if last:
                    mf = lambda PT: P.tt(PT[:], PT[:], _bc(CMT, CMT.t[:, var, :], 1, [128, 2, 128]), ALU.mult)
                elif var == 0 and ct == nct - 2:
                    mf = lambda PT: P.tt(PT[:], PT[:], _bc(CMT, CMT.t[:, 16, :], 1, [128, 2, 128]), ALU.mult)
                else:
                    mf = None
                tiles.append((ct == 0, last, OB[0], KCTb[0:64, ct * 128:(ct + 1) * 128], 0, VCMP[:, ct, :], mf, None))
            kts = [kt for kt in range(qb - 4, qb + 1) if kt >= 0]
            for kt in kts:
                if kt == qb:
                    mf = lambda PT: P.tt(PT[:], PT[:], _bc(TRIU, TRIU.t[:, :], 1, [128, 2, 128]), ALU.mult)
                elif kt == qb - 4:
                    mf = lambda PT: P.tt(PT[:], PT[:], _bc(TRIL, TRIL.t[:, :], 1, [128, 2, 128]), ALU.mult)
                else:
                    mf = None
                tiles.append((kt == kts[0], kt == kts[-1], OB[2], KT[64:128, kt * 128:(kt + 1) * 128], 64, VSW[:, kt, 1, :], mf, None))
            nj = 2 * (qb + 1)
            P.ts(SELN[:, 0:nj], SEL[:, 0:nj], -1.0, 30000.0, ALU.add, ALU.mult)
            P.copy(SELXb.v(SELXb.t[:, 0:nj * 64].rearrange("p (j f) -> p j f", f=64)),
                   SELN.v(SELN.t[:, 0:nj].unsqueeze(2).to_broadcast([128, nj, 64])))
            for kt in range(qb + 1):
                tiles.append((kt == 0, kt == qb, OB[1], KT[0:64, kt * 128:(kt + 1) * 128], 0, VSW[:, kt, 0, :], None, kt))
            LA = 3
            pend = []
            sbanks = [banks[0], banks[1], banks[2], banks[3]]
            mbanks = [banks[2], banks[3]]
            for i in range(len(tiles) + LA):
                if i < len(tiles):
                    first, last, ob, kT_ap, part, v1_ap, mf, kt = tiles[i]
                    sb = sbanks[sb_i[0] % 4]; sb_i[0] += 1
                    PT = PTt[pt_i[0] % 6]; pt_i[0] += 1
                    P.mm(sb[:, 0:256], kT_ap, QTb.v(QTb.t[part:part + 64, :, :].rearrange("p a b -> p (a b)")),
                         start=True, stop=(kt is None))
                    if kt is not None:
                        for r in range(2):
                            P.mm(sb[:, r * 128:(r + 1) * 128], SELXb[:, kt * 128:(kt + 1) * 128], IDTb[:], start=False, stop=(r == 1))
                    P.act(PT.v(PT.t[:, :, :].rearrange("p a b -> p (a b)")), sb[:, 0:256], AF.Exp)
                    if kt is not None:
                        if kt == qb:
                            P.tt(PT[:], PT[:], _bc(TRIU, TRIU.t[:, :], 1, [128, 2, 128]), ALU.mult, e="pool")
                    elif mf is not None:
                        mf(PT)
                    pend.append((PT, ob, v1_ap, first, last))
                    drainA(gA, 2)
                j = i - LA
                if j >= 0:
                    PT, ob, v1_ap, first, last = pend[j]
                    for r in range(2):
                        P.mm(banks[6 + r][:, ob * 65:(ob + 1) * 65], PT[:, r, :], v1_ap, start=first, stop=last)
            drainA(gA, None)
            RI = RIt[k]; OUT = OUTt[k]; OT = OTt[k]
            for bi in range(3):
                for r in range(2):
                    ob = banks[6 + r]
                    c0 = bi * 65
                    P.ts(RI[:, bi, r:r + 1], ob[:, c0 + 64:c0 + 65], 1.1754944e-38, None, ALU.max)
                    P.recip(RI[:, bi, r:r + 1], RI[:, bi, r:r + 1])
                    P.tt(RI[:, bi, r:r + 1], RI[:, bi, r:r + 1], G[:, r, bi:bi + 1], ALU.mult)
                    if bi == 0:
                        P.ts(OUT[:, r, :], ob[:, c0:c0 + 64], RI[:, bi, r:r + 1], None, ALU.mult)
                    else:
                        P.stt(OUT[:, r, :], ob[:, c0:c0 + 64], RI[:, bi, r:r + 1], OUT[:, r, :], ALU.mult, ALU.add)
            P.store("sp", yb[jq * 128:(jq + 1) * 128, :], OUT.v(OUT.t[:, :, :].rearrange("p a b -> p (a b)")))
        P.finish()
        print("k3 ninst", P.ninst, "nsem", P.nsem)
    return nc


def k3_inputs(p_b, g, hh, prm, l, TT):
    q = p_b[:TT, 1792 + g * 256: 1792 + (g + 1) * 256].reshape(TT, 4, 64)
    order = [2 * hh, 2 * hh + 1, 2 * (1 - hh), 2 * (1 - hh) + 1]
    q = q[:, order, :].reshape(TT, 256)
    kv = p_b[:TT, 2304:3072].reshape(TT, 6, 2, 64)[:, :, g, :].reshape(TT, 384)
    gt = p_b[:TT, 3072:3096].reshape(TT, 8, 3)[:, g * 4 + 2 * hh: g * 4 + 2 * hh + 2, :].reshape(TT, 6)
    cp = prm["cmp_pos"][l]
    cpos = np.concatenate([cp[0].T, cp[1].T], 0)
    w1 = prm["cmp_w1"][l].reshape(2, 32, 64, 256).transpose(0, 2, 1, 3).reshape(128, 32, 256)
    w2 = prm["cmp_w2"][l].reshape(2, 2, 128, 64).transpose(2, 0, 1, 3)
    d = {"qin": np.ascontiguousarray(q), "gin": np.ascontiguousarray(gt), "kvin": np.ascontiguousarray(kv),
         "qkg": np.ascontiguousarray(prm["qk_norm_g"][l].reshape(1, 256)), "cpos": np.ascontiguousarray(cpos),
         "w1": np.ascontiguousarray(w1), "w2": np.ascontiguousarray(w2)}
    d.update(k3_consts(TT))
    return d


def run_k3(p, prm, l, TT=T):
    key = ("k3", TT)
    if key not in _CACHE:
        _CACHE[key] = build_k3(TT)
    nc = _CACHE[key]
    maps = []
    for ci in range(NCORES):
        b, rem = divmod(ci, 4)
        g, hh = divmod(rem, 2)
        maps.append(k3_inputs(p[b], g, hh, prm, l, TT))
    res = run_bass_kernel_spmd(nc, maps, core_ids=list(range(NCORES)))
    out = np.empty((NB, TT, 512), np.float32)
    for ci in range(NCORES):
        b, rem = divmod(ci, 4)
        g, hh = divmod(rem, 2)
        h0 = g * 4 + 2 * hh
        out[b, :, h0 * 64:(h0 + 2) * 64] = res.results[ci]["yb"]
    return out


def build_k4a():
    nc = _new_nc()
    NT = 16
    dr = lambda n, s, kind="ExternalInput": nc.dram_tensor(n, s, F32, kind=kind).ap()
    x = dr("x", [NT * 128, D]); ya = dr("ya", [NT * 128, 512]); yb = dr("yb", [NT * 128, 512]); pm = dr("pm", [NT * 128, 2048])
    gm = dr("gm", [1, D]); wua = dr("wua", [512, D]); wub = dr("wub", [512, D]); wo = dr("wo", [D, D]); ident = dr("ident", [128, 128])
    x1 = dr("x1", [NT * 128, D], "ExternalOutput")
    with ExitStack() as es, nc.allow_low_precision("bf16 projection operands, fp32 PSUM accumulation"):
        P = Prog2(nc, es)
        IDT = P.tile("IDT", [128, 128]); GMB = P.tile("GMB", [128, D])
        WUA = P.tile("WUA", [128, 4, D]); WUB = P.tile("WUB", [128, 4, D]); WO = P.tile("WO", [128, 8, D])
        P.load("sp", IDT[:], ident[:, :]); P.load("act", GMB[:], gm.partition_broadcast(128))
        P.load("sp", WUA[:], wua.rearrange("(kc kp) n -> kp kc n", kp=128))
        P.load("act", WUB[:], wub.rearrange("(kc kp) n -> kp kc n", kp=128))
        P.load("sp", WO[:, 0:4, :], wo[0:512, :].rearrange("(kc kp) n -> kp kc n", kp=128))
        P.load("act", WO[:, 4:8, :], wo[512:1024, :].rearrange("(kc kp) n -> kp kc n", kp=128))
        WUAb = P.tile("WUAb", [128, 4, D], BF16); WUBb = P.tile("WUBb", [128, 4, D], BF16); WOb = P.tile("WOb", [128, 8, D], BF16)
        P.copy(WUAb[:], WUA[:], e="pool"); P.copy(WUBb[:], WUB[:], e="act")
        P.copy(WOb[:, 0:4, :], WO[:, 0:4, :], e="pool"); P.copy(WOb[:, 4:8, :], WO[:, 4:8, :], e="act")
        banks = [P.tile(f"bank{i}", [128, 512], psum=True) for i in range(8)]
        bi = [0]

        def nb():
            b = banks[bi[0] % 8]; bi[0] += 1
            return b
        Xt = [P.tile(f"X{i}", [128, D]) for i in range(2)]
        YAt = [P.tile(f"YA{i}", [128, 512]) for i in range(2)]
        YBt = [P.tile(f"YB{i}", [128, 512]) for i in range(2)]
        PMt = [P.tile(f"PM{i}", [128, 2048]) for i in range(2)]
        YTt = [P.tile(f"YT{i}", [128, 8, 128], BF16) for i in range(2)]
        MIXt = [P.tile(f"MIX{i}", [128, D]) for i in range(2)]
        TMPt = [P.tile(f"TMP{i}", [128, 512]) for i in range(2)]
        MTt = [P.tile(f"MT{i}", [128, 8, 128], BF16) for i in range(2)]
        for ti in range(NT):
            k = ti % 2
            rs = slice(ti * 128, (ti + 1) * 128)
            X = Xt[k]; YA = YAt[k]; YB = YBt[k]; PM = PMt[k]; YT = YTt[k]; MIX = MIXt[k]; MT = MTt[k]
            P.load("sp", X[:], x[rs, :]); P.load("act", YA[:], ya[rs, :]); P.load("sp", YB[:], yb[rs, :]); P.load("act", PM[:], pm[rs, :])
            for j, Y in enumerate((YA, YB)):
                b = nb()
                for c in range(4):
                    P.tr(b[:, c * 128:(c + 1) * 128], Y[:, c * 128:(c + 1) * 128], IDT[:])
                P.copy(YT.v(YT.t[:, j * 4:(j + 1) * 4, :].rearrange("p a b -> p (a b)")), b[:], e="act" if j else "dve")
            P.act(PM[:], PM[:], AF.Sigmoid)
            for nh in range(2):
                cs = slice(nh * 512, (nh + 1) * 512)
                ba = nb(); bb = nb()
                for kc in range(4):
                    P.mm(ba[:], YT[:, kc, :], WUAb[:, kc, cs], start=(kc == 0), stop=(kc == 3))
                for kc in range(4):
                    P.mm(bb[:], YT[:, 4 + kc, :], WUBb[:, kc, cs], start=(kc == 0), stop=(kc == 3))
                TMP = TMPt[nh]
                P.tt(MIX[:, cs], ba[:], PM[:, nh * 512:(nh + 1) * 512], ALU.mult)
                P.tt(TMP[:], bb[:], PM[:, 1024 + nh * 512:1024 + (nh + 1) * 512], ALU.mult)
                P.tt(MIX[:, cs], MIX[:, cs], TMP[:], ALU.add, e="pool")
            for half in range(2):
                b = nb()
                for c in range(4):
                    kc = half * 4 + c
                    P.tr(b[:, c * 128:(c + 1) * 128], MIX[:, kc * 128:(kc + 1) * 128], IDT[:])
                P.copy(MT.v(MT.t[:, half * 4:(half + 1) * 4, :].rearrange("p a b -> p (a b)")), b[:], e="act" if half else "dve")
            for nh in range(2):
                cs = slice(nh * 512, (nh + 1) * 512)
                b = nb()
                for kc in range(8):
                    P.mm(b[:], MT[:, kc, :], WOb[:, kc, cs], start=(kc == 0), stop=(kc == 7))
                TMP = TMPt[nh]
                P.tt(TMP[:], b[:], GMB[:, cs], ALU.mult)
                P.tt(X[:, cs], X[:, cs], TMP[:], ALU.add, e="pool")
            P.store("sp" if k else "act", x1[rs, :], X[:])
        P.finish()
        print("k4a ninst", P.ninst, "nsem", P.nsem)
    return nc


def run_k4a(xfull, ya, yb, p, gate_mix, prm, l):
    if "k4a" not in _CACHE:
        _CACHE["k4a"] = build_k4a()
    nc = _CACHE["k4a"]
    ident = np.eye(128, dtype=np.float32)
    maps = []
    for i in range(NCORES):
        b, tq = divmod(i, 4)
        ts_ = slice(tq * 2048, (tq + 1) * 2048)
        maps.append({"x": np.ascontiguousarray(xfull[b, ts_]), "ya": np.ascontiguousarray(ya[b, ts_]),
                     "yb": np.ascontiguousarray(yb[b, ts_]), "pm": np.ascontiguousarray(p[b, ts_, 3096:5144]),
                     "gm": gate_mix[b][None].copy(), "wua": prm["w_up_rwkv"][l], "wub": prm["w_up_nsa"][l],
                     "wo": prm["w_out"][l], "ident": ident})
    res = run_bass_kernel_spmd(nc, maps, core_ids=list(range(NCORES)))
    out = np.empty((NB, T, D), np.float32)
    for i in range(NCORES):
        b, tq = divmod(i, 4)
        out[b, tq * 2048:(tq + 1) * 2048] = res.results[i]["x1"]
    return out


def build_k4b():
    nc = _new_nc()
    NT = 16; NE = 16
    dr = lambda n, s, kind="ExternalInput": nc.dram_tensor(n, s, F32, kind=kind).ap()
    x1 = dr("x1", [NT * 128, D]); g = dr("g", [1, D]); sc = dr("sc", [1, D]); sh = dr("sh", [1, D]); gf = dr("gf", [1, D])
    rw = dr("rw", [D, 16]); rb = dr("rb", [1, 16]); ident = dr("ident", [128, 128])
    wg = dr("wg", [NE, D, 512]); wu = dr("wu", [NE, D, 512]); wd = dr("wd", [NE, 512, D])
    xo = dr("xo", [NT * 128, D], "ExternalOutput")
    with ExitStack() as es, nc.allow_low_precision("bf16 expert matmuls, fp32 accumulation"):
        P = Prog2(nc, es)
        IDT = P.tile("IDT", [128, 128]); G2S = P.tile("G2S", [128, D]); SC = P.tile("SC", [128, D]); SH = P.tile("SH", [128, D])
        GF = P.tile("GF", [128, D]); RW = P.tile("RW", [128, 8, 16]); RB = P.tile("RB", [128, 16])
        P.load("sp", IDT[:], ident[:, :]); P.load("act", G2S[:], g.partition_broadcast(128)); P.load("sp", SC[:], sc.partition_broadcast(128))
        P.load("act", SH[:], sh.partition_broadcast(128)); P.load("sp", GF[:], gf.partition_broadcast(128))
        P.load("act", RW[:], rw.rearrange("(kc kp) n -> kp kc n", kp=128)); P.load("sp", RB[:], rb.partition_broadcast(128))
        P.stt(G2S[:], SC[:], 1.0, G2S[:], ALU.add, ALU.mult)
        banks = [P.tile(f"bank{i}", [128, 512], psum=True) for i in range(8)]
        bi = [0]

        def nb():
            b = banks[bi[0] % 8]; bi[0] += 1
            return b
        ACC = [P.tile(f"ACC{t}", [128, D]) for t in range(8)]
        H2T = P.tile("H2T", [128, 8, 1024], BF16)
        CW = P.tile("CW", [128, 8, 16])
        STG = [P.tile(f"STG{i}", [128, 4096]) for i in range(2)]
        WB = [[P.tile(f"WB{m}_{p}", [128, 4096], BF16) for p in range(2)] for m in range(3)]
        H2t = [P.tile(f"H2{i}", [128, D]) for i in range(2)]
        H2Tf = [P.tile(f"H2Tf{i}", [128, 8, 128]) for i in range(2)]
        junk = P.tile("junk", [128, D])
        ST = [P.tile(f"ST{i}", [128, 2]) for i in range(2)]
        SCO = [P.tile(f"SCO{i}", [128, 16]) for i in range(2)]
        SS = [P.tile(f"SS{i}", [128, 4, 4]) for i in range(2)]
        PSm = [P.tile(f"PSm{i}", [128, 4, 6]) for i in range(2)]
        GS = [P.tile(f"GS{i}", [128, 4]) for i in range(2)]
        GMx = [P.tile(f"GMx{i}", [128, 2]) for i in range(2)]
        E2 = [P.tile(f"E2{i}", [128, 4, 4]) for i in range(2)]
        SIL = [P.tile(f"SIL{i}", [128, 512]) for i in range(2)]
        HID = [P.tile(f"HID{i}", [128, 4, 512], BF16) for i in range(2)]
        stg_i = [0]
        for tg in range(2):
            for t in range(8):
                ti = tg * 8 + t
                k = t % 2
                A = ACC[t]
                P.load("sp" if k else "act", A[:], x1[ti * 128:(ti + 1) * 128, :])
                S = ST[k]; H2 = H2t[k]; HF = H2Tf[k]
                P.act(junk[:], A[:], AF.Square, accum_out=S[:, 0:1])
                P.ts(S[:, 1:2], S[:, 0:1], 1.0 / D, 1e-6, ALU.mult, ALU.add)
                P.act(S[:, 1:2], S[:, 1:2], AF.Sqrt)
                P.recip(S[:, 1:2], S[:, 1:2])
                P.stt(H2[:], A[:], S[:, 1:2], G2S[:], ALU.mult, ALU.mult)
                P.tt(H2[:], H2[:], SH[:], ALU.add, e="pool")
                for half in range(2):
                    b = nb()
                    for c in range(4):
                        kc = half * 4 + c
                        P.tr(b[:, c * 128:(c + 1) * 128], H2[:, kc * 128:(kc + 1) * 128], IDT[:])
                    bv = b.v(b.t[:, :].rearrange("p (a b) -> p a b", a=4))
                    P.copy(HF[:, half * 4:(half + 1) * 4, :], bv)
                    P.copy(H2T[:, half * 4:(half + 1) * 4, t * 128:(t + 1) * 128], bv, e="act")
                b = nb()
                for kc in range(8):
                    P.mm(b[:, 0:16], HF[:, kc, :], RW[:, kc, :], start=(kc == 0), stop=(kc == 7))
                sco = SCO[k]; ss = SS[k]; psm = PSm[k]; gs = GS[k]; gmx = GMx[k]; e2 = E2[k]
                P.act(sco[:], b[:, 0:16], AF.Sigmoid)
                ssf = ss.v(ss.t[:, :, :].rearrange("p a b -> p (a b)"))
                P.tt(ssf, sco[:], RB[:], ALU.add)
                P.tt(psm[:, :, 0:3], ss[:, :, 0:3], ss[:, :, 1:4], ALU.add)
                P.tt(psm[:, :, 3:5], ss[:, :, 0:2], ss[:, :, 2:4], ALU.add)
                P.tt(psm[:, :, 5:6], ss[:, :, 0:1], ss[:, :, 3:4], ALU.add)
                P.red(gs[:], psm[:], ALU.max)
                P.red(gmx[:, 0:1], gs[:], ALU.max)
                P.ts(gs[:], gs[:], gmx[:, 0:1], None, ALU.is_ge)
                P.tt(psm[:, :, 0:3], ss[:, :, 0:3], ss[:, :, 1:4], ALU.min)
                P.tt(psm[:, :, 3:5], ss[:, :, 0:2], ss[:, :, 2:4], ALU.min)
                P.tt(psm[:, :, 5:6], ss[:, :, 0:1], ss[:, :, 3:4], ALU.min)
                P.red(e2[:, :, 0], psm[:], ALU.max)
                thr = E2[k].v(E2[k].t[:, :, 0:1].to_broadcast([128, 4, 4]))
                P.tt(psm[:, :, 0:4], ss[:], thr, ALU.is_ge)
                P.tt(psm[:, :, 0:4], psm[:, :, 0:4], _bc(gs, gs.t[:, :], 2, [128, 4, 4]), ALU.mult)
                scv = sco.v(sco.t[:, :].rearrange("p (a b) -> p a b", b=4))
                P.tt(e2[:], psm[:, :, 0:4], scv, ALU.mult)
                P.red(gmx[:, 1:2], E2[k].v(E2[k].t[:, :, :].rearrange("p a b -> p (a b)")), ALU.add)
                P.recip(gmx[:, 1:2], gmx[:, 1:2])
                P.ts(CW[:, t, :], E2[k].v(E2[k].t[:, :, :].rearrange("p a b -> p (a b)")), gmx[:, 1:2], None, ALU.mult)
            for e in range(NE):
                p_ = e % 2
                for m, (wsrc, pat) in enumerate(((wg, 8), (wu, 8), (wd, 4))):
                    S_ = STG[stg_i[0] % 2]; stg_i[0] += 1
                    sv = S_.v(S_.t[:, :].rearrange("p (kc n) -> p kc n", kc=pat))
                    P.load("sp" if m % 2 == 0 else "act", sv, wsrc[e].rearrange("(kc kp) n -> kp kc n", kp=128))
                    Wb = WB[m][p_]
                    if m < 2:
                        P.copy(Wb[:], S_[:], e="pool")
                    else:
                        P.tt(Wb.v(Wb.t[:, :].rearrange("p (kc n) -> p kc n", kc=4)), sv, _bc(GF, GF.t[:, :], 1, [128, 4, D]), ALU.mult, e="pool")
                WG = WB[0][p_].v(WB[0][p_].t[:, :].rearrange("p (kc n) -> p kc n", kc=8))
                WU = WB[1][p_].v(WB[1][p_].t[:, :].rearrange("p (kc n) -> p kc n", kc=8))
                WD = WB[2][p_].v(WB[2][p_].t[:, :].rearrange("p (kc n) -> p kc n", kc=4))
                for sub in range(2):
                    hid = HID[sub]
                    tsl = slice(sub * 512, (sub + 1) * 512)
                    for c in range(4):
                        bg = nb(); bu = nb()
                        for kc in range(8):
                            P.mm(bg[:], WG[:, kc, c * 128:(c + 1) * 128], H2T[:, kc, tsl], start=(kc == 0), stop=(kc == 7))
                        for kc in range(8):
                            P.mm(bu[:], WU[:, kc, c * 128:(c + 1) * 128], H2T[:, kc, tsl], start=(kc == 0), stop=(kc == 7))
                        sil = SIL[c % 2]
                        P.act(sil[:], bg[:], AF.Silu)
                        P.tt(hid[:, c, :], sil[:], bu[:], ALU.mult)
                    for t4 in range(4):
                        t = sub * 4 + t4
                        for nh in range(2):
                            cs = slice(nh * 512, (nh + 1) * 512)
                            bd = nb()
                            for c in range(4):
                                P.mm(bd[:], hid[:, c, t4 * 128:(t4 + 1) * 128], WD[:, c, cs], start=(c == 0), stop=(c == 3))
                            P.stt(ACC[t][:, cs], bd[:], CW[:, t, e:e + 1], ACC[t][:, cs], ALU.mult, ALU.add)
            for t in range(8):
                ti = tg * 8 + t
                P.store("sp" if t % 2 else "act", xo[ti * 128:(ti + 1) * 128, :], ACC[t][:])
        P.finish()
        print("k4b ninst", P.ninst, "nsem", P.nsem)
    return nc


def run_k4b(x1, g2, sc2, sh2, gate_ffn, prm, l):
    if "k4b" not in _CACHE:
        _CACHE["k4b"] = build_k4b()
    nc = _CACHE["k4b"]
    ident = np.eye(128, dtype=np.float32)
    maps = []
    for i in range(NCORES):
        b, tq = divmod(i, 4)
        ts_ = slice(tq * 2048, (tq + 1) * 2048)
        maps.append({"x1": np.ascontiguousarray(x1[b, ts_]), "g": g2[None].copy(), "sc": sc2[b][None].copy(),
                     "sh": sh2[b][None].copy(), "gf": gate_ffn[b][None].copy(), "rw": prm["router_w"],
                     "rb": prm["router_b"][None].copy(), "ident": ident,
                     "wg": prm["exp_w_gate"][l], "wu": prm["exp_w_up"][l], "wd": prm["exp_w_down"][l]})
    res = run_bass_kernel_spmd(nc, maps, core_ids=list(range(NCORES)))
    out = np.empty((NB, T, D), np.float32)
    for i in range(NCORES):
        b, tq = divmod(i, 4)
        out[b, tq * 2048:(tq + 1) * 2048] = res.results[i]["xo"]
    return out


def kernel(**inputs):
    prm = {k: np.asarray(v) for k, v in inputs.items()}
    x = prm["x"].astype(np.float32, copy=False)
    mod = run_k0(prm["c"], prm["w_ada"], prm["b_ada"])
    for l in range(2):
        sh1, sc1, gate_mix, sh2, sc2, gate_ffn = [np.ascontiguousarray(a) for a in np.split(mod[l], 6, axis=-1)]
        p = run_k1(x, prm["norm_g"][l, 0], sc1, sh1, prm["w_in"][l], prm["b_in"][l])
        ya = run_k2(p, prm, l)
        yb = run_k3(p, prm, l)
        x1 = run_k4a(x, ya, yb, p, gate_mix, prm, l)
        x = run_k4b(x1, prm["norm_g"][l, 1], sc2, sh2, gate_ffn, prm, l)
    return x
```
